# Optimizing a Trainium2 kernel written in Bass

```python
import math
import jax, jax.numpy as jnp
from jax import lax
import numpy as np

D_MODEL = 1024
BATCH = 4
SEQ = 8192
DEPTH = 2

GRID_W = 64
CTX_LEN = 256
N_MIXERS = 2
N_POOL_LAYERS = (DEPTH + N_MIXERS - 1) // N_MIXERS
N_DN_LAYERS = DEPTH // N_MIXERS
POOL_WINDOWS = (2, 4, 8, 16)
N_POOL_GROUPS = 4
POOL_GC = D_MODEL // N_POOL_GROUPS
DN_HEADS = 8
DN_DK = D_MODEL // DN_HEADS
DN_DV = D_MODEL // DN_HEADS
DN_CONV_W = 4
DN_CHUNK = 64
DN_IN = 4 * D_MODEL + 4 * DN_HEADS
N_GROUPS = 4
EXPERTS_PER_GROUP = 8
N_EXPERTS = N_GROUPS * EXPERTS_PER_GROUP
TOP_K_EXPERT = 2
D_EXPERT = 512
EPS = 1e-6

kernel_name = "hybrid_pool_deltanet_hmoe_dit"


def rmsnorm(x, w):
    x32 = x.astype(jnp.float32)
    return x32 * lax.rsqrt(jnp.mean(x32 * x32, axis=-1, keepdims=True) + EPS) * w.astype(jnp.float32)


def l2norm(t):
    return t * lax.rsqrt(jnp.sum(t * t, axis=-1, keepdims=True) + EPS)


def adaln_mod(cond, w, b):
    m = jax.nn.silu(cond.astype(jnp.float32)) @ w + b
    return jnp.split(m, 6, axis=-1)


def box_mean(x, win, axis):
    n = x.shape[axis]
    cs = jnp.cumsum(x.astype(jnp.float32), axis=axis)
    cs = jnp.concatenate([jnp.zeros_like(lax.slice_in_dim(cs, 0, 1, axis=axis)), cs], axis=axis)
    t = np.arange(n)
    lo = np.maximum(t - win // 2, 0)
    hi = np.minimum(t + win // 2, n)
    shape = [1] * x.ndim
    shape[axis] = n
    cnt = jnp.asarray((hi - lo).astype(np.float32)).reshape(shape)
    return (jnp.take(cs, hi, axis=axis) - jnp.take(cs, lo, axis=axis)) / cnt


def pool_mixer(h, w_pool, b_pool, scale, rows):
    B, L, _ = h.shape
    hg = h.astype(jnp.float32).reshape(B, L, N_POOL_GROUPS, POOL_GC)
    diffs = []
    for gi, win in enumerate(POOL_WINDOWS):
        u = hg[:, :, gi, :]
        if rows is not None:
            ug = u.reshape(B, rows, GRID_W, POOL_GC)
            m = box_mean(box_mean(ug, win, 1), win, 2).reshape(B, L, POOL_GC)
        else:
            m = box_mean(u, win, 1)
        diffs.append(m - u)
    d = jnp.stack(diffs, axis=2)
    y = jnp.einsum('blgc,gce->blge', d, w_pool).reshape(B, L, D_MODEL)
    return (y + b_pool) * scale


def short_conv(u, w):
    L = u.shape[1]
    left = DN_CONV_W // 2
    right = DN_CONV_W - 1 - left
    up = jnp.pad(u, ((0, 0), (left, right), (0, 0)))
    out = up[:, 0:L] * w[0]
    for k in range(1, DN_CONV_W):
        out = out + up[:, k:k + L] * w[k]
    return out


def dn_project(h, w_in, w_conv):
    B, L, _ = h.shape
    D = D_MODEL
    p = h @ w_in
    qkv = jax.nn.silu(short_conv(p[..., :3 * D], w_conv))
    z = p[..., 3 * D:4 * D]
    ab = p[..., 4 * D:].astype(jnp.float32).reshape(B, L, 4, DN_HEADS).transpose(2, 0, 3, 1)

    def heads(t, dh):
        return t.reshape(B, L, DN_HEADS, dh).transpose(0, 2, 1, 3).astype(jnp.float32)

    q = l2norm(heads(qkv[..., :D], DN_DK)) * (DN_DK ** -0.5)
    k = l2norm(heads(qkv[..., D:2 * D], DN_DK))
    v = heads(qkv[..., 2 * D:], DN_DV)
    return q, k, v, z, ab


def gated_delta_chunked(q, k, v, beta, g, s0, need_output):
    B, H, L, dk = q.shape
    dv = v.shape[-1]
    C = DN_CHUNK
    n = L // C
    q = q.reshape(B, H, n, C, dk)
    k = k.reshape(B, H, n, C, dk)
    v = v.reshape(B, H, n, C, dv)
    beta = beta.reshape(B, H, n, C)
    gc = jnp.cumsum(g.reshape(B, H, n, C), axis=-1)
    strict = np.tril(np.ones((C, C), dtype=bool), -1)
    diff = gc[..., :, None] - gc[..., None, :]
    kb = k * beta[..., None]
    lmat = jnp.where(strict, jnp.einsum('bhnid,bhnjd->bhnij', kb, k) * jnp.exp(jnp.where(strict, diff, 0.0)), 0.0)
    rhs = jnp.concatenate([v * beta[..., None], kb * jnp.exp(gc)[..., None]], axis=-1)
    sol = lax.linalg.triangular_solve(lmat, rhs, left_side=True, lower=True, unit_diagonal=True)
    u, w = sol[..., :dv], sol[..., dv:]
    g_last = gc[..., -1]
    k_dec = k * jnp.exp(g_last[..., None] - gc)[..., None]
    xs = {'u': u, 'w': w, 'k_dec': k_dec}
    if need_output:
        incl = np.tril(np.ones((C, C), dtype=bool), 0)
        xs['qk'] = jnp.where(incl, jnp.einsum('bhnid,bhnjd->bhnij', q, k) * jnp.exp(jnp.where(incl, diff, 0.0)), 0.0)
        xs['q_dec'] = q * jnp.exp(gc)[..., None]
    xs = {name: jnp.moveaxis(t, 2, 0) for name, t in xs.items()}
    xs['g_last'] = jnp.moveaxis(g_last, 2, 0)

    def step(S, xc):
        v_new = xc['u'] - jnp.einsum('bhck,bhkv->bhcv', xc['w'], S)
        S_next = S * jnp.exp(xc['g_last'])[..., None, None] + jnp.einsum('bhck,bhcv->bhkv', xc['k_dec'], v_new)
        if need_output:
            o = jnp.einsum('bhck,bhkv->bhcv', xc['q_dec'], S) + jnp.einsum('bhij,bhjv->bhiv', xc['qk'], v_new)
            return S_next, o
        return S_next, None

    S_final, o = lax.scan(step, s0, xs)
    if need_output:
        o = jnp.moveaxis(o, 0, 2).reshape(B, H, L, dv)
    return o, S_final


def run_direction(q, k, v, a, b, a_log, dt_bias, s0, reverse, need_output):
    g = -jnp.exp(a_log)[None, :, None] * jax.nn.softplus(a + dt_bias[None, :, None])
    beta = jax.nn.sigmoid(b)
    if reverse:
        q, k, v = jnp.flip(q, 2), jnp.flip(k, 2), jnp.flip(v, 2)
        g, beta = jnp.flip(g, 2), jnp.flip(beta, 2)
    o, S = gated_delta_chunked(q, k, v, beta, g, s0, need_output)
    if reverse and need_output:
        o = jnp.flip(o, 2)
    return o, S


def dn_output(o, z, norm_w, w_out):
    B, H, L, dv = o.shape
    o = o.transpose(0, 2, 1, 3)
    o = o * lax.rsqrt(jnp.mean(o * o, axis=-1, keepdims=True) + EPS) * norm_w
    o = o * jax.nn.silu(z.astype(jnp.float32).reshape(B, L, H, dv))
    return o.reshape(B, L, H * dv) @ w_out


def deltanet_mixer(h, hc, w_in, w_conv, a_log, dt_bias, norm_w, w_out, ctx_out):
    q, k, v, z, ab = dn_project(h, w_in, w_conv)
    qc, kc, vc, zc, abc = dn_project(hc, w_in, w_conv)
    s0 = jnp.zeros((h.shape[0], DN_HEADS, DN_DK, DN_DV), jnp.float32)
    o_lat, o_ctx = None, None
    for d in range(2):
        oc_d, s_ctx = run_direction(qc, kc, vc, abc[2 * d], abc[2 * d + 1], a_log[d], dt_bias[d], s0, d == 1, ctx_out)
        o_d, _ = run_direction(q, k, v, ab[2 * d], ab[2 * d + 1], a_log[d], dt_bias[d], s_ctx, d == 1, True)
        o_lat = o_d if o_lat is None else o_lat + o_d
        if ctx_out:
            o_ctx = oc_d if o_ctx is None else o_ctx + oc_d
    y = dn_output(o_lat, z, norm_w, w_out)
    yc = dn_output(o_ctx, zc, norm_w, w_out) if ctx_out else None
    return y, yc


def hier_moe(h, w_rg, b_rg, w_re, b_re, w_gate, w_up, w_down):
    shp = h.shape
    t = h.reshape(-1, D_MODEL)
    pg = jax.nn.softmax((t @ w_rg + b_rg).astype(jnp.float32), axis=-1)
    pg_top, g_idx = lax.top_k(pg, 1)
    elog = (t @ w_re + b_re).astype(jnp.float32).reshape(-1, N_GROUPS, EXPERTS_PER_GROUP)
    elog_sel = jnp.einsum('tge,tg->te', elog, jax.nn.one_hot(g_idx[:, 0], N_GROUPS, dtype=jnp.float32))
    e_top, e_idx = lax.top_k(elog_sel, TOP_K_EXPERT)
    wts = pg_top * jax.nn.softmax(e_top, axis=-1)
    ids = g_idx * EXPERTS_PER_GROUP + e_idx
    gates = jnp.sum(jax.nn.one_hot(ids, N_EXPERTS, dtype=jnp.float32) * wts[..., None], axis=1)

    def expert(acc, xs):
        wg, wu, wd, gt = xs
        hid = jax.nn.silu(t @ wg) * (t @ wu)
        return acc + gt[:, None] * (hid @ wd), None

    acc, _ = lax.scan(expert, jnp.zeros(t.shape, jnp.float32), (w_gate, w_up, w_down, gates.T))
    return acc.reshape(shp)


def setup_inputs(seed: int = 0) -> dict:
    key = jax.random.key(seed)
    ks = jax.random.split(key, 32)
    D = D_MODEL

    def nrm(k, shape, s):
        return jax.random.normal(k, shape, jnp.float32) * s

    dt = jnp.exp(jax.random.uniform(ks[15], (N_DN_LAYERS, 2, DN_HEADS), jnp.float32,
                                    minval=math.log(1e-3), maxval=math.log(1e-1)))
    return {
        'x': nrm(ks[0], (BATCH, SEQ, D), 1.0),
        'c': nrm(ks[1], (BATCH, D), 1.0),
        'ctx': nrm(ks[2], (BATCH, CTX_LEN, D), 1.0),
        'c_ctx': nrm(ks[3], (D,), 1.0),
        'w_ada': nrm(ks[4], (DEPTH, D, 6 * D), 0.5 * D ** -0.5),
        'b_ada': nrm(ks[5], (DEPTH, 6 * D), 0.02),
        'norm_mix': 1.0 + nrm(ks[6], (DEPTH, D), 0.1),
        'norm_ffn': 1.0 + nrm(ks[7], (DEPTH, D), 0.1),
        'w_pool': nrm(ks[8], (N_POOL_LAYERS, N_POOL_GROUPS, POOL_GC, POOL_GC), POOL_GC ** -0.5),
        'b_pool': nrm(ks[9], (N_POOL_LAYERS, D), 0.02),
        'pool_scale': 1.0 + nrm(ks[10], (N_POOL_LAYERS, D), 0.1),
        'w_dn_in': nrm(ks[11], (N_DN_LAYERS, D, DN_IN), D ** -0.5),
        'w_dn_conv': nrm(ks[12], (N_DN_LAYERS, DN_CONV_W, 3 * D), DN_CONV_W ** -0.5),
        'dn_a_log': jnp.log(jax.random.uniform(ks[13], (N_DN_LAYERS, 2, DN_HEADS), jnp.float32, minval=1.0, maxval=16.0)),
        'dn_dt_bias': dt + jnp.log(-jnp.expm1(-dt)),
        'dn_norm': 1.0 + nrm(ks[14], (N_DN_LAYERS, DN_DV), 0.1),
        'w_dn_out': nrm(ks[16], (N_DN_LAYERS, D, D), D ** -0.5),
        'w_rg': nrm(ks[17], (DEPTH, D, N_GROUPS), D ** -0.5),
        'b_rg': nrm(ks[18], (DEPTH, N_GROUPS), 0.01),
        'w_re': nrm(ks[19], (DEPTH, D, N_EXPERTS), D ** -0.5),
        'b_re': nrm(ks[20], (DEPTH, N_EXPERTS), 0.01),
        'w_e_gate': nrm(ks[21], (DEPTH, N_EXPERTS, D, D_EXPERT), D ** -0.5),
        'w_e_up': nrm(ks[22], (DEPTH, N_EXPERTS, D, D_EXPERT), D ** -0.5),
        'w_e_down': nrm(ks[23], (DEPTH, N_EXPERTS, D_EXPERT, D), D_EXPERT ** -0.5),
        'norm_final': 1.0 + nrm(ks[24], (D,), 0.1),
    }


def reference(x, c, ctx, c_ctx, w_ada, b_ada, norm_mix, norm_ffn, w_pool, b_pool, pool_scale,
              w_dn_in, w_dn_conv, dn_a_log, dn_dt_bias, dn_norm, w_dn_out,
              w_rg, b_rg, w_re, b_re, w_e_gate, w_e_up, w_e_down, norm_final):
    out_dtype = x.dtype
    rows = x.shape[1] // GRID_W
    xs = x.astype(jnp.float32)
    cs = ctx.astype(jnp.float32)
    for i in range(DEPTH):
        last = i == DEPTH - 1
        j = i // N_MIXERS
        is_pool = i % N_MIXERS == 0
        sh_m, sc_m, gt_m, sh_f, sc_f, gt_f = [m[:, None, :] for m in adaln_mod(c, w_ada[i], b_ada[i])]
        csh_m, csc_m, cgt_m, csh_f, csc_f, cgt_f = adaln_mod(c_ctx, w_ada[i], b_ada[i])
        hx = rmsnorm(xs, norm_mix[i]) * (1.0 + sc_m) + sh_m
        if is_pool:
            xs = xs + gt_m * pool_mixer(hx, w_pool[j], b_pool[j], pool_scale[j], rows)
            if not last:
                hc = rmsnorm(cs, norm_mix[i]) * (1.0 + csc_m) + csh_m
                cs = cs + cgt_m * pool_mixer(hc, w_pool[j], b_pool[j], pool_scale[j], None)
        else:
            hc = rmsnorm(cs, norm_mix[i]) * (1.0 + csc_m) + csh_m
            y, yc = deltanet_mixer(hx, hc, w_dn_in[j], w_dn_conv[j], dn_a_log[j], dn_dt_bias[j],
                                   dn_norm[j], w_dn_out[j], not last)
            xs = xs + gt_m * y
            if not last:
                cs = cs + cgt_m * yc
        hx = rmsnorm(xs, norm_ffn[i]) * (1.0 + sc_f) + sh_f
        if last:
            xs = xs + gt_f * hier_moe(hx, w_rg[i], b_rg[i], w_re[i], b_re[i], w_e_gate[i], w_e_up[i], w_e_down[i])
        else:
            hc = rmsnorm(cs, norm_ffn[i]) * (1.0 + csc_f) + csh_f
            n_ctx = hc.shape[1]
            f = hier_moe(jnp.concatenate([hc, hx], axis=1), w_rg[i], b_rg[i], w_re[i], b_re[i],
                         w_e_gate[i], w_e_up[i], w_e_down[i])
            cs = cs + cgt_f * f[:, :n_ctx]
            xs = xs + gt_f * f[:, n_ctx:]
    return rmsnorm(xs, norm_final).astype(out_dtype)
```

```python
import os
from concourse.bass_utils import run_bass_kernel_spmd
import numpy as np, contextlib
import concourse.bass as bass
import concourse.mybir as mybir

F32 = mybir.dt.float32
BF16 = mybir.dt.bfloat16
AF = mybir.ActivationFunctionType
ALU = mybir.AluOpType
AX = mybir.AxisListType


class Prog:
    NS = 16

    def __init__(self, nc, es):
        self.nc = nc
        self.E = {'pe': nc.tensor, 'act': nc.scalar, 'dve': nc.vector, 'pool': nc.gpsimd, 'sp': nc.sync}
        self.sem = {e: es.enter_context(nc.semaphore("s_" + e)) for e in ('pe', 'act', 'dve', 'pool')}
        self.dsem = {q: [es.enter_context(nc.semaphore("d_%s%d" % (q, i))) for i in range(self.NS)]
                     for q in ('sp', 'act', 'pool')}
        self.sigcount = {e: 0 for e in self.sem}
        self.dmacount = {q: 0 for q in self.dsem}
        self.waited = {}
        self.ops = []
        self.last_w = {}
        self.readers = {}
        self.n_inst = 0

    def op(self, eng, fn, r=(), w=(), dma=False):
        self.ops.append((eng, fn, tuple(r), tuple(w), dma))

    def dma(self, q, out, in_, r, w, **kw):
        e = self.E[q]
        self.op(q, lambda: e.dma_start(out=out, in_=in_, **kw), r, w, dma=True)

    @staticmethod
    def _needs_wait(oj_eng, oj_dma, oi_eng, oi_dma, typ):
        if oj_dma:
            return True
        if oj_eng == oi_eng and not oi_dma:
            if oi_eng == 'pe':
                return False
            return typ == 'raw'
        return True

    def flush(self):
        ops = self.ops
        n = len(ops)
        deps = [None] * n
        last_w, readers = self.last_w, self.readers
        for i, (eng, fn, r, w, dma) in enumerate(ops):
            d = {}
            for k in r:
                t = last_w.get(k)
                if t is not None:
                    d[t] = 'raw'
                if isinstance(k, tuple) and k and k[0] == 'ps':
                    for e2, t in readers.get(k, {}).items():
                        if e2 != eng and t not in d:
                            d[t] = 'raw'
            for k in w:
                t = last_w.get(k)
                if t is not None and t not in d:
                    d[t] = 'waw'
                for t in readers.get(k, {}).values():
                    if t not in d:
                        d[t] = 'war'
            d.pop(('p', i), None)
            deps[i] = d
            me = ('p', i)
            for k in r:
                rk = readers.setdefault(k, {})
                if dma:
                    rk[('d', i)] = me
                else:
                    rk[eng] = me
            for k in w:
                last_w[k] = me
                readers[k] = {}
        need_sig = [False] * n
        for i, (eng, fn, r, w, dma) in enumerate(ops):
            for t, typ in deps[i].items():
                if t[0] == 'p':
                    j = t[1]
                    ej, _, _, _, dj = ops[j]
                    if not dj and self._needs_wait(ej, dj, eng, dma, typ):
                        need_sig[j] = True
        last_of = {}
        for i, (eng, fn, r, w, dma) in enumerate(ops):
            if not dma:
                last_of[eng] = i
        for e, i in last_of.items():
            need_sig[i] = True
        resolved = [None] * n
        for i, (eng, fn, r, w, dma) in enumerate(ops):
            E = self.E[eng]
            waits = {}
            for t, typ in deps[i].items():
                if t[0] == 'p':
                    t2 = resolved[t[1]]
                    ej, dj = ops[t[1]][0], ops[t[1]][4]
                else:
                    t2 = t
                    ej, dj = t[1], t[0] == 'd'
                if not self._needs_wait(ej, dj, eng, dma, typ):
                    continue
                if t2[0] == 'c':
                    key = ('c', t2[1]); val = t2[2]
                else:
                    key = ('d', t2[1], t2[2]); val = t2[3]
                if waits.get(key, 0) < val:
                    waits[key] = val
            if dma:
                k = self.dmacount[eng]
                slot = k % self.NS
                if k >= self.NS:
                    key = ('d', eng, slot); val = 16 * (k // self.NS)
                    if waits.get(key, 0) < val:
                        waits[key] = val
            for key, val in waits.items():
                wk = (eng, key)
                if self.waited.get(wk, 0) >= val:
                    continue
                self.waited[wk] = val
                s = self.sem[key[1]] if key[0] == 'c' else self.dsem[key[1]][key[2]]
                E.wait_ge(s, val)
                self.n_inst += 1
            inst = fn()
            self.n_inst += 1
            if dma:
                k = self.dmacount[eng]
                slot = k % self.NS
                val = 16 * (k // self.NS + 1)
                inst.then_inc(self.dsem[eng][slot], 16)
                self.dmacount[eng] = k + 1
                resolved[i] = ('d', eng, slot, val)
            else:
                if need_sig[i]:
                    self.sigcount[eng] += 1
                    inst.then_inc(self.sem[eng], 1)
                    resolved[i] = ('c', eng, self.sigcount[eng])
        nxt = {}
        for i in range(n - 1, -1, -1):
            eng, dma = ops[i][0], ops[i][4]
            if dma:
                continue
            if resolved[i] is not None:
                nxt[eng] = resolved[i]
            else:
                resolved[i] = nxt[eng]
        for k in list(last_w.keys()):
            t = last_w[k]
            if t[0] == 'p':
                last_w[k] = resolved[t[1]]
        for k in list(readers.keys()):
            rk = readers[k]
            for kk in list(rk.keys()):
                t = rk[kk]
                if t[0] == 'p':
                    rk[kk] = resolved[t[1]]
        self.ops = []

    def barrier(self):
        self.flush()
        for eng in ('pe', 'act', 'dve', 'pool', 'sp'):
            E = self.E[eng]
            for e, c in self.sigcount.items():
                if c > 0 and e != eng and self.waited.get((eng, ('c', e)), 0) < c:
                    E.wait_ge(self.sem[e], c)
                    self.waited[(eng, ('c', e))] = c
            for q, k in self.dmacount.items():
                for slot in range(self.NS):
                    if k > slot:
                        val = 16 * ((k - 1 - slot) // self.NS + 1)
                        if self.waited.get((eng, ('d', q, slot)), 0) < val:
                            E.wait_ge(self.dsem[q][slot], val)
                            self.waited[(eng, ('d', q, slot))] = val

    def finish(self):
        self.flush()
        sp = self.E['sp']
        for e, c in self.sigcount.items():
            if c > 0:
                sp.wait_ge(self.sem[e], c)
        for q, k in self.dmacount.items():
            for slot in range(self.NS):
                if k > slot:
                    cnt = (k - 1 - slot) // self.NS + 1
                    sp.wait_ge(self.dsem[q][slot], 16 * cnt)


D = 1024
NCT = 2
NLT = 64
NT = NCT + NLT
EPS = 1e-6
POOL_WINDOWS = (2, 4, 8, 16)
JREL = {0: list(range(-1, 4)), 1: list(range(-1, 5)), 2: list(range(-2, 6)), 3: list(range(-4, 8))}
BAND_IDX = {}
_i = 0
for _g in range(4):
    for _j in JREL[_g]:
        BAND_IDX[(_g, _j)] = _i
        _i += 1
NBAND = _i


def host_constants():
    c = {}
    def axis_w(n, win):
        t = np.arange(n)
        lo = np.maximum(t - win // 2, 0); hi = np.minimum(t + win // 2, n)
        W = np.zeros((n, n), np.float64)
        for o in range(n):
            W[lo[o]:hi[o], o] = 1.0 / (hi[o] - lo[o])
        return W
    band = np.zeros((3, 128, NBAND, 512), np.float32)
    for g, win in enumerate(POOL_WINDOWS):
        Wr = axis_w(128, win); Wc = axis_w(64, win)
        for ti, b in enumerate((0, 7, 15)):
            for j in JREL[g]:
                jt = 4 * b + j
                if jt < 0 or jt >= 64:
                    continue
                M = np.einsum('ab,cd->acbd', Wr[2 * jt:2 * jt + 2, 8 * b:8 * b + 8], Wc).reshape(128, 512)
                if 0 <= j < 4:
                    M[:, j * 128:(j + 1) * 128] -= np.eye(128)
                band[ti, :, BAND_IDX[(g, j)], :] = M
    c['band'] = band
    cb = np.zeros((128, 8, 256), np.float32)
    for g, win in enumerate(POOL_WINDOWS):
        W = axis_w(256, win) - np.eye(256)
        for j in range(2):
            cb[:, g * 2 + j, :] = W[j * 128:(j + 1) * 128, :]
    c['cband'] = cb
    idx = np.arange(128)
    m = np.zeros((128, 13, 128), np.float32)
    m[:, 0] = np.eye(128)
    m[:, 1] = (idx[:, None] <= idx[None, :])
    m[:, 2] = (idx[:, None] >= idx[None, :])
    m[:, 3] = 1.0
    m[:, 4] = np.where(idx[:, None] <= idx[None, :], 0.0, -30000.0)
    m[:, 5] = np.where(idx[:, None] >= idx[None, :], 0.0, -30000.0)
    m[:, 6] = (idx[:, None] < idx[None, :])
    m[:, 7] = (idx[:, None] > idx[None, :])
    m[:, 8] = np.where(idx[:, None] > idx[None, :], 0.0, -30000.0)
    m[:, 9] = np.where(idx[:, None] < idx[None, :], 0.0, -30000.0)
    bd32 = (idx[:, None] // 32 == idx[None, :] // 32); bd64 = (idx[:, None] // 64 == idx[None, :] // 64)
    m[:, 10] = bd32; m[:, 11] = bd64 & ~bd32; m[:, 12] = ~bd64
    c['masks'] = m
    bm = np.zeros((16, 8, 128), np.float32)
    for r in range(16):
        bm[r, r % 8, :] = 1.0
    c['blockmask'] = bm
    cm = np.zeros((128, 160), np.float32)
    cm[:, 0:32] = np.arange(32)[None, :]
    cm[:, 32:40] = np.arange(8)[None, :] * 128 + np.arange(128)[:, None]
    cm[:, 40:120] = np.arange(80)[None, :] * 512
    c['cmisc'] = cm
    return c


_UNIQ = [0]


class Ring:
    def __init__(self, nc, es, name, n, shape, dtype):
        _UNIQ[0] += 1
        self.tiles = [es.enter_context(nc.sbuf_tensor("%s%d_u%d" % (name, i, _UNIQ[0]), shape, dtype)) for i in range(n)]
        self.name = name
        self.i = 0

    def next(self):
        k = self.i % len(self.tiles)
        self.i += 1
        return self.tiles[k], "%s%d" % (self.name, k)


def build(stop_after=None, debug=False):
    nc = bass.Bass("TRN2", target_bir_lowering=False)
    es = contextlib.ExitStack()

    def din(name, shape, dt=F32):
        return nc.dram_tensor(name, list(shape), dt, kind="ExternalInput").ap()

    def dscr(name, shape, dt=F32):
        kind = "ExternalOutput" if debug else "Internal"
        return nc.dram_tensor(name, list(shape), dt, kind=kind).ap()

    xin = din("xin", [NT * 128, D])
    ccol = din("ccol", [128, 2, 8])
    w_ada = din("w_ada", [2, D, 6 * D]); b_ada = din("b_ada", [2, 6 * D])
    norm_mix = din("norm_mix", [2, D]); norm_ffn = din("norm_ffn", [2, D])
    w_pool = din("w_pool", [4, 256, 256]); b_pool = din("b_pool", [D]); pool_scale = din("pool_scale", [D])
    w_dn_in = din("w_dn_in", [D, 4128]); wconv = din("wconv", [128, 24, 4])
    dn_a_log = din("dn_a_log", [16]); dn_dt_bias = din("dn_dt_bias", [16]); dn_norm = din("dn_norm", [128])
    w_dn_out = din("w_dn_out", [D, D])
    w_r = din("w_r", [2, D, 36]); b_r = din("b_r", [2, 36])
    w_e_gate = din("w_e_gate", [2, 32, D, 512]); w_e_up = din("w_e_up", [2, 32, D, 512])
    w_e_down = din("w_e_down", [2, 32, 512, D]); norm_final = din("norm_final", [D])
    band = din("band", [3, 128, NBAND, 512]); cband = din("cband", [128, 8, 256])
    masks = din("masks", [128, 13, 128]); blockmask = din("blockmask", [16, 8, 128])
    cmisc = din("cmisc", [128, 160])
    out = nc.dram_tensor("out", [NLT * 128, D], F32, kind="ExternalOutput").ap()
    XS1 = dscr("XS1", [NT * 128, D])
    XS2 = dscr("XS2", [NT * 128, D])
    XS3 = dscr("XS3", [NT * 128, D])

    with es:
        P = Prog(nc, es)
        psum = es.enter_context(nc.psum_tensor("psum", [128, 4096], F32))
        PS = [psum[:, b * 512:(b + 1) * 512] for b in range(8)]
        PK = [("ps", b) for b in range(8)]

        def sb(name, shape, dt=F32, stack=es):
            _UNIQ[0] += 1
            return stack.enter_context(nc.sbuf_tensor("%s_u%d" % (name, _UNIQ[0]), list(shape), dt))

        msk = sb("msk", [128, 13, 128])
        P.dma('sp', msk[:], masks, r=["d_masks"], w=["msk"])
        ident = msk[:, 0, :]
        identb = sb("identb", [128, 128], BF16)
        P.op('dve', lambda: nc.vector.tensor_copy(out=identb[:], in_=msk[:, 0, :]), r=["msk"], w=["identb"])
        modL = sb("modL", [128, 6 * D]); modC = sb("modC", [128, 6 * D])
        csb = sb("csb", [128, 2, 8]); sil = sb("sil", [128, 2, 8])
        rep = sb("rep", [128, 2, 8, 128], BF16)
        P.dma('sp', csb[:], ccol, r=["d_ccol"], w=["csb"])
        P.op('act', lambda: nc.scalar.activation(out=sil[:], in_=csb[:], func=AF.Silu), r=["csb"], w=["sil"])
        P.op('dve', lambda: nc.vector.tensor_copy(out=rep[:], in_=sil[:].unsqueeze(3).to_broadcast([128, 2, 8, 128])),
             r=["sil"], w=["rep"])

        def adaln(layer, ph):
            wr = Ring(nc, ph, "adaw", 2, [128, 8, 512], BF16)
            nw = sb("nw", [128, 2, D], F32, ph)
            P.dma('sp', modL[:], b_ada[layer].partition_broadcast(128), r=["d_b_ada"], w=["modL"])
            P.dma('sp', modC[:], b_ada[layer].partition_broadcast(128), r=["d_b_ada"], w=["modC"])
            P.dma('sp', nw[:, 0, :], norm_mix[layer].partition_broadcast(128), r=["d_nm"], w=["nw"])
            P.dma('sp', nw[:, 1, :], norm_ffn[layer].partition_broadcast(128), r=["d_nm"], w=["nw"])
            wv = w_ada[layer].rearrange("(k p) n -> p k n", p=128)
            for blk in range(12):
                wt, wk = wr.next()
                P.dma('pool', wt[:], wv[:, :, blk * 512:(blk + 1) * 512], r=["d_w_ada"], w=[wk])
                for s, (mod, mk) in enumerate(((modL, "modL"), (modC, "modC"))):
                    b = (blk * 2 + s) % 8
                    for k in range(8):
                        P.op('pe', (lambda b=b, s=s, k=k, wt=wt: nc.tensor.matmul(
                            PS[b], rep[:, s, k, :], wt[:, k, :], start=(k == 0), stop=(k == 7))),
                            r=["rep", wk], w=[PK[b]])
                    sl = slice(blk * 512, (blk + 1) * 512)
                    P.op('dve', (lambda b=b, mod=mod, sl=sl: nc.vector.tensor_tensor(
                        out=mod[:, sl], in0=PS[b], in1=mod[:, sl], op=ALU.add)), r=[PK[b], mk], w=[mk])
            for s, (mod, mk) in enumerate(((modL, "modL"), (modC, "modC"))):
                for j, col in enumerate((1, 4)):
                    sl = slice(col * D, (col + 1) * D)
                    P.op('dve', (lambda mod=mod, sl=sl, j=j: nc.vector.scalar_tensor_tensor(
                        out=mod[:, sl], in0=mod[:, sl], scalar=1.0, in1=nw[:, j, :], op0=ALU.add, op1=ALU.mult)),
                        r=[mk, "nw"], w=[mk])

        def mv(mod, i):
            return mod[:, i * D:(i + 1) * D]

        def rms_mod(ph_rings, xs_ap, xs_key, A_ap, sh_ap, mod_keys, out_ap, out_key, eps_scale=1.0 / D):
            ss, sk = ph_rings['ss'].next()
            tmp, tk = ph_rings['hxtmp'].next()
            P.op('act', lambda: nc.scalar.activation(out=tmp[:], in_=xs_ap, func=AF.Square), r=[xs_key], w=[tk])
            P.op('dve', lambda: nc.vector.reduce_sum(out=ss[:, 0:1], in_=tmp[:], axis=AX.X), r=[tk], w=[sk])
            P.op('dve', lambda: nc.vector.tensor_scalar(out=ss[:, 1:2], in0=ss[:, 0:1], scalar1=eps_scale, scalar2=EPS,
                                                        op0=ALU.mult, op1=ALU.add), r=[sk], w=[sk])
            P.op('act', lambda: nc.scalar.activation(out=ss[:, 2:3], in_=ss[:, 1:2], func=AF.Sqrt), r=[sk], w=[sk])
            P.op('dve', lambda: nc.vector.reciprocal(out=ss[:, 3:4], in_=ss[:, 2:3]), r=[sk], w=[sk])
            P.op('dve', lambda: nc.vector.scalar_tensor_tensor(out=tmp[:], in0=xs_ap, scalar=ss[:, 3:4], in1=A_ap,
                                                               op0=ALU.mult, op1=ALU.mult),
                 r=[xs_key, sk] + mod_keys, w=[tk])
            if sh_ap is None:
                P.op('pool', lambda: nc.gpsimd.tensor_copy(out=out_ap, in_=tmp[:]), r=[tk], w=[out_key])
            else:
                P.op('pool', lambda: nc.gpsimd.tensor_tensor(out=out_ap, in0=tmp[:], in1=sh_ap, op=ALU.add),
                     r=[tk] + mod_keys, w=[out_key])

        def mk_rings(ph):
            return {'ss': Ring(nc, ph, "ss", 4, [128, 4], F32),
                    'hxtmp': Ring(nc, ph, "hxtmp", 2, [128, D], F32),
                    'xs': Ring(nc, ph, "xsr", 2, [128, D], F32)}

        xin_t = xin.rearrange("(t p) d -> t p d", p=128)
        XS1_t = XS1.rearrange("(t p) d -> t p d", p=128)
        XS2_t = XS2.rearrange("(t p) d -> t p d", p=128)
        XS3_t = XS3.rearrange("(t p) d -> t p d", p=128)
        out_t = out.rearrange("(t p) d -> t p d", p=128)

        with contextlib.ExitStack() as ph:
            adaln(0, ph)
            P.barrier()
        def dump(name, ap, keys):
            shape = list(ap.shape)
            t = nc.dram_tensor(name, shape, ap.dtype, kind="ExternalOutput").ap()
            P.dma('sp', t, ap, r=keys, w=["dbg_" + name])
        if stop_after == "ada":
            dump("dbg_modL", modL[:], ["modL"]); dump("dbg_modC", modC[:], ["modC"]); dump("dbg_rep", rep[:], ["rep"])
            P.finish()
            return nc
        with contextlib.ExitStack() as ph:
            R = mk_rings(ph)
            hx0 = sb("hx0", [128, 24, D], BF16, ph)
            bandsb = sb("bandsb", [128, NBAND, 512], BF16, ph)
            cbandsb = sb("cbandsb", [128, 8, 256], BF16, ph)
            wpl = sb("wpl", [128, 8, 256], BF16, ph)
            vecs = sb("vecs", [128, 2, D], F32, ph)
            AB = sb("AB", [128, 4, D], F32, ph)
            dT = Ring(nc, ph, "dT", 1, [128, 8, 512], BF16)
            yt = Ring(nc, ph, "yt", 2, [128, D], F32)
            P.dma('pool', cbandsb[:], cband, r=["d_cband"], w=["cbandsb"])
            P.dma('pool', wpl[:], w_pool.rearrange("g (c p) e -> p (g c) e", p=128), r=["d_wpool"], w=["wpl"])
            P.dma('sp', vecs[:, 0, :], pool_scale.partition_broadcast(128), r=["d_ps"], w=["vecs"])
            P.dma('sp', vecs[:, 1, :], b_pool.partition_broadcast(128), r=["d_bp"], w=["vecs"])
            for s, (mod, mkey) in enumerate(((modL, "modL"), (modC, "modC"))):
                P.op('dve', (lambda s=s, mod=mod: nc.vector.tensor_tensor(out=AB[:, 2 * s, :], in0=mv(mod, 2), in1=vecs[:, 0, :],
                                                                          op=ALU.mult)), r=[mkey, "vecs"], w=["AB"])
                P.op('dve', (lambda s=s: nc.vector.tensor_tensor(out=AB[:, 2 * s + 1, :], in0=AB[:, 2 * s, :], in1=vecs[:, 1, :],
                                                                 op=ALU.mult)), r=["AB", "vecs"], w=["AB"])

            def pool_segment(is_ctx, in_tiles, base, out_blocks):
                mod, mkey = (modC, "modC") if is_ctx else (modL, "modL")
                seq0 = 0 if is_ctx else NCT
                for lt in in_tiles:
                    xt, xk = R['xs'].next()
                    P.dma('sp', xt[:], xin_t[seq0 + lt], r=["d_xin"], w=[xk])
                    rms_mod(R, xt[:], xk, mv(mod, 1), mv(mod, 0), [mkey], hx0[:, lt - base, :], ("hx0", lt - base))
                cur_band = [None]
                for (ot0, ntl, btype) in out_blocks:
                    ncol = ntl * 128
                    if not is_ctx and cur_band[0] != btype:
                        P.dma('pool', bandsb[:], band[btype], r=["d_band"], w=["bandsb"])
                        cur_band[0] = btype
                    dt_, dk = dT.next()
                    for g in range(4):
                        for cc in range(2):
                            b = 2 * g + cc
                            if is_ctx:
                                lst = [(j, cbandsb[:, g * 2 + j, 0:ncol]) for j in range(2)]
                                bk = "cbandsb"
                            else:
                                lst = []
                                for j in JREL[g]:
                                    jt = ot0 + j
                                    if jt < 0 or jt >= NLT:
                                        continue
                                    lst.append((jt, bandsb[:, BAND_IDX[(g, j)], 0:ncol]))
                                bk = "bandsb"
                            for n_, (jt, rhs) in enumerate(lst):
                                P.op('pe', (lambda b=b, jt=jt, rhs=rhs, n_=n_, L=len(lst), ch=b, ncol=ncol: nc.tensor.matmul(
                                    PS[b][:, 0:ncol], hx0[:, jt - base, ch * 128:(ch + 1) * 128], rhs,
                                    start=(n_ == 0), stop=(n_ == L - 1))),
                                    r=[("hx0", jt - base), bk], w=[PK[b]])
                            if b % 2 == 0:
                                P.op('act', (lambda b=b, ncol=ncol, dt_=dt_: nc.scalar.copy(out=dt_[:, b, 0:ncol], in_=PS[b][:, 0:ncol])),
                                     r=[PK[b]], w=[(dk, b)])
                            else:
                                P.op('dve', (lambda b=b, ncol=ncol, dt_=dt_: nc.vector.tensor_copy(out=dt_[:, b, 0:ncol], in_=PS[b][:, 0:ncol])),
                                     r=[PK[b]], w=[(dk, b)])
                    for t in range(ntl):
                        gt = seq0 + ot0 + t
                        b0 = 2 * (t % 4)
                        for g in range(4):
                            pb = b0 + g // 2
                            for cc in range(2):
                                P.op('pe', (lambda pb=pb, g=g, cc=cc, t=t, dt_=dt_: nc.tensor.matmul(
                                    PS[pb][:, (g % 2) * 256:(g % 2) * 256 + 256], dt_[:, 2 * g + cc, t * 128:(t + 1) * 128],
                                    wpl[:, 2 * g + cc, :], start=(cc == 0), stop=(cc == 1))),
                                    r=[(dk, 2 * g + cc), "wpl"], w=[PK[pb]])
                        xt, xk = R['xs'].next()
                        P.dma('sp', xt[:], xin_t[gt], r=["d_xin"], w=[xk])
                        ai = 2 if is_ctx else 0
                        y, yk = yt.next()
                        for h in range(2):
                            sl = slice(h * 512, (h + 1) * 512)
                            P.op('dve', (lambda y=y, sl=sl, h=h, b0=b0, ai=ai: nc.vector.tensor_tensor(
                                out=y[:, sl], in0=PS[b0 + h], in1=AB[:, ai, sl], op=ALU.mult)), r=[PK[b0 + h], "AB"], w=[(yk, h)])
                            P.op('pool', (lambda y=y, sl=sl, xt=xt: nc.gpsimd.tensor_tensor(
                                out=y[:, sl], in0=y[:, sl], in1=xt[:, sl], op=ALU.add)), r=[(yk, h), xk], w=[(yk, h)])
                            P.op('pool', (lambda y=y, sl=sl, ai=ai: nc.gpsimd.tensor_tensor(
                                out=y[:, sl], in0=y[:, sl], in1=AB[:, ai + 1, sl], op=ALU.add)), r=[(yk, h), "AB"], w=[(yk, h)])
                        P.dma('sp', XS1_t[gt], y[:], r=[(yk, 0), (yk, 1)], w=["d_XS1"])

            pool_segment(True, [0, 1], 0, [(0, 2, 0)])
            if stop_after == "pool_dbg":
                dump("dbg_hx0", hx0[:, 0:2, :], [("hx0", 0), ("hx0", 1)])
                dump("dbg_dT", dT.tiles[0][:], [("dT0", b) for b in range(8)])
                dump("dbg_AB", AB[:], ["AB"])
                dump("dbg_cb", cbandsb[:], ["cbandsb"])
                dump("dbg_wpl", wpl[:], ["wpl"])
                P.finish()
                return nc
            for seg in range(4):
                lo = max(0, 16 * seg - 4); hi = min(NLT, 16 * seg + 20)
                blocks = [(4 * b, 4, 0 if b == 0 else (2 if b == 15 else 1)) for b in range(4 * seg, 4 * seg + 4)]
                pool_segment(False, list(range(lo, hi)), lo, blocks)
            P.flush()
        if stop_after == "pool":
            P.finish()
            return nc
        P.barrier()

        def moe_phase(layer, src_t, dst_t, tiles, final):
            SBT = 8 if final else 10
            with contextlib.ExitStack() as ph:
                R = mk_rings(ph)
                hxf = Ring(nc, ph, "hxf", 2, [128, D], F32)
                hxTf = Ring(nc, ph, "hxTf", 1, [128, 8, 128], F32)
                hxTb = sb("hxTb", [128, 8, SBT * 128], BF16, ph)
                acc = sb("acc", [128, SBT, D], F32, ph)
                gates = sb("gates", [128, SBT, 32], F32, ph)
                wrs = sb("wrs", [128, 8, 36], F32, ph)
                brb = sb("brb", [128, 36], F32, ph)
                rt = Ring(nc, ph, "rt", 2, [128, 96], F32)
                wg = Ring(nc, ph, "wg", 2, [128, 8, 512], BF16)
                wu = Ring(nc, ph, "wu", 2, [128, 8, 512], BF16)
                wd = Ring(nc, ph, "wd", 2, [128, 4, D], BF16)
                sgb = Ring(nc, ph, "sgb", 1, [128, 512], BF16)
                hidT = Ring(nc, ph, "hidT", 2, [128, 4, 512], BF16)
                nfb = None
                if final:
                    nfb = sb("nfb", [128, D], F32, ph)
                    P.dma('sp', nfb[:], norm_final.partition_broadcast(128), r=["d_nf"], w=["nfb"])
                P.dma('sp', wrs[:], w_r[layer].rearrange("(k p) n -> p k n", p=128), r=["d_wr"], w=["wrs"])
                P.dma('sp', brb[:], b_r[layer].partition_broadcast(128), r=["d_br"], w=["brb"])
                def route(i, r_, rk, lvl, sparse_info=None):
                    lg = r_[:, 0:36]; m4 = r_[:, 36:37]; nm4 = r_[:, 37:38]; e4 = r_[:, 40:44]; s4 = r_[:, 38:39]
                    pg = r_[:, 39:40]; ohg = r_[:, 44:48]; sel = r_[:, 48:56]; m8 = r_[:, 56:64]; d21 = r_[:, 64:65]
                    e21 = r_[:, 65:66]; w1 = r_[:, 66:67]; w2 = r_[:, 67:68]; c1 = r_[:, 72:80]; c2 = r_[:, 80:88]
                    V = nc.vector
                    def dv(fn, rk=rk):
                        P.op('dve', fn, r=[rk], w=[rk])
                    P.op('dve', lambda lg=lg: V.tensor_tensor(out=lg, in0=PS[5][:, 0:36], in1=brb[:], op=ALU.add),
                         r=[PK[5], "brb"], w=[rk])
                    if lvl <= 2:
                        P.op('dve', (lambda i=i, lg=lg: V.tensor_copy(out=gates[:, i, :], in_=lg[:, 0:32])), r=[rk], w=[("gates", i)])
                        return
                    dv(lambda: V.reduce_max(out=m4, in_=lg[:, 0:4], axis=AX.X))
                    dv(lambda: V.tensor_scalar(out=nm4, in0=m4, scalar1=-1.0, scalar2=None, op0=ALU.mult))
                    P.op('act', lambda: nc.scalar.activation(out=e4, in_=lg[:, 0:4], func=AF.Exp, bias=nm4, scale=1.0),
                         r=[rk], w=[rk])
                    dv(lambda: V.reduce_sum(out=s4, in_=e4, axis=AX.X))
                    dv(lambda: V.reciprocal(out=pg, in_=s4))
                    dv(lambda: V.tensor_scalar(out=ohg, in0=lg[:, 0:4], scalar1=m4, scalar2=None, op0=ALU.is_equal))
                    dv(lambda: V.tensor_scalar(out=sel, in0=lg[:, 4:12], scalar1=ohg[:, 0:1], scalar2=None, op0=ALU.mult))
                    for g in range(1, 4):
                        dv(lambda g=g: V.scalar_tensor_tensor(out=sel, in0=lg[:, 4 + 8 * g:12 + 8 * g], scalar=ohg[:, g:g + 1],
                                                              in1=sel, op0=ALU.mult, op1=ALU.add))
                    dv(lambda: V.max(out=m8, in_=sel))
                    dv(lambda: V.tensor_tensor(out=d21, in0=m8[:, 1:2], in1=m8[:, 0:1], op=ALU.subtract))
                    P.op('act', lambda: nc.scalar.activation(out=e21, in_=d21, func=AF.Exp), r=[rk], w=[rk])
                    dv(lambda: V.tensor_scalar(out=e21, in0=e21, scalar1=1.0, scalar2=None, op0=ALU.add))
                    dv(lambda: V.reciprocal(out=w1, in_=e21))
                    dv(lambda: V.tensor_tensor(out=w1, in0=w1, in1=pg, op=ALU.mult))
                    dv(lambda: V.tensor_tensor(out=w2, in0=pg, in1=w1, op=ALU.subtract))
                    if sparse_info is not None:
                        sparse_info(r_, rk, sel, m8, ohg, w1, w2, dv)
                        return
                    dv(lambda: V.tensor_scalar(out=c1, in0=sel, scalar1=m8[:, 0:1], scalar2=w1, op0=ALU.is_equal, op1=ALU.mult))
                    dv(lambda: V.tensor_scalar(out=c2, in0=sel, scalar1=m8[:, 1:2], scalar2=w2, op0=ALU.is_equal, op1=ALU.mult))
                    dv(lambda: V.tensor_tensor(out=c1, in0=c1, in1=c2, op=ALU.add))
                    P.op('dve', (lambda i=i, c1=c1, ohg=ohg: V.tensor_tensor(
                        out=gates[:, i, :].rearrange("p (g e) -> p g e", g=4),
                        in0=c1.unsqueeze(1).to_broadcast([128, 4, 8]), in1=ohg.unsqueeze(2).to_broadcast([128, 4, 8]),
                        op=ALU.mult)), r=[rk], w=[("gates", i)])
                for s0 in range(0, len(tiles), SBT):
                    sbt = tiles[s0:s0 + SBT]
                    import os
                    if os.environ.get("MOE_NT"):
                        sbt = sbt[:int(os.environ["MOE_NT"])]
                    n_sb = len(sbt)
                    for i, gt in enumerate(sbt):
                        mod, mkey = (modC, "modC") if gt < NCT else (modL, "modL")
                        xt, xk = R['xs'].next()
                        P.dma('sp', xt[:], src_t[gt], r=["d_XS1" if layer == 0 else "d_XS3"], w=[xk])
                        hx, hk = hxf.next()
                        rms_mod(R, xt[:], xk, mv(mod, 4), mv(mod, 3), [mkey], hx[:], hk)
                        import os
                        if int(os.environ.get("MOE_LVL", "9")) == 0:
                            dump("dbg_hx%d" % i, hx[:], [hk])
                            continue
                        for k in range(8):
                            b = 6 + k // 4
                            P.op('pe', (lambda b=b, k=k, hx=hx: nc.tensor.transpose(
                                PS[b][:, (k % 4) * 128:(k % 4) * 128 + 128], hx[:, k * 128:(k + 1) * 128], ident)),
                                r=[hk, "msk"], w=[PK[b]])
                        hT, hTk = hxTf.next()
                        for h in range(2):
                            b = 6 + h
                            P.op('act', (lambda b=b, h=h, hT=hT: nc.scalar.copy(
                                out=hT[:, 4 * h:4 * h + 4, :], in_=PS[b].rearrange("p (k t) -> p k t", k=4))),
                                r=[PK[b]], w=[hTk])
                        P.op('pool', (lambda i=i, hT=hT: nc.gpsimd.tensor_copy(out=hxTb[:, :, i * 128:(i + 1) * 128], in_=hT[:])),
                             r=[hTk], w=[("hxTb", i)])
                        import os
                        lvl = int(os.environ.get("MOE_LVL", "9"))
                        if lvl <= 1:
                            continue
                        for k in range(8):
                            P.op('pe', (lambda k=k, hT=hT: nc.tensor.matmul(PS[5][:, 0:36], hT[:, k, :], wrs[:, k, :],
                                                                          start=(k == 0), stop=(k == 7))),
                                 r=[hTk, "wrs"], w=[PK[5]])
                        r_, rk = rt.next()
                        route(i, r_, rk, lvl)
                    if stop_after == "moe_b1":
                        lvl = int(os.environ.get("MOE_LVL", "9"))
                        if lvl == 0:
                            P.finish()
                            return "STOP"
                        if lvl > 1:
                            dump("dbg_rt0", rt.tiles[0][:], [rt.name + "0"]); dump("dbg_rt1", rt.tiles[1][:], [rt.name + "1"])
                            dump("dbg_gates", gates[:], [("gates", i) for i in range(n_sb)])
                        if os.environ.get("NO_HXTB") is None:
                            dump("dbg_hxTb", hxTb[:], [("hxTb", i) for i in range(n_sb)])
                        else:
                            dump("dbg_hxTf", hxTf.tiles[0][:], [hxTf.name + "0"])
                        P.finish()
                        return "STOP"
                    blocks = [(t0, min(4, n_sb - t0)) for t0 in range(0, n_sb, 4)]
                    hcn = 0
                    yn = 0
                    for e in range(32):
                        wgt, wgk = wg.next(); wut, wuk = wu.next(); wdt, wdk = wd.next()
                        P.dma('pool', wgt[:], w_e_gate[layer, e].rearrange("(k p) n -> p k n", p=128), r=["d_weg"], w=[wgk])
                        P.dma('pool', wut[:], w_e_up[layer, e].rearrange("(k p) n -> p k n", p=128), r=["d_weu"], w=[wuk])
                        P.dma('pool', wdt[:], w_e_down[layer, e].rearrange("(k p) n -> p k n", p=128), r=["d_wed"], w=[wdk])
                        for (t0, ntl) in blocks:
                            ncol = ntl * 128
                            cs = slice(t0 * 128, t0 * 128 + ncol)
                            rk_h = [("hxTb", t0 + j) for j in range(ntl)]
                            hid, hidk = hidT.next()
                            for hc in range(4):
                                gb = (hcn % 2) * 2; ub = gb + 1; hcn += 1
                                for k in range(8):
                                    P.op('pe', (lambda gb=gb, k=k, hc=hc, wgt=wgt, cs=cs, ncol=ncol: nc.tensor.matmul(
                                        PS[gb][:, 0:ncol], wgt[:, k, hc * 128:(hc + 1) * 128], hxTb[:, k, cs],
                                        start=(k == 0), stop=(k == 7))), r=[wgk] + rk_h, w=[PK[gb]])
                                for k in range(8):
                                    P.op('pe', (lambda ub=ub, k=k, hc=hc, wut=wut, cs=cs, ncol=ncol: nc.tensor.matmul(
                                        PS[ub][:, 0:ncol], wut[:, k, hc * 128:(hc + 1) * 128], hxTb[:, k, cs],
                                        start=(k == 0), stop=(k == 7))), r=[wuk] + rk_h, w=[PK[ub]])
                                sg, sgk = sgb.next()
                                P.op('act', (lambda sg=sg, gb=gb, ncol=ncol: nc.scalar.activation(
                                    out=sg[:, 0:ncol], in_=PS[gb][:, 0:ncol], func=AF.Silu)), r=[PK[gb]], w=[sgk])
                                P.op('dve', (lambda sg=sg, ub=ub, ncol=ncol, hid=hid, hc=hc: nc.vector.tensor_tensor(
                                    out=hid[:, hc, 0:ncol], in0=sg[:, 0:ncol], in1=PS[ub][:, 0:ncol], op=ALU.mult)),
                                    r=[sgk, PK[ub]], w=[(hidk, hc)])
                            for t in range(ntl):
                                i = t0 + t
                                for half in range(2):
                                    yb = 4 + yn % 2; yn += 1
                                    for hc in range(4):
                                        P.op('pe', (lambda yb=yb, hc=hc, t=t, half=half, hid=hid, wdt=wdt: nc.tensor.matmul(
                                            PS[yb], hid[:, hc, t * 128:(t + 1) * 128], wdt[:, hc, half * 512:(half + 1) * 512],
                                            start=(hc == 0), stop=(hc == 3))), r=[(hidk, hc), wdk], w=[PK[yb]])
                                    sl = slice(half * 512, (half + 1) * 512)
                                    if e == 0:
                                        P.op('dve', (lambda yb=yb, i=i, sl=sl, e=e: nc.vector.tensor_scalar(
                                            out=acc[:, i, sl], in0=PS[yb], scalar1=gates[:, i, e:e + 1], scalar2=None, op0=ALU.mult)),
                                            r=[PK[yb], ("gates", i)], w=[("acc", i, half)])
                                    else:
                                        P.op('dve', (lambda yb=yb, i=i, sl=sl, e=e: nc.vector.scalar_tensor_tensor(
                                            out=acc[:, i, sl], in0=PS[yb], scalar=gates[:, i, e:e + 1], in1=acc[:, i, sl],
                                            op0=ALU.mult, op1=ALU.add)), r=[PK[yb], ("gates", i), ("acc", i, half)], w=[("acc", i, half)])
                    for i, gt in enumerate(sbt):
                        mod, mkey = (modC, "modC") if gt < NCT else (modL, "modL")
                        xt, xk = R['xs'].next()
                        P.dma('sp', xt[:], src_t[gt], r=["d_XS1" if layer == 0 else "d_XS3"], w=[xk])
                        P.op('pool', (lambda i=i, mod=mod: nc.gpsimd.tensor_tensor(out=acc[:, i, :], in0=acc[:, i, :], in1=mv(mod, 5), op=ALU.mult)),
                             r=[("acc", i, 0), ("acc", i, 1), mkey], w=[("acc", i, 0), ("acc", i, 1)])
                        P.op('pool', (lambda i=i, xt=xt: nc.gpsimd.tensor_tensor(out=acc[:, i, :], in0=acc[:, i, :], in1=xt[:], op=ALU.add)),
                             r=[("acc", i, 0), ("acc", i, 1), xk], w=[("acc", i, 0), ("acc", i, 1)])
                        if not final:
                            P.dma('sp', dst_t[gt], acc[:, i, :], r=[("acc", i, 0), ("acc", i, 1)], w=["d_dst%d" % layer])
                        else:
                            o_, ok = hxf.next()
                            rms_mod(R, acc[:, i, :], ("acc", i, 0), nfb[:], None, ["nfb", ("acc", i, 1)], o_[:], ok)
                            P.dma('sp', dst_t[gt - NCT], o_[:], r=[ok], w=["d_out"])
                P.barrier()

        I32 = mybir.dt.int32
        NTS = 65
        NSLOT = NTS * 512
        HXB = dscr("HXB", [NT * 128, D], BF16); XG = dscr("XG", [NSLOT, D], BF16)
        WG = dscr("WG", [NSLOT, 1]); YG = dscr("YG", [NSLOT, D])
        HXB_t = HXB.rearrange("(t p) d -> t p d", p=128)
        W2 = {"g": w_e_gate.rearrange("l e (p k) n -> (l e p) (k n)", k=8), "u": w_e_up.rearrange("l e (p k) n -> (l e p) (k n)", k=8),
              "d": w_e_down.rearrange("l e d n -> (l e d) n")}

        def moe_sparse(layer, src_t, dst_t, tiles, final):
            T_ = len(tiles)
            skey = "d_XS1" if layer == 0 else "d_XS3"
            es2 = contextlib.ExitStack()
            with es2:
                cms = sb("cms", [128, 160], F32, es2)
                info = sb("info", [128, NT, 8], F32, es2)
                posi = sb("posi", [128, NT, 2], I32, es2)
                cum = sb("cum", [128, 32], F32, es2)
                offs = sb("offs", [128, 32], F32, es2)
                te = sb("te", [128, 80], F32, es2)
                P.dma('sp', cms[:], cmisc, r=["d_cm"], w=["cms"])
                P.op('pool', lambda: nc.gpsimd.memset(cum[:], 0.0), r=[], w=["cum"])
                iota = cms[:, 0:32]; base = cms[:, 32:40]; svals = cms[:, 40:120]
                V = nc.vector; G = nc.gpsimd; A = nc.scalar
                with contextlib.ExitStack() as ph:
                    R = mk_rings(ph)
                    hxf = Ring(nc, ph, "hxf", 2, [128, D], F32)
                    hxb = Ring(nc, ph, "hxb", 2, [128, D], BF16)
                    hxTf = Ring(nc, ph, "hxTf", 2, [128, 8, 128], F32)
                    wrs = sb("wrs", [128, 8, 36], F32, ph); brb = sb("brb", [128, 36], F32, ph)
                    rt = Ring(nc, ph, "rt", 2, [128, 288], F32)
                    gates = None
                    P.dma('sp', wrs[:], w_r[layer].rearrange("(k p) n -> p k n", p=128), r=["d_wr"], w=["wrs"])
                    P.dma('sp', brb[:], b_r[layer].partition_broadcast(128), r=["d_br"], w=["brb"])

                    def route(i, r_, rk):
                        lg = r_[:, 0:36]; m4 = r_[:, 36:37]; nm4 = r_[:, 37:38]; e4 = r_[:, 40:44]; s4 = r_[:, 38:39]
                        pg = r_[:, 39:40]; ohg = r_[:, 44:48]; sel = r_[:, 48:56]; m8 = r_[:, 56:64]; d21 = r_[:, 64:65]
                        e21 = r_[:, 65:66]; w1 = r_[:, 66:67]; w2 = r_[:, 67:68]; eq = r_[:, 72:88]
                        oh1 = r_[:, 96:128]; oh2 = r_[:, 128:160]; ohs = r_[:, 160:192]; rkt = r_[:, 192:224]; tmp = r_[:, 224:256]

                        def dv(fn):
                            P.op('dve', fn, r=[rk], w=[rk])
                        P.op('dve', lambda: V.tensor_tensor(out=lg, in0=PS[5][:, 0:36], in1=brb[:], op=ALU.add), r=[PK[5], "brb"], w=[rk])
                        dv(lambda: V.reduce_max(out=m4, in_=lg[:, 0:4], axis=AX.X))
                        dv(lambda: V.tensor_scalar(out=nm4, in0=m4, scalar1=-1.0, scalar2=None, op0=ALU.mult))
                        P.op('act', lambda: A.activation(out=e4, in_=lg[:, 0:4], func=AF.Exp, bias=nm4, scale=1.0), r=[rk], w=[rk])
                        dv(lambda: V.reduce_sum(out=s4, in_=e4, axis=AX.X))
                        dv(lambda: V.reciprocal(out=pg, in_=s4))
                        dv(lambda: V.tensor_scalar(out=ohg, in0=lg[:, 0:4], scalar1=m4, scalar2=None, op0=ALU.is_equal))
                        dv(lambda: V.tensor_scalar(out=sel, in0=lg[:, 4:12], scalar1=ohg[:, 0:1], scalar2=None, op0=ALU.mult))
                        for g in range(1, 4):
                            dv(lambda g=g: V.scalar_tensor_tensor(out=sel, in0=lg[:, 4 + 8 * g:12 + 8 * g], scalar=ohg[:, g:g + 1],
                                                                  in1=sel, op0=ALU.mult, op1=ALU.add))
                        dv(lambda: V.max(out=m8, in_=sel))
                        dv(lambda: V.tensor_tensor(out=d21, in0=m8[:, 1:2], in1=m8[:, 0:1], op=ALU.subtract))
                        P.op('act', lambda: A.activation(out=e21, in_=d21, func=AF.Exp), r=[rk], w=[rk])
                        dv(lambda: V.tensor_scalar(out=e21, in0=e21, scalar1=1.0, scalar2=None, op0=ALU.add))
                        dv(lambda: V.reciprocal(out=w1, in_=e21))
                        dv(lambda: V.tensor_tensor(out=info[:, i, 2:3], in0=w1, in1=pg, op=ALU.mult))
                        dv(lambda: V.tensor_tensor(out=info[:, i, 5:6], in0=pg, in1=info[:, i, 2:3], op=ALU.subtract))
                        dv(lambda: V.tensor_scalar(out=eq[:, 0:8], in0=sel, scalar1=m8[:, 0:1], scalar2=None, op0=ALU.is_equal))
                        dv(lambda: V.tensor_scalar(out=eq[:, 8:16], in0=sel, scalar1=m8[:, 1:2], scalar2=None, op0=ALU.is_equal))
                        for j, oh in enumerate((oh1, oh2)):
                            dv(lambda j=j, oh=oh: V.tensor_tensor(out=oh.rearrange("p (g e) -> p g e", g=4),
                                                                  in0=eq[:, 8 * j:8 * j + 8].unsqueeze(1).to_broadcast([128, 4, 8]),
                                                                  in1=ohg.unsqueeze(2).to_broadcast([128, 4, 8]), op=ALU.mult))
                        dv(lambda: V.tensor_tensor(out=ohs, in0=oh1, in1=oh2, op=ALU.add))
                        P.op('pe', lambda: nc.tensor.matmul(PS[4][:, 0:32], msk[:, 6, :], ohs, start=True, stop=True), r=[rk, "msk"], w=[PK[4]])
                        P.op('pe', lambda: nc.tensor.matmul(PS[4][:, 32:64], msk[:, 3, :], ohs, start=True, stop=True), r=[rk, "msk"], w=[PK[4]])
                        P.op('dve', lambda: V.tensor_tensor(out=rkt, in0=PS[4][:, 0:32], in1=cum[:], op=ALU.add), r=[PK[4], "cum", rk], w=[rk])
                        P.op('dve', lambda: V.tensor_tensor(out=cum[:], in0=cum[:], in1=PS[4][:, 32:64], op=ALU.add), r=[PK[4], "cum", rk], w=["cum"])
                        for j, oh in enumerate((oh1, oh2)):
                            dv(lambda oh=oh: V.tensor_tensor(out=tmp, in0=oh, in1=rkt, op=ALU.mult))
                            dv(lambda j=j: V.reduce_sum(out=info[:, i, 3 * j + 1:3 * j + 2], in_=tmp, axis=AX.X))
                            dv(lambda oh=oh: V.tensor_tensor(out=tmp, in0=oh, in1=iota, op=ALU.mult))
                            dv(lambda j=j: V.reduce_sum(out=info[:, i, 3 * j:3 * j + 1], in_=tmp, axis=AX.X))

                    for i, gt in enumerate(tiles):
                        mod, mkey = (modC, "modC") if gt < NCT else (modL, "modL")
                        xt, xk = R['xs'].next()
                        P.dma('sp', xt[:], src_t[gt], r=[skey], w=[xk])
                        hx, hk = hxf.next()
                        rms_mod(R, xt[:], xk, mv(mod, 4), mv(mod, 3), [mkey], hx[:], hk)
                        hb, hbk = hxb.next()
                        P.op('pool', (lambda hb=hb, hx=hx: G.tensor_copy(out=hb[:], in_=hx[:])), r=[hk], w=[hbk])
                        P.dma('sp', HXB_t[gt], hb[:], r=[hbk], w=["d_HXB"])
                        for k in range(8):
                            b = 6 + k // 4
                            P.op('pe', (lambda b=b, k=k, hx=hx: nc.tensor.transpose(
                                PS[b][:, (k % 4) * 128:(k % 4) * 128 + 128], hx[:, k * 128:(k + 1) * 128], ident)),
                                r=[hk, "msk"], w=[PK[b]])
                        hT, hTk = hxTf.next()
                        for h in range(2):
                            b = 6 + h
                            P.op('act', (lambda b=b, h=h, hT=hT: A.copy(
                                out=hT[:, 4 * h:4 * h + 4, :], in_=PS[b].rearrange("p (k t) -> p k t", k=4))), r=[PK[b]], w=[hTk])
                        for k in range(8):
                            P.op('pe', (lambda k=k, hT=hT: nc.tensor.matmul(PS[5][:, 0:36], hT[:, k, :], wrs[:, k, :],
                                                                          start=(k == 0), stop=(k == 7))), r=[hTk, "wrs"], w=[PK[5]])
                        r_, rk = rt.next()
                        route(i, r_, rk)
                        P.flush()
                    ci = sb("ci", [128, 32], I32, ph); pn = sb("pn", [128, 32], F32, ph)
                    sa = sb("sa", [128, 32], F32, ph); sb_ = sb("sb_", [128, 32], F32, ph)
                    P.op('dve', lambda: V.tensor_copy(out=ci[:], in_=cum[:]), r=["cum"], w=["ci"])
                    P.op('dve', lambda: V.tensor_scalar(out=ci[:], in0=ci[:], scalar1=511, scalar2=None, op0=ALU.add), r=["ci"], w=["ci"])
                    P.op('dve', lambda: V.tensor_scalar(out=ci[:], in0=ci[:], scalar1=9, scalar2=None, op0=ALU.arith_shift_right), r=["ci"], w=["ci"])
                    P.op('dve', lambda: V.tensor_scalar(out=ci[:], in0=ci[:], scalar1=9, scalar2=None, op0=ALU.logical_shift_left), r=["ci"], w=["ci"])
                    P.op('dve', lambda: V.tensor_copy(out=pn[:], in_=ci[:]), r=["ci"], w=["pn"])
                    P.op('dve', lambda: V.tensor_copy(out=sa[:], in_=pn[:]), r=["pn"], w=["sa"])
                    a_, b_ = sa, sb_
                    ak, bk = "sa", "sb_"
                    for sh in (1, 2, 4, 8, 16):
                        P.op('dve', (lambda a_=a_, b_=b_, sh=sh: V.tensor_copy(out=b_[:, 0:sh], in_=a_[:, 0:sh])), r=[ak], w=[bk])
                        P.op('dve', (lambda a_=a_, b_=b_, sh=sh: V.tensor_tensor(out=b_[:, sh:32], in0=a_[:, sh:32], in1=a_[:, 0:32 - sh], op=ALU.add)),
                             r=[ak, bk], w=[bk])
                        a_, b_, ak, bk = b_, a_, bk, ak
                    incl, inclk = a_, ak
                    P.op('dve', lambda: V.tensor_tensor(out=offs[:], in0=incl[:], in1=pn[:], op=ALU.subtract), r=[inclk, "pn"], w=["offs"])
                    P.op('pool', lambda: G.memset(te[:], 0.0), r=[], w=["te"])
                    for e in range(32):
                        P.op('dve', (lambda e=e: V.scalar_tensor_tensor(out=te[:], in0=svals, scalar=incl[:, e:e + 1], in1=te[:], op0=ALU.is_ge, op1=ALU.add)),
                             r=[inclk, "te", "cms"], w=["te"])
                    P.op('dve', lambda: V.tensor_scalar(out=te[:], in0=te[:], scalar1=31.0, scalar2=None, op0=ALU.min), r=["te"], w=["te"])
                    P.barrier()
                with contextlib.ExitStack() as ph:
                    zt = sb("zt", [128, 4096], BF16, ph)
                    hb2 = Ring(nc, ph, "hb2", 3, [128, D], BF16)
                    pt = Ring(nc, ph, "pt", 2, [128, 72], F32)
                    P.op('pool', lambda: G.memset(zt[:], 0.0), r=[], w=["zt"])
                    XGz = XG.rearrange("(q p f) d -> q p (f d)", p=128, f=4)
                    for q in range(NTS):
                        P.dma('sp', XGz[q], zt[:], r=["zt"], w=["d_XG"])
                    for i, gt in enumerate(tiles):
                        p_, pk_ = pt.next()
                        for j in range(2):
                            P.op('dve', (lambda p_=p_, i=i, j=j: V.tensor_scalar(out=p_[:, 0:32], in0=iota, scalar1=info[:, i, 3 * j:3 * j + 1], scalar2=None, op0=ALU.is_equal)),
                                 r=["info", "cms", pk_], w=[pk_])
                            P.op('dve', (lambda p_=p_: V.tensor_tensor(out=p_[:, 0:32], in0=p_[:, 0:32], in1=offs[:], op=ALU.mult)), r=[pk_, "offs"], w=[pk_])
                            P.op('dve', (lambda p_=p_, j=j: V.reduce_sum(out=p_[:, 32 + j:33 + j], in_=p_[:, 0:32], axis=AX.X)), r=[pk_], w=[pk_])
                            P.op('dve', (lambda p_=p_, i=i, j=j: V.tensor_tensor(out=p_[:, 32 + j:33 + j], in0=p_[:, 32 + j:33 + j], in1=info[:, i, 3 * j + 1:3 * j + 2], op=ALU.add)),
                                 r=[pk_, "info"], w=[pk_])
                        P.op('dve', (lambda p_=p_, i=i: V.tensor_copy(out=posi[:, i, :], in_=p_[:, 32:34])), r=[pk_], w=[("posi", i)])
                        hb, hbk = hb2.next()
                        P.dma('sp', hb[:], HXB_t[gt], r=["d_HXB"], w=[hbk])
                        for j in range(2):
                            P.op('pool', (lambda hb=hb, i=i, j=j: G.indirect_dma_start(
                                out=XG[:, :], out_offset=bass.IndirectOffsetOnAxis(ap=posi[:, i, j:j + 1], axis=0), in_=hb[:, :], in_offset=None)),
                                r=[hbk, ("posi", i), "d_XG"], w=["d_XGs"], dma=True)
                            P.op('pool', (lambda i=i, j=j: G.indirect_dma_start(
                                out=WG[:, :], out_offset=bass.IndirectOffsetOnAxis(ap=posi[:, i, j:j + 1], axis=0), in_=info[:, i, 3 * j + 2:3 * j + 3], in_offset=None)),
                                r=["info", ("posi", i)], w=["d_WG"], dma=True)
                        P.flush()
                    P.barrier()
                with contextlib.ExitStack() as ph:
                    wg = Ring(nc, ph, "wg", 2, [128, 8, 512], BF16); wu = Ring(nc, ph, "wu", 2, [128, 8, 512], BF16)
                    wd = Ring(nc, ph, "wd", 2, [128, 4, D], BF16)
                    xg = Ring(nc, ph, "xg", 2, [128, 4, D], BF16); xT = Ring(nc, ph, "xT", 2, [128, 8, 512], BF16)
                    wgt = Ring(nc, ph, "wgt", 2, [128, 4], F32)
                    idf = Ring(nc, ph, "idf", 2, [128, 12], F32); idi = Ring(nc, ph, "idi", 2, [128, 12], I32)
                    sgb = Ring(nc, ph, "sgb", 2, [128, 512], BF16); hidT = Ring(nc, ph, "hidT", 2, [128, 4, 512], BF16)
                    yg = Ring(nc, ph, "yg", 2, [128, D], F32)
                    hcn = [0]; yn = [0]

                    def slot_tile(s_):
                        f_, fk = idf.next(); ii, ik = idi.next()
                        P.op('dve', lambda: V.scalar_tensor_tensor(out=f_[:, 0:1], in0=te[:, s_:s_ + 1], scalar=128.0, in1=base[:, 0:1],
                                                                   op0=ALU.mult, op1=ALU.add), r=["te", "cms"], w=[fk])
                        P.op('dve', lambda: V.scalar_tensor_tensor(out=f_[:, 8:12], in0=te[:, s_:s_ + 1].to_broadcast([128, 4]), scalar=512.0, in1=base[:, 0:4],
                                                                   op0=ALU.mult, op1=ALU.add), r=["te", "cms", fk], w=[fk])
                        if layer > 0:
                            P.op('dve', lambda: V.tensor_scalar(out=f_[:, 0:1], in0=f_[:, 0:1], scalar1=float(layer * 32 * 128), scalar2=None, op0=ALU.add), r=[fk], w=[fk])
                            P.op('dve', lambda: V.tensor_scalar(out=f_[:, 8:12], in0=f_[:, 8:12], scalar1=float(layer * 32 * 512), scalar2=None, op0=ALU.add), r=[fk], w=[fk])
                        P.op('dve', lambda: V.tensor_copy(out=ii[:], in_=f_[:]), r=[fk], w=[ik])
                        wgt_, wgk = wg.next(); wut, wuk = wu.next(); wdt, wdk = wd.next()
                        P.op('pool', lambda: G.indirect_dma_start(out=wgt_[:].rearrange("p k n -> p (k n)"), out_offset=None, in_=W2["g"],
                                                                  in_offset=bass.IndirectOffsetOnAxis(ap=ii[:, 0:1], axis=0)),
                             r=[ik], w=[(wgk, k) for k in range(8)], dma=True)
                        P.op('pool', lambda: G.indirect_dma_start(out=wut[:].rearrange("p k n -> p (k n)"), out_offset=None, in_=W2["u"],
                                                                  in_offset=bass.IndirectOffsetOnAxis(ap=ii[:, 0:1], axis=0)),
                             r=[ik], w=[(wuk, k) for k in range(8)], dma=True)
                        for k in range(4):
                            P.op('pool', (lambda k=k: G.indirect_dma_start(out=wdt[:, k, :], out_offset=None, in_=W2["d"],
                                                                         in_offset=bass.IndirectOffsetOnAxis(ap=ii[:, 8 + k:9 + k], axis=0))),
                                 r=[ik], w=[(wdk, k)], dma=True)
                        x_, xk_ = xg.next(); xt_, xtk = xT.next(); g4, g4k = wgt.next()
                        P.dma('sp', x_[:], XG[s_ * 512:(s_ + 1) * 512, :].rearrange("(t p) d -> p t d", p=128), r=["d_XGs", "d_XG"], w=[xk_])
                        P.dma('sp', g4[:], WG[s_ * 512:(s_ + 1) * 512, :].rearrange("(t p) o -> p (t o)", p=128), r=["d_WG"], w=[g4k], allow_slow_non_contiguous=True)
                        for t in range(4):
                            b = 6 + t % 2
                            psb = PS[b].bitcast(BF16)
                            for k in range(8):
                                P.op('pe', (lambda psb=psb, k=k, t=t: nc.tensor.transpose(psb[:, k * 128:(k + 1) * 128], x_[:, t, k:D:8], identb[:])),
                                     r=[xk_, "identb"], w=[PK[b]])
                            P.op('act', (lambda psb=psb, t=t: A.copy(out=xt_[:, :, t * 128:(t + 1) * 128], in_=psb.rearrange("p (k c) -> p k c", k=8))),
                                 r=[PK[b]], w=[(xtk, t)])
                        xtkeys = [(xtk, t) for t in range(4)]
                        hid, hidk = hidT.next()
                        for hc in range(4):
                            gb = (hcn[0] % 2) * 2; ub = gb + 1; hcn[0] += 1
                            for k in range(8):
                                P.op('pe', (lambda gb=gb, k=k, hc=hc: nc.tensor.matmul(PS[gb], wgt_[:, k, hc * 128:(hc + 1) * 128], xt_[:, k, :],
                                                                                    start=(k == 0), stop=(k == 7))), r=[(wgk, k)] + xtkeys, w=[PK[gb]])
                            for k in range(8):
                                P.op('pe', (lambda ub=ub, k=k, hc=hc: nc.tensor.matmul(PS[ub], wut[:, k, hc * 128:(hc + 1) * 128], xt_[:, k, :],
                                                                                    start=(k == 0), stop=(k == 7))), r=[(wuk, k)] + xtkeys, w=[PK[ub]])
                            sg, sgk = sgb.next()
                            P.op('act', (lambda sg=sg, gb=gb: A.activation(out=sg[:], in_=PS[gb], func=AF.Silu)), r=[PK[gb]], w=[sgk])
                            P.op('dve', (lambda sg=sg, ub=ub, hc=hc: V.tensor_tensor(out=hid[:, hc, :], in0=sg[:], in1=PS[ub], op=ALU.mult)),
                                 r=[sgk, PK[ub]], w=[(hidk, hc)])
                        for t in range(4):
                            y_, yk = yg.next()
                            for half in range(2):
                                yb_ = 4 + yn[0] % 2; yn[0] += 1
                                for hc in range(4):
                                    P.op('pe', (lambda yb_=yb_, hc=hc, t=t, half=half: nc.tensor.matmul(
                                        PS[yb_], hid[:, hc, t * 128:(t + 1) * 128], wdt[:, hc, half * 512:(half + 1) * 512],
                                        start=(hc == 0), stop=(hc == 3))), r=[(hidk, hc), (wdk, hc)], w=[PK[yb_]])
                                P.op('act', (lambda yb_=yb_, half=half, t=t, y_=y_: A.activation(out=y_[:, half * 512:(half + 1) * 512], in_=PS[yb_], func=AF.Copy,
                                                                                            scale=g4[:, t:t + 1])), r=[PK[yb_], g4k], w=[(yk, half)])
                            P.dma('sp', YG[s_ * 512 + t * 128:s_ * 512 + (t + 1) * 128, :], y_[:], r=[(yk, 0), (yk, 1)], w=["d_YG"])

                    for s_ in range(NTS):
                        slot_tile(s_)
                        P.flush()
                    P.barrier()
                with contextlib.ExitStack() as ph:
                    R = mk_rings(ph)
                    y1 = Ring(nc, ph, "y1", 2, [128, D], F32); y2 = Ring(nc, ph, "y2", 2, [128, D], F32)
                    ofin = Ring(nc, ph, "ofin", 2, [128, D], F32)
                    nfb = None
                    if final:
                        nfb = sb("nfb", [128, D], F32, ph)
                        P.dma('sp', nfb[:], norm_final.partition_broadcast(128), r=["d_nf"], w=["nfb"])

                    def comb(i, gt):
                        mod, mkey = (modC, "modC") if gt < NCT else (modL, "modL")
                        a_, ak_ = y1.next(); b_, bk_ = y2.next()
                        for j, (dst, dk_) in enumerate(((a_, ak_), (b_, bk_))):
                            P.op('pool', (lambda dst=dst, j=j: G.indirect_dma_start(out=dst[:, :], out_offset=None, in_=YG[:, :],
                                                                                  in_offset=bass.IndirectOffsetOnAxis(ap=posi[:, i, j:j + 1], axis=0))),
                                 r=[("posi", i), "d_YG"], w=[dk_], dma=True)
                        xt, xk = R['xs'].next()
                        P.dma('sp', xt[:], src_t[gt], r=[skey], w=[xk])
                        P.op('pool', lambda: G.tensor_tensor(out=a_[:], in0=a_[:], in1=b_[:], op=ALU.add), r=[ak_, bk_], w=[ak_])
                        P.op('dve', lambda: V.tensor_tensor(out=a_[:], in0=a_[:], in1=mv(mod, 5), op=ALU.mult), r=[ak_, mkey], w=[ak_])
                        P.op('pool', lambda: G.tensor_tensor(out=a_[:], in0=a_[:], in1=xt[:], op=ALU.add), r=[ak_, xk], w=[ak_])
                        if not final:
                            P.dma('sp', dst_t[gt], a_[:], r=[ak_], w=["d_dst%d" % layer])
                        else:
                            o_, ok = ofin.next()
                            rms_mod(R, a_[:], ak_, nfb[:], None, ["nfb"], o_[:], ok)
                            P.dma('sp', dst_t[gt - NCT], o_[:], r=[ok], w=["d_out"])

                    for i, gt in enumerate(tiles):
                        comb(i, gt)
                    P.barrier()

        import os
        MOE = moe_sparse if os.environ.get("DENSE_MOE") is None else moe_phase
        if MOE(0, XS1_t, XS2_t, list(range(NT)), False) == "STOP":
            return nc
        if stop_after == "moe0":
            P.finish()
            return nc

        bfd = lambda name, shape: dscr(name, shape, BF16)
        QT_d = bfd("QT_d", [NT, 128, 8, 128]); KT_d = bfd("KT_d", [NT, 128, 8, 128])
        KK_d = bfd("KK_d", [NT, 128, 8, 128]); VV_d = bfd("VV_d", [NT, 128, 8, 128])
        ZZ_d = bfd("ZZ_d", [NT, 128, D]); GB_d = dscr("GB_d", [NT, 128, 32])
        OF_d = dscr("OF_d", [2, NLT, 128, D])
        with contextlib.ExitStack() as ph:
            adaln(1, ph)
            P.barrier()

        with contextlib.ExitStack() as ph:
            R = mk_rings(ph)
            SBT1 = 4
            hxf = Ring(nc, ph, "c1hx", 1, [128, D], F32)
            hxT = sb("c1hxT", [128, 8, (SBT1 + 2) * 128], BF16, ph)
            pT = Ring(nc, ph, "pT", 2, [128, SBT1 * 128 + 4], F32)
            cv = Ring(nc, ph, "cv", 2, [128, SBT1 * 128], F32)
            sqb = Ring(nc, ph, "sqb", 1, [128, SBT1 * 128], BF16)
            rsb = Ring(nc, ph, "rsb", 1, [128, SBT1 * 128], F32)
            kf = Ring(nc, ph, "kf", 1, [128, SBT1 * 128], F32)
            qst = sb("qst", [128, SBT1, 8, 128], BF16, ph); kst = sb("kst", [128, SBT1, 8, 128], BF16, ph)
            ktst = sb("ktst", [128, SBT1, 8, 128], BF16, ph); vtst = sb("vtst", [128, SBT1, 8, 128], BF16, ph)
            win_sb = sb("win_sb", [128, 8, 3072], BF16, ph)
            wz = sb("wz", [128, 8, 1056], BF16, ph)
            wcs = sb("wcs", [128, 24, 4], F32, ph)
            onesb = sb("onesb", [128, 128], BF16, ph)
            c16 = sb("c16", [128, 2, 16], F32, ph)
            zsb = Ring(nc, ph, "zsb", 1, [128, D], BF16)
            gbr = Ring(nc, ph, "gbr", 2, [128, 64], F32)
            P.dma('pool', wz[:], w_dn_in.rearrange("(k p) n -> p k n", p=128)[:, :, 3072:4128], r=["d_win"], w=["wz"])
            P.dma('sp', wcs[:], wconv, r=["d_wconv"], w=["wcs"])
            P.op('dve', lambda: nc.vector.tensor_copy(out=onesb[:], in_=msk[:, 3, :]), r=["msk"], w=["onesb"])
            P.dma('sp', c16[:, 0, :], dn_dt_bias.partition_broadcast(128), r=["d_dtb"], w=["c16"])
            P.dma('sp', c16[:, 1, :], dn_a_log.partition_broadcast(128), r=["d_alog"], w=["c16"])
            P.op('act', lambda: nc.scalar.activation(out=c16[:, 1, :], in_=c16[:, 1, :], func=AF.Exp), r=["c16"], w=["c16"])
            P.op('dve', lambda: nc.vector.tensor_scalar(out=c16[:, 1, :], in0=c16[:, 1, :], scalar1=-1.0, scalar2=None, op0=ALU.mult),
                 r=["c16"], w=["c16"])
            win_v = w_dn_in.rearrange("(k p) n -> p k n", p=128)
            for j6 in range(6):
                P.dma('pool', win_sb[:, :, j6 * 512:(j6 + 1) * 512], win_v[:, :, j6 * 512:(j6 + 1) * 512], r=["d_win"], w=[("win_sb", j6)])

            def c1_tile_norm(gt, slot, mod, mkey):
                xt, xk = R['xs'].next()
                P.dma('sp', xt[:], XS2_t[gt], r=["d_dst0"], w=[xk])
                hx, hk = hxf.next()
                rms_mod(R, xt[:], xk, mv(mod, 1), mv(mod, 0), [mkey], hx[:], hk)
                for k in range(8):
                    b = 6 + k // 4
                    P.op('pe', (lambda b=b, k=k, hx=hx: nc.tensor.transpose(
                        PS[b][:, (k % 4) * 128:(k % 4) * 128 + 128], hx[:, k * 128:(k + 1) * 128], ident)),
                        r=[hk, "msk"], w=[PK[b]])
                for h in range(2):
                    b = 6 + h
                    P.op('act', (lambda b=b, h=h, slot=slot: nc.scalar.copy(
                        out=hxT[:, 4 * h:4 * h + 4, slot * 128:(slot + 1) * 128],
                        in_=PS[b].rearrange("p (k t) -> p k t", k=4))), r=[PK[b]], w=[("hxT", slot)])

            def c1_zab(gt, slot, is_ctx):
                hk = [("hxT", slot)]
                if not is_ctx:
                    zs, zk = zsb.next()
                    for half in range(2):
                        b = 4 + half
                        for k in range(8):
                            P.op('pe', (lambda b=b, k=k, half=half, slot=slot: nc.tensor.matmul(
                                PS[b], hxT[:, k, slot * 128:(slot + 1) * 128], wz[:, k, half * 512:(half + 1) * 512],
                                start=(k == 0), stop=(k == 7))), r=hk + ["wz"], w=[PK[b]])
                        P.op('act', (lambda b=b, half=half, zs=zs: nc.scalar.activation(
                            out=zs[:, half * 512:(half + 1) * 512], in_=PS[b], func=AF.Silu)), r=[PK[b]], w=[(zk, half)])
                    P.dma('sp', ZZ_d[gt], zs[:], r=[(zk, 0), (zk, 1)], w=["d_ZZ"])
                for k in range(8):
                    P.op('pe', (lambda k=k, slot=slot: nc.tensor.matmul(
                        PS[3][:, 0:32], hxT[:, k, slot * 128:(slot + 1) * 128], wz[:, k, 1024:1056],
                        start=(k == 0), stop=(k == 7))), r=hk + ["wz"], w=[PK[3]])
                g_, gk = gbr.next()
                V = nc.vector
                ab = g_[:, 0:32].rearrange("p (f h) -> p f h", f=4)
                o4 = g_[:, 32:64].rearrange("p (f h) -> p f h", f=4)
                P.op('dve', lambda: V.tensor_copy(out=g_[:, 0:32], in_=PS[3][:, 0:32]), r=[PK[3]], w=[gk])
                P.op('dve', lambda: V.tensor_tensor(out=o4[:, 0::2, :], in0=ab[:, 0::2, :],
                                                    in1=c16[:, 0, :].rearrange("p (d h) -> p d h", d=2), op=ALU.add),
                     r=[gk, "c16"], w=[gk])
                P.op('act', lambda: nc.scalar.activation(out=o4[:, 0::2, :], in_=o4[:, 0::2, :], func=AF.Exp), r=[gk], w=[gk])
                P.op('dve', lambda: V.tensor_scalar(out=o4[:, 0::2, :], in0=o4[:, 0::2, :], scalar1=1.0, scalar2=None, op0=ALU.add),
                     r=[gk], w=[gk])
                P.op('act', lambda: nc.scalar.activation(out=o4[:, 0::2, :], in_=o4[:, 0::2, :], func=AF.Ln), r=[gk], w=[gk])
                P.op('dve', lambda: V.tensor_tensor(out=o4[:, 0::2, :], in0=o4[:, 0::2, :],
                                                    in1=c16[:, 1, :].rearrange("p (d h) -> p d h", d=2), op=ALU.mult),
                     r=[gk, "c16"], w=[gk])
                P.op('act', lambda: nc.scalar.activation(out=o4[:, 1::2, :], in_=ab[:, 1::2, :], func=AF.Sigmoid), r=[gk], w=[gk])
                P.dma('sp', GB_d[gt], g_[:, 32:64], r=[gk], w=["d_GB"])

            def c1_chunk(cc, seq0, nseq, t0, t1, hbase_tile):
                W = (t1 - t0) * 128
                ntile = t1 - t0
                wk = ("win_sb", cc // 4)
                p_, pk = pT.next()
                tok0 = t0 * 128 - 2
                lo = max(tok0, 0); hi = min(t1 * 128 + 1, nseq * 128)
                if lo > tok0:
                    P.op('pool', lambda: nc.gpsimd.memset(p_[:, 0:lo - tok0], 0.0), r=[], w=[(pk, 'l')])
                if hi < t1 * 128 + 1:
                    P.op('pool', lambda: nc.gpsimd.memset(p_[:, hi - tok0:W + 3], 0.0), r=[], w=[(pk, 'r')])
                a = lo
                wi = 0
                while a < hi:
                    b_ = min(a + 512, hi)
                    bank = wi % 3
                    hk = [("hxT", s_) for s_ in range((a // 128) - hbase_tile, ((b_ - 1) // 128) - hbase_tile + 1)]
                    for k in range(8):
                        P.op('pe', (lambda k=k, bank=bank, a=a, b_=b_: nc.tensor.matmul(
                            PS[bank][:, 0:b_ - a], win_sb[:, k, cc * 128:(cc + 1) * 128], hxT[:, k, a - hbase_tile * 128:b_ - hbase_tile * 128],
                            start=(k == 0), stop=(k == 7))), r=[wk] + hk, w=[PK[bank]])
                    P.op('act', (lambda bank=bank, a=a, b_=b_: nc.scalar.copy(out=p_[:, a - tok0:b_ - tok0], in_=PS[bank][:, 0:b_ - a])),
                         r=[PK[bank]], w=[(pk, wi)])
                    a = b_; wi += 1
                pkeys = [(pk, j) for j in range(wi)] + [(pk, 'l'), (pk, 'r')]
                c_, ck = cv.next()
                P.op('dve', lambda: nc.vector.tensor_scalar(out=c_[:, 0:W], in0=p_[:, 0:W], scalar1=wcs[:, cc, 0:1], scalar2=None, op0=ALU.mult),
                     r=pkeys + ["wcs"], w=[ck])
                for tap in range(1, 4):
                    eng = 'dve'
                    E_ = nc.gpsimd if eng == 'pool' else nc.vector
                    P.op(eng, (lambda tap=tap, E_=E_: E_.scalar_tensor_tensor(out=c_[:, 0:W], in0=p_[:, tap:tap + W], scalar=wcs[:, cc, tap:tap + 1],
                                                                             in1=c_[:, 0:W], op0=ALU.mult, op1=ALU.add)),
                         r=pkeys + ["wcs", ck], w=[ck])
                P.op('act', lambda: nc.scalar.activation(out=c_[:, 0:W], in_=c_[:, 0:W], func=AF.Silu), r=[ck], w=[ck])
                kind = cc // 8; h = cc % 8
                src = c_
                srck = ck
                if kind < 2:
                    sq, sqk = sqb.next(); rs, rk_ = rsb.next()
                    P.op('pool', lambda: nc.gpsimd.tensor_tensor(out=sq[:, 0:W], in0=c_[:, 0:W], in1=c_[:, 0:W], op=ALU.mult), r=[ck], w=[sqk])
                    for j in range(0, W, 512):
                        n_ = min(512, W - j)
                        bank = 3 + (j // 512) % 2
                        P.op('pe', (lambda j=j, n_=n_, bank=bank: nc.tensor.matmul(PS[bank][:, 0:n_], onesb[:], sq[:, j:j + n_], start=True, stop=True)),
                             r=[sqk, "onesb"], w=[PK[bank]])
                        P.op('dve', (lambda j=j, n_=n_, bank=bank: nc.vector.tensor_scalar(out=rs[:, j:j + n_], in0=PS[bank][:, 0:n_], scalar1=EPS, scalar2=None, op0=ALU.add)),
                             r=[PK[bank]], w=[(rk_, j)])
                    rkeys = [(rk_, j) for j in range(0, W, 512)]
                    P.op('act', lambda: nc.scalar.activation(out=rs[:, 0:W], in_=rs[:, 0:W], func=AF.Sqrt), r=rkeys, w=rkeys)
                    P.op('dve', lambda: nc.vector.reciprocal(out=rs[:, 0:W], in_=rs[:, 0:W]), r=rkeys, w=rkeys)
                    if kind == 0:
                        P.op('dve', lambda: nc.vector.scalar_tensor_tensor(
                            out=qst[:, 0:ntile, h, :], in0=c_[:, 0:W].rearrange("p (t k) -> p t k", k=128), scalar=float(128 ** -0.5),
                            in1=rs[:, 0:W].rearrange("p (t k) -> p t k", k=128), op0=ALU.mult, op1=ALU.mult), r=[ck] + rkeys, w=[("qst", h)])
                        return
                    kf_, kfk = kf.next()
                    P.op('dve', lambda: nc.vector.tensor_tensor(out=kf_[:, 0:W], in0=c_[:, 0:W], in1=rs[:, 0:W], op=ALU.mult), r=[ck] + rkeys, w=[kfk])
                    P.op('pool', lambda: nc.gpsimd.tensor_copy(out=kst[:, 0:ntile, h, :], in_=kf_[:, 0:W].rearrange("p (t k) -> p t k", k=128)),
                         r=[kfk], w=[("kst", h)])
                    src = kf_; srck = kfk
                dst = ktst if kind == 1 else vtst
                dkey = "ktst" if kind == 1 else "vtst"
                for j in range(0, ntile, 4):
                    n_ = min(4, ntile - j)
                    bank = 5 + (j // 4) % 2
                    for t in range(n_):
                        P.op('pe', (lambda t=t, j=j, bank=bank: nc.tensor.transpose(
                            PS[bank][:, t * 128:(t + 1) * 128], src[:, (j + t) * 128:(j + t + 1) * 128], ident)),
                            r=[srck, "msk"], w=[PK[bank]])
                    P.op('act', (lambda j=j, n_=n_, bank=bank: nc.scalar.copy(
                        out=dst[:, j:j + n_, h, :], in_=PS[bank][:, 0:n_ * 128].rearrange("p (t k) -> p t k", k=128))),
                        r=[PK[bank]], w=[(dkey, h, j)])

            def c1_superblock(seq0, nseq, t0, t1, is_ctx):
                mod, mkey = (modC, "modC") if is_ctx else (modL, "modL")
                hb = max(t0 - 1, 0); he = min(t1 + 1, nseq)
                for lt in range(hb, he):
                    c1_tile_norm(seq0 + lt, lt - hb, mod, mkey)
                for lt in range(t0, t1):
                    c1_zab(seq0 + lt, lt - hb, is_ctx)
                for cc in range(24):
                    c1_chunk(cc, seq0, nseq, t0, t1, hb)
                nt_ = t1 - t0
                g0 = seq0 + t0
                for (dst, st, key) in ((QT_d, qst, "qst"), (KT_d, kst, "kst")):
                    P.dma('sp', dst[g0:g0 + nt_].rearrange("t p h k -> p t h k"), st[:, 0:nt_, :, :],
                          r=[(key, h) for h in range(8)], w=["d_" + key])
                for (dst, st, key) in ((KK_d, ktst, "ktst"), (VV_d, vtst, "vtst")):
                    P.dma('sp', dst[g0:g0 + nt_].rearrange("t p h k -> p t h k"), st[:, 0:nt_, :, :],
                          r=[(key, h, j) for h in range(8) for j in range(0, nt_, 4)], w=["d_" + key])

            c1_superblock(0, NCT, 0, NCT, True)
            for t0 in range(0, NLT, SBT1):
                c1_superblock(NCT, NLT, t0, t0 + SBT1, False)
            P.barrier()
        if stop_after == "c1":
            P.finish()
            return nc

        with contextlib.ExitStack() as ph:
            HS = [128, 8, 128]
            lKT = Ring(nc, ph, "lKT", 2, HS, BF16); lQT = Ring(nc, ph, "lQT", 2, HS, BF16)
            lKK = Ring(nc, ph, "lKK", 2, HS, BF16); lVV = Ring(nc, ph, "lVV", 2, HS, BF16)
            gbl = Ring(nc, ph, "gbl", 2, [128, 32], F32)
            scr = Ring(nc, ph, "scr", 2, [128, 80], F32)
            L16 = Ring(nc, ph, "L16", 2, [16, 4, 128], F32)
            rE = Ring(nc, ph, "rE", 2, [16, 2, 8, 128], F32)
            bmask = sb("bmask", [16, 8, 128], F32, ph)
            Ei = Ring(nc, ph, "Ei", 2, HS, F32); Es = Ring(nc, ph, "Es", 1, HS, F32)
            SBm = Ring(nc, ph, "SBm", 1, HS, F32); tf = Ring(nc, ph, "tf", 2, HS, F32)
            Ak = Ring(nc, ph, "Ak", 2, HS, BF16); Bk = Ring(nc, ph, "Bk", 2, HS, BF16); Tk = Ring(nc, ph, "Tk", 3, HS, BF16)
            TTk = Ring(nc, ph, "TTk", 3, HS, BF16); A0r = Ring(nc, ph, "A0r", 2, HS, BF16); B0r = Ring(nc, ph, "B0r", 2, HS, BF16)
            Of = Ring(nc, ph, "Of", 4, HS, BF16); P1r = Ring(nc, ph, "P1r", 2, HS, BF16)
            qkT = Ring(nc, ph, "qkT", 2, HS, BF16); KG = Ring(nc, ph, "KG", 2, HS, BF16); Kd = Ring(nc, ph, "Kd", 2, HS, BF16)
            up = Ring(nc, ph, "up", 1, HS, F32); wT = Ring(nc, ph, "wT", 2, HS, BF16); vn = Ring(nc, ph, "vn", 2, HS, BF16)
            ob = Ring(nc, ph, "ob", 1, HS, F32)
            Sst = [sb("S%d" % d, HS, F32, ph) for d in range(2)]
            Sbf = [sb("Sb%d" % d, HS, BF16, ph) for d in range(2)]
            P.dma('sp', bmask[:], blockmask, r=["d_bm"], w=["bmask"])
            for d in range(2):
                P.op('pool', (lambda d=d: nc.gpsimd.memset(Sst[d][:], 0.0)), r=[], w=[("S", d)])
                P.op('pool', (lambda d=d: nc.gpsimd.memset(Sbf[d][:], 0.0)), r=[], w=[("Sb", d)])
            for t_ in scr.tiles:
                P.op('pool', (lambda t_=t_: nc.gpsimd.memset(t_[:], 1.0)), r=[], w=[])
            P.flush()
            P.barrier()
            ppc = [0]

            def pair():
                p = ppc[0] % 4
                ppc[0] += 1
                v = psum[:, p * 1024:(p + 1) * 1024]
                return p, v, v.rearrange("p (h k) -> p h k", h=8), [PK[2 * p], PK[2 * p + 1]]

            def bc_mid(ap2d, n=128):
                return ap2d.unsqueeze(1).to_broadcast([ap2d.shape[0], 8, ap2d.shape[1]])

            def bc_last(ap2d):
                return ap2d.unsqueeze(2).to_broadcast([ap2d.shape[0], 8, 128])

            def dn_step(d, gt, lt, need_o):
                V = nc.vector; G = nc.gpsimd; A = nc.scalar
                kt, ktk = lKT.next(); kk, kkk = lKK.next(); vv, vvk = lVV.next(); gb, gbk = gbl.next()
                P.dma('sp', kt[:], KT_d[gt], r=["d_kst"], w=[ktk])
                P.dma('sp', kk[:], KK_d[gt], r=["d_ktst"], w=[kkk])
                P.dma('sp', vv[:], VV_d[gt], r=["d_vtst"], w=[vvk])
                P.dma('sp', gb[:], GB_d[gt], r=["d_GB"], w=[gbk])
                if need_o:
                    qt, qtk = lQT.next()
                    P.dma('sp', qt[:], QT_d[gt], r=["d_qst"], w=[qtk])
                g = gb[:, 16 * d:16 * d + 8]; beta = gb[:, 16 * d + 8:16 * d + 16]
                sc, sk = scr.next()
                p, pv, pv3, pk = pair()
                P.op('pe', lambda: nc.tensor.matmul(pv[:, 0:8], msk[:, 1 + d, :], g, start=True, stop=True), r=[gbk, "msk"], w=pk)
                P.op('pe', lambda: nc.tensor.matmul(pv[:, 8:16], msk[:, 3, :], g, start=True, stop=True), r=[gbk, "msk"], w=pk)
                P.op('dve', lambda: V.tensor_copy(out=sc[:, 0:16], in_=pv[:, 0:16]), r=pk, w=[sk])
                P.op('act', lambda: A.activation(out=sc[:, 16:24], in_=sc[:, 0:8], func=AF.Exp), r=[sk], w=[sk])
                P.op('dve', lambda: V.tensor_tensor(out=sc[:, 24:32], in0=sc[:, 8:16], in1=sc[:, 0:8], op=ALU.subtract), r=[sk], w=[sk])
                P.op('act', lambda: A.activation(out=sc[:, 24:32], in_=sc[:, 24:32], func=AF.Exp), r=[sk], w=[sk])
                P.op('act', lambda: A.activation(out=sc[:, 32:40], in_=sc[:, 8:16], func=AF.Exp), r=[sk], w=[sk])
                P.op('dve', lambda: V.tensor_scalar(out=sc[:, 40:48], in0=sc[:, 0:8], scalar1=-1.0, scalar2=None, op0=ALU.mult), r=[sk], w=[sk])
                P.op('dve', lambda: V.tensor_copy(out=sc[:, 56:64], in_=sc[:, 0:8]), r=[sk], w=[sk])
                P.op('act', lambda: A.activation(out=sc[:, 72:80], in_=beta, func=AF.Ln), r=[gbk], w=[sk])
                P.op('dve', lambda: V.tensor_tensor(out=sc[:, 72:80], in0=sc[:, 72:80], in1=sc[:, 40:48], op=ALU.add), r=[sk], w=[sk])
                egc = sc[:, 16:24]; edec = sc[:, 24:32]; egl = sc[:, 32:40]
                l16, lk = L16.next(); re, rek = rE.next()
                p, pv, pv3, pk = pair()
                for q in range(4):
                    P.op('pe', (lambda q=q: nc.tensor.transpose(pv[0:16, q * 128:(q + 1) * 128], sc[:, 40 + 8 * q:56 + 8 * q], ident)),
                         r=[sk, "msk"], w=pk)
                P.op('act', lambda: A.copy(out=l16[:], in_=pv[0:16, 0:512].rearrange("p (q k) -> p q k", q=4)), r=pk, w=[lk])
                P.op('pool', lambda: G.tensor_tensor(out=re[:, 0, :, :], in0=bc_mid(l16[:, 1, :]), in1=bmask[:], op=ALU.mult), r=[lk, "bmask"], w=[(rek, 0)])
                P.op('pool', lambda: G.tensor_tensor(out=re[:, 1, :, :], in0=bc_mid(l16[:, 3, :]), in1=bmask[:], op=ALU.mult), r=[lk, "bmask"], w=[(rek, 1)])
                ei, eik = Ei.next(); es_, esk = Es.next()
                for which, (lq, dst, dk_, mi) in enumerate(((0, ei, eik, 4 + d), (2, es_, esk, 8 + d))):
                    p, pv, pv3, pk = pair()
                    for half in range(2):
                        P.op('pe', (lambda half=half, which=which, lq=lq, pv=pv: nc.tensor.matmul(
                            pv[:, half * 512:(half + 1) * 512], l16[:, lq, :],
                            re[:, which, 4 * half:4 * half + 4, :].rearrange("p h k -> p (h k)"), start=True, stop=True)),
                            r=[lk, (rek, which)], w=pk)
                    P.op('dve', (lambda pv3=pv3, dst=dst, mi=mi: V.scalar_tensor_tensor(
                        out=dst[:], in0=pv3, scalar=0.0, in1=bc_mid(msk[:, mi, :]), op0=ALU.min, op1=ALU.add)), r=pk + ["msk"], w=[dk_])
                    P.op('act', (lambda dst=dst: A.activation(out=dst[:], in_=dst[:], func=AF.Exp)), r=[dk_], w=[dk_])
                sbm, sbk = SBm.next()
                P.op('pool', lambda: G.tensor_tensor(out=sbm[:], in0=bc_mid(msk[:, 6 + d, :]), in1=bc_last(beta), op=ALU.mult), r=["msk", gbk], w=[sbk])
                p, pv, pv3, pk = pair()
                for h in range(8):
                    P.op('pe', (lambda h=h, pv=pv: nc.tensor.matmul(pv[:, h * 128:(h + 1) * 128], kt[:, h, :], kt[:, h, :], start=True, stop=True)),
                         r=[ktk], w=pk)
                t1, t1k = tf.next()
                a0, a0k = A0r.next(); b0, b0k = B0r.next()
                P.op('dve', (lambda pv3=pv3: V.tensor_tensor(out=t1[:], in0=pv3, in1=ei[:], op=ALU.mult)), r=pk + [eik], w=[t1k])
                P.op('pool', lambda: G.tensor_tensor(out=a0[:], in0=t1[:], in1=sbm[:], op=ALU.mult), r=[t1k, sbk], w=[a0k])
                P.op('dve', (lambda pv3=pv3: V.tensor_tensor(out=b0[:], in0=pv3, in1=es_[:], op=ALU.mult)), r=pk + [esk], w=[b0k])
                if need_o:
                    qk_, qkk = qkT.next()
                    p, pv, pv3, pk = pair()
                    for h in range(8):
                        P.op('pe', (lambda h=h, pv=pv: nc.tensor.matmul(pv[:, h * 128:(h + 1) * 128], kt[:, h, :], qt[:, h, :], start=True, stop=True)),
                             r=[ktk, qtk], w=pk)
                    P.op('dve', (lambda pv3=pv3: V.tensor_tensor(out=qk_[:], in0=pv3, in1=ei[:], op=ALU.mult)), r=pk + [eik], w=[qkk])
                def mm8(lhs, rhs, keys):
                    p, pv, pv3, pk = pair()
                    for h in range(8):
                        P.op('pe', (lambda h=h, pv=pv: nc.tensor.matmul(pv[:, h * 128:(h + 1) * 128], lhs[:, h, :], rhs[:, h, :], start=True, stop=True)),
                             r=keys, w=pk)
                    return pv3, pk
                ca, cak = Ak.next(); cb, cbk = Bk.next(); cT, cTk = Tk.next(); cTT, cTTk = TTk.next()
                P.op('pool', lambda ca=ca: G.tensor_tensor(out=ca[:], in0=a0[:], in1=bc_mid(msk[:, 10, :]), op=ALU.mult), r=[a0k, "msk"], w=[cak])
                P.op('pool', lambda cb=cb: G.tensor_tensor(out=cb[:], in0=b0[:], in1=bc_mid(msk[:, 10, :]), op=ALU.mult), r=[b0k, "msk"], w=[cbk])
                P.op('pool', lambda ca=ca, cT=cT: G.tensor_tensor(out=cT[:], in0=bc_mid(identb[:]), in1=ca[:], op=ALU.subtract), r=["identb", cak], w=[cTk])
                P.op('pool', lambda cb=cb, cTT=cTT: G.tensor_tensor(out=cTT[:], in0=bc_mid(identb[:]), in1=cb[:], op=ALU.subtract), r=["identb", cbk], w=[cTTk])
                offs = []
                for mi in (11, 12):
                    of_, ofk = Of.next(); oft, oftk = Of.next()
                    P.op('pool', (lambda of_=of_, mi=mi: G.tensor_tensor(out=of_[:], in0=a0[:], in1=bc_mid(msk[:, mi, :]), op=ALU.mult)), r=[a0k, "msk"], w=[ofk])
                    P.op('pool', (lambda oft=oft, mi=mi: G.tensor_tensor(out=oft[:], in0=b0[:], in1=bc_mid(msk[:, mi, :]), op=ALU.mult)), r=[b0k, "msk"], w=[oftk])
                    offs.append((of_, ofk, oft, oftk))
                for lev in range(4):
                    na, nak = Ak.next(); nb, nbk = Bk.next(); nT, nTk = Tk.next(); nTT, nTTk = TTk.next()
                    pv3, pk = mm8(cb, ca, [cak, cbk])
                    P.op('act', (lambda pv3=pv3, na=na: A.copy(out=na[:], in_=pv3)), r=pk, w=[nak])
                    pv3, pk = mm8(ca, cb, [cak, cbk])
                    P.op('dve', (lambda pv3=pv3, nb=nb: V.tensor_copy(out=nb[:], in_=pv3)), r=pk, w=[nbk])
                    pv3, pk = mm8(nb, cT, [nbk, cTk])
                    P.op('dve', (lambda pv3=pv3, nT=nT, cT=cT: V.tensor_tensor(out=nT[:], in0=pv3, in1=cT[:], op=ALU.add)), r=pk + [cTk], w=[nTk])
                    pv3, pk = mm8(na, cTT, [nak, cTTk])
                    P.op('dve', (lambda pv3=pv3, nTT=nTT, cTT=cTT: V.tensor_tensor(out=nTT[:], in0=pv3, in1=cTT[:], op=ALU.add)), r=pk + [cTTk], w=[nTTk])
                    ca, cak, cb, cbk, cT, cTk, cTT, cTTk = na, nak, nb, nbk, nT, nTk, nTT, nTTk
                for si, (of_, ofk, oft, oftk) in enumerate(offs):
                    p1, p1k = P1r.next()
                    pv3, pk = mm8(oft, cT, [oftk, cTk])
                    P.op('act', (lambda pv3=pv3, p1=p1: A.copy(out=p1[:], in_=pv3)), r=pk, w=[p1k])
                    pv3, pk = mm8(cTT, p1, [cTTk, p1k])
                    nT, nTk = Tk.next()
                    P.op('dve', (lambda pv3=pv3, nT=nT, cT=cT: V.tensor_tensor(out=nT[:], in0=cT[:], in1=pv3, op=ALU.subtract)), r=pk + [cTk], w=[nTk])
                    if si == 0:
                        p1t, p1tk = P1r.next()
                        pv3, pk = mm8(of_, cTT, [ofk, cTTk])
                        P.op('act', (lambda pv3=pv3, p1t=p1t: A.copy(out=p1t[:], in_=pv3)), r=pk, w=[p1tk])
                        pv3, pk = mm8(cT, p1t, [cTk, p1tk])
                        nTT, nTTk = TTk.next()
                        P.op('dve', (lambda pv3=pv3, nTT=nTT, cTT=cTT: V.tensor_tensor(out=nTT[:], in0=cTT[:], in1=pv3, op=ALU.subtract)), r=pk + [cTTk], w=[nTTk])
                        cTT, cTTk = nTT, nTTk
                    cT, cTk = nT, nTk
                u_, uk = up.next(); w_, wk_ = wT.next(); kg, kgk = KG.next(); kd, kdk = Kd.next()
                P.op('pool', lambda: G.tensor_tensor(out=kg[:], in0=kk[:], in1=bc_last(egc), op=ALU.mult), r=[kkk, sk], w=[kgk])
                P.op('pool', lambda: G.tensor_tensor(out=kd[:], in0=kk[:], in1=bc_last(edec), op=ALU.mult), r=[kkk, sk], w=[kdk])
                p, pv, pv3, pk = pair()
                for h in range(8):
                    P.op('pe', (lambda h=h, pv=pv: nc.tensor.matmul(pv[:, h * 128:(h + 1) * 128], cT[:, h, :], vv[:, h, :], start=True, stop=True)),
                         r=[cTk, vvk], w=pk)
                P.op('act', (lambda pv3=pv3: A.copy(out=u_[:], in_=pv3)), r=pk, w=[uk])
                p, pv, pv3, pk = pair()
                for h in range(8):
                    P.op('pe', (lambda h=h, pv=pv: nc.tensor.matmul(pv[:, h * 128:(h + 1) * 128], kg[:, h, :], cT[:, h, :], start=True, stop=True)),
                         r=[cTk, kgk], w=pk)
                P.op('act', (lambda pv3=pv3: A.copy(out=w_[:], in_=pv3)), r=pk, w=[wk_])
                S = Sst[d]; Sb = Sbf[d]; Sk = ("S", d); Sbk = ("Sb", d)
                vn_, vnk = vn.next(); t2, t2k = tf.next()
                p, pv, pv3, pk = pair()
                for h in range(8):
                    P.op('pe', (lambda h=h, pv=pv: nc.tensor.matmul(pv[:, h * 128:(h + 1) * 128], w_[:, h, :], Sb[:, h, :], start=True, stop=True)),
                         r=[wk_, Sbk], w=pk)
                P.op('dve', (lambda pv3=pv3: V.tensor_tensor(out=t2[:], in0=u_[:], in1=pv3, op=ALU.subtract)), r=pk + [uk], w=[t2k])
                P.op('pool', lambda: G.tensor_tensor(out=vn_[:], in0=t2[:], in1=bc_last(beta), op=ALU.mult), r=[t2k, gbk], w=[vnk])
                if need_o:
                    o_, ok_ = ob.next()
                    p, pv, pv3, pk = pair()
                    for h in range(8):
                        P.op('pe', (lambda h=h, pv=pv: nc.tensor.matmul(pv[:, h * 128:(h + 1) * 128], qt[:, h, :], Sb[:, h, :], start=True, stop=True)),
                             r=[qtk, Sbk], w=pk)
                    P.op('dve', (lambda pv3=pv3: V.tensor_tensor(out=o_[:], in0=pv3, in1=bc_last(egc), op=ALU.mult)), r=pk + [sk], w=[ok_])
                    p, pv, pv3, pk = pair()
                    for h in range(8):
                        P.op('pe', (lambda h=h, pv=pv: nc.tensor.matmul(pv[:, h * 128:(h + 1) * 128], qk_[:, h, :], vn_[:, h, :], start=True, stop=True)),
                             r=[qkk, vnk], w=pk)
                    P.op('dve', (lambda pv3=pv3: V.tensor_tensor(out=o_[:], in0=o_[:], in1=pv3, op=ALU.add)), r=pk + [ok_], w=[ok_])
                    P.dma('sp', OF_d[d, lt], o_[:].rearrange("p h k -> p (h k)"), r=[ok_], w=["d_OF"])
                p, pv, pv3, pk = pair()
                for h in range(8):
                    P.op('pe', (lambda h=h, pv=pv: nc.tensor.matmul(pv[:, h * 128:(h + 1) * 128], kd[:, h, :], vn_[:, h, :], start=True, stop=True)),
                         r=[kdk, vnk], w=pk)
                P.op('dve', lambda: V.tensor_tensor(out=S[:], in0=S[:], in1=bc_last(egl), op=ALU.mult), r=[Sk, sk], w=[Sk])
                P.op('dve', (lambda pv3=pv3: V.tensor_tensor(out=S[:], in0=S[:], in1=pv3, op=ALU.add)), r=pk + [Sk], w=[Sk])
                P.op('act', lambda: A.copy(out=Sb[:], in_=S[:]), r=[Sk], w=[Sbk])
                import os
                if os.environ.get("DN_DBG") and d == int(os.environ["DN_DBG"]) and gt == int(os.environ.get("DN_DBG_GT", "0")):
                    dump("dbg_sc", sc[:], [sk]); dump("dbg_Ei", ei[:], [eik]); dump("dbg_Es", es_[:], [esk])
                    dump("dbg_a0", a0[:], [a0k]); dump("dbg_b0", b0[:], [b0k]); dump("dbg_T", cT[:], [cTk])
                    dump("dbg_up", u_[:], [uk]); dump("dbg_wT", w_[:], [wk_]); dump("dbg_vn", vn_[:], [vnk]); dump("dbg_S", S[:], [Sk])
                    dump("dbg_l16", l16[:], [lk])
                    if need_o:
                        dump("dbg_qk", qk_[:], [qkk]); dump("dbg_o", o_[:], [ok_])

            order_f = [(t, t, False) for t in range(NCT)] + [(NCT + t, t, True) for t in range(NLT)]
            order_b = [(t, t, False) for t in reversed(range(NCT))] + [(NCT + t, t, True) for t in reversed(range(NLT))]
            import os
            nstep = int(os.environ.get("DN_STEPS", str(NT)))
            for i in range(nstep):
                gt, lt, no = order_f[i]
                dn_step(0, gt, lt, no)
                gt, lt, no = order_b[i]
                dn_step(1, gt, lt, no)
                P.flush()
            P.barrier()
        if stop_after == "c2":
            P.finish()
            return nc

        with contextlib.ExitStack() as ph:
            R = mk_rings(ph)
            of0 = Ring(nc, ph, "of0", 2, [128, D], F32); of1 = Ring(nc, ph, "of1", 2, [128, D], F32)
            sqt = Ring(nc, ph, "sqt", 1, [128, D], F32)
            zl = Ring(nc, ph, "zl", 2, [128, D], BF16)
            ms = Ring(nc, ph, "ms", 2, [128, 8], F32)
            onT = Ring(nc, ph, "onT", 2, [128, 8, 128], BF16)
            yb = Ring(nc, ph, "yb", 2, [128, D], F32)
            wo = sb("wo", [128, 8, D], BF16, ph)
            dnw = sb("dnw", [128, 128], F32, ph)
            P.dma('pool', wo[:], w_dn_out.rearrange("(k p) n -> p k n", p=128), r=["d_wo"], w=["wo"])
            P.dma('sp', dnw[:], dn_norm.partition_broadcast(128), r=["d_dnn"], w=["dnw"])

            def c3_tile(lt):
                gt = NCT + lt
                V = nc.vector; G = nc.gpsimd; A = nc.scalar
                a0_, a0k = of0.next(); a1_, a1k = of1.next(); z_, zk = zl.next(); m_, mk_ = ms.next(); sq, sqk = sqt.next()
                P.dma('sp', a0_[:], OF_d[0, lt], r=["d_OF"], w=[a0k])
                P.dma('sp', a1_[:], OF_d[1, lt], r=["d_OF"], w=[a1k])
                P.dma('sp', z_[:], ZZ_d[gt], r=["d_ZZ"], w=[zk])
                P.op('pool', lambda: G.tensor_tensor(out=a0_[:], in0=a0_[:], in1=a1_[:], op=ALU.add), r=[a0k, a1k], w=[a0k])
                P.op('act', lambda: A.activation(out=sq[:], in_=a0_[:], func=AF.Square), r=[a0k], w=[sqk])
                P.op('dve', lambda: V.reduce_sum(out=m_[:], in_=sq[:].rearrange("p (h k) -> p h k", h=8), axis=AX.X), r=[sqk], w=[mk_])
                P.op('dve', lambda: V.tensor_scalar(out=m_[:], in0=m_[:], scalar1=1.0 / 128, scalar2=EPS, op0=ALU.mult, op1=ALU.add), r=[mk_], w=[mk_])
                P.op('act', lambda: A.activation(out=m_[:], in_=m_[:], func=AF.Sqrt), r=[mk_], w=[mk_])
                P.op('dve', lambda: V.reciprocal(out=m_[:], in_=m_[:]), r=[mk_], w=[mk_])
                o3 = a0_[:].rearrange("p (h k) -> p h k", h=8)
                P.op('dve', lambda: V.tensor_tensor(out=o3, in0=o3, in1=m_[:].unsqueeze(2).to_broadcast([128, 8, 128]), op=ALU.mult), r=[a0k, mk_], w=[a0k])
                P.op('pool', lambda: G.tensor_tensor(out=o3, in0=o3, in1=dnw[:].unsqueeze(1).to_broadcast([128, 8, 128]), op=ALU.mult), r=[a0k, "dnw"], w=[a0k])
                P.op('pool', lambda: G.tensor_tensor(out=a0_[:], in0=a0_[:], in1=z_[:], op=ALU.mult), r=[a0k, zk], w=[a0k])
                for k in range(8):
                    b = 6 + k // 4
                    P.op('pe', (lambda b=b, k=k: nc.tensor.transpose(PS[b][:, (k % 4) * 128:(k % 4) * 128 + 128], a0_[:, k * 128:(k + 1) * 128], ident)),
                         r=[a0k, "msk"], w=[PK[b]])
                t_, tk = onT.next()
                for h in range(2):
                    b = 6 + h
                    P.op('act', (lambda b=b, h=h: A.copy(out=t_[:, 4 * h:4 * h + 4, :], in_=PS[b].rearrange("p (k t) -> p k t", k=4))),
                         r=[PK[b]], w=[(tk, h)])
                y_, yk = yb.next()
                xt, xk = R['xs'].next()
                P.dma('sp', xt[:], XS2_t[gt], r=["d_dst0"], w=[xk])
                for half in range(2):
                    b = 4 + half
                    for k in range(8):
                        P.op('pe', (lambda b=b, k=k, half=half: nc.tensor.matmul(PS[b], t_[:, k, :], wo[:, k, half * 512:(half + 1) * 512],
                                                                             start=(k == 0), stop=(k == 7))), r=[(tk, 0), (tk, 1), "wo"], w=[PK[b]])
                    sl = slice(half * 512, (half + 1) * 512)
                    P.op('dve', (lambda b=b, sl=sl: V.tensor_tensor(out=y_[:, sl], in0=PS[b], in1=mv(modL, 2)[:, sl], op=ALU.mult)),
                         r=[PK[b], "modL"], w=[(yk, half)])
                    P.op('pool', (lambda sl=sl: G.tensor_tensor(out=y_[:, sl], in0=y_[:, sl], in1=xt[:, sl], op=ALU.add)), r=[(yk, half), xk], w=[(yk, half)])
                P.dma('sp', XS3_t[gt], y_[:], r=[(yk, 0), (yk, 1)], w=["d_XS3"])

            for lt in range(NLT):
                c3_tile(lt)
            P.barrier()
        if stop_after == "c3":
            P.finish()
            return nc
        MOE(1, XS3_t, out_t, list(range(NCT, NT)), True)
        P.finish()
    return nc


_CACHE = {}


def _core_inputs(b, inp, consts):
    m = {}
    m['xin'] = np.ascontiguousarray(np.concatenate([inp['ctx'][b], inp['x'][b]], 0), dtype=np.float32)
    cc = np.stack([inp['c'][b], inp['c_ctx']], 0)
    m['ccol'] = np.ascontiguousarray(cc.reshape(2, 8, 128).transpose(2, 0, 1), dtype=np.float32)
    for k in ('w_ada', 'b_ada', 'norm_mix', 'norm_ffn', 'w_e_gate', 'w_e_up', 'w_e_down', 'norm_final'):
        m[k] = np.ascontiguousarray(inp[k], dtype=np.float32)
    m['w_pool'] = np.ascontiguousarray(inp['w_pool'][0]); m['b_pool'] = np.ascontiguousarray(inp['b_pool'][0])
    m['pool_scale'] = np.ascontiguousarray(inp['pool_scale'][0])
    m['w_dn_in'] = np.ascontiguousarray(inp['w_dn_in'][0])
    m['wconv'] = np.ascontiguousarray(inp['w_dn_conv'][0].reshape(4, 24, 128).transpose(2, 1, 0))
    m['dn_a_log'] = np.ascontiguousarray(inp['dn_a_log'][0].reshape(16))
    m['dn_dt_bias'] = np.ascontiguousarray(inp['dn_dt_bias'][0].reshape(16))
    m['dn_norm'] = np.ascontiguousarray(inp['dn_norm'][0]); m['w_dn_out'] = np.ascontiguousarray(inp['w_dn_out'][0])
    m['w_r'] = np.ascontiguousarray(np.concatenate([inp['w_rg'], inp['w_re']], -1))
    m['b_r'] = np.ascontiguousarray(np.concatenate([inp['b_rg'], inp['b_re']], -1))
    m.update(consts)
    return m


def kernel(**inputs):
    inp = {k: np.asarray(v) for k, v in inputs.items()}
    if 'nc' not in _CACHE:
        _CACHE['nc'] = build()
        _CACHE['consts'] = host_constants()
    nc = _CACHE['nc']
    consts = _CACHE['consts']
    B = inp['x'].shape[0]
    in_maps = [_core_inputs(b, inp, consts) for b in range(B)]
    res = run_bass_kernel_spmd(nc, in_maps, core_ids=list(range(B)))
    out = np.stack([np.asarray(res.results[b]['out']).reshape(NLT * 128, D) for b in range(B)], 0)
    return out.astype(inp['x'].dtype)
```

```python
import os
from concourse.bass_utils import run_bass_kernel_spmd
import numpy as np, contextlib
import concourse.bass as bass
import concourse.mybir as mybir

F32 = mybir.dt.float32
BF16 = mybir.dt.bfloat16
AF = mybir.ActivationFunctionType
ALU = mybir.AluOpType
AX = mybir.AxisListType


class Prog:
    NS = 16

    def __init__(self, nc, es):
        self.nc = nc
        self.E = {'pe': nc.tensor, 'act': nc.scalar, 'dve': nc.vector, 'pool': nc.gpsimd, 'sp': nc.sync}
        self.sem = {e: es.enter_context(nc.semaphore("s_" + e)) for e in ('pe', 'act', 'dve', 'pool')}
        self.dsem = {q: [es.enter_context(nc.semaphore("d_%s%d" % (q, i))) for i in range(self.NS)]
                     for q in ('sp', 'act', 'pool')}
        self.sigcount = {e: 0 for e in self.sem}
        self.dmacount = {q: 0 for q in self.dsem}
        self.waited = {}
        self.ops = []
        self.last_w = {}
        self.readers = {}
        self.n_inst = 0

    def op(self, eng, fn, r=(), w=(), dma=False):
        self.ops.append((eng, fn, tuple(r), tuple(w), dma))

    def dma(self, q, out, in_, r, w, **kw):
        e = self.E[q]
        self.op(q, lambda: e.dma_start(out=out, in_=in_, **kw), r, w, dma=True)

    @staticmethod
    def _needs_wait(oj_eng, oj_dma, oi_eng, oi_dma, typ):
        if oj_dma:
            return True
        if oj_eng == oi_eng and not oi_dma:
            if oi_eng == 'pe':
                return False
            return typ == 'raw'
        return True

    def flush(self):
        ops = self.ops
        n = len(ops)
        deps = [None] * n
        last_w, readers = self.last_w, self.readers
        for i, (eng, fn, r, w, dma) in enumerate(ops):
            d = {}
            for k in r:
                t = last_w.get(k)
                if t is not None:
                    d[t] = 'raw'
                if isinstance(k, tuple) and k and k[0] == 'ps':
                    for e2, t in readers.get(k, {}).items():
                        if e2 != eng and t not in d:
                            d[t] = 'raw'
            for k in w:
                t = last_w.get(k)
                if t is not None and t not in d:
                    d[t] = 'waw'
                for t in readers.get(k, {}).values():
                    if t not in d:
                        d[t] = 'war'
            d.pop(('p', i), None)
            deps[i] = d
            me = ('p', i)
            for k in r:
                rk = readers.setdefault(k, {})
                if dma:
                    rk[('d', i)] = me
                else:
                    rk[eng] = me
            for k in w:
                last_w[k] = me
                readers[k] = {}
        need_sig = [False] * n
        for i, (eng, fn, r, w, dma) in enumerate(ops):
            for t, typ in deps[i].items():
                if t[0] == 'p':
                    j = t[1]
                    ej, _, _, _, dj = ops[j]
                    if not dj and self._needs_wait(ej, dj, eng, dma, typ):
                        need_sig[j] = True
        last_of = {}
        for i, (eng, fn, r, w, dma) in enumerate(ops):
            if not dma:
                last_of[eng] = i
        for e, i in last_of.items():
            need_sig[i] = True
        resolved = [None] * n
        for i, (eng, fn, r, w, dma) in enumerate(ops):
            E = self.E[eng]
            waits = {}
            for t, typ in deps[i].items():
                if t[0] == 'p':
                    t2 = resolved[t[1]]
                    ej, dj = ops[t[1]][0], ops[t[1]][4]
                else:
                    t2 = t
                    ej, dj = t[1], t[0] == 'd'
                if not self._needs_wait(ej, dj, eng, dma, typ):
                    continue
                if t2[0] == 'c':
                    key = ('c', t2[1]); val = t2[2]
                else:
                    key = ('d', t2[1], t2[2]); val = t2[3]
                if waits.get(key, 0) < val:
                    waits[key] = val
            if dma:
                k = self.dmacount[eng]
                slot = k % self.NS
                if k >= self.NS:
                    key = ('d', eng, slot); val = 16 * (k // self.NS)
                    if waits.get(key, 0) < val:
                        waits[key] = val
            for key, val in waits.items():
                wk = (eng, key)
                if self.waited.get(wk, 0) >= val:
                    continue
                self.waited[wk] = val
                s = self.sem[key[1]] if key[0] == 'c' else self.dsem[key[1]][key[2]]
                E.wait_ge(s, val)
                self.n_inst += 1
            inst = fn()
            self.n_inst += 1
            if dma:
                k = self.dmacount[eng]
                slot = k % self.NS
                val = 16 * (k // self.NS + 1)
                inst.then_inc(self.dsem[eng][slot], 16)
                self.dmacount[eng] = k + 1
                resolved[i] = ('d', eng, slot, val)
            else:
                if need_sig[i]:
                    self.sigcount[eng] += 1
                    inst.then_inc(self.sem[eng], 1)
                    resolved[i] = ('c', eng, self.sigcount[eng])
        nxt = {}
        for i in range(n - 1, -1, -1):
            eng, dma = ops[i][0], ops[i][4]
            if dma:
                continue
            if resolved[i] is not None:
                nxt[eng] = resolved[i]
            else:
                resolved[i] = nxt[eng]
        for k in list(last_w.keys()):
            t = last_w[k]
            if t[0] == 'p':
                last_w[k] = resolved[t[1]]
        for k in list(readers.keys()):
            rk = readers[k]
            for kk in list(rk.keys()):
                t = rk[kk]
                if t[0] == 'p':
                    rk[kk] = resolved[t[1]]
        self.ops = []

    def barrier(self):
        self.flush()
        for eng in ('pe', 'act', 'dve', 'pool', 'sp'):
            E = self.E[eng]
            for e, c in self.sigcount.items():
                if c > 0 and e != eng and self.waited.get((eng, ('c', e)), 0) < c:
                    E.wait_ge(self.sem[e], c)
                    self.waited[(eng, ('c', e))] = c
            for q, k in self.dmacount.items():
                for slot in range(self.NS):
                    if k > slot:
                        val = 16 * ((k - 1 - slot) // self.NS + 1)
                        if self.waited.get((eng, ('d', q, slot)), 0) < val:
                            E.wait_ge(self.dsem[q][slot], val)
                            self.waited[(eng, ('d', q, slot))] = val

    def finish(self):
        self.flush()
        sp = self.E['sp']
        for e, c in self.sigcount.items():
            if c > 0:
                sp.wait_ge(self.sem[e], c)
        for q, k in self.dmacount.items():
            for slot in range(self.NS):
                if k > slot:
                    cnt = (k - 1 - slot) // self.NS + 1
                    sp.wait_ge(self.dsem[q][slot], 16 * cnt)


D = 1024
NCT = 2
NLT = 64
NT = NCT + NLT
EPS = 1e-6
POOL_WINDOWS = (2, 4, 8, 16)
JREL = {0: list(range(-1, 4)), 1: list(range(-1, 5)), 2: list(range(-2, 6)), 3: list(range(-4, 8))}
BAND_IDX = {}
_i = 0
for _g in range(4):
    for _j in JREL[_g]:
        BAND_IDX[(_g, _j)] = _i
        _i += 1
NBAND = _i


def host_constants():
    c = {}
    def axis_w(n, win):
        t = np.arange(n)
        lo = np.maximum(t - win // 2, 0); hi = np.minimum(t + win // 2, n)
        W = np.zeros((n, n), np.float64)
        for o in range(n):
            W[lo[o]:hi[o], o] = 1.0 / (hi[o] - lo[o])
        return W
    band = np.zeros((3, 128, NBAND, 512), np.float32)
    for g, win in enumerate(POOL_WINDOWS):
        Wr = axis_w(128, win); Wc = axis_w(64, win)
        for ti, b in enumerate((0, 7, 15)):
            for j in JREL[g]:
                jt = 4 * b + j
                if jt < 0 or jt >= 64:
                    continue
                M = np.einsum('ab,cd->acbd', Wr[2 * jt:2 * jt + 2, 8 * b:8 * b + 8], Wc).reshape(128, 512)
                if 0 <= j < 4:
                    M[:, j * 128:(j + 1) * 128] -= np.eye(128)
                band[ti, :, BAND_IDX[(g, j)], :] = M
    c['band'] = band
    cb = np.zeros((128, 8, 256), np.float32)
    for g, win in enumerate(POOL_WINDOWS):
        W = axis_w(256, win) - np.eye(256)
        for j in range(2):
            cb[:, g * 2 + j, :] = W[j * 128:(j + 1) * 128, :]
    c['cband'] = cb
    idx = np.arange(128)
    m = np.zeros((128, 13, 128), np.float32)
    m[:, 0] = np.eye(128)
    m[:, 1] = (idx[:, None] <= idx[None, :])
    m[:, 2] = (idx[:, None] >= idx[None, :])
    m[:, 3] = 1.0
    m[:, 4] = np.where(idx[:, None] <= idx[None, :], 0.0, -30000.0)
    m[:, 5] = np.where(idx[:, None] >= idx[None, :], 0.0, -30000.0)
    m[:, 6] = (idx[:, None] < idx[None, :])
    m[:, 7] = (idx[:, None] > idx[None, :])
    m[:, 8] = np.where(idx[:, None] > idx[None, :], 0.0, -30000.0)
    m[:, 9] = np.where(idx[:, None] < idx[None, :], 0.0, -30000.0)
    bd32 = (idx[:, None] // 32 == idx[None, :] // 32); bd64 = (idx[:, None] // 64 == idx[None, :] // 64)
    m[:, 10] = bd32; m[:, 11] = bd64 & ~bd32; m[:, 12] = ~bd64
    c['masks'] = m
    bm = np.zeros((16, 8, 128), np.float32)
    for r in range(16):
        bm[r, r % 8, :] = 1.0
    c['blockmask'] = bm
    cm = np.zeros((128, 160), np.float32)
    cm[:, 0:32] = np.arange(32)[None, :]
    cm[:, 32:40] = np.arange(8)[None, :] * 128 + np.arange(128)[:, None]
    cm[:, 40:120] = np.arange(80)[None, :] * 512
    c['cmisc'] = cm
    return c


_UNIQ = [0]


def run_interleaved(P, gens, width):
    active = []
    it = iter(gens)
    while True:
        while len(active) < width:
            try:
                active.append(next(it))
            except StopIteration:
                break
        if not active:
            break
        for g in list(active):
            try:
                next(g)
            except StopIteration:
                active.remove(g)
        P.flush()


class Ring:
    def __init__(self, nc, es, name, n, shape, dtype):
        _UNIQ[0] += 1
        self.tiles = [es.enter_context(nc.sbuf_tensor("%s%d_u%d" % (name, i, _UNIQ[0]), shape, dtype)) for i in range(n)]
        self.name = name
        self.i = 0

    def next(self):
        k = self.i % len(self.tiles)
        self.i += 1
        return self.tiles[k], "%s%d" % (self.name, k)


def build(stop_after=None, debug=False):
    nc = bass.Bass("TRN2", target_bir_lowering=False)
    es = contextlib.ExitStack()

    def din(name, shape, dt=F32):
        return nc.dram_tensor(name, list(shape), dt, kind="ExternalInput").ap()

    def dscr(name, shape, dt=F32):
        kind = "ExternalOutput" if debug else "Internal"
        return nc.dram_tensor(name, list(shape), dt, kind=kind).ap()

    xin = din("xin", [NT * 128, D])
    ccol = din("ccol", [128, 2, 8])
    w_ada = din("w_ada", [2, D, 6 * D]); b_ada = din("b_ada", [2, 6 * D])
    norm_mix = din("norm_mix", [2, D]); norm_ffn = din("norm_ffn", [2, D])
    w_pool = din("w_pool", [4, 256, 256]); b_pool = din("b_pool", [D]); pool_scale = din("pool_scale", [D])
    w_dn_in = din("w_dn_in", [D, 4128]); wconv = din("wconv", [128, 24, 4])
    dn_a_log = din("dn_a_log", [16]); dn_dt_bias = din("dn_dt_bias", [16]); dn_norm = din("dn_norm", [128])
    w_dn_out = din("w_dn_out", [D, D])
    w_r = din("w_r", [2, D, 36]); b_r = din("b_r", [2, 36])
    w_e_gate = din("w_e_gate", [2, 32, D, 512]); w_e_up = din("w_e_up", [2, 32, D, 512])
    w_e_down = din("w_e_down", [2, 32, 512, D]); norm_final = din("norm_final", [D])
    band = din("band", [3, 128, NBAND, 512]); cband = din("cband", [128, 8, 256])
    masks = din("masks", [128, 13, 128]); blockmask = din("blockmask", [16, 8, 128])
    cmisc = din("cmisc", [128, 160])
    out = nc.dram_tensor("out", [NLT * 128, D], F32, kind="ExternalOutput").ap()
    XS1 = dscr("XS1", [NT * 128, D])
    XS2 = dscr("XS2", [NT * 128, D])
    XS3 = dscr("XS3", [NT * 128, D])

    with es:
        P = Prog(nc, es)
        psum = es.enter_context(nc.psum_tensor("psum", [128, 4096], F32))
        PS = [psum[:, b * 512:(b + 1) * 512] for b in range(8)]
        PK = [("ps", b) for b in range(8)]

        def sb(name, shape, dt=F32, stack=es):
            _UNIQ[0] += 1
            return stack.enter_context(nc.sbuf_tensor("%s_u%d" % (name, _UNIQ[0]), list(shape), dt))

        msk = sb("msk", [128, 13, 128])
        P.dma('sp', msk[:], masks, r=["d_masks"], w=["msk"])
        ident = msk[:, 0, :]
        identb = sb("identb", [128, 128], BF16)
        P.op('dve', lambda: nc.vector.tensor_copy(out=identb[:], in_=msk[:, 0, :]), r=["msk"], w=["identb"])
        MODS_d = dscr("MODS_d", [2, 128, 6 * D])
        MVEC = {}

        def load_mods(ph, need):
            buf = sb("mvec", [128, len(need), D], F32, ph)
            MVEC.clear()
            for j, (st, ix) in enumerate(need):
                P.dma('sp', buf[:, j, :], MODS_d[0 if st == 'L' else 1][:, ix * D:(ix + 1) * D], r=["d_mods"], w=["mvec"])
                MVEC[(st, ix)] = buf[:, j, :]
        csb = sb("csb", [128, 2, 8]); sil = sb("sil", [128, 2, 8])
        rep = sb("rep", [128, 2, 8, 128], BF16)
        P.dma('sp', csb[:], ccol, r=["d_ccol"], w=["csb"])
        P.op('act', lambda: nc.scalar.activation(out=sil[:], in_=csb[:], func=AF.Silu), r=["csb"], w=["sil"])
        P.op('dve', lambda: nc.vector.tensor_copy(out=rep[:], in_=sil[:].unsqueeze(3).to_broadcast([128, 2, 8, 128])),
             r=["sil"], w=["rep"])

        def adaln(layer, ph):
            wr = Ring(nc, ph, "adaw", 2, [128, 8, 512], BF16)
            nw = sb("nw", [128, 2, D], F32, ph)
            modL = sb("modL", [128, 6 * D], F32, ph); modC = sb("modC", [128, 6 * D], F32, ph)
            P.dma('sp', modL[:], b_ada[layer].partition_broadcast(128), r=["d_b_ada"], w=["modL"])
            P.dma('sp', modC[:], b_ada[layer].partition_broadcast(128), r=["d_b_ada"], w=["modC"])
            P.dma('sp', nw[:, 0, :], norm_mix[layer].partition_broadcast(128), r=["d_nm"], w=["nw"])
            P.dma('sp', nw[:, 1, :], norm_ffn[layer].partition_broadcast(128), r=["d_nm"], w=["nw"])
            wv = w_ada[layer].rearrange("(k p) n -> p k n", p=128)
            for blk in range(12):
                wt, wk = wr.next()
                P.dma('pool', wt[:], wv[:, :, blk * 512:(blk + 1) * 512], r=["d_w_ada"], w=[wk])
                for s, (mod, mk) in enumerate(((modL, "modL"), (modC, "modC"))):
                    b = (blk * 2 + s) % 8
                    for k in range(8):
                        P.op('pe', (lambda b=b, s=s, k=k, wt=wt: nc.tensor.matmul(
                            PS[b], rep[:, s, k, :], wt[:, k, :], start=(k == 0), stop=(k == 7))),
                            r=["rep", wk], w=[PK[b]])
                    sl = slice(blk * 512, (blk + 1) * 512)
                    P.op('dve', (lambda b=b, mod=mod, sl=sl: nc.vector.tensor_tensor(
                        out=mod[:, sl], in0=PS[b], in1=mod[:, sl], op=ALU.add)), r=[PK[b], mk], w=[mk])
            for s, (mod, mk) in enumerate(((modL, "modL"), (modC, "modC"))):
                for j, col in enumerate((1, 4)):
                    sl = slice(col * D, (col + 1) * D)
                    P.op('dve', (lambda mod=mod, sl=sl, j=j: nc.vector.scalar_tensor_tensor(
                        out=mod[:, sl], in0=mod[:, sl], scalar=1.0, in1=nw[:, j, :], op0=ALU.add, op1=ALU.mult)),
                        r=[mk, "nw"], w=[mk])
            P.dma('sp', MODS_d[0], modL[:], r=["modL"], w=["d_mods"])
            P.dma('sp', MODS_d[1], modC[:], r=["modC"], w=["d_mods"])

        def mv(mod, i):
            return MVEC[(mod, i)]

        def rms_mod(ph_rings, xs_ap, xs_key, A_ap, sh_ap, mod_keys, out_ap, out_key, eps_scale=1.0 / D):
            ss, sk = ph_rings['ss'].next()
            tmp, tk = ph_rings['hxtmp'].next()
            P.op('act', lambda: nc.scalar.activation(out=tmp[:], in_=xs_ap, func=AF.Square), r=[xs_key], w=[tk])
            P.op('dve', lambda: nc.vector.reduce_sum(out=ss[:, 0:1], in_=tmp[:], axis=AX.X), r=[tk], w=[sk])
            P.op('dve', lambda: nc.vector.tensor_scalar(out=ss[:, 1:2], in0=ss[:, 0:1], scalar1=eps_scale, scalar2=EPS,
                                                        op0=ALU.mult, op1=ALU.add), r=[sk], w=[sk])
            P.op('act', lambda: nc.scalar.activation(out=ss[:, 2:3], in_=ss[:, 1:2], func=AF.Sqrt), r=[sk], w=[sk])
            P.op('dve', lambda: nc.vector.reciprocal(out=ss[:, 3:4], in_=ss[:, 2:3]), r=[sk], w=[sk])
            P.op('dve', lambda: nc.vector.scalar_tensor_tensor(out=tmp[:], in0=xs_ap, scalar=ss[:, 3:4], in1=A_ap,
                                                               op0=ALU.mult, op1=ALU.mult),
                 r=[xs_key, sk] + mod_keys, w=[tk])
            if sh_ap is None:
                P.op('pool', lambda: nc.gpsimd.tensor_copy(out=out_ap, in_=tmp[:]), r=[tk], w=[out_key])
            else:
                P.op('pool', lambda: nc.gpsimd.tensor_tensor(out=out_ap, in0=tmp[:], in1=sh_ap, op=ALU.add),
                     r=[tk] + mod_keys, w=[out_key])

        def mk_rings(ph):
            return {'ss': Ring(nc, ph, "ss", 4, [128, 4], F32),
                    'hxtmp': Ring(nc, ph, "hxtmp", 2, [128, D], F32),
                    'xs': Ring(nc, ph, "xsr", 2, [128, D], F32)}

        xin_t = xin.rearrange("(t p) d -> t p d", p=128)
        XS1_t = XS1.rearrange("(t p) d -> t p d", p=128)
        XS2_t = XS2.rearrange("(t p) d -> t p d", p=128)
        XS3_t = XS3.rearrange("(t p) d -> t p d", p=128)
        out_t = out.rearrange("(t p) d -> t p d", p=128)

        with contextlib.ExitStack() as ph:
            adaln(0, ph)
            P.barrier()
        def dump(name, ap, keys):
            shape = list(ap.shape)
            t = nc.dram_tensor(name, shape, ap.dtype, kind="ExternalOutput").ap()
            P.dma('sp', t, ap, r=keys, w=["dbg_" + name])
        if stop_after == "ada":
            dump("dbg_modL", modL[:], ["modL"]); dump("dbg_modC", modC[:], ["modC"]); dump("dbg_rep", rep[:], ["rep"])
            P.finish()
            return nc
        with contextlib.ExitStack() as ph:
            R = mk_rings(ph)
            hx0 = sb("hx0", [128, 24, D], BF16, ph)
            bandsb = sb("bandsb", [128, NBAND, 512], BF16, ph)
            cbandsb = sb("cbandsb", [128, 8, 256], BF16, ph)
            wpl = sb("wpl", [128, 8, 256], BF16, ph)
            vecs = sb("vecs", [128, 2, D], F32, ph)
            AB = sb("AB", [128, 4, D], F32, ph)
            dT = Ring(nc, ph, "dT", 1, [128, 8, 512], BF16)
            yt = Ring(nc, ph, "yt", 2, [128, D], F32)
            P.dma('pool', cbandsb[:], cband, r=["d_cband"], w=["cbandsb"])
            P.dma('pool', wpl[:], w_pool.rearrange("g (c p) e -> p (g c) e", p=128), r=["d_wpool"], w=["wpl"])
            P.dma('sp', vecs[:, 0, :], pool_scale.partition_broadcast(128), r=["d_ps"], w=["vecs"])
            P.dma('sp', vecs[:, 1, :], b_pool.partition_broadcast(128), r=["d_bp"], w=["vecs"])
            load_mods(ph, [(st, ix) for st in "LC" for ix in (0, 1, 2)])
            for s, (mod, mkey) in enumerate((("L", "mvec"), ("C", "mvec"))):
                P.op('dve', (lambda s=s, mod=mod: nc.vector.tensor_tensor(out=AB[:, 2 * s, :], in0=mv(mod, 2), in1=vecs[:, 0, :],
                                                                          op=ALU.mult)), r=[mkey, "vecs"], w=["AB"])
                P.op('dve', (lambda s=s: nc.vector.tensor_tensor(out=AB[:, 2 * s + 1, :], in0=AB[:, 2 * s, :], in1=vecs[:, 1, :],
                                                                 op=ALU.mult)), r=["AB", "vecs"], w=["AB"])

            def pool_segment(is_ctx, in_tiles, base, out_blocks):
                mod, mkey = ("C", "mvec") if is_ctx else ("L", "mvec")
                seq0 = 0 if is_ctx else NCT
                for lt in in_tiles:
                    xt, xk = R['xs'].next()
                    P.dma('sp', xt[:], xin_t[seq0 + lt], r=["d_xin"], w=[xk])
                    rms_mod(R, xt[:], xk, mv(mod, 1), mv(mod, 0), [mkey], hx0[:, lt - base, :], ("hx0", lt - base))
                cur_band = [None]
                for (ot0, ntl, btype) in out_blocks:
                    ncol = ntl * 128
                    if not is_ctx and cur_band[0] != btype:
                        P.dma('pool', bandsb[:], band[btype], r=["d_band"], w=["bandsb"])
                        cur_band[0] = btype
                    dt_, dk = dT.next()
                    for g in range(4):
                        for cc in range(2):
                            b = 2 * g + cc
                            if is_ctx:
                                lst = [(j, cbandsb[:, g * 2 + j, 0:ncol]) for j in range(2)]
                                bk = "cbandsb"
                            else:
                                lst = []
                                for j in JREL[g]:
                                    jt = ot0 + j
                                    if jt < 0 or jt >= NLT:
                                        continue
                                    lst.append((jt, bandsb[:, BAND_IDX[(g, j)], 0:ncol]))
                                bk = "bandsb"
                            for n_, (jt, rhs) in enumerate(lst):
                                P.op('pe', (lambda b=b, jt=jt, rhs=rhs, n_=n_, L=len(lst), ch=b, ncol=ncol: nc.tensor.matmul(
                                    PS[b][:, 0:ncol], hx0[:, jt - base, ch * 128:(ch + 1) * 128], rhs,
                                    start=(n_ == 0), stop=(n_ == L - 1))),
                                    r=[("hx0", jt - base), bk], w=[PK[b]])
                            if b % 2 == 0:
                                P.op('act', (lambda b=b, ncol=ncol, dt_=dt_: nc.scalar.copy(out=dt_[:, b, 0:ncol], in_=PS[b][:, 0:ncol])),
                                     r=[PK[b]], w=[(dk, b)])
                            else:
                                P.op('dve', (lambda b=b, ncol=ncol, dt_=dt_: nc.vector.tensor_copy(out=dt_[:, b, 0:ncol], in_=PS[b][:, 0:ncol])),
                                     r=[PK[b]], w=[(dk, b)])
                    for t in range(ntl):
                        gt = seq0 + ot0 + t
                        b0 = 2 * (t % 4)
                        for g in range(4):
                            pb = b0 + g // 2
                            for cc in range(2):
                                P.op('pe', (lambda pb=pb, g=g, cc=cc, t=t, dt_=dt_: nc.tensor.matmul(
                                    PS[pb][:, (g % 2) * 256:(g % 2) * 256 + 256], dt_[:, 2 * g + cc, t * 128:(t + 1) * 128],
                                    wpl[:, 2 * g + cc, :], start=(cc == 0), stop=(cc == 1))),
                                    r=[(dk, 2 * g + cc), "wpl"], w=[PK[pb]])
                        xt, xk = R['xs'].next()
                        P.dma('sp', xt[:], xin_t[gt], r=["d_xin"], w=[xk])
                        ai = 2 if is_ctx else 0
                        y, yk = yt.next()
                        for h in range(2):
                            sl = slice(h * 512, (h + 1) * 512)
                            P.op('dve', (lambda y=y, sl=sl, h=h, b0=b0, ai=ai: nc.vector.tensor_tensor(
                                out=y[:, sl], in0=PS[b0 + h], in1=AB[:, ai, sl], op=ALU.mult)), r=[PK[b0 + h], "AB"], w=[(yk, h)])
                            P.op('pool', (lambda y=y, sl=sl, xt=xt: nc.gpsimd.tensor_tensor(
                                out=y[:, sl], in0=y[:, sl], in1=xt[:, sl], op=ALU.add)), r=[(yk, h), xk], w=[(yk, h)])
                            P.op('pool', (lambda y=y, sl=sl, ai=ai: nc.gpsimd.tensor_tensor(
                                out=y[:, sl], in0=y[:, sl], in1=AB[:, ai + 1, sl], op=ALU.add)), r=[(yk, h), "AB"], w=[(yk, h)])
                        P.dma('sp', XS1_t[gt], y[:], r=[(yk, 0), (yk, 1)], w=["d_XS1"])

            pool_segment(True, [0, 1], 0, [(0, 2, 0)])
            if stop_after == "pool_dbg":
                dump("dbg_hx0", hx0[:, 0:2, :], [("hx0", 0), ("hx0", 1)])
                dump("dbg_dT", dT.tiles[0][:], [("dT0", b) for b in range(8)])
                dump("dbg_AB", AB[:], ["AB"])
                dump("dbg_cb", cbandsb[:], ["cbandsb"])
                dump("dbg_wpl", wpl[:], ["wpl"])
                P.finish()
                return nc
            for seg in range(4):
                lo = max(0, 16 * seg - 4); hi = min(NLT, 16 * seg + 20)
                blocks = [(4 * b, 4, 0 if b == 0 else (2 if b == 15 else 1)) for b in range(4 * seg, 4 * seg + 4)]
                pool_segment(False, list(range(lo, hi)), lo, blocks)
            P.flush()
        if stop_after == "pool":
            P.finish()
            return nc
        P.barrier()

        def moe_phase(layer, src_t, dst_t, tiles, final):
            SBT = 8 if final else 10
            with contextlib.ExitStack() as ph:
                R = mk_rings(ph)
                load_mods(ph, [(st, ix) for st in ("LC" if not final else "L") for ix in (3, 4, 5)])
                hxf = Ring(nc, ph, "hxf", 2, [128, D], F32)
                hxTf = Ring(nc, ph, "hxTf", 1, [128, 8, 128], F32)
                hxTb = sb("hxTb", [128, 8, SBT * 128], BF16, ph)
                acc = sb("acc", [128, SBT, D], F32, ph)
                gates = sb("gates", [128, SBT, 32], F32, ph)
                wrs = sb("wrs", [128, 8, 36], F32, ph)
                brb = sb("brb", [128, 36], F32, ph)
                rt = Ring(nc, ph, "rt", 2, [128, 96], F32)
                wg = Ring(nc, ph, "wg", 2, [128, 8, 512], BF16)
                wu = Ring(nc, ph, "wu", 2, [128, 8, 512], BF16)
                wd = Ring(nc, ph, "wd", 2, [128, 4, D], BF16)
                sgb = Ring(nc, ph, "sgb", 1, [128, 512], BF16)
                hidT = Ring(nc, ph, "hidT", 2, [128, 4, 512], BF16)
                nfb = None
                if final:
                    nfb = sb("nfb", [128, D], F32, ph)
                    P.dma('sp', nfb[:], norm_final.partition_broadcast(128), r=["d_nf"], w=["nfb"])
                P.dma('sp', wrs[:], w_r[layer].rearrange("(k p) n -> p k n", p=128), r=["d_wr"], w=["wrs"])
                P.dma('sp', brb[:], b_r[layer].partition_broadcast(128), r=["d_br"], w=["brb"])
                def route(i, r_, rk, lvl, sparse_info=None):
                    lg = r_[:, 0:36]; m4 = r_[:, 36:37]; nm4 = r_[:, 37:38]; e4 = r_[:, 40:44]; s4 = r_[:, 38:39]
                    pg = r_[:, 39:40]; ohg = r_[:, 44:48]; sel = r_[:, 48:56]; m8 = r_[:, 56:64]; d21 = r_[:, 64:65]
                    e21 = r_[:, 65:66]; w1 = r_[:, 66:67]; w2 = r_[:, 67:68]; c1 = r_[:, 72:80]; c2 = r_[:, 80:88]
                    V = nc.vector
                    def dv(fn, rk=rk):
                        P.op('dve', fn, r=[rk], w=[rk])
                    P.op('dve', lambda lg=lg: V.tensor_tensor(out=lg, in0=PS[5][:, 0:36], in1=brb[:], op=ALU.add),
                         r=[PK[5], "brb"], w=[rk])
                    if lvl <= 2:
                        P.op('dve', (lambda i=i, lg=lg: V.tensor_copy(out=gates[:, i, :], in_=lg[:, 0:32])), r=[rk], w=[("gates", i)])
                        return
                    dv(lambda: V.reduce_max(out=m4, in_=lg[:, 0:4], axis=AX.X))
                    dv(lambda: V.tensor_scalar(out=nm4, in0=m4, scalar1=-1.0, scalar2=None, op0=ALU.mult))
                    P.op('act', lambda: nc.scalar.activation(out=e4, in_=lg[:, 0:4], func=AF.Exp, bias=nm4, scale=1.0),
                         r=[rk], w=[rk])
                    dv(lambda: V.reduce_sum(out=s4, in_=e4, axis=AX.X))
                    dv(lambda: V.reciprocal(out=pg, in_=s4))
                    dv(lambda: V.tensor_scalar(out=ohg, in0=lg[:, 0:4], scalar1=m4, scalar2=None, op0=ALU.is_equal))
                    dv(lambda: V.tensor_scalar(out=sel, in0=lg[:, 4:12], scalar1=ohg[:, 0:1], scalar2=None, op0=ALU.mult))
                    for g in range(1, 4):
                        dv(lambda g=g: V.scalar_tensor_tensor(out=sel, in0=lg[:, 4 + 8 * g:12 + 8 * g], scalar=ohg[:, g:g + 1],
                                                              in1=sel, op0=ALU.mult, op1=ALU.add))
                    dv(lambda: V.max(out=m8, in_=sel))
                    dv(lambda: V.tensor_tensor(out=d21, in0=m8[:, 1:2], in1=m8[:, 0:1], op=ALU.subtract))
                    P.op('act', lambda: nc.scalar.activation(out=e21, in_=d21, func=AF.Exp), r=[rk], w=[rk])
                    dv(lambda: V.tensor_scalar(out=e21, in0=e21, scalar1=1.0, scalar2=None, op0=ALU.add))
                    dv(lambda: V.reciprocal(out=w1, in_=e21))
                    dv(lambda: V.tensor_tensor(out=w1, in0=w1, in1=pg, op=ALU.mult))
                    dv(lambda: V.tensor_tensor(out=w2, in0=pg, in1=w1, op=ALU.subtract))
                    if sparse_info is not None:
                        sparse_info(r_, rk, sel, m8, ohg, w1, w2, dv)
                        return
                    dv(lambda: V.tensor_scalar(out=c1, in0=sel, scalar1=m8[:, 0:1], scalar2=w1, op0=ALU.is_equal, op1=ALU.mult))
                    dv(lambda: V.tensor_scalar(out=c2, in0=sel, scalar1=m8[:, 1:2], scalar2=w2, op0=ALU.is_equal, op1=ALU.mult))
                    dv(lambda: V.tensor_tensor(out=c1, in0=c1, in1=c2, op=ALU.add))
                    P.op('dve', (lambda i=i, c1=c1, ohg=ohg: V.tensor_tensor(
                        out=gates[:, i, :].rearrange("p (g e) -> p g e", g=4),
                        in0=c1.unsqueeze(1).to_broadcast([128, 4, 8]), in1=ohg.unsqueeze(2).to_broadcast([128, 4, 8]),
                        op=ALU.mult)), r=[rk], w=[("gates", i)])
                for s0 in range(0, len(tiles), SBT):
                    sbt = tiles[s0:s0 + SBT]
                    import os
                    if os.environ.get("MOE_NT"):
                        sbt = sbt[:int(os.environ["MOE_NT"])]
                    n_sb = len(sbt)
                    for i, gt in enumerate(sbt):
                        mod, mkey = ("C", "mvec") if gt < NCT else ("L", "mvec")
                        xt, xk = R['xs'].next()
                        P.dma('sp', xt[:], src_t[gt], r=["d_XS1" if layer == 0 else "d_XS3"], w=[xk])
                        hx, hk = hxf.next()
                        rms_mod(R, xt[:], xk, mv(mod, 4), mv(mod, 3), [mkey], hx[:], hk)
                        import os
                        if int(os.environ.get("MOE_LVL", "9")) == 0:
                            dump("dbg_hx%d" % i, hx[:], [hk])
                            continue
                        for k in range(8):
                            b = 6 + k // 4
                            P.op('pe', (lambda b=b, k=k, hx=hx: nc.tensor.transpose(
                                PS[b][:, (k % 4) * 128:(k % 4) * 128 + 128], hx[:, k * 128:(k + 1) * 128], ident)),
                                r=[hk, "msk"], w=[PK[b]])
                        hT, hTk = hxTf.next()
                        for h in range(2):
                            b = 6 + h
                            P.op('act', (lambda b=b, h=h, hT=hT: nc.scalar.copy(
                                out=hT[:, 4 * h:4 * h + 4, :], in_=PS[b].rearrange("p (k t) -> p k t", k=4))),
                                r=[PK[b]], w=[hTk])
                        P.op('pool', (lambda i=i, hT=hT: nc.gpsimd.tensor_copy(out=hxTb[:, :, i * 128:(i + 1) * 128], in_=hT[:])),
                             r=[hTk], w=[("hxTb", i)])
                        import os
                        lvl = int(os.environ.get("MOE_LVL", "9"))
                        if lvl <= 1:
                            continue
                        for k in range(8):
                            P.op('pe', (lambda k=k, hT=hT: nc.tensor.matmul(PS[5][:, 0:36], hT[:, k, :], wrs[:, k, :],
                                                                          start=(k == 0), stop=(k == 7))),
                                 r=[hTk, "wrs"], w=[PK[5]])
                        r_, rk = rt.next()
                        route(i, r_, rk, lvl)
                    if stop_after == "moe_b1":
                        lvl = int(os.environ.get("MOE_LVL", "9"))
                        if lvl == 0:
                            P.finish()
                            return "STOP"
                        if lvl > 1:
                            dump("dbg_rt0", rt.tiles[0][:], [rt.name + "0"]); dump("dbg_rt1", rt.tiles[1][:], [rt.name + "1"])
                            dump("dbg_gates", gates[:], [("gates", i) for i in range(n_sb)])
                        if os.environ.get("NO_HXTB") is None:
                            dump("dbg_hxTb", hxTb[:], [("hxTb", i) for i in range(n_sb)])
                        else:
                            dump("dbg_hxTf", hxTf.tiles[0][:], [hxTf.name + "0"])
                        P.finish()
                        return "STOP"
                    blocks = [(t0, min(4, n_sb - t0)) for t0 in range(0, n_sb, 4)]
                    hcn = 0
                    yn = 0
                    for e in range(32):
                        wgt, wgk = wg.next(); wut, wuk = wu.next(); wdt, wdk = wd.next()
                        P.dma('pool', wgt[:], w_e_gate[layer, e].rearrange("(k p) n -> p k n", p=128), r=["d_weg"], w=[wgk])
                        P.dma('pool', wut[:], w_e_up[layer, e].rearrange("(k p) n -> p k n", p=128), r=["d_weu"], w=[wuk])
                        P.dma('pool', wdt[:], w_e_down[layer, e].rearrange("(k p) n -> p k n", p=128), r=["d_wed"], w=[wdk])
                        for (t0, ntl) in blocks:
                            ncol = ntl * 128
                            cs = slice(t0 * 128, t0 * 128 + ncol)
                            rk_h = [("hxTb", t0 + j) for j in range(ntl)]
                            hid, hidk = hidT.next()
                            for hc in range(4):
                                gb = (hcn % 2) * 2; ub = gb + 1; hcn += 1
                                for k in range(8):
                                    P.op('pe', (lambda gb=gb, k=k, hc=hc, wgt=wgt, cs=cs, ncol=ncol: nc.tensor.matmul(
                                        PS[gb][:, 0:ncol], wgt[:, k, hc * 128:(hc + 1) * 128], hxTb[:, k, cs],
                                        start=(k == 0), stop=(k == 7))), r=[wgk] + rk_h, w=[PK[gb]])
                                for k in range(8):
                                    P.op('pe', (lambda ub=ub, k=k, hc=hc, wut=wut, cs=cs, ncol=ncol: nc.tensor.matmul(
                                        PS[ub][:, 0:ncol], wut[:, k, hc * 128:(hc + 1) * 128], hxTb[:, k, cs],
                                        start=(k == 0), stop=(k == 7))), r=[wuk] + rk_h, w=[PK[ub]])
                                sg, sgk = sgb.next()
                                P.op('act', (lambda sg=sg, gb=gb, ncol=ncol: nc.scalar.activation(
                                    out=sg[:, 0:ncol], in_=PS[gb][:, 0:ncol], func=AF.Silu)), r=[PK[gb]], w=[sgk])
                                P.op('dve', (lambda sg=sg, ub=ub, ncol=ncol, hid=hid, hc=hc: nc.vector.tensor_tensor(
                                    out=hid[:, hc, 0:ncol], in0=sg[:, 0:ncol], in1=PS[ub][:, 0:ncol], op=ALU.mult)),
                                    r=[sgk, PK[ub]], w=[(hidk, hc)])
                            for t in range(ntl):
                                i = t0 + t
                                for half in range(2):
                                    yb = 4 + yn % 2; yn += 1
                                    for hc in range(4):
                                        P.op('pe', (lambda yb=yb, hc=hc, t=t, half=half, hid=hid, wdt=wdt: nc.tensor.matmul(
                                            PS[yb], hid[:, hc, t * 128:(t + 1) * 128], wdt[:, hc, half * 512:(half + 1) * 512],
                                            start=(hc == 0), stop=(hc == 3))), r=[(hidk, hc), wdk], w=[PK[yb]])
                                    sl = slice(half * 512, (half + 1) * 512)
                                    if e == 0:
                                        P.op('dve', (lambda yb=yb, i=i, sl=sl, e=e: nc.vector.tensor_scalar(
                                            out=acc[:, i, sl], in0=PS[yb], scalar1=gates[:, i, e:e + 1], scalar2=None, op0=ALU.mult)),
                                            r=[PK[yb], ("gates", i)], w=[("acc", i, half)])
                                    else:
                                        P.op('dve', (lambda yb=yb, i=i, sl=sl, e=e: nc.vector.scalar_tensor_tensor(
                                            out=acc[:, i, sl], in0=PS[yb], scalar=gates[:, i, e:e + 1], in1=acc[:, i, sl],
                                            op0=ALU.mult, op1=ALU.add)), r=[PK[yb], ("gates", i), ("acc", i, half)], w=[("acc", i, half)])
                    for i, gt in enumerate(sbt):
                        mod, mkey = ("C", "mvec") if gt < NCT else ("L", "mvec")
                        xt, xk = R['xs'].next()
                        P.dma('sp', xt[:], src_t[gt], r=["d_XS1" if layer == 0 else "d_XS3"], w=[xk])
                        P.op('pool', (lambda i=i, mod=mod: nc.gpsimd.tensor_tensor(out=acc[:, i, :], in0=acc[:, i, :], in1=mv(mod, 5), op=ALU.mult)),
                             r=[("acc", i, 0), ("acc", i, 1), mkey], w=[("acc", i, 0), ("acc", i, 1)])
                        P.op('pool', (lambda i=i, xt=xt: nc.gpsimd.tensor_tensor(out=acc[:, i, :], in0=acc[:, i, :], in1=xt[:], op=ALU.add)),
                             r=[("acc", i, 0), ("acc", i, 1), xk], w=[("acc", i, 0), ("acc", i, 1)])
                        if not final:
                            P.dma('sp', dst_t[gt], acc[:, i, :], r=[("acc", i, 0), ("acc", i, 1)], w=["d_dst%d" % layer])
                        else:
                            o_, ok = hxf.next()
                            rms_mod(R, acc[:, i, :], ("acc", i, 0), nfb[:], None, ["nfb", ("acc", i, 1)], o_[:], ok)
                            P.dma('sp', dst_t[gt - NCT], o_[:], r=[ok], w=["d_out"])
                P.barrier()

        I32 = mybir.dt.int32
        NTS = 65
        NSLOT = NTS * 512
        HXB = dscr("HXB", [NT * 128, D], BF16); XG = dscr("XG", [NSLOT, D], BF16)
        WG = dscr("WG", [NSLOT, 1]); YG = dscr("YG", [NSLOT, D])
        HXB_t = HXB.rearrange("(t p) d -> t p d", p=128)
        W2 = {"g": w_e_gate.rearrange("l e (p k) n -> (l e p) (k n)", k=8), "u": w_e_up.rearrange("l e (p k) n -> (l e p) (k n)", k=8),
              "d": w_e_down.rearrange("l e d n -> (l e d) n")}

        def moe_sparse(layer, src_t, dst_t, tiles, final):
            T_ = len(tiles)
            skey = "d_XS1" if layer == 0 else "d_XS3"
            es2 = contextlib.ExitStack()
            with es2:
                cms = sb("cms", [128, 160], F32, es2)
                info = sb("info", [128, NT, 8], F32, es2)
                posi = sb("posi", [128, NT, 2], I32, es2)
                cum = sb("cum", [128, 32], F32, es2)
                offs = sb("offs", [128, 32], F32, es2)
                te = sb("te", [128, 80], F32, es2)
                tec = sb("tec", [128, 80], F32, es2)
                P.dma('sp', cms[:], cmisc, r=["d_cm"], w=["cms"])
                P.op('pool', lambda: nc.gpsimd.memset(cum[:], 0.0), r=[], w=["cum"])
                iota = cms[:, 0:32]; base = cms[:, 32:40]; svals = cms[:, 40:120]
                V = nc.vector; G = nc.gpsimd; A = nc.scalar
                with contextlib.ExitStack() as ph:
                    R = mk_rings(ph)
                    load_mods(ph, [(st, ix) for st in ("LC" if not final else "L") for ix in (3, 4)])
                    hxf = Ring(nc, ph, "hxf", 2, [128, D], F32)
                    hxb = Ring(nc, ph, "hxb", 2, [128, D], BF16)
                    hxTf = Ring(nc, ph, "hxTf", 2, [128, 8, 128], F32)
                    wrs = sb("wrs", [128, 8, 36], F32, ph); brb = sb("brb", [128, 36], F32, ph)
                    rt = Ring(nc, ph, "rt", 2, [128, 288], F32)
                    gates = None
                    P.dma('sp', wrs[:], w_r[layer].rearrange("(k p) n -> p k n", p=128), r=["d_wr"], w=["wrs"])
                    P.dma('sp', brb[:], b_r[layer].partition_broadcast(128), r=["d_br"], w=["brb"])

                    def route(i, r_, rk):
                        lg = r_[:, 0:36]; m4 = r_[:, 36:37]; nm4 = r_[:, 37:38]; e4 = r_[:, 40:44]; s4 = r_[:, 38:39]
                        pg = r_[:, 39:40]; ohg = r_[:, 44:48]; sel = r_[:, 48:56]; m8 = r_[:, 56:64]; d21 = r_[:, 64:65]
                        e21 = r_[:, 65:66]; w1 = r_[:, 66:67]; w2 = r_[:, 67:68]; eq = r_[:, 72:88]
                        oh1 = r_[:, 96:128]; oh2 = r_[:, 128:160]; ohs = r_[:, 160:192]; rkt = r_[:, 192:224]; tmp = r_[:, 224:256]

                        def dv(fn):
                            P.op('dve', fn, r=[rk], w=[rk])
                        P.op('dve', lambda: V.tensor_tensor(out=lg, in0=PS[5 - 4 * (i % 2)][:, 0:36], in1=brb[:], op=ALU.add), r=[PK[5 - 4 * (i % 2)], "brb"], w=[rk])
                        dv(lambda: V.reduce_max(out=m4, in_=lg[:, 0:4], axis=AX.X))
                        dv(lambda: V.tensor_scalar(out=nm4, in0=m4, scalar1=-1.0, scalar2=None, op0=ALU.mult))
                        yield
                        P.op('act', lambda: A.activation(out=e4, in_=lg[:, 0:4], func=AF.Exp, bias=nm4, scale=1.0), r=[rk], w=[rk])
                        yield
                        dv(lambda: V.reduce_sum(out=s4, in_=e4, axis=AX.X))
                        dv(lambda: V.reciprocal(out=pg, in_=s4))
                        dv(lambda: V.tensor_scalar(out=ohg, in0=lg[:, 0:4], scalar1=m4, scalar2=None, op0=ALU.is_equal))
                        dv(lambda: V.tensor_scalar(out=sel, in0=lg[:, 4:12], scalar1=ohg[:, 0:1], scalar2=None, op0=ALU.mult))
                        for g in range(1, 4):
                            dv(lambda g=g: V.scalar_tensor_tensor(out=sel, in0=lg[:, 4 + 8 * g:12 + 8 * g], scalar=ohg[:, g:g + 1],
                                                                  in1=sel, op0=ALU.mult, op1=ALU.add))
                        yield
                        dv(lambda: V.max(out=m8, in_=sel))
                        dv(lambda: V.tensor_tensor(out=d21, in0=m8[:, 1:2], in1=m8[:, 0:1], op=ALU.subtract))
                        yield
                        P.op('act', lambda: A.activation(out=e21, in_=d21, func=AF.Exp), r=[rk], w=[rk])
                        yield
                        dv(lambda: V.tensor_scalar(out=e21, in0=e21, scalar1=1.0, scalar2=None, op0=ALU.add))
                        dv(lambda: V.reciprocal(out=w1, in_=e21))
                        dv(lambda: V.tensor_tensor(out=info[:, i, 2:3], in0=w1, in1=pg, op=ALU.mult))
                        dv(lambda: V.tensor_tensor(out=info[:, i, 5:6], in0=pg, in1=info[:, i, 2:3], op=ALU.subtract))
                        dv(lambda: V.tensor_scalar(out=eq[:, 0:8], in0=sel, scalar1=m8[:, 0:1], scalar2=None, op0=ALU.is_equal))
                        dv(lambda: V.tensor_scalar(out=eq[:, 8:16], in0=sel, scalar1=m8[:, 1:2], scalar2=None, op0=ALU.is_equal))
                        for j, oh in enumerate((oh1, oh2)):
                            dv(lambda j=j, oh=oh: V.tensor_tensor(out=oh.rearrange("p (g e) -> p g e", g=4),
                                                                  in0=eq[:, 8 * j:8 * j + 8].unsqueeze(1).to_broadcast([128, 4, 8]),
                                                                  in1=ohg.unsqueeze(2).to_broadcast([128, 4, 8]), op=ALU.mult))
                        dv(lambda: V.tensor_tensor(out=ohs, in0=oh1, in1=oh2, op=ALU.add))
                        yield
                        P.op('pe', lambda: nc.tensor.matmul(PS[4 - 4 * (i % 2)][:, 0:32], msk[:, 6, :], ohs, start=True, stop=True), r=[rk, "msk"], w=[PK[4 - 4 * (i % 2)]])
                        P.op('pe', lambda: nc.tensor.matmul(PS[4 - 4 * (i % 2)][:, 32:64], msk[:, 3, :], ohs, start=True, stop=True), r=[rk, "msk"], w=[PK[4 - 4 * (i % 2)]])
                        yield
                        P.op('dve', lambda: V.tensor_tensor(out=rkt, in0=PS[4 - 4 * (i % 2)][:, 0:32], in1=cum[:], op=ALU.add), r=[PK[4 - 4 * (i % 2)], "cum", rk], w=[rk])
                        P.op('dve', lambda: V.tensor_tensor(out=cum[:], in0=cum[:], in1=PS[4 - 4 * (i % 2)][:, 32:64], op=ALU.add), r=[PK[4 - 4 * (i % 2)], "cum", rk], w=["cum"])
                        for j, oh in enumerate((oh1, oh2)):
                            dv(lambda oh=oh: V.tensor_tensor(out=tmp, in0=oh, in1=rkt, op=ALU.mult))
                            dv(lambda j=j: V.reduce_sum(out=info[:, i, 3 * j + 1:3 * j + 2], in_=tmp, axis=AX.X))
                            dv(lambda oh=oh: V.tensor_tensor(out=tmp, in0=oh, in1=iota, op=ALU.mult))
                            dv(lambda j=j: V.reduce_sum(out=info[:, i, 3 * j:3 * j + 1], in_=tmp, axis=AX.X))

                    def sa_tile(i, gt):
                        mod, mkey = ("C", "mvec") if gt < NCT else ("L", "mvec")
                        xt, xk = R['xs'].next()
                        P.dma('sp', xt[:], src_t[gt], r=[skey], w=[xk])
                        hx, hk = hxf.next()
                        rms_mod(R, xt[:], xk, mv(mod, 4), mv(mod, 3), [mkey], hx[:], hk)
                        yield
                        hb, hbk = hxb.next()
                        P.op('pool', (lambda hb=hb, hx=hx: G.tensor_copy(out=hb[:], in_=hx[:])), r=[hk], w=[hbk])
                        P.dma('sp', HXB_t[gt], hb[:], r=[hbk], w=["d_HXB"])
                        for k in range(8):
                            b = (6 if i % 2 == 0 else 2) + k // 4
                            P.op('pe', (lambda b=b, k=k, hx=hx: nc.tensor.transpose(
                                PS[b][:, (k % 4) * 128:(k % 4) * 128 + 128], hx[:, k * 128:(k + 1) * 128], ident)),
                                r=[hk, "msk"], w=[PK[b]])
                        yield
                        hT, hTk = hxTf.next()
                        for h in range(2):
                            b = (6 if i % 2 == 0 else 2) + h
                            P.op('act', (lambda b=b, h=h, hT=hT: A.copy(
                                out=hT[:, 4 * h:4 * h + 4, :], in_=PS[b].rearrange("p (k t) -> p k t", k=4))), r=[PK[b]], w=[hTk])
                        for k in range(8):
                            P.op('pe', (lambda k=k, hT=hT: nc.tensor.matmul(PS[5 - 4 * (i % 2)][:, 0:36], hT[:, k, :], wrs[:, k, :],
                                                                          start=(k == 0), stop=(k == 7))), r=[hTk, "wrs"], w=[PK[5 - 4 * (i % 2)]])
                        yield
                        r_, rk = rt.next()
                        yield from route(i, r_, rk)

                    run_interleaved(P, (sa_tile(i, gt) for i, gt in enumerate(tiles)), 2)
                    ci = sb("ci", [128, 32], I32, ph); pn = sb("pn", [128, 32], F32, ph)
                    sa = sb("sa", [128, 32], F32, ph); sb_ = sb("sb_", [128, 32], F32, ph)
                    P.op('dve', lambda: V.tensor_copy(out=ci[:], in_=cum[:]), r=["cum"], w=["ci"])
                    P.op('dve', lambda: V.tensor_scalar(out=ci[:], in0=ci[:], scalar1=511, scalar2=None, op0=ALU.add), r=["ci"], w=["ci"])
                    P.op('dve', lambda: V.tensor_scalar(out=ci[:], in0=ci[:], scalar1=9, scalar2=None, op0=ALU.arith_shift_right), r=["ci"], w=["ci"])
                    P.op('dve', lambda: V.tensor_scalar(out=ci[:], in0=ci[:], scalar1=9, scalar2=None, op0=ALU.logical_shift_left), r=["ci"], w=["ci"])
                    P.op('dve', lambda: V.tensor_copy(out=pn[:], in_=ci[:]), r=["ci"], w=["pn"])
                    P.op('dve', lambda: V.tensor_copy(out=sa[:], in_=pn[:]), r=["pn"], w=["sa"])
                    a_, b_ = sa, sb_
                    ak, bk = "sa", "sb_"
                    for sh in (1, 2, 4, 8, 16):
                        P.op('dve', (lambda a_=a_, b_=b_, sh=sh: V.tensor_copy(out=b_[:, 0:sh], in_=a_[:, 0:sh])), r=[ak], w=[bk])
                        P.op('dve', (lambda a_=a_, b_=b_, sh=sh: V.tensor_tensor(out=b_[:, sh:32], in0=a_[:, sh:32], in1=a_[:, 0:32 - sh], op=ALU.add)),
                             r=[ak, bk], w=[bk])
                        a_, b_, ak, bk = b_, a_, bk, ak
                    incl, inclk = a_, ak
                    P.op('dve', lambda: V.tensor_tensor(out=offs[:], in0=incl[:], in1=pn[:], op=ALU.subtract), r=[inclk, "pn"], w=["offs"])
                    P.op('pool', lambda: G.memset(te[:], 0.0), r=[], w=["te"])
                    for e in range(32):
                        P.op('dve', (lambda e=e: V.scalar_tensor_tensor(out=te[:], in0=svals, scalar=incl[:, e:e + 1], in1=te[:], op0=ALU.is_ge, op1=ALU.add)),
                             r=[inclk, "te", "cms"], w=["te"])
                    P.op('dve', lambda: V.tensor_scalar(out=tec[:], in0=te[:], scalar1=31.0, scalar2=None, op0=ALU.min), r=["te"], w=["tec"])
                    P.barrier()
                if stop_after == "sA":
                    P.finish(); return "STOP"
                with contextlib.ExitStack() as ph:
                    zt = sb("zt", [128, 4096], BF16, ph)
                    hb2 = Ring(nc, ph, "hb2", 3, [128, D], BF16)
                    pt = Ring(nc, ph, "pt", 2, [128, 72], F32)
                    P.op('pool', lambda: G.memset(zt[:], 0.0), r=[], w=["zt"])
                    XGz = XG.rearrange("(q p f) d -> q p (f d)", p=128, f=4)
                    for q in range(NTS):
                        P.dma('sp', XGz[q], zt[:], r=["zt"], w=["d_XG"])
                    for i, gt in enumerate(tiles):
                        p_, pk_ = pt.next()
                        for j in range(2):
                            P.op('dve', (lambda p_=p_, i=i, j=j: V.tensor_scalar(out=p_[:, 0:32], in0=iota, scalar1=info[:, i, 3 * j:3 * j + 1], scalar2=None, op0=ALU.is_equal)),
                                 r=["info", "cms", pk_], w=[pk_])
                            P.op('dve', (lambda p_=p_: V.tensor_tensor(out=p_[:, 0:32], in0=p_[:, 0:32], in1=offs[:], op=ALU.mult)), r=[pk_, "offs"], w=[pk_])
                            P.op('dve', (lambda p_=p_, j=j: V.reduce_sum(out=p_[:, 32 + j:33 + j], in_=p_[:, 0:32], axis=AX.X)), r=[pk_], w=[pk_])
                            P.op('dve', (lambda p_=p_, i=i, j=j: V.tensor_tensor(out=p_[:, 32 + j:33 + j], in0=p_[:, 32 + j:33 + j], in1=info[:, i, 3 * j + 1:3 * j + 2], op=ALU.add)),
                                 r=[pk_, "info"], w=[pk_])
                        P.op('dve', (lambda p_=p_, i=i: V.tensor_copy(out=posi[:, i, :], in_=p_[:, 32:34])), r=[pk_], w=[("posi", i)])
                        hb, hbk = hb2.next()
                        P.dma('sp', hb[:], HXB_t[gt], r=["d_HXB"], w=[hbk])
                        for j in range(2):
                            P.op('pool', (lambda hb=hb, i=i, j=j: G.indirect_dma_start(
                                out=XG[:, :], out_offset=bass.IndirectOffsetOnAxis(ap=posi[:, i, j:j + 1], axis=0), in_=hb[:, :], in_offset=None)),
                                r=[hbk, ("posi", i), "d_XG"], w=["d_XGs"], dma=True)
                            P.op('pool', (lambda i=i, j=j: G.indirect_dma_start(
                                out=WG[:, :], out_offset=bass.IndirectOffsetOnAxis(ap=posi[:, i, j:j + 1], axis=0), in_=info[:, i, 3 * j + 2:3 * j + 3], in_offset=None)),
                                r=["info", ("posi", i)], w=["d_WG"], dma=True)
                        P.flush()
                    P.barrier()
                if stop_after == "sC":
                    P.finish(); return "STOP"
                with contextlib.ExitStack() as ph:
                    wg = Ring(nc, ph, "wg", 2, [128, 8, 512], BF16); wu = Ring(nc, ph, "wu", 2, [128, 8, 512], BF16)
                    wd = Ring(nc, ph, "wd", 2, [128, 4, D], BF16)
                    xg = Ring(nc, ph, "xg", 2, [128, 4, D], BF16); xT = Ring(nc, ph, "xT", 2, [128, 8, 512], BF16)
                    wgt = Ring(nc, ph, "wgt", 2, [128, 4], F32)
                    idf = Ring(nc, ph, "idf", 2, [128, 12], F32); idi = Ring(nc, ph, "idi", 2, [128, 12], I32)
                    sgb = Ring(nc, ph, "sgb", 2, [128, 512], BF16); hidT = Ring(nc, ph, "hidT", 2, [128, 4, 512], BF16)
                    yg = Ring(nc, ph, "yg", 2, [128, D], F32)
                    hcn = [0]; yn = [0]

                    def slot_tile(s_):
                        f_, fk = idf.next(); ii, ik = idi.next()
                        P.op('dve', lambda: V.scalar_tensor_tensor(out=f_[:, 0:1], in0=tec[:, s_:s_ + 1], scalar=128.0, in1=base[:, 0:1],
                                                                   op0=ALU.mult, op1=ALU.add), r=["tec", "cms"], w=[fk])
                        P.op('dve', lambda: V.scalar_tensor_tensor(out=f_[:, 8:12], in0=tec[:, s_:s_ + 1].to_broadcast([128, 4]), scalar=512.0, in1=base[:, 0:4],
                                                                   op0=ALU.mult, op1=ALU.add), r=["tec", "cms", fk], w=[fk])
                        if layer > 0:
                            P.op('dve', lambda: V.tensor_scalar(out=f_[:, 0:1], in0=f_[:, 0:1], scalar1=float(layer * 32 * 128), scalar2=None, op0=ALU.add), r=[fk], w=[fk])
                            P.op('dve', lambda: V.tensor_scalar(out=f_[:, 8:12], in0=f_[:, 8:12], scalar1=float(layer * 32 * 512), scalar2=None, op0=ALU.add), r=[fk], w=[fk])
                        P.op('dve', lambda: V.tensor_copy(out=ii[:], in_=f_[:]), r=[fk], w=[ik])
                        wgt_, wgk = wg.next(); wut, wuk = wu.next(); wdt, wdk = wd.next()
                        P.op('pool', lambda: G.indirect_dma_start(out=wgt_[:].rearrange("p k n -> p (k n)"), out_offset=None, in_=W2["g"],
                                                                  in_offset=bass.IndirectOffsetOnAxis(ap=ii[:, 0:1], axis=0)),
                             r=[ik], w=[(wgk, k) for k in range(8)], dma=True)
                        P.op('pool', lambda: G.indirect_dma_start(out=wut[:].rearrange("p k n -> p (k n)"), out_offset=None, in_=W2["u"],
                                                                  in_offset=bass.IndirectOffsetOnAxis(ap=ii[:, 0:1], axis=0)),
                             r=[ik], w=[(wuk, k) for k in range(8)], dma=True)
                        for k in range(4):
                            P.op('pool', (lambda k=k: G.indirect_dma_start(out=wdt[:, k, :], out_offset=None, in_=W2["d"],
                                                                         in_offset=bass.IndirectOffsetOnAxis(ap=ii[:, 8 + k:9 + k], axis=0))),
                                 r=[ik], w=[(wdk, k)], dma=True)
                        yield
                        x_, xk_ = xg.next(); xt_, xtk = xT.next(); g4, g4k = wgt.next()
                        P.dma('sp', x_[:], XG[s_ * 512:(s_ + 1) * 512, :].rearrange("(t p) d -> p t d", p=128), r=["d_XGs", "d_XG"], w=[xk_])
                        P.dma('sp', g4[:], WG[s_ * 512:(s_ + 1) * 512, :].rearrange("(t p) o -> p (t o)", p=128), r=["d_WG"], w=[g4k], allow_slow_non_contiguous=True)
                        yield
                        for t in range(4):
                            b = 6 + t % 2
                            psb = PS[b].bitcast(BF16)
                            for k in range(8):
                                P.op('pe', (lambda psb=psb, k=k, t=t: nc.tensor.transpose(psb[:, k * 128:(k + 1) * 128], x_[:, t, k:D:8], identb[:])),
                                     r=[xk_, "identb"], w=[PK[b]])
                            P.op('act', (lambda psb=psb, t=t: A.copy(out=xt_[:, :, t * 128:(t + 1) * 128], in_=psb.rearrange("p (k c) -> p k c", k=8))),
                                 r=[PK[b]], w=[(xtk, t)])
                        yield
                        xtkeys = [(xtk, t) for t in range(4)]
                        hid, hidk = hidT.next()
                        for hc in range(4):
                            gb = (hcn[0] % 2) * 2; ub = gb + 1; hcn[0] += 1
                            for k in range(8):
                                P.op('pe', (lambda gb=gb, k=k, hc=hc: nc.tensor.matmul(PS[gb], wgt_[:, k, hc * 128:(hc + 1) * 128], xt_[:, k, :],
                                                                                    start=(k == 0), stop=(k == 7))), r=[(wgk, k)] + xtkeys, w=[PK[gb]])
                            for k in range(8):
                                P.op('pe', (lambda ub=ub, k=k, hc=hc: nc.tensor.matmul(PS[ub], wut[:, k, hc * 128:(hc + 1) * 128], xt_[:, k, :],
                                                                                    start=(k == 0), stop=(k == 7))), r=[(wuk, k)] + xtkeys, w=[PK[ub]])
                            yield
                            sg, sgk = sgb.next()
                            P.op('act', (lambda sg=sg, gb=gb: A.activation(out=sg[:], in_=PS[gb], func=AF.Silu)), r=[PK[gb]], w=[sgk])
                            P.op('dve', (lambda sg=sg, ub=ub, hc=hc: V.tensor_tensor(out=hid[:, hc, :], in0=sg[:], in1=PS[ub], op=ALU.mult)),
                                 r=[sgk, PK[ub]], w=[(hidk, hc)])
                        for t in range(4):
                            yield
                            y_, yk = yg.next()
                            for half in range(2):
                                yb_ = 4 + yn[0] % 2; yn[0] += 1
                                for hc in range(4):
                                    P.op('pe', (lambda yb_=yb_, hc=hc, t=t, half=half: nc.tensor.matmul(
                                        PS[yb_], hid[:, hc, t * 128:(t + 1) * 128], wdt[:, hc, half * 512:(half + 1) * 512],
                                        start=(hc == 0), stop=(hc == 3))), r=[(hidk, hc), (wdk, hc)], w=[PK[yb_]])
                                P.op('act', (lambda yb_=yb_, half=half, t=t, y_=y_: A.activation(out=y_[:, half * 512:(half + 1) * 512], in_=PS[yb_], func=AF.Copy,
                                                                                            scale=g4[:, t:t + 1])), r=[PK[yb_], g4k], w=[(yk, half)])
                            P.dma('sp', YG[s_ * 512 + t * 128:s_ * 512 + (t + 1) * 128, :], y_[:], r=[(yk, 0), (yk, 1)], w=["d_YG"])

                    run_interleaved(P, (slot_tile(s_) for s_ in range(NTS)), 2)
                    P.barrier()
                if stop_after == "sD":
                    P.finish(); return "STOP"
                with contextlib.ExitStack() as ph:
                    R = mk_rings(ph)
                    load_mods(ph, [(st, 5) for st in ("LC" if not final else "L")])
                    y1 = Ring(nc, ph, "y1", 2, [128, D], F32); y2 = Ring(nc, ph, "y2", 2, [128, D], F32)
                    ofin = Ring(nc, ph, "ofin", 2, [128, D], F32)
                    nfb = None
                    if final:
                        nfb = sb("nfb", [128, D], F32, ph)
                        P.dma('sp', nfb[:], norm_final.partition_broadcast(128), r=["d_nf"], w=["nfb"])

                    def comb(i, gt):
                        mod, mkey = ("C", "mvec") if gt < NCT else ("L", "mvec")
                        a_, ak_ = y1.next(); b_, bk_ = y2.next()
                        for j, (dst, dk_) in enumerate(((a_, ak_), (b_, bk_))):
                            P.op('pool', (lambda dst=dst, j=j: G.indirect_dma_start(out=dst[:, :], out_offset=None, in_=YG[:, :],
                                                                                  in_offset=bass.IndirectOffsetOnAxis(ap=posi[:, i, j:j + 1], axis=0))),
                                 r=[("posi", i), "d_YG"], w=[dk_], dma=True)
                        xt, xk = R['xs'].next()
                        P.dma('sp', xt[:], src_t[gt], r=[skey], w=[xk])
                        P.op('pool', lambda: G.tensor_tensor(out=a_[:], in0=a_[:], in1=b_[:], op=ALU.add), r=[ak_, bk_], w=[ak_])
                        P.op('dve', lambda: V.tensor_tensor(out=a_[:], in0=a_[:], in1=mv(mod, 5), op=ALU.mult), r=[ak_, mkey], w=[ak_])
                        P.op('pool', lambda: G.tensor_tensor(out=a_[:], in0=a_[:], in1=xt[:], op=ALU.add), r=[ak_, xk], w=[ak_])
                        if not final:
                            P.dma('sp', dst_t[gt], a_[:], r=[ak_], w=["d_dst%d" % layer])
                        else:
                            o_, ok = ofin.next()
                            rms_mod(R, a_[:], ak_, nfb[:], None, ["nfb"], o_[:], ok)
                            P.dma('sp', dst_t[gt - NCT], o_[:], r=[ok], w=["d_out"])

                    for i, gt in enumerate(tiles):
                        comb(i, gt)
                    P.barrier()

        import os
        MOE = moe_sparse if os.environ.get("DENSE_MOE") is None else moe_phase
        if MOE(0, XS1_t, XS2_t, list(range(NT)), False) == "STOP":
            return nc
        if stop_after == "moe0":
            P.finish()
            return nc

        bfd = lambda name, shape: dscr(name, shape, BF16)
        QT_d = bfd("QT_d", [NT, 128, 8, 128]); KT_d = bfd("KT_d", [NT, 128, 8, 128])
        KK_d = bfd("KK_d", [NT, 128, 8, 128]); VV_d = bfd("VV_d", [NT, 128, 8, 128])
        ZZ_d = bfd("ZZ_d", [NT, 128, D]); GB_d = dscr("GB_d", [NT, 128, 32])
        OF_d = dscr("OF_d", [2, NLT, 128, D])
        with contextlib.ExitStack() as ph:
            adaln(1, ph)
            P.barrier()

        with contextlib.ExitStack() as ph:
            R = mk_rings(ph)
            load_mods(ph, [(st, ix) for st in "LC" for ix in (0, 1)])
            SBT1 = 4
            hxf = Ring(nc, ph, "c1hx", 1, [128, D], F32)
            hxT = sb("c1hxT", [128, 8, (SBT1 + 2) * 128], BF16, ph)
            pT = Ring(nc, ph, "pT", 2, [128, SBT1 * 128 + 4], F32)
            cv = Ring(nc, ph, "cv", 2, [128, SBT1 * 128], F32)
            sqb = Ring(nc, ph, "sqb", 1, [128, SBT1 * 128], BF16)
            rsb = Ring(nc, ph, "rsb", 1, [128, SBT1 * 128], F32)
            kf = Ring(nc, ph, "kf", 1, [128, SBT1 * 128], F32)
            qst = sb("qst", [128, SBT1, 8, 128], BF16, ph); kst = sb("kst", [128, SBT1, 8, 128], BF16, ph)
            ktst = sb("ktst", [128, SBT1, 8, 128], BF16, ph); vtst = sb("vtst", [128, SBT1, 8, 128], BF16, ph)
            win_sb = sb("win_sb", [128, 8, 3072], BF16, ph)
            wz = sb("wz", [128, 8, 1056], BF16, ph)
            wcs = sb("wcs", [128, 24, 4], F32, ph)
            onesb = sb("onesb", [128, 128], BF16, ph)
            c16 = sb("c16", [128, 2, 16], F32, ph)
            zsb = Ring(nc, ph, "zsb", 1, [128, D], BF16)
            gbr = Ring(nc, ph, "gbr", 2, [128, 64], F32)
            P.dma('pool', wz[:], w_dn_in.rearrange("(k p) n -> p k n", p=128)[:, :, 3072:4128], r=["d_win"], w=["wz"])
            P.dma('sp', wcs[:], wconv, r=["d_wconv"], w=["wcs"])
            P.op('dve', lambda: nc.vector.tensor_copy(out=onesb[:], in_=msk[:, 3, :]), r=["msk"], w=["onesb"])
            P.dma('sp', c16[:, 0, :], dn_dt_bias.partition_broadcast(128), r=["d_dtb"], w=["c16"])
            P.dma('sp', c16[:, 1, :], dn_a_log.partition_broadcast(128), r=["d_alog"], w=["c16"])
            P.op('act', lambda: nc.scalar.activation(out=c16[:, 1, :], in_=c16[:, 1, :], func=AF.Exp), r=["c16"], w=["c16"])
            P.op('dve', lambda: nc.vector.tensor_scalar(out=c16[:, 1, :], in0=c16[:, 1, :], scalar1=-1.0, scalar2=None, op0=ALU.mult),
                 r=["c16"], w=["c16"])
            win_v = w_dn_in.rearrange("(k p) n -> p k n", p=128)
            for j6 in range(6):
                P.dma('pool', win_sb[:, :, j6 * 512:(j6 + 1) * 512], win_v[:, :, j6 * 512:(j6 + 1) * 512], r=["d_win"], w=[("win_sb", j6)])

            def c1_tile_norm(gt, slot, mod, mkey):
                xt, xk = R['xs'].next()
                P.dma('sp', xt[:], XS2_t[gt], r=["d_dst0"], w=[xk])
                hx, hk = hxf.next()
                rms_mod(R, xt[:], xk, mv(mod, 1), mv(mod, 0), [mkey], hx[:], hk)
                for k in range(8):
                    b = 6 + k // 4
                    P.op('pe', (lambda b=b, k=k, hx=hx: nc.tensor.transpose(
                        PS[b][:, (k % 4) * 128:(k % 4) * 128 + 128], hx[:, k * 128:(k + 1) * 128], ident)),
                        r=[hk, "msk"], w=[PK[b]])
                for h in range(2):
                    b = 6 + h
                    P.op('act', (lambda b=b, h=h, slot=slot: nc.scalar.copy(
                        out=hxT[:, 4 * h:4 * h + 4, slot * 128:(slot + 1) * 128],
                        in_=PS[b].rearrange("p (k t) -> p k t", k=4))), r=[PK[b]], w=[("hxT", slot)])

            def c1_zab(gt, slot, is_ctx):
                hk = [("hxT", slot)]
                if not is_ctx:
                    zs, zk = zsb.next()
                    for half in range(2):
                        b = 4 + half
                        for k in range(8):
                            P.op('pe', (lambda b=b, k=k, half=half, slot=slot: nc.tensor.matmul(
                                PS[b], hxT[:, k, slot * 128:(slot + 1) * 128], wz[:, k, half * 512:(half + 1) * 512],
                                start=(k == 0), stop=(k == 7))), r=hk + ["wz"], w=[PK[b]])
                        P.op('act', (lambda b=b, half=half, zs=zs: nc.scalar.activation(
                            out=zs[:, half * 512:(half + 1) * 512], in_=PS[b], func=AF.Silu)), r=[PK[b]], w=[(zk, half)])
                    P.dma('sp', ZZ_d[gt], zs[:], r=[(zk, 0), (zk, 1)], w=["d_ZZ"])
                for k in range(8):
                    P.op('pe', (lambda k=k, slot=slot: nc.tensor.matmul(
                        PS[3][:, 0:32], hxT[:, k, slot * 128:(slot + 1) * 128], wz[:, k, 1024:1056],
                        start=(k == 0), stop=(k == 7))), r=hk + ["wz"], w=[PK[3]])
                g_, gk = gbr.next()
                V = nc.vector
                ab = g_[:, 0:32].rearrange("p (f h) -> p f h", f=4)
                o4 = g_[:, 32:64].rearrange("p (f h) -> p f h", f=4)
                P.op('dve', lambda: V.tensor_copy(out=g_[:, 0:32], in_=PS[3][:, 0:32]), r=[PK[3]], w=[gk])
                P.op('dve', lambda: V.tensor_tensor(out=o4[:, 0::2, :], in0=ab[:, 0::2, :],
                                                    in1=c16[:, 0, :].rearrange("p (d h) -> p d h", d=2), op=ALU.add),
                     r=[gk, "c16"], w=[gk])
                P.op('act', lambda: nc.scalar.activation(out=o4[:, 0::2, :], in_=o4[:, 0::2, :], func=AF.Exp), r=[gk], w=[gk])
                P.op('dve', lambda: V.tensor_scalar(out=o4[:, 0::2, :], in0=o4[:, 0::2, :], scalar1=1.0, scalar2=None, op0=ALU.add),
                     r=[gk], w=[gk])
                P.op('act', lambda: nc.scalar.activation(out=o4[:, 0::2, :], in_=o4[:, 0::2, :], func=AF.Ln), r=[gk], w=[gk])
                P.op('dve', lambda: V.tensor_tensor(out=o4[:, 0::2, :], in0=o4[:, 0::2, :],
                                                    in1=c16[:, 1, :].rearrange("p (d h) -> p d h", d=2), op=ALU.mult),
                     r=[gk, "c16"], w=[gk])
                P.op('act', lambda: nc.scalar.activation(out=o4[:, 1::2, :], in_=ab[:, 1::2, :], func=AF.Sigmoid), r=[gk], w=[gk])
                P.dma('sp', GB_d[gt], g_[:, 32:64], r=[gk], w=["d_GB"])

            def c1_chunk(cc, seq0, nseq, t0, t1, hbase_tile):
                W = (t1 - t0) * 128
                ntile = t1 - t0
                wk = ("win_sb", cc // 4)
                p_, pk = pT.next()
                tok0 = t0 * 128 - 2
                lo = max(tok0, 0); hi = min(t1 * 128 + 1, nseq * 128)
                if lo > tok0:
                    P.op('pool', lambda: nc.gpsimd.memset(p_[:, 0:lo - tok0], 0.0), r=[], w=[(pk, 'l')])
                if hi < t1 * 128 + 1:
                    P.op('pool', lambda: nc.gpsimd.memset(p_[:, hi - tok0:W + 3], 0.0), r=[], w=[(pk, 'r')])
                a = lo
                wi = 0
                while a < hi:
                    b_ = min(a + 512, hi)
                    bank = wi % 3
                    hk = [("hxT", s_) for s_ in range((a // 128) - hbase_tile, ((b_ - 1) // 128) - hbase_tile + 1)]
                    for k in range(8):
                        P.op('pe', (lambda k=k, bank=bank, a=a, b_=b_: nc.tensor.matmul(
                            PS[bank][:, 0:b_ - a], win_sb[:, k, cc * 128:(cc + 1) * 128], hxT[:, k, a - hbase_tile * 128:b_ - hbase_tile * 128],
                            start=(k == 0), stop=(k == 7))), r=[wk] + hk, w=[PK[bank]])
                    P.op('act', (lambda bank=bank, a=a, b_=b_: nc.scalar.copy(out=p_[:, a - tok0:b_ - tok0], in_=PS[bank][:, 0:b_ - a])),
                         r=[PK[bank]], w=[(pk, wi)])
                    a = b_; wi += 1
                pkeys = [(pk, j) for j in range(wi)] + [(pk, 'l'), (pk, 'r')]
                c_, ck = cv.next()
                P.op('dve', lambda: nc.vector.tensor_scalar(out=c_[:, 0:W], in0=p_[:, 0:W], scalar1=wcs[:, cc, 0:1], scalar2=None, op0=ALU.mult),
                     r=pkeys + ["wcs"], w=[ck])
                for tap in range(1, 4):
                    eng = 'dve'
                    E_ = nc.gpsimd if eng == 'pool' else nc.vector
                    P.op(eng, (lambda tap=tap, E_=E_: E_.scalar_tensor_tensor(out=c_[:, 0:W], in0=p_[:, tap:tap + W], scalar=wcs[:, cc, tap:tap + 1],
                                                                             in1=c_[:, 0:W], op0=ALU.mult, op1=ALU.add)),
                         r=pkeys + ["wcs", ck], w=[ck])
                P.op('act', lambda: nc.scalar.activation(out=c_[:, 0:W], in_=c_[:, 0:W], func=AF.Silu), r=[ck], w=[ck])
                kind = cc // 8; h = cc % 8
                src = c_
                srck = ck
                if kind < 2:
                    sq, sqk = sqb.next(); rs, rk_ = rsb.next()
                    P.op('pool', lambda: nc.gpsimd.tensor_tensor(out=sq[:, 0:W], in0=c_[:, 0:W], in1=c_[:, 0:W], op=ALU.mult), r=[ck], w=[sqk])
                    for j in range(0, W, 512):
                        n_ = min(512, W - j)
                        bank = 3 + (j // 512) % 2
                        P.op('pe', (lambda j=j, n_=n_, bank=bank: nc.tensor.matmul(PS[bank][:, 0:n_], onesb[:], sq[:, j:j + n_], start=True, stop=True)),
                             r=[sqk, "onesb"], w=[PK[bank]])
                        P.op('dve', (lambda j=j, n_=n_, bank=bank: nc.vector.tensor_scalar(out=rs[:, j:j + n_], in0=PS[bank][:, 0:n_], scalar1=EPS, scalar2=None, op0=ALU.add)),
                             r=[PK[bank]], w=[(rk_, j)])
                    rkeys = [(rk_, j) for j in range(0, W, 512)]
                    P.op('act', lambda: nc.scalar.activation(out=rs[:, 0:W], in_=rs[:, 0:W], func=AF.Sqrt), r=rkeys, w=rkeys)
                    P.op('dve', lambda: nc.vector.reciprocal(out=rs[:, 0:W], in_=rs[:, 0:W]), r=rkeys, w=rkeys)
                    if kind == 0:
                        P.op('dve', lambda: nc.vector.scalar_tensor_tensor(
                            out=qst[:, 0:ntile, h, :], in0=c_[:, 0:W].rearrange("p (t k) -> p t k", k=128), scalar=float(128 ** -0.5),
                            in1=rs[:, 0:W].rearrange("p (t k) -> p t k", k=128), op0=ALU.mult, op1=ALU.mult), r=[ck] + rkeys, w=[("qst", h)])
                        return
                    kf_, kfk = kf.next()
                    P.op('dve', lambda: nc.vector.tensor_tensor(out=kf_[:, 0:W], in0=c_[:, 0:W], in1=rs[:, 0:W], op=ALU.mult), r=[ck] + rkeys, w=[kfk])
                    P.op('pool', lambda: nc.gpsimd.tensor_copy(out=kst[:, 0:ntile, h, :], in_=kf_[:, 0:W].rearrange("p (t k) -> p t k", k=128)),
                         r=[kfk], w=[("kst", h)])
                    src = kf_; srck = kfk
                dst = ktst if kind == 1 else vtst
                dkey = "ktst" if kind == 1 else "vtst"
                for j in range(0, ntile, 4):
                    n_ = min(4, ntile - j)
                    bank = 5 + (j // 4) % 2
                    for t in range(n_):
                        P.op('pe', (lambda t=t, j=j, bank=bank: nc.tensor.transpose(
                            PS[bank][:, t * 128:(t + 1) * 128], src[:, (j + t) * 128:(j + t + 1) * 128], ident)),
                            r=[srck, "msk"], w=[PK[bank]])
                    P.op('act', (lambda j=j, n_=n_, bank=bank: nc.scalar.copy(
                        out=dst[:, j:j + n_, h, :], in_=PS[bank][:, 0:n_ * 128].rearrange("p (t k) -> p t k", k=128))),
                        r=[PK[bank]], w=[(dkey, h, j)])

            def c1_superblock(seq0, nseq, t0, t1, is_ctx):
                mod, mkey = ("C", "mvec") if is_ctx else ("L", "mvec")
                hb = max(t0 - 1, 0); he = min(t1 + 1, nseq)
                for lt in range(hb, he):
                    c1_tile_norm(seq0 + lt, lt - hb, mod, mkey)
                for lt in range(t0, t1):
                    c1_zab(seq0 + lt, lt - hb, is_ctx)
                for cc in range(24):
                    c1_chunk(cc, seq0, nseq, t0, t1, hb)
                nt_ = t1 - t0
                g0 = seq0 + t0
                for (dst, st, key) in ((QT_d, qst, "qst"), (KT_d, kst, "kst")):
                    P.dma('sp', dst[g0:g0 + nt_].rearrange("t p h k -> p t h k"), st[:, 0:nt_, :, :],
                          r=[(key, h) for h in range(8)], w=["d_" + key])
                for (dst, st, key) in ((KK_d, ktst, "ktst"), (VV_d, vtst, "vtst")):
                    P.dma('sp', dst[g0:g0 + nt_].rearrange("t p h k -> p t h k"), st[:, 0:nt_, :, :],
                          r=[(key, h, j) for h in range(8) for j in range(0, nt_, 4)], w=["d_" + key])

            c1_superblock(0, NCT, 0, NCT, True)
            for t0 in range(0, NLT, SBT1):
                c1_superblock(NCT, NLT, t0, t0 + SBT1, False)
            P.barrier()
        if stop_after == "c1":
            P.finish()
            return nc

        with contextlib.ExitStack() as ph:
            HS = [128, 8, 128]
            lKT = Ring(nc, ph, "lKT", 2, HS, BF16); lQT = Ring(nc, ph, "lQT", 2, HS, BF16)
            lKK = Ring(nc, ph, "lKK", 2, HS, BF16); lVV = Ring(nc, ph, "lVV", 2, HS, BF16)
            gbl = Ring(nc, ph, "gbl", 2, [128, 32], F32)
            scr = Ring(nc, ph, "scr", 2, [128, 80], F32)
            L16 = Ring(nc, ph, "L16", 2, [16, 4, 128], F32)
            rE = Ring(nc, ph, "rE", 2, [16, 2, 8, 128], F32)
            bmask = sb("bmask", [16, 8, 128], F32, ph)
            Ei = Ring(nc, ph, "Ei", 2, HS, F32); Es = Ring(nc, ph, "Es", 2, HS, F32)
            SBm = Ring(nc, ph, "SBm", 2, HS, F32); tf = Ring(nc, ph, "tf", 2, HS, F32)
            Ak = Ring(nc, ph, "Ak", 4, HS, BF16); Bk = Ring(nc, ph, "Bk", 4, HS, BF16); Tk = Ring(nc, ph, "Tk", 4, HS, BF16)
            TTk = Ring(nc, ph, "TTk", 4, HS, BF16); A0r = Ring(nc, ph, "A0r", 2, HS, BF16); B0r = Ring(nc, ph, "B0r", 2, HS, BF16)
            Of = Ring(nc, ph, "Of", 8, HS, BF16); P1r = Ring(nc, ph, "P1r", 4, HS, BF16)
            qkT = Ring(nc, ph, "qkT", 2, HS, BF16); KG = Ring(nc, ph, "KG", 2, HS, BF16); Kd = Ring(nc, ph, "Kd", 2, HS, BF16)
            up = Ring(nc, ph, "up", 2, HS, F32); wT = Ring(nc, ph, "wT", 2, HS, BF16); vn = Ring(nc, ph, "vn", 2, HS, BF16)
            ob = Ring(nc, ph, "ob", 2, HS, F32)
            Sst = [sb("S%d" % d, HS, F32, ph) for d in range(2)]
            Sbf = [sb("Sb%d" % d, HS, BF16, ph) for d in range(2)]
            P.dma('sp', bmask[:], blockmask, r=["d_bm"], w=["bmask"])
            for d in range(2):
                P.op('pool', (lambda d=d: nc.gpsimd.memset(Sst[d][:], 0.0)), r=[], w=[("S", d)])
                P.op('pool', (lambda d=d: nc.gpsimd.memset(Sbf[d][:], 0.0)), r=[], w=[("Sb", d)])
            for t_ in scr.tiles:
                P.op('pool', (lambda t_=t_: nc.gpsimd.memset(t_[:], 1.0)), r=[], w=[])
            P.flush()
            P.barrier()
            ppc = [0]

            def pair():
                p = ppc[0] % 4
                ppc[0] += 1
                v = psum[:, p * 1024:(p + 1) * 1024]
                return p, v, v.rearrange("p (h k) -> p h k", h=8), [PK[2 * p], PK[2 * p + 1]]

            def bc_mid(ap2d, n=128):
                return ap2d.unsqueeze(1).to_broadcast([ap2d.shape[0], 8, ap2d.shape[1]])

            def bc_last(ap2d):
                return ap2d.unsqueeze(2).to_broadcast([ap2d.shape[0], 8, 128])

            def dn_step(d, gt, lt, need_o):
                V = nc.vector; G = nc.gpsimd; A = nc.scalar
                kt, ktk = lKT.next(); kk, kkk = lKK.next(); vv, vvk = lVV.next(); gb, gbk = gbl.next()
                P.dma('sp', kt[:], KT_d[gt], r=["d_kst"], w=[ktk])
                P.dma('sp', kk[:], KK_d[gt], r=["d_ktst"], w=[kkk])
                P.dma('sp', vv[:], VV_d[gt], r=["d_vtst"], w=[vvk])
                P.dma('sp', gb[:], GB_d[gt], r=["d_GB"], w=[gbk])
                if need_o:
                    qt, qtk = lQT.next()
                    P.dma('sp', qt[:], QT_d[gt], r=["d_qst"], w=[qtk])
                g = gb[:, 16 * d:16 * d + 8]; beta = gb[:, 16 * d + 8:16 * d + 16]
                sc, sk = scr.next()
                p, pv, pv3, pk = pair()
                P.op('pe', lambda: nc.tensor.matmul(pv[:, 0:8], msk[:, 1 + d, :], g, start=True, stop=True), r=[gbk, "msk"], w=pk)
                P.op('pe', lambda: nc.tensor.matmul(pv[:, 8:16], msk[:, 3, :], g, start=True, stop=True), r=[gbk, "msk"], w=pk)
                P.op('dve', lambda: V.tensor_copy(out=sc[:, 0:16], in_=pv[:, 0:16]), r=pk, w=[sk])
                yield
                P.op('act', lambda: A.activation(out=sc[:, 16:24], in_=sc[:, 0:8], func=AF.Exp), r=[sk], w=[sk])
                P.op('dve', lambda: V.tensor_tensor(out=sc[:, 24:32], in0=sc[:, 8:16], in1=sc[:, 0:8], op=ALU.subtract), r=[sk], w=[sk])
                P.op('act', lambda: A.activation(out=sc[:, 24:32], in_=sc[:, 24:32], func=AF.Exp), r=[sk], w=[sk])
                P.op('act', lambda: A.activation(out=sc[:, 32:40], in_=sc[:, 8:16], func=AF.Exp), r=[sk], w=[sk])
                P.op('dve', lambda: V.tensor_scalar(out=sc[:, 40:48], in0=sc[:, 0:8], scalar1=-1.0, scalar2=None, op0=ALU.mult), r=[sk], w=[sk])
                P.op('dve', lambda: V.tensor_copy(out=sc[:, 56:64], in_=sc[:, 0:8]), r=[sk], w=[sk])
                P.op('act', lambda: A.activation(out=sc[:, 72:80], in_=beta, func=AF.Ln), r=[gbk], w=[sk])
                P.op('dve', lambda: V.tensor_tensor(out=sc[:, 72:80], in0=sc[:, 72:80], in1=sc[:, 40:48], op=ALU.add), r=[sk], w=[sk])
                yield
                egc = sc[:, 16:24]; edec = sc[:, 24:32]; egl = sc[:, 32:40]
                l16, lk = L16.next(); re, rek = rE.next()
                p, pv, pv3, pk = pair()
                for q in range(4):
                    P.op('pe', (lambda q=q: nc.tensor.transpose(pv[0:16, q * 128:(q + 1) * 128], sc[:, 40 + 8 * q:56 + 8 * q], ident)),
                         r=[sk, "msk"], w=pk)
                yield
                P.op('act', lambda: A.copy(out=l16[:], in_=pv[0:16, 0:512].rearrange("p (q k) -> p q k", q=4)), r=pk, w=[lk])
                P.op('pool', lambda: G.tensor_tensor(out=re[:, 0, :, :], in0=bc_mid(l16[:, 1, :]), in1=bmask[:], op=ALU.mult), r=[lk, "bmask"], w=[(rek, 0)])
                P.op('pool', lambda: G.tensor_tensor(out=re[:, 1, :, :], in0=bc_mid(l16[:, 3, :]), in1=bmask[:], op=ALU.mult), r=[lk, "bmask"], w=[(rek, 1)])
                yield
                ei, eik = Ei.next(); es_, esk = Es.next()
                for which, (lq, dst, dk_, mi) in enumerate(((0, ei, eik, 4 + d), (2, es_, esk, 8 + d))):
                    yield
                    p, pv, pv3, pk = pair()
                    for half in range(2):
                        P.op('pe', (lambda half=half, which=which, lq=lq, pv=pv: nc.tensor.matmul(
                            pv[:, half * 512:(half + 1) * 512], l16[:, lq, :],
                            re[:, which, 4 * half:4 * half + 4, :].rearrange("p h k -> p (h k)"), start=True, stop=True)),
                            r=[lk, (rek, which)], w=pk)
                    P.op('dve', (lambda pv3=pv3, dst=dst, mi=mi: V.scalar_tensor_tensor(
                        out=dst[:], in0=pv3, scalar=0.0, in1=bc_mid(msk[:, mi, :]), op0=ALU.min, op1=ALU.add)), r=pk + ["msk"], w=[dk_])
                    P.op('act', (lambda dst=dst: A.activation(out=dst[:], in_=dst[:], func=AF.Exp)), r=[dk_], w=[dk_])
                yield
                sbm, sbk = SBm.next()
                P.op('pool', lambda: G.tensor_tensor(out=sbm[:], in0=bc_mid(msk[:, 6 + d, :]), in1=bc_last(beta), op=ALU.mult), r=["msk", gbk], w=[sbk])
                p, pv, pv3, pk = pair()
                for h in range(8):
                    P.op('pe', (lambda h=h, pv=pv: nc.tensor.matmul(pv[:, h * 128:(h + 1) * 128], kt[:, h, :], kt[:, h, :], start=True, stop=True)),
                         r=[ktk], w=pk)
                yield
                t1, t1k = tf.next()
                a0, a0k = A0r.next(); b0, b0k = B0r.next()
                P.op('dve', (lambda pv3=pv3: V.tensor_tensor(out=t1[:], in0=pv3, in1=ei[:], op=ALU.mult)), r=pk + [eik], w=[t1k])
                P.op('pool', lambda: G.tensor_tensor(out=a0[:], in0=t1[:], in1=sbm[:], op=ALU.mult), r=[t1k, sbk], w=[a0k])
                P.op('dve', (lambda pv3=pv3: V.tensor_tensor(out=b0[:], in0=pv3, in1=es_[:], op=ALU.mult)), r=pk + [esk], w=[b0k])
                if need_o:
                    qk_, qkk = qkT.next()
                    p, pv, pv3, pk = pair()
                    for h in range(8):
                        P.op('pe', (lambda h=h, pv=pv: nc.tensor.matmul(pv[:, h * 128:(h + 1) * 128], kt[:, h, :], qt[:, h, :], start=True, stop=True)),
                             r=[ktk, qtk], w=pk)
                    P.op('dve', (lambda pv3=pv3: V.tensor_tensor(out=qk_[:], in0=pv3, in1=ei[:], op=ALU.mult)), r=pk + [eik], w=[qkk])
                yield
                def mm8(lhs, rhs, keys):
                    p, pv, pv3, pk = pair()
                    for h in range(8):
                        P.op('pe', (lambda h=h, pv=pv: nc.tensor.matmul(pv[:, h * 128:(h + 1) * 128], lhs[:, h, :], rhs[:, h, :], start=True, stop=True)),
                             r=keys, w=pk)
                    return pv3, pk
                ca, cak = Ak.next(); cb, cbk = Bk.next(); cT, cTk = Tk.next(); cTT, cTTk = TTk.next()
                P.op('pool', lambda ca=ca: G.tensor_tensor(out=ca[:], in0=a0[:], in1=bc_mid(msk[:, 10, :]), op=ALU.mult), r=[a0k, "msk"], w=[cak])
                P.op('pool', lambda cb=cb: G.tensor_tensor(out=cb[:], in0=b0[:], in1=bc_mid(msk[:, 10, :]), op=ALU.mult), r=[b0k, "msk"], w=[cbk])
                P.op('pool', lambda ca=ca, cT=cT: G.tensor_tensor(out=cT[:], in0=bc_mid(identb[:]), in1=ca[:], op=ALU.subtract), r=["identb", cak], w=[cTk])
                P.op('pool', lambda cb=cb, cTT=cTT: G.tensor_tensor(out=cTT[:], in0=bc_mid(identb[:]), in1=cb[:], op=ALU.subtract), r=["identb", cbk], w=[cTTk])
                yield
                offs = []
                for mi in (11, 12):
                    of_, ofk = Of.next(); oft, oftk = Of.next()
                    P.op('pool', (lambda of_=of_, mi=mi: G.tensor_tensor(out=of_[:], in0=a0[:], in1=bc_mid(msk[:, mi, :]), op=ALU.mult)), r=[a0k, "msk"], w=[ofk])
                    P.op('pool', (lambda oft=oft, mi=mi: G.tensor_tensor(out=oft[:], in0=b0[:], in1=bc_mid(msk[:, mi, :]), op=ALU.mult)), r=[b0k, "msk"], w=[oftk])
                    offs.append((of_, ofk, oft, oftk))
                for lev in range(4):
                    yield
                    na, nak = Ak.next(); nb, nbk = Bk.next(); nT, nTk = Tk.next(); nTT, nTTk = TTk.next()
                    pv3, pk = mm8(cb, ca, [cak, cbk])
                    P.op('act', (lambda pv3=pv3, na=na: A.copy(out=na[:], in_=pv3)), r=pk, w=[nak])
                    pv3, pk = mm8(ca, cb, [cak, cbk])
                    P.op('dve', (lambda pv3=pv3, nb=nb: V.tensor_copy(out=nb[:], in_=pv3)), r=pk, w=[nbk])
                    yield
                    pv3, pk = mm8(nb, cT, [nbk, cTk])
                    P.op('dve', (lambda pv3=pv3, nT=nT, cT=cT: V.tensor_tensor(out=nT[:], in0=pv3, in1=cT[:], op=ALU.add)), r=pk + [cTk], w=[nTk])
                    pv3, pk = mm8(na, cTT, [nak, cTTk])
                    P.op('dve', (lambda pv3=pv3, nTT=nTT, cTT=cTT: V.tensor_tensor(out=nTT[:], in0=pv3, in1=cTT[:], op=ALU.add)), r=pk + [cTTk], w=[nTTk])
                    ca, cak, cb, cbk, cT, cTk, cTT, cTTk = na, nak, nb, nbk, nT, nTk, nTT, nTTk
                for si, (of_, ofk, oft, oftk) in enumerate(offs):
                    yield
                    p1, p1k = P1r.next()
                    pv3, pk = mm8(oft, cT, [oftk, cTk])
                    P.op('act', (lambda pv3=pv3, p1=p1: A.copy(out=p1[:], in_=pv3)), r=pk, w=[p1k])
                    yield
                    pv3, pk = mm8(cTT, p1, [cTTk, p1k])
                    nT, nTk = Tk.next()
                    P.op('dve', (lambda pv3=pv3, nT=nT, cT=cT: V.tensor_tensor(out=nT[:], in0=cT[:], in1=pv3, op=ALU.subtract)), r=pk + [cTk], w=[nTk])
                    if si == 0:
                        p1t, p1tk = P1r.next()
                        pv3, pk = mm8(of_, cTT, [ofk, cTTk])
                        P.op('act', (lambda pv3=pv3, p1t=p1t: A.copy(out=p1t[:], in_=pv3)), r=pk, w=[p1tk])
                        pv3, pk = mm8(cT, p1t, [cTk, p1tk])
                        nTT, nTTk = TTk.next()
                        P.op('dve', (lambda pv3=pv3, nTT=nTT, cTT=cTT: V.tensor_tensor(out=nTT[:], in0=cTT[:], in1=pv3, op=ALU.subtract)), r=pk + [cTTk], w=[nTTk])
                        cTT, cTTk = nTT, nTTk
                    cT, cTk = nT, nTk
                yield
                u_, uk = up.next(); w_, wk_ = wT.next(); kg, kgk = KG.next(); kd, kdk = Kd.next()
                P.op('pool', lambda: G.tensor_tensor(out=kg[:], in0=kk[:], in1=bc_last(egc), op=ALU.mult), r=[kkk, sk], w=[kgk])
                P.op('pool', lambda: G.tensor_tensor(out=kd[:], in0=kk[:], in1=bc_last(edec), op=ALU.mult), r=[kkk, sk], w=[kdk])
                p, pv, pv3, pk = pair()
                for h in range(8):
                    P.op('pe', (lambda h=h, pv=pv: nc.tensor.matmul(pv[:, h * 128:(h + 1) * 128], cT[:, h, :], vv[:, h, :], start=True, stop=True)),
                         r=[cTk, vvk], w=pk)
                P.op('act', (lambda pv3=pv3: A.copy(out=u_[:], in_=pv3)), r=pk, w=[uk])
                p, pv, pv3, pk = pair()
                for h in range(8):
                    P.op('pe', (lambda h=h, pv=pv: nc.tensor.matmul(pv[:, h * 128:(h + 1) * 128], kg[:, h, :], cT[:, h, :], start=True, stop=True)),
                         r=[cTk, kgk], w=pk)
                P.op('act', (lambda pv3=pv3: A.copy(out=w_[:], in_=pv3)), r=pk, w=[wk_])
                yield
                S = Sst[d]; Sb = Sbf[d]; Sk = ("S", d); Sbk = ("Sb", d)
                vn_, vnk = vn.next(); t2, t2k = tf.next()
                p, pv, pv3, pk = pair()
                for h in range(8):
                    P.op('pe', (lambda h=h, pv=pv: nc.tensor.matmul(pv[:, h * 128:(h + 1) * 128], w_[:, h, :], Sb[:, h, :], start=True, stop=True)),
                         r=[wk_, Sbk], w=pk)
                yield
                P.op('dve', (lambda pv3=pv3: V.tensor_tensor(out=t2[:], in0=u_[:], in1=pv3, op=ALU.subtract)), r=pk + [uk], w=[t2k])
                P.op('pool', lambda: G.tensor_tensor(out=vn_[:], in0=t2[:], in1=bc_last(beta), op=ALU.mult), r=[t2k, gbk], w=[vnk])
                if need_o:
                    o_, ok_ = ob.next()
                    p, pv, pv3, pk = pair()
                    for h in range(8):
                        P.op('pe', (lambda h=h, pv=pv: nc.tensor.matmul(pv[:, h * 128:(h + 1) * 128], qt[:, h, :], Sb[:, h, :], start=True, stop=True)),
                             r=[qtk, Sbk], w=pk)
                    P.op('dve', (lambda pv3=pv3: V.tensor_tensor(out=o_[:], in0=pv3, in1=bc_last(egc), op=ALU.mult)), r=pk + [sk], w=[ok_])
                    p, pv, pv3, pk = pair()
                    for h in range(8):
                        P.op('pe', (lambda h=h, pv=pv: nc.tensor.matmul(pv[:, h * 128:(h + 1) * 128], qk_[:, h, :], vn_[:, h, :], start=True, stop=True)),
                             r=[qkk, vnk], w=pk)
                    P.op('dve', (lambda pv3=pv3: V.tensor_tensor(out=o_[:], in0=o_[:], in1=pv3, op=ALU.add)), r=pk + [ok_], w=[ok_])
                    P.dma('sp', OF_d[d, lt], o_[:].rearrange("p h k -> p (h k)"), r=[ok_], w=["d_OF"])
                yield
                p, pv, pv3, pk = pair()
                for h in range(8):
                    P.op('pe', (lambda h=h, pv=pv: nc.tensor.matmul(pv[:, h * 128:(h + 1) * 128], kd[:, h, :], vn_[:, h, :], start=True, stop=True)),
                         r=[kdk, vnk], w=pk)
                P.op('dve', lambda: V.tensor_tensor(out=S[:], in0=S[:], in1=bc_last(egl), op=ALU.mult), r=[Sk, sk], w=[Sk])
                P.op('dve', (lambda pv3=pv3: V.tensor_tensor(out=S[:], in0=S[:], in1=pv3, op=ALU.add)), r=pk + [Sk], w=[Sk])
                P.op('act', lambda: A.copy(out=Sb[:], in_=S[:]), r=[Sk], w=[Sbk])
                import os
                if os.environ.get("DN_DBG") and d == int(os.environ["DN_DBG"]) and gt == int(os.environ.get("DN_DBG_GT", "0")):
                    dump("dbg_sc", sc[:], [sk]); dump("dbg_Ei", ei[:], [eik]); dump("dbg_Es", es_[:], [esk])
                    dump("dbg_a0", a0[:], [a0k]); dump("dbg_b0", b0[:], [b0k]); dump("dbg_T", cT[:], [cTk])
                    dump("dbg_up", u_[:], [uk]); dump("dbg_wT", w_[:], [wk_]); dump("dbg_vn", vn_[:], [vnk]); dump("dbg_S", S[:], [Sk])
                    dump("dbg_l16", l16[:], [lk])
                    if need_o:
                        dump("dbg_qk", qk_[:], [qkk]); dump("dbg_o", o_[:], [ok_])

            order_f = [(t, t, False) for t in range(NCT)] + [(NCT + t, t, True) for t in range(NLT)]
            order_b = [(t, t, False) for t in reversed(range(NCT))] + [(NCT + t, t, True) for t in reversed(range(NLT))]
            import os
            nstep = int(os.environ.get("DN_STEPS", str(NT)))
            for i in range(nstep):
                g0 = dn_step(0, *order_f[i])
                g1 = dn_step(1, *order_b[i])
                run_interleaved(P, [g0, g1], 2)
            P.barrier()
        if stop_after == "c2":
            P.finish()
            return nc

        with contextlib.ExitStack() as ph:
            R = mk_rings(ph)
            load_mods(ph, [("L", 2)])
            of0 = Ring(nc, ph, "of0", 2, [128, D], F32); of1 = Ring(nc, ph, "of1", 2, [128, D], F32)
            sqt = Ring(nc, ph, "sqt", 1, [128, D], F32)
            zl = Ring(nc, ph, "zl", 2, [128, D], BF16)
            ms = Ring(nc, ph, "ms", 2, [128, 8], F32)
            onT = Ring(nc, ph, "onT", 2, [128, 8, 128], BF16)
            yb = Ring(nc, ph, "yb", 2, [128, D], F32)
            wo = sb("wo", [128, 8, D], BF16, ph)
            dnw = sb("dnw", [128, 128], F32, ph)
            P.dma('pool', wo[:], w_dn_out.rearrange("(k p) n -> p k n", p=128), r=["d_wo"], w=["wo"])
            P.dma('sp', dnw[:], dn_norm.partition_broadcast(128), r=["d_dnn"], w=["dnw"])

            def c3_tile(lt):
                gt = NCT + lt
                V = nc.vector; G = nc.gpsimd; A = nc.scalar
                a0_, a0k = of0.next(); a1_, a1k = of1.next(); z_, zk = zl.next(); m_, mk_ = ms.next(); sq, sqk = sqt.next()
                P.dma('sp', a0_[:], OF_d[0, lt], r=["d_OF"], w=[a0k])
                P.dma('sp', a1_[:], OF_d[1, lt], r=["d_OF"], w=[a1k])
                P.dma('sp', z_[:], ZZ_d[gt], r=["d_ZZ"], w=[zk])
                P.op('pool', lambda: G.tensor_tensor(out=a0_[:], in0=a0_[:], in1=a1_[:], op=ALU.add), r=[a0k, a1k], w=[a0k])
                P.op('act', lambda: A.activation(out=sq[:], in_=a0_[:], func=AF.Square), r=[a0k], w=[sqk])
                P.op('dve', lambda: V.reduce_sum(out=m_[:], in_=sq[:].rearrange("p (h k) -> p h k", h=8), axis=AX.X), r=[sqk], w=[mk_])
                P.op('dve', lambda: V.tensor_scalar(out=m_[:], in0=m_[:], scalar1=1.0 / 128, scalar2=EPS, op0=ALU.mult, op1=ALU.add), r=[mk_], w=[mk_])
                P.op('act', lambda: A.activation(out=m_[:], in_=m_[:], func=AF.Sqrt), r=[mk_], w=[mk_])
                P.op('dve', lambda: V.reciprocal(out=m_[:], in_=m_[:]), r=[mk_], w=[mk_])
                o3 = a0_[:].rearrange("p (h k) -> p h k", h=8)
                P.op('dve', lambda: V.tensor_tensor(out=o3, in0=o3, in1=m_[:].unsqueeze(2).to_broadcast([128, 8, 128]), op=ALU.mult), r=[a0k, mk_], w=[a0k])
                P.op('pool', lambda: G.tensor_tensor(out=o3, in0=o3, in1=dnw[:].unsqueeze(1).to_broadcast([128, 8, 128]), op=ALU.mult), r=[a0k, "dnw"], w=[a0k])
                P.op('pool', lambda: G.tensor_tensor(out=a0_[:], in0=a0_[:], in1=z_[:], op=ALU.mult), r=[a0k, zk], w=[a0k])
                for k in range(8):
                    b = 6 + k // 4
                    P.op('pe', (lambda b=b, k=k: nc.tensor.transpose(PS[b][:, (k % 4) * 128:(k % 4) * 128 + 128], a0_[:, k * 128:(k + 1) * 128], ident)),
                         r=[a0k, "msk"], w=[PK[b]])
                t_, tk = onT.next()
                for h in range(2):
                    b = 6 + h
                    P.op('act', (lambda b=b, h=h: A.copy(out=t_[:, 4 * h:4 * h + 4, :], in_=PS[b].rearrange("p (k t) -> p k t", k=4))),
                         r=[PK[b]], w=[(tk, h)])
                y_, yk = yb.next()
                xt, xk = R['xs'].next()
                P.dma('sp', xt[:], XS2_t[gt], r=["d_dst0"], w=[xk])
                for half in range(2):
                    b = 4 + half
                    for k in range(8):
                        P.op('pe', (lambda b=b, k=k, half=half: nc.tensor.matmul(PS[b], t_[:, k, :], wo[:, k, half * 512:(half + 1) * 512],
                                                                             start=(k == 0), stop=(k == 7))), r=[(tk, 0), (tk, 1), "wo"], w=[PK[b]])
                    sl = slice(half * 512, (half + 1) * 512)
                    P.op('dve', (lambda b=b, sl=sl: V.tensor_tensor(out=y_[:, sl], in0=PS[b], in1=mv("L", 2)[:, sl], op=ALU.mult)),
                         r=[PK[b], "mvec"], w=[(yk, half)])
                    P.op('pool', (lambda sl=sl: G.tensor_tensor(out=y_[:, sl], in0=y_[:, sl], in1=xt[:, sl], op=ALU.add)), r=[(yk, half), xk], w=[(yk, half)])
                P.dma('sp', XS3_t[gt], y_[:], r=[(yk, 0), (yk, 1)], w=["d_XS3"])

            for lt in range(NLT):
                c3_tile(lt)
            P.barrier()
        if stop_after == "c3":
            P.finish()
            return nc
        MOE(1, XS3_t, out_t, list(range(NCT, NT)), True)
        P.finish()
    return nc


_CACHE = {}


def _core_inputs(b, inp, consts):
    m = {}
    m['xin'] = np.ascontiguousarray(np.concatenate([inp['ctx'][b], inp['x'][b]], 0), dtype=np.float32)
    cc = np.stack([inp['c'][b], inp['c_ctx']], 0)
    m['ccol'] = np.ascontiguousarray(cc.reshape(2, 8, 128).transpose(2, 0, 1), dtype=np.float32)
    for k in ('w_ada', 'b_ada', 'norm_mix', 'norm_ffn', 'w_e_gate', 'w_e_up', 'w_e_down', 'norm_final'):
        m[k] = np.ascontiguousarray(inp[k], dtype=np.float32)
    m['w_pool'] = np.ascontiguousarray(inp['w_pool'][0]); m['b_pool'] = np.ascontiguousarray(inp['b_pool'][0])
    m['pool_scale'] = np.ascontiguousarray(inp['pool_scale'][0])
    m['w_dn_in'] = np.ascontiguousarray(inp['w_dn_in'][0])
    m['wconv'] = np.ascontiguousarray(inp['w_dn_conv'][0].reshape(4, 24, 128).transpose(2, 1, 0))
    m['dn_a_log'] = np.ascontiguousarray(inp['dn_a_log'][0].reshape(16))
    m['dn_dt_bias'] = np.ascontiguousarray(inp['dn_dt_bias'][0].reshape(16))
    m['dn_norm'] = np.ascontiguousarray(inp['dn_norm'][0]); m['w_dn_out'] = np.ascontiguousarray(inp['w_dn_out'][0])
    m['w_r'] = np.ascontiguousarray(np.concatenate([inp['w_rg'], inp['w_re']], -1))
    m['b_r'] = np.ascontiguousarray(np.concatenate([inp['b_rg'], inp['b_re']], -1))
    m.update(consts)
    return m


def kernel(**inputs):
    inp = {k: np.asarray(v) for k, v in inputs.items()}
    if 'nc' not in _CACHE:
        _CACHE['nc'] = build()
        _CACHE['consts'] = host_constants()
    nc = _CACHE['nc']
    consts = _CACHE['consts']
    B = inp['x'].shape[0]
    in_maps = [_core_inputs(b, inp, consts) for b in range(B)]
    res = run_bass_kernel_spmd(nc, in_maps, core_ids=list(range(B)))
    out = np.stack([np.asarray(res.results[b]['out']).reshape(NLT * 128, D) for b in range(B)], 0)
    return out.astype(inp['x'].dtype)
```

```python
import os
from concourse.bass_utils import run_bass_kernel_spmd
import numpy as np, contextlib
import concourse.bass as bass
import concourse.mybir as mybir

F32 = mybir.dt.float32
BF16 = mybir.dt.bfloat16
AF = mybir.ActivationFunctionType
ALU = mybir.AluOpType
AX = mybir.AxisListType


class Prog:
    NS = 16

    def __init__(self, nc, es):
        self.nc = nc
        self.E = {'pe': nc.tensor, 'act': nc.scalar, 'dve': nc.vector, 'pool': nc.gpsimd, 'sp': nc.sync}
        self.sem = {e: es.enter_context(nc.semaphore("s_" + e)) for e in ('pe', 'act', 'dve', 'pool')}
        self.dsem = {q: [es.enter_context(nc.semaphore("d_%s%d" % (q, i))) for i in range(self.NS)]
                     for q in ('sp', 'act', 'pool')}
        self.sigcount = {e: 0 for e in self.sem}
        self.dmacount = {q: 0 for q in self.dsem}
        self.waited = {}
        self.ops = []
        self.last_w = {}
        self.readers = {}
        self.n_inst = 0

    def op(self, eng, fn, r=(), w=(), dma=False):
        self.ops.append((eng, fn, tuple(r), tuple(w), dma))

    def dma(self, q, out, in_, r, w, **kw):
        e = self.E[q]
        self.op(q, lambda: e.dma_start(out=out, in_=in_, **kw), r, w, dma=True)

    @staticmethod
    def _needs_wait(oj_eng, oj_dma, oi_eng, oi_dma, typ):
        if oj_dma:
            return True
        if oj_eng == oi_eng and not oi_dma:
            if oi_eng == 'pe':
                return False
            return typ == 'raw'
        return True

    def flush(self):
        ops = self.ops
        n = len(ops)
        deps = [None] * n
        last_w, readers = self.last_w, self.readers
        for i, (eng, fn, r, w, dma) in enumerate(ops):
            d = {}
            for k in r:
                t = last_w.get(k)
                if t is not None:
                    d[t] = 'raw'
                if isinstance(k, tuple) and k and k[0] == 'ps':
                    for e2, t in readers.get(k, {}).items():
                        if e2 != eng and t not in d:
                            d[t] = 'raw'
            for k in w:
                t = last_w.get(k)
                if t is not None and t not in d:
                    d[t] = 'waw'
                for t in readers.get(k, {}).values():
                    if t not in d:
                        d[t] = 'war'
            d.pop(('p', i), None)
            deps[i] = d
            me = ('p', i)
            for k in r:
                rk = readers.setdefault(k, {})
                if dma:
                    rk[('d', i)] = me
                else:
                    rk[eng] = me
            for k in w:
                last_w[k] = me
                readers[k] = {}
        need_sig = [False] * n
        for i, (eng, fn, r, w, dma) in enumerate(ops):
            for t, typ in deps[i].items():
                if t[0] == 'p':
                    j = t[1]
                    ej, _, _, _, dj = ops[j]
                    if not dj and self._needs_wait(ej, dj, eng, dma, typ):
                        need_sig[j] = True
        last_of = {}
        for i, (eng, fn, r, w, dma) in enumerate(ops):
            if not dma:
                last_of[eng] = i
        for e, i in last_of.items():
            need_sig[i] = True
        resolved = [None] * n
        for i, (eng, fn, r, w, dma) in enumerate(ops):
            E = self.E[eng]
            waits = {}
            for t, typ in deps[i].items():
                if t[0] == 'p':
                    t2 = resolved[t[1]]
                    ej, dj = ops[t[1]][0], ops[t[1]][4]
                else:
                    t2 = t
                    ej, dj = t[1], t[0] == 'd'
                if not self._needs_wait(ej, dj, eng, dma, typ):
                    continue
                if t2[0] == 'c':
                    key = ('c', t2[1]); val = t2[2]
                else:
                    key = ('d', t2[1], t2[2]); val = t2[3]
                if waits.get(key, 0) < val:
                    waits[key] = val
            if dma:
                k = self.dmacount[eng]
                slot = k % self.NS
                if k >= self.NS:
                    key = ('d', eng, slot); val = 16 * (k // self.NS)
                    if waits.get(key, 0) < val:
                        waits[key] = val
            for key, val in waits.items():
                wk = (eng, key)
                if self.waited.get(wk, 0) >= val:
                    continue
                self.waited[wk] = val
                s = self.sem[key[1]] if key[0] == 'c' else self.dsem[key[1]][key[2]]
                E.wait_ge(s, val)
                self.n_inst += 1
            inst = fn()
            self.n_inst += 1
            if dma:
                k = self.dmacount[eng]
                slot = k % self.NS
                val = 16 * (k // self.NS + 1)
                inst.then_inc(self.dsem[eng][slot], 16)
                self.dmacount[eng] = k + 1
                resolved[i] = ('d', eng, slot, val)
            else:
                if need_sig[i]:
                    self.sigcount[eng] += 1
                    inst.then_inc(self.sem[eng], 1)
                    resolved[i] = ('c', eng, self.sigcount[eng])
        nxt = {}
        for i in range(n - 1, -1, -1):
            eng, dma = ops[i][0], ops[i][4]
            if dma:
                continue
            if resolved[i] is not None:
                nxt[eng] = resolved[i]
            else:
                resolved[i] = nxt[eng]
        for k in list(last_w.keys()):
            t = last_w[k]
            if t[0] == 'p':
                last_w[k] = resolved[t[1]]
        for k in list(readers.keys()):
            rk = readers[k]
            for kk in list(rk.keys()):
                t = rk[kk]
                if t[0] == 'p':
                    rk[kk] = resolved[t[1]]
        self.ops = []

    def barrier(self):
        self.flush()
        for eng in ('pe', 'act', 'dve', 'pool', 'sp'):
            E = self.E[eng]
            for e, c in self.sigcount.items():
                if c > 0 and e != eng and self.waited.get((eng, ('c', e)), 0) < c:
                    E.wait_ge(self.sem[e], c)
                    self.waited[(eng, ('c', e))] = c
            for q, k in self.dmacount.items():
                for slot in range(self.NS):
                    if k > slot:
                        val = 16 * ((k - 1 - slot) // self.NS + 1)
                        if self.waited.get((eng, ('d', q, slot)), 0) < val:
                            E.wait_ge(self.dsem[q][slot], val)
                            self.waited[(eng, ('d', q, slot))] = val

    def finish(self):
        self.flush()
        sp = self.E['sp']
        for e, c in self.sigcount.items():
            if c > 0:
                sp.wait_ge(self.sem[e], c)
        for q, k in self.dmacount.items():
            for slot in range(self.NS):
                if k > slot:
                    cnt = (k - 1 - slot) // self.NS + 1
                    sp.wait_ge(self.dsem[q][slot], 16 * cnt)


D = 1024
NCT = 2
NLT = 64
NT = NCT + NLT
EPS = 1e-6
POOL_WINDOWS = (2, 4, 8, 16)
JREL = {0: list(range(-1, 4)), 1: list(range(-1, 5)), 2: list(range(-2, 6)), 3: list(range(-4, 8))}
BAND_IDX = {}
_i = 0
for _g in range(4):
    for _j in JREL[_g]:
        BAND_IDX[(_g, _j)] = _i
        _i += 1
NBAND = _i


def host_constants():
    c = {}
    def axis_w(n, win):
        t = np.arange(n)
        lo = np.maximum(t - win // 2, 0); hi = np.minimum(t + win // 2, n)
        W = np.zeros((n, n), np.float64)
        for o in range(n):
            W[lo[o]:hi[o], o] = 1.0 / (hi[o] - lo[o])
        return W
    band = np.zeros((3, 128, NBAND, 512), np.float32)
    for g, win in enumerate(POOL_WINDOWS):
        Wr = axis_w(128, win); Wc = axis_w(64, win)
        for ti, b in enumerate((0, 7, 15)):
            for j in JREL[g]:
                jt = 4 * b + j
                if jt < 0 or jt >= 64:
                    continue
                M = np.einsum('ab,cd->acbd', Wr[2 * jt:2 * jt + 2, 8 * b:8 * b + 8], Wc).reshape(128, 512)
                if 0 <= j < 4:
                    M[:, j * 128:(j + 1) * 128] -= np.eye(128)
                band[ti, :, BAND_IDX[(g, j)], :] = M
    c['band'] = band
    cb = np.zeros((128, 8, 256), np.float32)
    for g, win in enumerate(POOL_WINDOWS):
        W = axis_w(256, win) - np.eye(256)
        for j in range(2):
            cb[:, g * 2 + j, :] = W[j * 128:(j + 1) * 128, :]
    c['cband'] = cb
    idx = np.arange(128)
    m = np.zeros((128, 13, 128), np.float32)
    m[:, 0] = np.eye(128)
    m[:, 1] = (idx[:, None] <= idx[None, :])
    m[:, 2] = (idx[:, None] >= idx[None, :])
    m[:, 3] = 1.0
    m[:, 4] = np.where(idx[:, None] <= idx[None, :], 0.0, -30000.0)
    m[:, 5] = np.where(idx[:, None] >= idx[None, :], 0.0, -30000.0)
    m[:, 6] = (idx[:, None] < idx[None, :])
    m[:, 7] = (idx[:, None] > idx[None, :])
    m[:, 8] = np.where(idx[:, None] > idx[None, :], 0.0, -30000.0)
    m[:, 9] = np.where(idx[:, None] < idx[None, :], 0.0, -30000.0)
    bd32 = (idx[:, None] // 32 == idx[None, :] // 32); bd64 = (idx[:, None] // 64 == idx[None, :] // 64)
    m[:, 10] = bd32; m[:, 11] = bd64 & ~bd32; m[:, 12] = ~bd64
    c['masks'] = m
    bm = np.zeros((16, 8, 128), np.float32)
    for r in range(16):
        bm[r, r % 8, :] = 1.0
    c['blockmask'] = bm
    cm = np.zeros((128, 160), np.float32)
    cm[:, 0:32] = np.arange(32)[None, :]
    cm[:, 32:40] = np.arange(8)[None, :] * 128 + np.arange(128)[:, None]
    cm[:, 40:120] = np.arange(80)[None, :] * 512
    c['cmisc'] = cm
    return c


_UNIQ = [0]


def run_interleaved(P, gens, width):
    active = []
    it = iter(gens)
    while True:
        while len(active) < width:
            try:
                active.append(next(it))
            except StopIteration:
                break
        if not active:
            break
        for g in list(active):
            try:
                next(g)
            except StopIteration:
                active.remove(g)
        P.flush()


class Ring:
    def __init__(self, nc, es, name, n, shape, dtype):
        _UNIQ[0] += 1
        self.tiles = [es.enter_context(nc.sbuf_tensor("%s%d_u%d" % (name, i, _UNIQ[0]), shape, dtype)) for i in range(n)]
        self.name = name
        self.i = 0

    def next(self):
        k = self.i % len(self.tiles)
        self.i += 1
        return self.tiles[k], "%s%d" % (self.name, k)


def build(stop_after=None, debug=False):
    nc = bass.Bass("TRN2", target_bir_lowering=False)
    es = contextlib.ExitStack()

    def din(name, shape, dt=F32):
        return nc.dram_tensor(name, list(shape), dt, kind="ExternalInput").ap()

    def dscr(name, shape, dt=F32):
        kind = "ExternalOutput" if debug else "Internal"
        return nc.dram_tensor(name, list(shape), dt, kind=kind).ap()

    xin = din("xin", [NT * 128, D])
    ccol = din("ccol", [128, 2, 8])
    w_ada = din("w_ada", [2, D, 6 * D]); b_ada = din("b_ada", [2, 6 * D])
    norm_mix = din("norm_mix", [2, D]); norm_ffn = din("norm_ffn", [2, D])
    w_pool = din("w_pool", [4, 256, 256]); b_pool = din("b_pool", [D]); pool_scale = din("pool_scale", [D])
    w_dn_in = din("w_dn_in", [D, 4128]); wconv = din("wconv", [128, 24, 4])
    dn_a_log = din("dn_a_log", [16]); dn_dt_bias = din("dn_dt_bias", [16]); dn_norm = din("dn_norm", [128])
    w_dn_out = din("w_dn_out", [D, D])
    w_r = din("w_r", [2, D, 36]); b_r = din("b_r", [2, 36])
    w_e_gate = din("w_e_gate", [2, 32, D, 512]); w_e_up = din("w_e_up", [2, 32, D, 512])
    w_e_down = din("w_e_down", [2, 32, 512, D]); norm_final = din("norm_final", [D])
    band = din("band", [3, 128, NBAND, 512]); cband = din("cband", [128, 8, 256])
    masks = din("masks", [128, 13, 128]); blockmask = din("blockmask", [16, 8, 128])
    cmisc = din("cmisc", [128, 160])
    out = nc.dram_tensor("out", [NLT * 128, D], F32, kind="ExternalOutput").ap()
    XS1 = dscr("XS1", [NT * 128, D])
    XS2 = dscr("XS2", [NT * 128, D])
    XS3 = dscr("XS3", [NT * 128, D])

    with es:
        P = Prog(nc, es)
        psum = es.enter_context(nc.psum_tensor("psum", [128, 4096], F32))
        PS = [psum[:, b * 512:(b + 1) * 512] for b in range(8)]
        PK = [("ps", b) for b in range(8)]

        def sb(name, shape, dt=F32, stack=es):
            _UNIQ[0] += 1
            return stack.enter_context(nc.sbuf_tensor("%s_u%d" % (name, _UNIQ[0]), list(shape), dt))

        msk = sb("msk", [128, 13, 128])
        P.dma('sp', msk[:], masks, r=["d_masks"], w=["msk"])
        ident = msk[:, 0, :]
        identb = sb("identb", [128, 128], BF16)
        P.op('dve', lambda: nc.vector.tensor_copy(out=identb[:], in_=msk[:, 0, :]), r=["msk"], w=["identb"])
        MODS_d = dscr("MODS_d", [2, 128, 6 * D])
        MVEC = {}

        def load_mods(ph, need):
            buf = sb("mvec", [128, len(need), D], F32, ph)
            MVEC.clear()
            for j, (st, ix) in enumerate(need):
                P.dma('sp', buf[:, j, :], MODS_d[0 if st == 'L' else 1][:, ix * D:(ix + 1) * D], r=["d_mods"], w=["mvec"])
                MVEC[(st, ix)] = buf[:, j, :]
        csb = sb("csb", [128, 2, 8]); sil = sb("sil", [128, 2, 8])
        rep = sb("rep", [128, 2, 8, 128], BF16)
        P.dma('sp', csb[:], ccol, r=["d_ccol"], w=["csb"])
        P.op('act', lambda: nc.scalar.activation(out=sil[:], in_=csb[:], func=AF.Silu), r=["csb"], w=["sil"])
        P.op('dve', lambda: nc.vector.tensor_copy(out=rep[:], in_=sil[:].unsqueeze(3).to_broadcast([128, 2, 8, 128])),
             r=["sil"], w=["rep"])

        def adaln(layer, ph):
            wr = Ring(nc, ph, "adaw", 2, [128, 8, 512], BF16)
            nw = sb("nw", [128, 2, D], F32, ph)
            modL = sb("modL", [128, 6 * D], F32, ph); modC = sb("modC", [128, 6 * D], F32, ph)
            P.dma('sp', modL[:], b_ada[layer].partition_broadcast(128), r=["d_b_ada"], w=["modL"])
            P.dma('sp', modC[:], b_ada[layer].partition_broadcast(128), r=["d_b_ada"], w=["modC"])
            P.dma('sp', nw[:, 0, :], norm_mix[layer].partition_broadcast(128), r=["d_nm"], w=["nw"])
            P.dma('sp', nw[:, 1, :], norm_ffn[layer].partition_broadcast(128), r=["d_nm"], w=["nw"])
            wv = w_ada[layer].rearrange("(k p) n -> p k n", p=128)
            for blk in range(12):
                wt, wk = wr.next()
                P.dma('pool', wt[:], wv[:, :, blk * 512:(blk + 1) * 512], r=["d_w_ada"], w=[wk])
                for s, (mod, mk) in enumerate(((modL, "modL"), (modC, "modC"))):
                    b = (blk * 2 + s) % 8
                    for k in range(8):
                        P.op('pe', (lambda b=b, s=s, k=k, wt=wt: nc.tensor.matmul(
                            PS[b], rep[:, s, k, :], wt[:, k, :], start=(k == 0), stop=(k == 7))),
                            r=["rep", wk], w=[PK[b]])
                    sl = slice(blk * 512, (blk + 1) * 512)
                    P.op('dve', (lambda b=b, mod=mod, sl=sl: nc.vector.tensor_tensor(
                        out=mod[:, sl], in0=PS[b], in1=mod[:, sl], op=ALU.add)), r=[PK[b], mk], w=[mk])
            for s, (mod, mk) in enumerate(((modL, "modL"), (modC, "modC"))):
                for j, col in enumerate((1, 4)):
                    sl = slice(col * D, (col + 1) * D)
                    P.op('dve', (lambda mod=mod, sl=sl, j=j: nc.vector.scalar_tensor_tensor(
                        out=mod[:, sl], in0=mod[:, sl], scalar=1.0, in1=nw[:, j, :], op0=ALU.add, op1=ALU.mult)),
                        r=[mk, "nw"], w=[mk])
            P.dma('sp', MODS_d[0], modL[:], r=["modL"], w=["d_mods"])
            P.dma('sp', MODS_d[1], modC[:], r=["modC"], w=["d_mods"])

        def mv(mod, i):
            return MVEC[(mod, i)]

        def rms_mod(ph_rings, xs_ap, xs_key, A_ap, sh_ap, mod_keys, out_ap, out_key, eps_scale=1.0 / D):
            ss, sk = ph_rings['ss'].next()
            tmp, tk = ph_rings['hxtmp'].next()
            P.op('act', lambda: nc.scalar.activation(out=tmp[:], in_=xs_ap, func=AF.Square), r=[xs_key], w=[tk])
            P.op('dve', lambda: nc.vector.reduce_sum(out=ss[:, 0:1], in_=tmp[:], axis=AX.X), r=[tk], w=[sk])
            P.op('dve', lambda: nc.vector.tensor_scalar(out=ss[:, 1:2], in0=ss[:, 0:1], scalar1=eps_scale, scalar2=EPS,
                                                        op0=ALU.mult, op1=ALU.add), r=[sk], w=[sk])
            P.op('act', lambda: nc.scalar.activation(out=ss[:, 2:3], in_=ss[:, 1:2], func=AF.Sqrt), r=[sk], w=[sk])
            P.op('dve', lambda: nc.vector.reciprocal(out=ss[:, 3:4], in_=ss[:, 2:3]), r=[sk], w=[sk])
            P.op('dve', lambda: nc.vector.scalar_tensor_tensor(out=tmp[:], in0=xs_ap, scalar=ss[:, 3:4], in1=A_ap,
                                                               op0=ALU.mult, op1=ALU.mult),
                 r=[xs_key, sk] + mod_keys, w=[tk])
            if sh_ap is None:
                P.op('pool', lambda: nc.gpsimd.tensor_copy(out=out_ap, in_=tmp[:]), r=[tk], w=[out_key])
            else:
                P.op('pool', lambda: nc.gpsimd.tensor_tensor(out=out_ap, in0=tmp[:], in1=sh_ap, op=ALU.add),
                     r=[tk] + mod_keys, w=[out_key])

        def mk_rings(ph):
            return {'ss': Ring(nc, ph, "ss", 4, [128, 4], F32),
                    'hxtmp': Ring(nc, ph, "hxtmp", 2, [128, D], F32),
                    'xs': Ring(nc, ph, "xsr", 2, [128, D], F32)}

        xin_t = xin.rearrange("(t p) d -> t p d", p=128)
        XS1_t = XS1.rearrange("(t p) d -> t p d", p=128)
        XS2_t = XS2.rearrange("(t p) d -> t p d", p=128)
        XS3_t = XS3.rearrange("(t p) d -> t p d", p=128)
        out_t = out.rearrange("(t p) d -> t p d", p=128)

        with contextlib.ExitStack() as ph:
            adaln(0, ph)
            P.barrier()
        def dump(name, ap, keys):
            shape = list(ap.shape)
            t = nc.dram_tensor(name, shape, ap.dtype, kind="ExternalOutput").ap()
            P.dma('sp', t, ap, r=keys, w=["dbg_" + name])
        if stop_after == "ada":
            dump("dbg_modL", modL[:], ["modL"]); dump("dbg_modC", modC[:], ["modC"]); dump("dbg_rep", rep[:], ["rep"])
            P.finish()
            return nc
        with contextlib.ExitStack() as ph:
            R = mk_rings(ph)
            hx0 = sb("hx0", [128, 24, D], BF16, ph)
            bandsb = sb("bandsb", [128, NBAND, 512], BF16, ph)
            cbandsb = sb("cbandsb", [128, 8, 256], BF16, ph)
            wpl = sb("wpl", [128, 8, 256], BF16, ph)
            vecs = sb("vecs", [128, 2, D], F32, ph)
            AB = sb("AB", [128, 4, D], F32, ph)
            dT = Ring(nc, ph, "dT", 1, [128, 8, 512], BF16)
            yt = Ring(nc, ph, "yt", 2, [128, D], F32)
            P.dma('pool', cbandsb[:], cband, r=["d_cband"], w=["cbandsb"])
            P.dma('pool', wpl[:], w_pool.rearrange("g (c p) e -> p (g c) e", p=128), r=["d_wpool"], w=["wpl"])
            P.dma('sp', vecs[:, 0, :], pool_scale.partition_broadcast(128), r=["d_ps"], w=["vecs"])
            P.dma('sp', vecs[:, 1, :], b_pool.partition_broadcast(128), r=["d_bp"], w=["vecs"])
            load_mods(ph, [(st, ix) for st in "LC" for ix in (0, 1, 2)])
            for s, (mod, mkey) in enumerate((("L", "mvec"), ("C", "mvec"))):
                P.op('dve', (lambda s=s, mod=mod: nc.vector.tensor_tensor(out=AB[:, 2 * s, :], in0=mv(mod, 2), in1=vecs[:, 0, :],
                                                                          op=ALU.mult)), r=[mkey, "vecs"], w=["AB"])
                P.op('dve', (lambda s=s: nc.vector.tensor_tensor(out=AB[:, 2 * s + 1, :], in0=AB[:, 2 * s, :], in1=vecs[:, 1, :],
                                                                 op=ALU.mult)), r=["AB", "vecs"], w=["AB"])

            def pool_segment(is_ctx, in_tiles, base, out_blocks):
                mod, mkey = ("C", "mvec") if is_ctx else ("L", "mvec")
                seq0 = 0 if is_ctx else NCT
                for lt in in_tiles:
                    xt, xk = R['xs'].next()
                    P.dma('sp', xt[:], xin_t[seq0 + lt], r=["d_xin"], w=[xk])
                    rms_mod(R, xt[:], xk, mv(mod, 1), mv(mod, 0), [mkey], hx0[:, lt - base, :], ("hx0", lt - base))
                cur_band = [None]
                for (ot0, ntl, btype) in out_blocks:
                    ncol = ntl * 128
                    if not is_ctx and cur_band[0] != btype:
                        P.dma('pool', bandsb[:], band[btype], r=["d_band"], w=["bandsb"])
                        cur_band[0] = btype
                    dt_, dk = dT.next()
                    for g in range(4):
                        for cc in range(2):
                            b = 2 * g + cc
                            if is_ctx:
                                lst = [(j, cbandsb[:, g * 2 + j, 0:ncol]) for j in range(2)]
                                bk = "cbandsb"
                            else:
                                lst = []
                                for j in JREL[g]:
                                    jt = ot0 + j
                                    if jt < 0 or jt >= NLT:
                                        continue
                                    lst.append((jt, bandsb[:, BAND_IDX[(g, j)], 0:ncol]))
                                bk = "bandsb"
                            for n_, (jt, rhs) in enumerate(lst):
                                P.op('pe', (lambda b=b, jt=jt, rhs=rhs, n_=n_, L=len(lst), ch=b, ncol=ncol: nc.tensor.matmul(
                                    PS[b][:, 0:ncol], hx0[:, jt - base, ch * 128:(ch + 1) * 128], rhs,
                                    start=(n_ == 0), stop=(n_ == L - 1))),
                                    r=[("hx0", jt - base), bk], w=[PK[b]])
                            if b % 2 == 0:
                                P.op('act', (lambda b=b, ncol=ncol, dt_=dt_: nc.scalar.copy(out=dt_[:, b, 0:ncol], in_=PS[b][:, 0:ncol])),
                                     r=[PK[b]], w=[(dk, b)])
                            else:
                                P.op('dve', (lambda b=b, ncol=ncol, dt_=dt_: nc.vector.tensor_copy(out=dt_[:, b, 0:ncol], in_=PS[b][:, 0:ncol])),
                                     r=[PK[b]], w=[(dk, b)])
                    for t in range(ntl):
                        gt = seq0 + ot0 + t
                        b0 = 2 * (t % 4)
                        for g in range(4):
                            pb = b0 + g // 2
                            for cc in range(2):
                                P.op('pe', (lambda pb=pb, g=g, cc=cc, t=t, dt_=dt_: nc.tensor.matmul(
                                    PS[pb][:, (g % 2) * 256:(g % 2) * 256 + 256], dt_[:, 2 * g + cc, t * 128:(t + 1) * 128],
                                    wpl[:, 2 * g + cc, :], start=(cc == 0), stop=(cc == 1))),
                                    r=[(dk, 2 * g + cc), "wpl"], w=[PK[pb]])
                        xt, xk = R['xs'].next()
                        P.dma('sp', xt[:], xin_t[gt], r=["d_xin"], w=[xk])
                        ai = 2 if is_ctx else 0
                        y, yk = yt.next()
                        for h in range(2):
                            sl = slice(h * 512, (h + 1) * 512)
                            P.op('dve', (lambda y=y, sl=sl, h=h, b0=b0, ai=ai: nc.vector.tensor_tensor(
                                out=y[:, sl], in0=PS[b0 + h], in1=AB[:, ai, sl], op=ALU.mult)), r=[PK[b0 + h], "AB"], w=[(yk, h)])
                            P.op('pool', (lambda y=y, sl=sl, xt=xt: nc.gpsimd.tensor_tensor(
                                out=y[:, sl], in0=y[:, sl], in1=xt[:, sl], op=ALU.add)), r=[(yk, h), xk], w=[(yk, h)])
                            P.op('pool', (lambda y=y, sl=sl, ai=ai: nc.gpsimd.tensor_tensor(
                                out=y[:, sl], in0=y[:, sl], in1=AB[:, ai + 1, sl], op=ALU.add)), r=[(yk, h), "AB"], w=[(yk, h)])
                        P.dma('sp', XS1_t[gt], y[:], r=[(yk, 0), (yk, 1)], w=["d_XS1"])

            pool_segment(True, [0, 1], 0, [(0, 2, 0)])
            if stop_after == "pool_dbg":
                dump("dbg_hx0", hx0[:, 0:2, :], [("hx0", 0), ("hx0", 1)])
                dump("dbg_dT", dT.tiles[0][:], [("dT0", b) for b in range(8)])
                dump("dbg_AB", AB[:], ["AB"])
                dump("dbg_cb", cbandsb[:], ["cbandsb"])
                dump("dbg_wpl", wpl[:], ["wpl"])
                P.finish()
                return nc
            for seg in range(4):
                lo = max(0, 16 * seg - 4); hi = min(NLT, 16 * seg + 20)
                blocks = [(4 * b, 4, 0 if b == 0 else (2 if b == 15 else 1)) for b in range(4 * seg, 4 * seg + 4)]
                pool_segment(False, list(range(lo, hi)), lo, blocks)
            P.flush()
        if stop_after == "pool":
            P.finish()
            return nc
        P.barrier()

        def moe_phase(layer, src_t, dst_t, tiles, final):
            SBT = 8 if final else 10
            with contextlib.ExitStack() as ph:
                R = mk_rings(ph)
                load_mods(ph, [(st, ix) for st in ("LC" if not final else "L") for ix in (3, 4, 5)])
                hxf = Ring(nc, ph, "hxf", 2, [128, D], F32)
                hxTf = Ring(nc, ph, "hxTf", 1, [128, 8, 128], F32)
                hxTb = sb("hxTb", [128, 8, SBT * 128], BF16, ph)
                acc = sb("acc", [128, SBT, D], F32, ph)
                gates = sb("gates", [128, SBT, 32], F32, ph)
                wrs = sb("wrs", [128, 8, 36], F32, ph)
                brb = sb("brb", [128, 36], F32, ph)
                rt = Ring(nc, ph, "rt", 2, [128, 96], F32)
                wg = Ring(nc, ph, "wg", 2, [128, 8, 512], BF16)
                wu = Ring(nc, ph, "wu", 2, [128, 8, 512], BF16)
                wd = Ring(nc, ph, "wd", 2, [128, 4, D], BF16)
                sgb = Ring(nc, ph, "sgb", 1, [128, 512], BF16)
                hidT = Ring(nc, ph, "hidT", 2, [128, 4, 512], BF16)
                nfb = None
                if final:
                    nfb = sb("nfb", [128, D], F32, ph)
                    P.dma('sp', nfb[:], norm_final.partition_broadcast(128), r=["d_nf"], w=["nfb"])
                P.dma('sp', wrs[:], w_r[layer].rearrange("(k p) n -> p k n", p=128), r=["d_wr"], w=["wrs"])
                P.dma('sp', brb[:], b_r[layer].partition_broadcast(128), r=["d_br"], w=["brb"])
                def route(i, r_, rk, lvl, sparse_info=None):
                    lg = r_[:, 0:36]; m4 = r_[:, 36:37]; nm4 = r_[:, 37:38]; e4 = r_[:, 40:44]; s4 = r_[:, 38:39]
                    pg = r_[:, 39:40]; ohg = r_[:, 44:48]; sel = r_[:, 48:56]; m8 = r_[:, 56:64]; d21 = r_[:, 64:65]
                    e21 = r_[:, 65:66]; w1 = r_[:, 66:67]; w2 = r_[:, 67:68]; c1 = r_[:, 72:80]; c2 = r_[:, 80:88]
                    V = nc.vector
                    def dv(fn, rk=rk):
                        P.op('dve', fn, r=[rk], w=[rk])
                    P.op('dve', lambda lg=lg: V.tensor_tensor(out=lg, in0=PS[5][:, 0:36], in1=brb[:], op=ALU.add),
                         r=[PK[5], "brb"], w=[rk])
                    if lvl <= 2:
                        P.op('dve', (lambda i=i, lg=lg: V.tensor_copy(out=gates[:, i, :], in_=lg[:, 0:32])), r=[rk], w=[("gates", i)])
                        return
                    dv(lambda: V.reduce_max(out=m4, in_=lg[:, 0:4], axis=AX.X))
                    dv(lambda: V.tensor_scalar(out=nm4, in0=m4, scalar1=-1.0, scalar2=None, op0=ALU.mult))
                    P.op('act', lambda: nc.scalar.activation(out=e4, in_=lg[:, 0:4], func=AF.Exp, bias=nm4, scale=1.0),
                         r=[rk], w=[rk])
                    dv(lambda: V.reduce_sum(out=s4, in_=e4, axis=AX.X))
                    dv(lambda: V.reciprocal(out=pg, in_=s4))
                    dv(lambda: V.tensor_scalar(out=ohg, in0=lg[:, 0:4], scalar1=m4, scalar2=None, op0=ALU.is_equal))
                    dv(lambda: V.tensor_scalar(out=sel, in0=lg[:, 4:12], scalar1=ohg[:, 0:1], scalar2=None, op0=ALU.mult))
                    for g in range(1, 4):
                        dv(lambda g=g: V.scalar_tensor_tensor(out=sel, in0=lg[:, 4 + 8 * g:12 + 8 * g], scalar=ohg[:, g:g + 1],
                                                              in1=sel, op0=ALU.mult, op1=ALU.add))
                    dv(lambda: V.max(out=m8, in_=sel))
                    dv(lambda: V.tensor_tensor(out=d21, in0=m8[:, 1:2], in1=m8[:, 0:1], op=ALU.subtract))
                    P.op('act', lambda: nc.scalar.activation(out=e21, in_=d21, func=AF.Exp), r=[rk], w=[rk])
                    dv(lambda: V.tensor_scalar(out=e21, in0=e21, scalar1=1.0, scalar2=None, op0=ALU.add))
                    dv(lambda: V.reciprocal(out=w1, in_=e21))
                    dv(lambda: V.tensor_tensor(out=w1, in0=w1, in1=pg, op=ALU.mult))
                    dv(lambda: V.tensor_tensor(out=w2, in0=pg, in1=w1, op=ALU.subtract))
                    if sparse_info is not None:
                        sparse_info(r_, rk, sel, m8, ohg, w1, w2, dv)
                        return
                    dv(lambda: V.tensor_scalar(out=c1, in0=sel, scalar1=m8[:, 0:1], scalar2=w1, op0=ALU.is_equal, op1=ALU.mult))
                    dv(lambda: V.tensor_scalar(out=c2, in0=sel, scalar1=m8[:, 1:2], scalar2=w2, op0=ALU.is_equal, op1=ALU.mult))
                    dv(lambda: V.tensor_tensor(out=c1, in0=c1, in1=c2, op=ALU.add))
                    P.op('dve', (lambda i=i, c1=c1, ohg=ohg: V.tensor_tensor(
                        out=gates[:, i, :].rearrange("p (g e) -> p g e", g=4),
                        in0=c1.unsqueeze(1).to_broadcast([128, 4, 8]), in1=ohg.unsqueeze(2).to_broadcast([128, 4, 8]),
                        op=ALU.mult)), r=[rk], w=[("gates", i)])
                for s0 in range(0, len(tiles), SBT):
                    sbt = tiles[s0:s0 + SBT]
                    import os
                    if os.environ.get("MOE_NT"):
                        sbt = sbt[:int(os.environ["MOE_NT"])]
                    n_sb = len(sbt)
                    for i, gt in enumerate(sbt):
                        mod, mkey = ("C", "mvec") if gt < NCT else ("L", "mvec")
                        xt, xk = R['xs'].next()
                        P.dma('sp', xt[:], src_t[gt], r=["d_XS1" if layer == 0 else "d_XS3"], w=[xk])
                        hx, hk = hxf.next()
                        rms_mod(R, xt[:], xk, mv(mod, 4), mv(mod, 3), [mkey], hx[:], hk)
                        import os
                        if int(os.environ.get("MOE_LVL", "9")) == 0:
                            dump("dbg_hx%d" % i, hx[:], [hk])
                            continue
                        for k in range(8):
                            b = 6 + k // 4
                            P.op('pe', (lambda b=b, k=k, hx=hx: nc.tensor.transpose(
                                PS[b][:, (k % 4) * 128:(k % 4) * 128 + 128], hx[:, k * 128:(k + 1) * 128], ident)),
                                r=[hk, "msk"], w=[PK[b]])
                        hT, hTk = hxTf.next()
                        for h in range(2):
                            b = 6 + h
                            P.op('act', (lambda b=b, h=h, hT=hT: nc.scalar.copy(
                                out=hT[:, 4 * h:4 * h + 4, :], in_=PS[b].rearrange("p (k t) -> p k t", k=4))),
                                r=[PK[b]], w=[hTk])
                        P.op('pool', (lambda i=i, hT=hT: nc.gpsimd.tensor_copy(out=hxTb[:, :, i * 128:(i + 1) * 128], in_=hT[:])),
                             r=[hTk], w=[("hxTb", i)])
                        import os
                        lvl = int(os.environ.get("MOE_LVL", "9"))
                        if lvl <= 1:
                            continue
                        for k in range(8):
                            P.op('pe', (lambda k=k, hT=hT: nc.tensor.matmul(PS[5][:, 0:36], hT[:, k, :], wrs[:, k, :],
                                                                          start=(k == 0), stop=(k == 7))),
                                 r=[hTk, "wrs"], w=[PK[5]])
                        r_, rk = rt.next()
                        route(i, r_, rk, lvl)
                    if stop_after == "moe_b1":
                        lvl = int(os.environ.get("MOE_LVL", "9"))
                        if lvl == 0:
                            P.finish()
                            return "STOP"
                        if lvl > 1:
                            dump("dbg_rt0", rt.tiles[0][:], [rt.name + "0"]); dump("dbg_rt1", rt.tiles[1][:], [rt.name + "1"])
                            dump("dbg_gates", gates[:], [("gates", i) for i in range(n_sb)])
                        if os.environ.get("NO_HXTB") is None:
                            dump("dbg_hxTb", hxTb[:], [("hxTb", i) for i in range(n_sb)])
                        else:
                            dump("dbg_hxTf", hxTf.tiles[0][:], [hxTf.name + "0"])
                        P.finish()
                        return "STOP"
                    blocks = [(t0, min(4, n_sb - t0)) for t0 in range(0, n_sb, 4)]
                    hcn = 0
                    yn = 0
                    for e in range(32):
                        wgt, wgk = wg.next(); wut, wuk = wu.next(); wdt, wdk = wd.next()
                        P.dma('pool', wgt[:], w_e_gate[layer, e].rearrange("(k p) n -> p k n", p=128), r=["d_weg"], w=[wgk])
                        P.dma('pool', wut[:], w_e_up[layer, e].rearrange("(k p) n -> p k n", p=128), r=["d_weu"], w=[wuk])
                        P.dma('pool', wdt[:], w_e_down[layer, e].rearrange("(k p) n -> p k n", p=128), r=["d_wed"], w=[wdk])
                        for (t0, ntl) in blocks:
                            ncol = ntl * 128
                            cs = slice(t0 * 128, t0 * 128 + ncol)
                            rk_h = [("hxTb", t0 + j) for j in range(ntl)]
                            hid, hidk = hidT.next()
                            for hc in range(4):
                                gb = (hcn % 2) * 2; ub = gb + 1; hcn += 1
                                for k in range(8):
                                    P.op('pe', (lambda gb=gb, k=k, hc=hc, wgt=wgt, cs=cs, ncol=ncol: nc.tensor.matmul(
                                        PS[gb][:, 0:ncol], wgt[:, k, hc * 128:(hc + 1) * 128], hxTb[:, k, cs],
                                        start=(k == 0), stop=(k == 7))), r=[wgk] + rk_h, w=[PK[gb]])
                                for k in range(8):
                                    P.op('pe', (lambda ub=ub, k=k, hc=hc, wut=wut, cs=cs, ncol=ncol: nc.tensor.matmul(
                                        PS[ub][:, 0:ncol], wut[:, k, hc * 128:(hc + 1) * 128], hxTb[:, k, cs],
                                        start=(k == 0), stop=(k == 7))), r=[wuk] + rk_h, w=[PK[ub]])
                                sg, sgk = sgb.next()
                                P.op('act', (lambda sg=sg, gb=gb, ncol=ncol: nc.scalar.activation(
                                    out=sg[:, 0:ncol], in_=PS[gb][:, 0:ncol], func=AF.Silu)), r=[PK[gb]], w=[sgk])
                                P.op('dve', (lambda sg=sg, ub=ub, ncol=ncol, hid=hid, hc=hc: nc.vector.tensor_tensor(
                                    out=hid[:, hc, 0:ncol], in0=sg[:, 0:ncol], in1=PS[ub][:, 0:ncol], op=ALU.mult)),
                                    r=[sgk, PK[ub]], w=[(hidk, hc)])
                            for t in range(ntl):
                                i = t0 + t
                                for half in range(2):
                                    yb = 4 + yn % 2; yn += 1
                                    for hc in range(4):
                                        P.op('pe', (lambda yb=yb, hc=hc, t=t, half=half, hid=hid, wdt=wdt: nc.tensor.matmul(
                                            PS[yb], hid[:, hc, t * 128:(t + 1) * 128], wdt[:, hc, half * 512:(half + 1) * 512],
                                            start=(hc == 0), stop=(hc == 3))), r=[(hidk, hc), wdk], w=[PK[yb]])
                                    sl = slice(half * 512, (half + 1) * 512)
                                    if e == 0:
                                        P.op('dve', (lambda yb=yb, i=i, sl=sl, e=e: nc.vector.tensor_scalar(
                                            out=acc[:, i, sl], in0=PS[yb], scalar1=gates[:, i, e:e + 1], scalar2=None, op0=ALU.mult)),
                                            r=[PK[yb], ("gates", i)], w=[("acc", i, half)])
                                    else:
                                        P.op('dve', (lambda yb=yb, i=i, sl=sl, e=e: nc.vector.scalar_tensor_tensor(
                                            out=acc[:, i, sl], in0=PS[yb], scalar=gates[:, i, e:e + 1], in1=acc[:, i, sl],
                                            op0=ALU.mult, op1=ALU.add)), r=[PK[yb], ("gates", i), ("acc", i, half)], w=[("acc", i, half)])
                    for i, gt in enumerate(sbt):
                        mod, mkey = ("C", "mvec") if gt < NCT else ("L", "mvec")
                        xt, xk = R['xs'].next()
                        P.dma('sp', xt[:], src_t[gt], r=["d_XS1" if layer == 0 else "d_XS3"], w=[xk])
                        P.op('pool', (lambda i=i, mod=mod: nc.gpsimd.tensor_tensor(out=acc[:, i, :], in0=acc[:, i, :], in1=mv(mod, 5), op=ALU.mult)),
                             r=[("acc", i, 0), ("acc", i, 1), mkey], w=[("acc", i, 0), ("acc", i, 1)])
                        P.op('pool', (lambda i=i, xt=xt: nc.gpsimd.tensor_tensor(out=acc[:, i, :], in0=acc[:, i, :], in1=xt[:], op=ALU.add)),
                             r=[("acc", i, 0), ("acc", i, 1), xk], w=[("acc", i, 0), ("acc", i, 1)])
                        if not final:
                            P.dma('sp', dst_t[gt], acc[:, i, :], r=[("acc", i, 0), ("acc", i, 1)], w=["d_dst%d" % layer])
                        else:
                            o_, ok = hxf.next()
                            rms_mod(R, acc[:, i, :], ("acc", i, 0), nfb[:], None, ["nfb", ("acc", i, 1)], o_[:], ok)
                            P.dma('sp', dst_t[gt - NCT], o_[:], r=[ok], w=["d_out"])
                P.barrier()

        I32 = mybir.dt.int32
        NTS = 65
        NSLOT = NTS * 512
        HXB = dscr("HXB", [NT * 128, D], BF16); XG = dscr("XG", [NSLOT, D], BF16)
        WG = dscr("WG", [NSLOT, 1]); YG = dscr("YG", [NSLOT, D])
        HXB_t = HXB.rearrange("(t p) d -> t p d", p=128)
        W2 = {"g": w_e_gate.rearrange("l e (p k) n -> (l e p) (k n)", k=8), "u": w_e_up.rearrange("l e (p k) n -> (l e p) (k n)", k=8),
              "d": w_e_down.rearrange("l e d n -> (l e d) n")}

        def moe_sparse(layer, src_t, dst_t, tiles, final):
            T_ = len(tiles)
            skey = "d_XS1" if layer == 0 else "d_XS3"
            es2 = contextlib.ExitStack()
            with es2:
                cms = sb("cms", [128, 160], F32, es2)
                info = sb("info", [128, NT, 8], F32, es2)
                posi = sb("posi", [128, NT, 2], I32, es2)
                cum = sb("cum", [128, 32], F32, es2)
                offs = sb("offs", [128, 32], F32, es2)
                te = sb("te", [128, 80], F32, es2)
                tec = sb("tec", [128, 80], F32, es2)
                P.dma('sp', cms[:], cmisc, r=["d_cm"], w=["cms"])
                P.op('pool', lambda: nc.gpsimd.memset(cum[:], 0.0), r=[], w=["cum"])
                iota = cms[:, 0:32]; base = cms[:, 32:40]; svals = cms[:, 40:120]
                V = nc.vector; G = nc.gpsimd; A = nc.scalar
                with contextlib.ExitStack() as ph:
                    R = mk_rings(ph)
                    load_mods(ph, [(st, ix) for st in ("LC" if not final else "L") for ix in (3, 4)])
                    hxf = Ring(nc, ph, "hxf", 2, [128, D], F32)
                    hxb = Ring(nc, ph, "hxb", 2, [128, D], BF16)
                    hxTf = Ring(nc, ph, "hxTf", 2, [128, 8, 128], F32)
                    wrs = sb("wrs", [128, 8, 36], F32, ph); brb = sb("brb", [128, 36], F32, ph)
                    rt = Ring(nc, ph, "rt", 2, [128, 288], F32)
                    gates = None
                    P.dma('sp', wrs[:], w_r[layer].rearrange("(k p) n -> p k n", p=128), r=["d_wr"], w=["wrs"])
                    P.dma('sp', brb[:], b_r[layer].partition_broadcast(128), r=["d_br"], w=["brb"])

                    def route(i, r_, rk):
                        lg = r_[:, 0:36]; m4 = r_[:, 36:37]; nm4 = r_[:, 37:38]; e4 = r_[:, 40:44]; s4 = r_[:, 38:39]
                        pg = r_[:, 39:40]; ohg = r_[:, 44:48]; sel = r_[:, 48:56]; m8 = r_[:, 56:64]; d21 = r_[:, 64:65]
                        e21 = r_[:, 65:66]; w1 = r_[:, 66:67]; w2 = r_[:, 67:68]; eq = r_[:, 72:88]
                        oh1 = r_[:, 96:128]; oh2 = r_[:, 128:160]; ohs = r_[:, 160:192]; rkt = r_[:, 192:224]; tmp = r_[:, 224:256]

                        def dv(fn):
                            P.op('dve', fn, r=[rk], w=[rk])
                        P.op('dve', lambda: V.tensor_tensor(out=lg, in0=PS[5 - 4 * (i % 2)][:, 0:36], in1=brb[:], op=ALU.add), r=[PK[5 - 4 * (i % 2)], "brb"], w=[rk])
                        dv(lambda: V.reduce_max(out=m4, in_=lg[:, 0:4], axis=AX.X))
                        dv(lambda: V.tensor_scalar(out=nm4, in0=m4, scalar1=-1.0, scalar2=None, op0=ALU.mult))
                        yield
                        P.op('act', lambda: A.activation(out=e4, in_=lg[:, 0:4], func=AF.Exp, bias=nm4, scale=1.0), r=[rk], w=[rk])
                        yield
                        dv(lambda: V.reduce_sum(out=s4, in_=e4, axis=AX.X))
                        dv(lambda: V.reciprocal(out=pg, in_=s4))
                        dv(lambda: V.tensor_scalar(out=ohg, in0=lg[:, 0:4], scalar1=m4, scalar2=None, op0=ALU.is_equal))
                        dv(lambda: V.tensor_scalar(out=sel, in0=lg[:, 4:12], scalar1=ohg[:, 0:1], scalar2=None, op0=ALU.mult))
                        for g in range(1, 4):
                            dv(lambda g=g: V.scalar_tensor_tensor(out=sel, in0=lg[:, 4 + 8 * g:12 + 8 * g], scalar=ohg[:, g:g + 1],
                                                                  in1=sel, op0=ALU.mult, op1=ALU.add))
                        yield
                        dv(lambda: V.max(out=m8, in_=sel))
                        dv(lambda: V.tensor_tensor(out=d21, in0=m8[:, 1:2], in1=m8[:, 0:1], op=ALU.subtract))
                        yield
                        P.op('act', lambda: A.activation(out=e21, in_=d21, func=AF.Exp), r=[rk], w=[rk])
                        yield
                        dv(lambda: V.tensor_scalar(out=e21, in0=e21, scalar1=1.0, scalar2=None, op0=ALU.add))
                        dv(lambda: V.reciprocal(out=w1, in_=e21))
                        dv(lambda: V.tensor_tensor(out=info[:, i, 2:3], in0=w1, in1=pg, op=ALU.mult))
                        dv(lambda: V.tensor_tensor(out=info[:, i, 5:6], in0=pg, in1=info[:, i, 2:3], op=ALU.subtract))
                        dv(lambda: V.tensor_scalar(out=eq[:, 0:8], in0=sel, scalar1=m8[:, 0:1], scalar2=None, op0=ALU.is_equal))
                        dv(lambda: V.tensor_scalar(out=eq[:, 8:16], in0=sel, scalar1=m8[:, 1:2], scalar2=None, op0=ALU.is_equal))
                        for j, oh in enumerate((oh1, oh2)):
                            dv(lambda j=j, oh=oh: V.tensor_tensor(out=oh.rearrange("p (g e) -> p g e", g=4),
                                                                  in0=eq[:, 8 * j:8 * j + 8].unsqueeze(1).to_broadcast([128, 4, 8]),
                                                                  in1=ohg.unsqueeze(2).to_broadcast([128, 4, 8]), op=ALU.mult))
                        dv(lambda: V.tensor_tensor(out=ohs, in0=oh1, in1=oh2, op=ALU.add))
                        yield
                        P.op('pe', lambda: nc.tensor.matmul(PS[4 - 4 * (i % 2)][:, 0:32], msk[:, 6, :], ohs, start=True, stop=True), r=[rk, "msk"], w=[PK[4 - 4 * (i % 2)]])
                        P.op('pe', lambda: nc.tensor.matmul(PS[4 - 4 * (i % 2)][:, 32:64], msk[:, 3, :], ohs, start=True, stop=True), r=[rk, "msk"], w=[PK[4 - 4 * (i % 2)]])
                        yield
                        P.op('dve', lambda: V.tensor_tensor(out=rkt, in0=PS[4 - 4 * (i % 2)][:, 0:32], in1=cum[:], op=ALU.add), r=[PK[4 - 4 * (i % 2)], "cum", rk], w=[rk])
                        P.op('dve', lambda: V.tensor_tensor(out=cum[:], in0=cum[:], in1=PS[4 - 4 * (i % 2)][:, 32:64], op=ALU.add), r=[PK[4 - 4 * (i % 2)], "cum", rk], w=["cum"])
                        for j, oh in enumerate((oh1, oh2)):
                            dv(lambda oh=oh: V.tensor_tensor(out=tmp, in0=oh, in1=rkt, op=ALU.mult))
                            dv(lambda j=j: V.reduce_sum(out=info[:, i, 3 * j + 1:3 * j + 2], in_=tmp, axis=AX.X))
                            dv(lambda oh=oh: V.tensor_tensor(out=tmp, in0=oh, in1=iota, op=ALU.mult))
                            dv(lambda j=j: V.reduce_sum(out=info[:, i, 3 * j:3 * j + 1], in_=tmp, axis=AX.X))

                    def sa_tile(i, gt):
                        mod, mkey = ("C", "mvec") if gt < NCT else ("L", "mvec")
                        xt, xk = R['xs'].next()
                        P.dma('sp', xt[:], src_t[gt], r=[skey], w=[xk])
                        hx, hk = hxf.next()
                        rms_mod(R, xt[:], xk, mv(mod, 4), mv(mod, 3), [mkey], hx[:], hk)
                        yield
                        hb, hbk = hxb.next()
                        P.op('pool', (lambda hb=hb, hx=hx: G.tensor_copy(out=hb[:], in_=hx[:])), r=[hk], w=[hbk])
                        P.dma('sp', HXB_t[gt], hb[:], r=[hbk], w=["d_HXB"])
                        for k in range(8):
                            b = (6 if i % 2 == 0 else 2) + k // 4
                            P.op('pe', (lambda b=b, k=k, hx=hx: nc.tensor.transpose(
                                PS[b][:, (k % 4) * 128:(k % 4) * 128 + 128], hx[:, k * 128:(k + 1) * 128], ident)),
                                r=[hk, "msk"], w=[PK[b]])
                        yield
                        hT, hTk = hxTf.next()
                        for h in range(2):
                            b = (6 if i % 2 == 0 else 2) + h
                            P.op('act', (lambda b=b, h=h, hT=hT: A.copy(
                                out=hT[:, 4 * h:4 * h + 4, :], in_=PS[b].rearrange("p (k t) -> p k t", k=4))), r=[PK[b]], w=[hTk])
                        for k in range(8):
                            P.op('pe', (lambda k=k, hT=hT: nc.tensor.matmul(PS[5 - 4 * (i % 2)][:, 0:36], hT[:, k, :], wrs[:, k, :],
                                                                          start=(k == 0), stop=(k == 7))), r=[hTk, "wrs"], w=[PK[5 - 4 * (i % 2)]])
                        yield
                        r_, rk = rt.next()
                        yield from route(i, r_, rk)

                    run_interleaved(P, (sa_tile(i, gt) for i, gt in enumerate(tiles)), 2)
                    ci = sb("ci", [128, 32], I32, ph); pn = sb("pn", [128, 32], F32, ph)
                    sa = sb("sa", [128, 32], F32, ph); sb_ = sb("sb_", [128, 32], F32, ph)
                    P.op('dve', lambda: V.tensor_copy(out=ci[:], in_=cum[:]), r=["cum"], w=["ci"])
                    P.op('dve', lambda: V.tensor_scalar(out=ci[:], in0=ci[:], scalar1=511, scalar2=None, op0=ALU.add), r=["ci"], w=["ci"])
                    P.op('dve', lambda: V.tensor_scalar(out=ci[:], in0=ci[:], scalar1=9, scalar2=None, op0=ALU.arith_shift_right), r=["ci"], w=["ci"])
                    P.op('dve', lambda: V.tensor_scalar(out=ci[:], in0=ci[:], scalar1=9, scalar2=None, op0=ALU.logical_shift_left), r=["ci"], w=["ci"])
                    P.op('dve', lambda: V.tensor_copy(out=pn[:], in_=ci[:]), r=["ci"], w=["pn"])
                    P.op('dve', lambda: V.tensor_copy(out=sa[:], in_=pn[:]), r=["pn"], w=["sa"])
                    a_, b_ = sa, sb_
                    ak, bk = "sa", "sb_"
                    for sh in (1, 2, 4, 8, 16):
                        P.op('dve', (lambda a_=a_, b_=b_, sh=sh: V.tensor_copy(out=b_[:, 0:sh], in_=a_[:, 0:sh])), r=[ak], w=[bk])
                        P.op('dve', (lambda a_=a_, b_=b_, sh=sh: V.tensor_tensor(out=b_[:, sh:32], in0=a_[:, sh:32], in1=a_[:, 0:32 - sh], op=ALU.add)),
                             r=[ak, bk], w=[bk])
                        a_, b_, ak, bk = b_, a_, bk, ak
                    incl, inclk = a_, ak
                    P.op('dve', lambda: V.tensor_tensor(out=offs[:], in0=incl[:], in1=pn[:], op=ALU.subtract), r=[inclk, "pn"], w=["offs"])
                    P.op('pool', lambda: G.memset(te[:], 0.0), r=[], w=["te"])
                    for e in range(32):
                        P.op('dve', (lambda e=e: V.scalar_tensor_tensor(out=te[:], in0=svals, scalar=incl[:, e:e + 1], in1=te[:], op0=ALU.is_ge, op1=ALU.add)),
                             r=[inclk, "te", "cms"], w=["te"])
                    P.op('dve', lambda: V.tensor_scalar(out=tec[:], in0=te[:], scalar1=31.0, scalar2=None, op0=ALU.min), r=["te"], w=["tec"])
                    P.barrier()
                if stop_after == "sA":
                    P.finish(); return "STOP"
                with contextlib.ExitStack() as ph:
                    zt = sb("zt", [128, 4096], BF16, ph)
                    hb2 = Ring(nc, ph, "hb2", 3, [128, D], BF16)
                    pt = Ring(nc, ph, "pt", 2, [128, 72], F32)
                    P.op('pool', lambda: G.memset(zt[:], 0.0), r=[], w=["zt"])
                    XGz = XG.rearrange("(q p f) d -> q p (f d)", p=128, f=4)
                    for q in range(NTS):
                        P.dma('sp', XGz[q], zt[:], r=["zt"], w=["d_XG"])
                    for i, gt in enumerate(tiles):
                        p_, pk_ = pt.next()
                        for j in range(2):
                            P.op('dve', (lambda p_=p_, i=i, j=j: V.tensor_scalar(out=p_[:, 0:32], in0=iota, scalar1=info[:, i, 3 * j:3 * j + 1], scalar2=None, op0=ALU.is_equal)),
                                 r=["info", "cms", pk_], w=[pk_])
                            P.op('dve', (lambda p_=p_: V.tensor_tensor(out=p_[:, 0:32], in0=p_[:, 0:32], in1=offs[:], op=ALU.mult)), r=[pk_, "offs"], w=[pk_])
                            P.op('dve', (lambda p_=p_, j=j: V.reduce_sum(out=p_[:, 32 + j:33 + j], in_=p_[:, 0:32], axis=AX.X)), r=[pk_], w=[pk_])
                            P.op('dve', (lambda p_=p_, i=i, j=j: V.tensor_tensor(out=p_[:, 32 + j:33 + j], in0=p_[:, 32 + j:33 + j], in1=info[:, i, 3 * j + 1:3 * j + 2], op=ALU.add)),
                                 r=[pk_, "info"], w=[pk_])
                        P.op('dve', (lambda p_=p_, i=i: V.tensor_copy(out=posi[:, i, :], in_=p_[:, 32:34])), r=[pk_], w=[("posi", i)])
                        hb, hbk = hb2.next()
                        P.dma('sp', hb[:], HXB_t[gt], r=["d_HXB"], w=[hbk])
                        for j in range(2):
                            P.op('pool', (lambda hb=hb, i=i, j=j: G.indirect_dma_start(
                                out=XG[:, :], out_offset=bass.IndirectOffsetOnAxis(ap=posi[:, i, j:j + 1], axis=0), in_=hb[:, :], in_offset=None)),
                                r=[hbk, ("posi", i), "d_XG"], w=["d_XGs"], dma=True)
                            P.op('pool', (lambda i=i, j=j: G.indirect_dma_start(
                                out=WG[:, :], out_offset=bass.IndirectOffsetOnAxis(ap=posi[:, i, j:j + 1], axis=0), in_=info[:, i, 3 * j + 2:3 * j + 3], in_offset=None)),
                                r=["info", ("posi", i)], w=["d_WG"], dma=True)
                        P.flush()
                    P.barrier()
                if stop_after == "sC":
                    P.finish(); return "STOP"
                with contextlib.ExitStack() as ph:
                    wg = Ring(nc, ph, "wg", 2, [128, 8, 512], BF16); wu = Ring(nc, ph, "wu", 2, [128, 8, 512], BF16)
                    wd = Ring(nc, ph, "wd", 2, [128, 4, D], BF16)
                    xg = Ring(nc, ph, "xg", 2, [128, 4, D], BF16); xT = Ring(nc, ph, "xT", 2, [128, 8, 512], BF16)
                    wgt = Ring(nc, ph, "wgt", 2, [128, 4], F32)
                    idf = Ring(nc, ph, "idf", 2, [128, 12], F32); idi = Ring(nc, ph, "idi", 2, [128, 12], I32)
                    sgb = Ring(nc, ph, "sgb", 2, [128, 512], BF16); hidT = Ring(nc, ph, "hidT", 2, [128, 4, 512], BF16)
                    yg = Ring(nc, ph, "yg", 2, [128, D], F32)
                    hcn = [0]; yn = [0]

                    def slot_tile(s_):
                        f_, fk = idf.next(); ii, ik = idi.next()
                        P.op('dve', lambda: V.scalar_tensor_tensor(out=f_[:, 0:1], in0=tec[:, s_:s_ + 1], scalar=128.0, in1=base[:, 0:1],
                                                                   op0=ALU.mult, op1=ALU.add), r=["tec", "cms"], w=[fk])
                        P.op('dve', lambda: V.scalar_tensor_tensor(out=f_[:, 8:12], in0=tec[:, s_:s_ + 1].to_broadcast([128, 4]), scalar=512.0, in1=base[:, 0:4],
                                                                   op0=ALU.mult, op1=ALU.add), r=["tec", "cms", fk], w=[fk])
                        if layer > 0:
                            P.op('dve', lambda: V.tensor_scalar(out=f_[:, 0:1], in0=f_[:, 0:1], scalar1=float(layer * 32 * 128), scalar2=None, op0=ALU.add), r=[fk], w=[fk])
                            P.op('dve', lambda: V.tensor_scalar(out=f_[:, 8:12], in0=f_[:, 8:12], scalar1=float(layer * 32 * 512), scalar2=None, op0=ALU.add), r=[fk], w=[fk])
                        P.op('dve', lambda: V.tensor_copy(out=ii[:], in_=f_[:]), r=[fk], w=[ik])
                        wgt_, wgk = wg.next(); wut, wuk = wu.next(); wdt, wdk = wd.next()
                        P.op('pool', lambda: G.indirect_dma_start(out=wgt_[:].rearrange("p k n -> p (k n)"), out_offset=None, in_=W2["g"],
                                                                  in_offset=bass.IndirectOffsetOnAxis(ap=ii[:, 0:1], axis=0)),
                             r=[ik], w=[(wgk, k) for k in range(8)], dma=True)
                        P.op('pool', lambda: G.indirect_dma_start(out=wut[:].rearrange("p k n -> p (k n)"), out_offset=None, in_=W2["u"],
                                                                  in_offset=bass.IndirectOffsetOnAxis(ap=ii[:, 0:1], axis=0)),
                             r=[ik], w=[(wuk, k) for k in range(8)], dma=True)
                        for k in range(4):
                            P.op('pool', (lambda k=k: G.indirect_dma_start(out=wdt[:, k, :], out_offset=None, in_=W2["d"],
                                                                         in_offset=bass.IndirectOffsetOnAxis(ap=ii[:, 8 + k:9 + k], axis=0))),
                                 r=[ik], w=[(wdk, k)], dma=True)
                        yield
                        x_, xk_ = xg.next(); xt_, xtk = xT.next(); g4, g4k = wgt.next()
                        P.dma('sp', x_[:], XG[s_ * 512:(s_ + 1) * 512, :].rearrange("(t p) d -> p t d", p=128), r=["d_XGs", "d_XG"], w=[xk_])
                        P.dma('sp', g4[:], WG[s_ * 512:(s_ + 1) * 512, :].rearrange("(t p) o -> p (t o)", p=128), r=["d_WG"], w=[g4k], allow_slow_non_contiguous=True)
                        yield
                        for t in range(4):
                            b = 6 + t % 2
                            psb = PS[b].bitcast(BF16)
                            for k in range(8):
                                P.op('pe', (lambda psb=psb, k=k, t=t: nc.tensor.transpose(psb[:, k * 128:(k + 1) * 128], x_[:, t, k:D:8], identb[:])),
                                     r=[xk_, "identb"], w=[PK[b]])
                            P.op('act', (lambda psb=psb, t=t: A.copy(out=xt_[:, :, t * 128:(t + 1) * 128], in_=psb.rearrange("p (k c) -> p k c", k=8))),
                                 r=[PK[b]], w=[(xtk, t)])
                        yield
                        xtkeys = [(xtk, t) for t in range(4)]
                        hid, hidk = hidT.next()
                        for hc in range(4):
                            gb = (hcn[0] % 2) * 2; ub = gb + 1; hcn[0] += 1
                            for k in range(8):
                                P.op('pe', (lambda gb=gb, k=k, hc=hc: nc.tensor.matmul(PS[gb], wgt_[:, k, hc * 128:(hc + 1) * 128], xt_[:, k, :],
                                                                                    start=(k == 0), stop=(k == 7))), r=[(wgk, k)] + xtkeys, w=[PK[gb]])
                            for k in range(8):
                                P.op('pe', (lambda ub=ub, k=k, hc=hc: nc.tensor.matmul(PS[ub], wut[:, k, hc * 128:(hc + 1) * 128], xt_[:, k, :],
                                                                                    start=(k == 0), stop=(k == 7))), r=[(wuk, k)] + xtkeys, w=[PK[ub]])
                            yield
                            sg, sgk = sgb.next()
                            P.op('act', (lambda sg=sg, gb=gb: A.activation(out=sg[:], in_=PS[gb], func=AF.Silu)), r=[PK[gb]], w=[sgk])
                            P.op('dve', (lambda sg=sg, ub=ub, hc=hc: V.tensor_tensor(out=hid[:, hc, :], in0=sg[:], in1=PS[ub], op=ALU.mult)),
                                 r=[sgk, PK[ub]], w=[(hidk, hc)])
                        for t in range(4):
                            yield
                            y_, yk = yg.next()
                            for half in range(2):
                                yb_ = 4 + yn[0] % 2; yn[0] += 1
                                for hc in range(4):
                                    P.op('pe', (lambda yb_=yb_, hc=hc, t=t, half=half: nc.tensor.matmul(
                                        PS[yb_], hid[:, hc, t * 128:(t + 1) * 128], wdt[:, hc, half * 512:(half + 1) * 512],
                                        start=(hc == 0), stop=(hc == 3))), r=[(hidk, hc), (wdk, hc)], w=[PK[yb_]])
                                P.op('act', (lambda yb_=yb_, half=half, t=t, y_=y_: A.activation(out=y_[:, half * 512:(half + 1) * 512], in_=PS[yb_], func=AF.Copy,
                                                                                            scale=g4[:, t:t + 1])), r=[PK[yb_], g4k], w=[(yk, half)])
                            P.dma('sp', YG[s_ * 512 + t * 128:s_ * 512 + (t + 1) * 128, :], y_[:], r=[(yk, 0), (yk, 1)], w=["d_YG"])

                    run_interleaved(P, (slot_tile(s_) for s_ in range(NTS)), 2)
                    P.barrier()
                if stop_after == "sD":
                    P.finish(); return "STOP"
                with contextlib.ExitStack() as ph:
                    R = mk_rings(ph)
                    load_mods(ph, [(st, 5) for st in ("LC" if not final else "L")])
                    y1 = Ring(nc, ph, "y1", 2, [128, D], F32); y2 = Ring(nc, ph, "y2", 2, [128, D], F32)
                    ofin = Ring(nc, ph, "ofin", 2, [128, D], F32)
                    nfb = None
                    if final:
                        nfb = sb("nfb", [128, D], F32, ph)
                        P.dma('sp', nfb[:], norm_final.partition_broadcast(128), r=["d_nf"], w=["nfb"])

                    def comb(i, gt):
                        mod, mkey = ("C", "mvec") if gt < NCT else ("L", "mvec")
                        a_, ak_ = y1.next(); b_, bk_ = y2.next()
                        for j, (dst, dk_) in enumerate(((a_, ak_), (b_, bk_))):
                            P.op('pool', (lambda dst=dst, j=j: G.indirect_dma_start(out=dst[:, :], out_offset=None, in_=YG[:, :],
                                                                                  in_offset=bass.IndirectOffsetOnAxis(ap=posi[:, i, j:j + 1], axis=0))),
                                 r=[("posi", i), "d_YG"], w=[dk_], dma=True)
                        xt, xk = R['xs'].next()
                        P.dma('sp', xt[:], src_t[gt], r=[skey], w=[xk])
                        P.op('pool', lambda: G.tensor_tensor(out=a_[:], in0=a_[:], in1=b_[:], op=ALU.add), r=[ak_, bk_], w=[ak_])
                        P.op('dve', lambda: V.tensor_tensor(out=a_[:], in0=a_[:], in1=mv(mod, 5), op=ALU.mult), r=[ak_, mkey], w=[ak_])
                        P.op('pool', lambda: G.tensor_tensor(out=a_[:], in0=a_[:], in1=xt[:], op=ALU.add), r=[ak_, xk], w=[ak_])
                        if not final:
                            P.dma('sp', dst_t[gt], a_[:], r=[ak_], w=["d_dst%d" % layer])
                        else:
                            o_, ok = ofin.next()
                            rms_mod(R, a_[:], ak_, nfb[:], None, ["nfb"], o_[:], ok)
                            P.dma('sp', dst_t[gt - NCT], o_[:], r=[ok], w=["d_out"])

                    for i, gt in enumerate(tiles):
                        comb(i, gt)
                    P.barrier()

        import os
        MOE = moe_sparse if os.environ.get("DENSE_MOE") is None else moe_phase
        if MOE(0, XS1_t, XS2_t, list(range(NT)), False) == "STOP":
            return nc
        if stop_after == "moe0":
            P.finish()
            return nc

        bfd = lambda name, shape: dscr(name, shape, BF16)
        QT_d = bfd("QT_d", [NT, 128, 8, 128]); KT_d = bfd("KT_d", [NT, 128, 8, 128])
        KK_d = bfd("KK_d", [NT, 128, 8, 128]); VV_d = bfd("VV_d", [NT, 128, 8, 128])
        ZZ_d = bfd("ZZ_d", [NT, 128, D]); GB_d = dscr("GB_d", [NT, 128, 32])
        OF_d = dscr("OF_d", [2, NLT, 128, D])
        with contextlib.ExitStack() as ph:
            adaln(1, ph)
            P.barrier()

        with contextlib.ExitStack() as ph:
            R = mk_rings(ph)
            load_mods(ph, [(st, ix) for st in "LC" for ix in (0, 1)])
            SBT1 = 4
            hxf = Ring(nc, ph, "c1hx", 1, [128, D], F32)
            hxT = sb("c1hxT", [128, 8, (SBT1 + 2) * 128], BF16, ph)
            pT = Ring(nc, ph, "pT", 4, [128, SBT1 * 128 + 4], F32)
            cv = Ring(nc, ph, "cv", 4, [128, SBT1 * 128], F32)
            sqb = Ring(nc, ph, "sqb", 3, [128, SBT1 * 128], BF16)
            rsb = Ring(nc, ph, "rsb", 3, [128, SBT1 * 128], F32)
            kf = Ring(nc, ph, "kf", 3, [128, SBT1 * 128], F32)
            qst = sb("qst", [128, SBT1, 8, 128], BF16, ph); kst = sb("kst", [128, SBT1, 8, 128], BF16, ph)
            ktst = sb("ktst", [128, SBT1, 8, 128], BF16, ph); vtst = sb("vtst", [128, SBT1, 8, 128], BF16, ph)
            win_sb = sb("win_sb", [128, 8, 3072], BF16, ph)
            wz = sb("wz", [128, 8, 1056], BF16, ph)
            wcs = sb("wcs", [128, 24, 4], F32, ph)
            onesb = sb("onesb", [128, 128], BF16, ph)
            c16 = sb("c16", [128, 2, 16], F32, ph)
            zsb = Ring(nc, ph, "zsb", 1, [128, D], BF16)
            gbr = Ring(nc, ph, "gbr", 2, [128, 64], F32)
            P.dma('pool', wz[:], w_dn_in.rearrange("(k p) n -> p k n", p=128)[:, :, 3072:4128], r=["d_win"], w=["wz"])
            P.dma('sp', wcs[:], wconv, r=["d_wconv"], w=["wcs"])
            P.op('dve', lambda: nc.vector.tensor_copy(out=onesb[:], in_=msk[:, 3, :]), r=["msk"], w=["onesb"])
            P.dma('sp', c16[:, 0, :], dn_dt_bias.partition_broadcast(128), r=["d_dtb"], w=["c16"])
            P.dma('sp', c16[:, 1, :], dn_a_log.partition_broadcast(128), r=["d_alog"], w=["c16"])
            P.op('act', lambda: nc.scalar.activation(out=c16[:, 1, :], in_=c16[:, 1, :], func=AF.Exp), r=["c16"], w=["c16"])
            P.op('dve', lambda: nc.vector.tensor_scalar(out=c16[:, 1, :], in0=c16[:, 1, :], scalar1=-1.0, scalar2=None, op0=ALU.mult),
                 r=["c16"], w=["c16"])
            win_v = w_dn_in.rearrange("(k p) n -> p k n", p=128)
            for j6 in range(6):
                P.dma('pool', win_sb[:, :, j6 * 512:(j6 + 1) * 512], win_v[:, :, j6 * 512:(j6 + 1) * 512], r=["d_win"], w=[("win_sb", j6)])

            def c1_tile_norm(gt, slot, mod, mkey):
                xt, xk = R['xs'].next()
                P.dma('sp', xt[:], XS2_t[gt], r=["d_dst0"], w=[xk])
                hx, hk = hxf.next()
                rms_mod(R, xt[:], xk, mv(mod, 1), mv(mod, 0), [mkey], hx[:], hk)
                for k in range(8):
                    b = 6 + k // 4
                    P.op('pe', (lambda b=b, k=k, hx=hx: nc.tensor.transpose(
                        PS[b][:, (k % 4) * 128:(k % 4) * 128 + 128], hx[:, k * 128:(k + 1) * 128], ident)),
                        r=[hk, "msk"], w=[PK[b]])
                for h in range(2):
                    b = 6 + h
                    P.op('act', (lambda b=b, h=h, slot=slot: nc.scalar.copy(
                        out=hxT[:, 4 * h:4 * h + 4, slot * 128:(slot + 1) * 128],
                        in_=PS[b].rearrange("p (k t) -> p k t", k=4))), r=[PK[b]], w=[("hxT", slot)])

            def c1_zab(gt, slot, is_ctx):
                hk = [("hxT", slot)]
                if not is_ctx:
                    zs, zk = zsb.next()
                    for half in range(2):
                        b = 4 + half
                        for k in range(8):
                            P.op('pe', (lambda b=b, k=k, half=half, slot=slot: nc.tensor.matmul(
                                PS[b], hxT[:, k, slot * 128:(slot + 1) * 128], wz[:, k, half * 512:(half + 1) * 512],
                                start=(k == 0), stop=(k == 7))), r=hk + ["wz"], w=[PK[b]])
                        P.op('act', (lambda b=b, half=half, zs=zs: nc.scalar.activation(
                            out=zs[:, half * 512:(half + 1) * 512], in_=PS[b], func=AF.Silu)), r=[PK[b]], w=[(zk, half)])
                    P.dma('sp', ZZ_d[gt], zs[:], r=[(zk, 0), (zk, 1)], w=["d_ZZ"])
                for k in range(8):
                    P.op('pe', (lambda k=k, slot=slot: nc.tensor.matmul(
                        PS[3][:, 0:32], hxT[:, k, slot * 128:(slot + 1) * 128], wz[:, k, 1024:1056],
                        start=(k == 0), stop=(k == 7))), r=hk + ["wz"], w=[PK[3]])
                g_, gk = gbr.next()
                V = nc.vector
                ab = g_[:, 0:32].rearrange("p (f h) -> p f h", f=4)
                o4 = g_[:, 32:64].rearrange("p (f h) -> p f h", f=4)
                P.op('dve', lambda: V.tensor_copy(out=g_[:, 0:32], in_=PS[3][:, 0:32]), r=[PK[3]], w=[gk])
                P.op('dve', lambda: V.tensor_tensor(out=o4[:, 0::2, :], in0=ab[:, 0::2, :],
                                                    in1=c16[:, 0, :].rearrange("p (d h) -> p d h", d=2), op=ALU.add),
                     r=[gk, "c16"], w=[gk])
                P.op('act', lambda: nc.scalar.activation(out=o4[:, 0::2, :], in_=o4[:, 0::2, :], func=AF.Exp), r=[gk], w=[gk])
                P.op('dve', lambda: V.tensor_scalar(out=o4[:, 0::2, :], in0=o4[:, 0::2, :], scalar1=1.0, scalar2=None, op0=ALU.add),
                     r=[gk], w=[gk])
                P.op('act', lambda: nc.scalar.activation(out=o4[:, 0::2, :], in_=o4[:, 0::2, :], func=AF.Ln), r=[gk], w=[gk])
                P.op('dve', lambda: V.tensor_tensor(out=o4[:, 0::2, :], in0=o4[:, 0::2, :],
                                                    in1=c16[:, 1, :].rearrange("p (d h) -> p d h", d=2), op=ALU.mult),
                     r=[gk, "c16"], w=[gk])
                P.op('act', lambda: nc.scalar.activation(out=o4[:, 1::2, :], in_=ab[:, 1::2, :], func=AF.Sigmoid), r=[gk], w=[gk])
                P.dma('sp', GB_d[gt], g_[:, 32:64], r=[gk], w=["d_GB"])

            def c1_chunk(cc, seq0, nseq, t0, t1, hbase_tile):
                W = (t1 - t0) * 128
                ntile = t1 - t0
                wk = ("win_sb", cc // 4)
                p_, pk = pT.next()
                tok0 = t0 * 128 - 2
                lo = max(tok0, 0); hi = min(t1 * 128 + 1, nseq * 128)
                if lo > tok0:
                    P.op('pool', lambda: nc.gpsimd.memset(p_[:, 0:lo - tok0], 0.0), r=[], w=[(pk, 'l')])
                if hi < t1 * 128 + 1:
                    P.op('pool', lambda: nc.gpsimd.memset(p_[:, hi - tok0:W + 3], 0.0), r=[], w=[(pk, 'r')])
                a = lo
                wi = 0
                while a < hi:
                    b_ = min(a + 512, hi)
                    bank = (wi + cc) % 3
                    hk = [("hxT", s_) for s_ in range((a // 128) - hbase_tile, ((b_ - 1) // 128) - hbase_tile + 1)]
                    for k in range(8):
                        P.op('pe', (lambda k=k, bank=bank, a=a, b_=b_: nc.tensor.matmul(
                            PS[bank][:, 0:b_ - a], win_sb[:, k, cc * 128:(cc + 1) * 128], hxT[:, k, a - hbase_tile * 128:b_ - hbase_tile * 128],
                            start=(k == 0), stop=(k == 7))), r=[wk] + hk, w=[PK[bank]])
                    P.op('act', (lambda bank=bank, a=a, b_=b_: nc.scalar.copy(out=p_[:, a - tok0:b_ - tok0], in_=PS[bank][:, 0:b_ - a])),
                         r=[PK[bank]], w=[(pk, wi)])
                    a = b_; wi += 1
                yield
                pkeys = [(pk, j) for j in range(wi)] + [(pk, 'l'), (pk, 'r')]
                c_, ck = cv.next()
                P.op('dve', lambda: nc.vector.tensor_scalar(out=c_[:, 0:W], in0=p_[:, 0:W], scalar1=wcs[:, cc, 0:1], scalar2=None, op0=ALU.mult),
                     r=pkeys + ["wcs"], w=[ck])
                for tap in range(1, 4):
                    eng = 'dve'
                    E_ = nc.gpsimd if eng == 'pool' else nc.vector
                    P.op(eng, (lambda tap=tap, E_=E_: E_.scalar_tensor_tensor(out=c_[:, 0:W], in0=p_[:, tap:tap + W], scalar=wcs[:, cc, tap:tap + 1],
                                                                             in1=c_[:, 0:W], op0=ALU.mult, op1=ALU.add)),
                         r=pkeys + ["wcs", ck], w=[ck])
                yield
                P.op('act', lambda: nc.scalar.activation(out=c_[:, 0:W], in_=c_[:, 0:W], func=AF.Silu), r=[ck], w=[ck])
                yield
                kind = cc // 8; h = cc % 8
                src = c_
                srck = ck
                if kind < 2:
                    sq, sqk = sqb.next(); rs, rk_ = rsb.next()
                    P.op('pool', lambda: nc.gpsimd.tensor_tensor(out=sq[:, 0:W], in0=c_[:, 0:W], in1=c_[:, 0:W], op=ALU.mult), r=[ck], w=[sqk])
                    for j in range(0, W, 512):
                        n_ = min(512, W - j)
                        bank = 3 + (j // 512) % 2
                        P.op('pe', (lambda j=j, n_=n_, bank=bank: nc.tensor.matmul(PS[bank][:, 0:n_], onesb[:], sq[:, j:j + n_], start=True, stop=True)),
                             r=[sqk, "onesb"], w=[PK[bank]])
                        P.op('dve', (lambda j=j, n_=n_, bank=bank: nc.vector.tensor_scalar(out=rs[:, j:j + n_], in0=PS[bank][:, 0:n_], scalar1=EPS, scalar2=None, op0=ALU.add)),
                             r=[PK[bank]], w=[(rk_, j)])
                    yield
                    rkeys = [(rk_, j) for j in range(0, W, 512)]
                    P.op('act', lambda: nc.scalar.activation(out=rs[:, 0:W], in_=rs[:, 0:W], func=AF.Sqrt), r=rkeys, w=rkeys)
                    yield
                    P.op('dve', lambda: nc.vector.reciprocal(out=rs[:, 0:W], in_=rs[:, 0:W]), r=rkeys, w=rkeys)
                    if kind == 0:
                        P.op('dve', lambda: nc.vector.scalar_tensor_tensor(
                            out=qst[:, 0:ntile, h, :], in0=c_[:, 0:W].rearrange("p (t k) -> p t k", k=128), scalar=float(128 ** -0.5),
                            in1=rs[:, 0:W].rearrange("p (t k) -> p t k", k=128), op0=ALU.mult, op1=ALU.mult), r=[ck] + rkeys, w=[("qst", h)])
                        return
                    kf_, kfk = kf.next()
                    P.op('dve', lambda: nc.vector.tensor_tensor(out=kf_[:, 0:W], in0=c_[:, 0:W], in1=rs[:, 0:W], op=ALU.mult), r=[ck] + rkeys, w=[kfk])
                    P.op('pool', lambda: nc.gpsimd.tensor_copy(out=kst[:, 0:ntile, h, :], in_=kf_[:, 0:W].rearrange("p (t k) -> p t k", k=128)),
                         r=[kfk], w=[("kst", h)])
                    src = kf_; srck = kfk
                yield
                dst = ktst if kind == 1 else vtst
                dkey = "ktst" if kind == 1 else "vtst"
                for j in range(0, ntile, 4):
                    n_ = min(4, ntile - j)
                    bank = 5 + (j // 4) % 2
                    for t in range(n_):
                        P.op('pe', (lambda t=t, j=j, bank=bank: nc.tensor.transpose(
                            PS[bank][:, t * 128:(t + 1) * 128], src[:, (j + t) * 128:(j + t + 1) * 128], ident)),
                            r=[srck, "msk"], w=[PK[bank]])
                    P.op('act', (lambda j=j, n_=n_, bank=bank: nc.scalar.copy(
                        out=dst[:, j:j + n_, h, :], in_=PS[bank][:, 0:n_ * 128].rearrange("p (t k) -> p t k", k=128))),
                        r=[PK[bank]], w=[(dkey, h, j)])

            def c1_superblock(seq0, nseq, t0, t1, is_ctx):
                mod, mkey = ("C", "mvec") if is_ctx else ("L", "mvec")
                hb = max(t0 - 1, 0); he = min(t1 + 1, nseq)
                for lt in range(hb, he):
                    c1_tile_norm(seq0 + lt, lt - hb, mod, mkey)
                for lt in range(t0, t1):
                    c1_zab(seq0 + lt, lt - hb, is_ctx)
                run_interleaved(P, (c1_chunk(cc, seq0, nseq, t0, t1, hb) for cc in range(24)), 3)
                nt_ = t1 - t0
                g0 = seq0 + t0
                for (dst, st, key) in ((QT_d, qst, "qst"), (KT_d, kst, "kst")):
                    P.dma('sp', dst[g0:g0 + nt_].rearrange("t p h k -> p t h k"), st[:, 0:nt_, :, :],
                          r=[(key, h) for h in range(8)], w=["d_" + key])
                for (dst, st, key) in ((KK_d, ktst, "ktst"), (VV_d, vtst, "vtst")):
                    P.dma('sp', dst[g0:g0 + nt_].rearrange("t p h k -> p t h k"), st[:, 0:nt_, :, :],
                          r=[(key, h, j) for h in range(8) for j in range(0, nt_, 4)], w=["d_" + key])

            c1_superblock(0, NCT, 0, NCT, True)
            for t0 in range(0, NLT, SBT1):
                c1_superblock(NCT, NLT, t0, t0 + SBT1, False)
            P.barrier()
        if stop_after == "c1":
            P.finish()
            return nc

        with contextlib.ExitStack() as ph:
            HS = [128, 8, 128]
            lKT = Ring(nc, ph, "lKT", 2, HS, BF16); lQT = Ring(nc, ph, "lQT", 2, HS, BF16)
            lKK = Ring(nc, ph, "lKK", 2, HS, BF16); lVV = Ring(nc, ph, "lVV", 2, HS, BF16)
            gbl = Ring(nc, ph, "gbl", 2, [128, 32], F32)
            scr = Ring(nc, ph, "scr", 2, [128, 80], F32)
            L16 = Ring(nc, ph, "L16", 2, [16, 4, 128], F32)
            rE = Ring(nc, ph, "rE", 2, [16, 2, 8, 128], F32)
            bmask = sb("bmask", [16, 8, 128], F32, ph)
            Ei = Ring(nc, ph, "Ei", 2, HS, F32); Es = Ring(nc, ph, "Es", 2, HS, F32)
            SBm = Ring(nc, ph, "SBm", 2, HS, F32); tf = Ring(nc, ph, "tf", 2, HS, F32)
            Ak = Ring(nc, ph, "Ak", 4, HS, BF16); Bk = Ring(nc, ph, "Bk", 4, HS, BF16); Tk = Ring(nc, ph, "Tk", 4, HS, BF16)
            TTk = Ring(nc, ph, "TTk", 4, HS, BF16); A0r = Ring(nc, ph, "A0r", 2, HS, BF16); B0r = Ring(nc, ph, "B0r", 2, HS, BF16)
            Of = Ring(nc, ph, "Of", 8, HS, BF16); P1r = Ring(nc, ph, "P1r", 4, HS, BF16)
            qkT = Ring(nc, ph, "qkT", 2, HS, BF16); KG = Ring(nc, ph, "KG", 2, HS, BF16); Kd = Ring(nc, ph, "Kd", 2, HS, BF16)
            up = Ring(nc, ph, "up", 2, HS, F32); wT = Ring(nc, ph, "wT", 2, HS, BF16); vn = Ring(nc, ph, "vn", 2, HS, BF16)
            ob = Ring(nc, ph, "ob", 2, HS, F32)
            Sst = [sb("S%d" % d, HS, F32, ph) for d in range(2)]
            Sbf = [sb("Sb%d" % d, HS, BF16, ph) for d in range(2)]
            P.dma('sp', bmask[:], blockmask, r=["d_bm"], w=["bmask"])
            for d in range(2):
                P.op('pool', (lambda d=d: nc.gpsimd.memset(Sst[d][:], 0.0)), r=[], w=[("S", d)])
                P.op('pool', (lambda d=d: nc.gpsimd.memset(Sbf[d][:], 0.0)), r=[], w=[("Sb", d)])
            for t_ in scr.tiles:
                P.op('pool', (lambda t_=t_: nc.gpsimd.memset(t_[:], 1.0)), r=[], w=[])
            P.flush()
            P.barrier()
            ppc = [0]

            def pair():
                p = ppc[0] % 4
                ppc[0] += 1
                v = psum[:, p * 1024:(p + 1) * 1024]
                return p, v, v.rearrange("p (h k) -> p h k", h=8), [PK[2 * p], PK[2 * p + 1]]

            def bc_mid(ap2d, n=128):
                return ap2d.unsqueeze(1).to_broadcast([ap2d.shape[0], 8, ap2d.shape[1]])

            def bc_last(ap2d):
                return ap2d.unsqueeze(2).to_broadcast([ap2d.shape[0], 8, 128])

            def dn_step(d, gt, lt, need_o):
                V = nc.vector; G = nc.gpsimd; A = nc.scalar
                kt, ktk = lKT.next(); kk, kkk = lKK.next(); vv, vvk = lVV.next(); gb, gbk = gbl.next()
                P.dma('sp', kt[:], KT_d[gt], r=["d_kst"], w=[ktk])
                P.dma('sp', kk[:], KK_d[gt], r=["d_ktst"], w=[kkk])
                P.dma('sp', vv[:], VV_d[gt], r=["d_vtst"], w=[vvk])
                P.dma('sp', gb[:], GB_d[gt], r=["d_GB"], w=[gbk])
                if need_o:
                    qt, qtk = lQT.next()
                    P.dma('sp', qt[:], QT_d[gt], r=["d_qst"], w=[qtk])
                g = gb[:, 16 * d:16 * d + 8]; beta = gb[:, 16 * d + 8:16 * d + 16]
                sc, sk = scr.next()
                p, pv, pv3, pk = pair()
                P.op('pe', lambda: nc.tensor.matmul(pv[:, 0:8], msk[:, 1 + d, :], g, start=True, stop=True), r=[gbk, "msk"], w=pk)
                P.op('pe', lambda: nc.tensor.matmul(pv[:, 8:16], msk[:, 3, :], g, start=True, stop=True), r=[gbk, "msk"], w=pk)
                P.op('dve', lambda: V.tensor_copy(out=sc[:, 0:16], in_=pv[:, 0:16]), r=pk, w=[sk])
                yield
                P.op('act', lambda: A.activation(out=sc[:, 16:24], in_=sc[:, 0:8], func=AF.Exp), r=[sk], w=[sk])
                P.op('dve', lambda: V.tensor_tensor(out=sc[:, 24:32], in0=sc[:, 8:16], in1=sc[:, 0:8], op=ALU.subtract), r=[sk], w=[sk])
                P.op('act', lambda: A.activation(out=sc[:, 24:32], in_=sc[:, 24:32], func=AF.Exp), r=[sk], w=[sk])
                P.op('act', lambda: A.activation(out=sc[:, 32:40], in_=sc[:, 8:16], func=AF.Exp), r=[sk], w=[sk])
                P.op('dve', lambda: V.tensor_scalar(out=sc[:, 40:48], in0=sc[:, 0:8], scalar1=-1.0, scalar2=None, op0=ALU.mult), r=[sk], w=[sk])
                P.op('dve', lambda: V.tensor_copy(out=sc[:, 56:64], in_=sc[:, 0:8]), r=[sk], w=[sk])
                P.op('act', lambda: A.activation(out=sc[:, 72:80], in_=beta, func=AF.Ln), r=[gbk], w=[sk])
                P.op('dve', lambda: V.tensor_tensor(out=sc[:, 72:80], in0=sc[:, 72:80], in1=sc[:, 40:48], op=ALU.add), r=[sk], w=[sk])
                yield
                egc = sc[:, 16:24]; edec = sc[:, 24:32]; egl = sc[:, 32:40]
                l16, lk = L16.next(); re, rek = rE.next()
                p, pv, pv3, pk = pair()
                for q in range(4):
                    P.op('pe', (lambda q=q: nc.tensor.transpose(pv[0:16, q * 128:(q + 1) * 128], sc[:, 40 + 8 * q:56 + 8 * q], ident)),
                         r=[sk, "msk"], w=pk)
                yield
                P.op('act', lambda: A.copy(out=l16[:], in_=pv[0:16, 0:512].rearrange("p (q k) -> p q k", q=4)), r=pk, w=[lk])
                P.op('pool', lambda: G.tensor_tensor(out=re[:, 0, :, :], in0=bc_mid(l16[:, 1, :]), in1=bmask[:], op=ALU.mult), r=[lk, "bmask"], w=[(rek, 0)])
                P.op('pool', lambda: G.tensor_tensor(out=re[:, 1, :, :], in0=bc_mid(l16[:, 3, :]), in1=bmask[:], op=ALU.mult), r=[lk, "bmask"], w=[(rek, 1)])
                yield
                ei, eik = Ei.next(); es_, esk = Es.next()
                for which, (lq, dst, dk_, mi) in enumerate(((0, ei, eik, 4 + d), (2, es_, esk, 8 + d))):
                    yield
                    p, pv, pv3, pk = pair()
                    for half in range(2):
                        P.op('pe', (lambda half=half, which=which, lq=lq, pv=pv: nc.tensor.matmul(
                            pv[:, half * 512:(half + 1) * 512], l16[:, lq, :],
                            re[:, which, 4 * half:4 * half + 4, :].rearrange("p h k -> p (h k)"), start=True, stop=True)),
                            r=[lk, (rek, which)], w=pk)
                    P.op('dve', (lambda pv3=pv3, dst=dst, mi=mi: V.scalar_tensor_tensor(
                        out=dst[:], in0=pv3, scalar=0.0, in1=bc_mid(msk[:, mi, :]), op0=ALU.min, op1=ALU.add)), r=pk + ["msk"], w=[dk_])
                    P.op('act', (lambda dst=dst: A.activation(out=dst[:], in_=dst[:], func=AF.Exp)), r=[dk_], w=[dk_])
                yield
                sbm, sbk = SBm.next()
                P.op('pool', lambda: G.tensor_tensor(out=sbm[:], in0=bc_mid(msk[:, 6 + d, :]), in1=bc_last(beta), op=ALU.mult), r=["msk", gbk], w=[sbk])
                p, pv, pv3, pk = pair()
                for h in range(8):
                    P.op('pe', (lambda h=h, pv=pv: nc.tensor.matmul(pv[:, h * 128:(h + 1) * 128], kt[:, h, :], kt[:, h, :], start=True, stop=True)),
                         r=[ktk], w=pk)
                yield
                t1, t1k = tf.next()
                a0, a0k = A0r.next(); b0, b0k = B0r.next()
                P.op('dve', (lambda pv3=pv3: V.tensor_tensor(out=t1[:], in0=pv3, in1=ei[:], op=ALU.mult)), r=pk + [eik], w=[t1k])
                P.op('pool', lambda: G.tensor_tensor(out=a0[:], in0=t1[:], in1=sbm[:], op=ALU.mult), r=[t1k, sbk], w=[a0k])
                P.op('dve', (lambda pv3=pv3: V.tensor_tensor(out=b0[:], in0=pv3, in1=es_[:], op=ALU.mult)), r=pk + [esk], w=[b0k])
                if need_o:
                    qk_, qkk = qkT.next()
                    p, pv, pv3, pk = pair()
                    for h in range(8):
                        P.op('pe', (lambda h=h, pv=pv: nc.tensor.matmul(pv[:, h * 128:(h + 1) * 128], kt[:, h, :], qt[:, h, :], start=True, stop=True)),
                             r=[ktk, qtk], w=pk)
                    P.op('dve', (lambda pv3=pv3: V.tensor_tensor(out=qk_[:], in0=pv3, in1=ei[:], op=ALU.mult)), r=pk + [eik], w=[qkk])
                yield
                def mm8(lhs, rhs, keys):
                    p, pv, pv3, pk = pair()
                    for h in range(8):
                        P.op('pe', (lambda h=h, pv=pv: nc.tensor.matmul(pv[:, h * 128:(h + 1) * 128], lhs[:, h, :], rhs[:, h, :], start=True, stop=True)),
                             r=keys, w=pk)
                    return pv3, pk
                ca, cak = Ak.next(); cb, cbk = Bk.next(); cT, cTk = Tk.next(); cTT, cTTk = TTk.next()
                P.op('pool', lambda ca=ca: G.tensor_tensor(out=ca[:], in0=a0[:], in1=bc_mid(msk[:, 10, :]), op=ALU.mult), r=[a0k, "msk"], w=[cak])
                P.op('pool', lambda cb=cb: G.tensor_tensor(out=cb[:], in0=b0[:], in1=bc_mid(msk[:, 10, :]), op=ALU.mult), r=[b0k, "msk"], w=[cbk])
                P.op('pool', lambda ca=ca, cT=cT: G.tensor_tensor(out=cT[:], in0=bc_mid(identb[:]), in1=ca[:], op=ALU.subtract), r=["identb", cak], w=[cTk])
                P.op('pool', lambda cb=cb, cTT=cTT: G.tensor_tensor(out=cTT[:], in0=bc_mid(identb[:]), in1=cb[:], op=ALU.subtract), r=["identb", cbk], w=[cTTk])
                yield
                offs = []
                for mi in (11, 12):
                    of_, ofk = Of.next(); oft, oftk = Of.next()
                    P.op('pool', (lambda of_=of_, mi=mi: G.tensor_tensor(out=of_[:], in0=a0[:], in1=bc_mid(msk[:, mi, :]), op=ALU.mult)), r=[a0k, "msk"], w=[ofk])
                    P.op('pool', (lambda oft=oft, mi=mi: G.tensor_tensor(out=oft[:], in0=b0[:], in1=bc_mid(msk[:, mi, :]), op=ALU.mult)), r=[b0k, "msk"], w=[oftk])
                    offs.append((of_, ofk, oft, oftk))
                for lev in range(4):
                    yield
                    na, nak = Ak.next(); nb, nbk = Bk.next(); nT, nTk = Tk.next(); nTT, nTTk = TTk.next()
                    pv3, pk = mm8(cb, ca, [cak, cbk])
                    P.op('act', (lambda pv3=pv3, na=na: A.copy(out=na[:], in_=pv3)), r=pk, w=[nak])
                    pv3, pk = mm8(ca, cb, [cak, cbk])
                    P.op('dve', (lambda pv3=pv3, nb=nb: V.tensor_copy(out=nb[:], in_=pv3)), r=pk, w=[nbk])
                    yield
                    pv3, pk = mm8(nb, cT, [nbk, cTk])
                    P.op('dve', (lambda pv3=pv3, nT=nT, cT=cT: V.tensor_tensor(out=nT[:], in0=pv3, in1=cT[:], op=ALU.add)), r=pk + [cTk], w=[nTk])
                    pv3, pk = mm8(na, cTT, [nak, cTTk])
                    P.op('dve', (lambda pv3=pv3, nTT=nTT, cTT=cTT: V.tensor_tensor(out=nTT[:], in0=pv3, in1=cTT[:], op=ALU.add)), r=pk + [cTTk], w=[nTTk])
                    ca, cak, cb, cbk, cT, cTk, cTT, cTTk = na, nak, nb, nbk, nT, nTk, nTT, nTTk
                for si, (of_, ofk, oft, oftk) in enumerate(offs):
                    yield
                    p1, p1k = P1r.next()
                    pv3, pk = mm8(oft, cT, [oftk, cTk])
                    P.op('act', (lambda pv3=pv3, p1=p1: A.copy(out=p1[:], in_=pv3)), r=pk, w=[p1k])
                    yield
                    pv3, pk = mm8(cTT, p1, [cTTk, p1k])
                    nT, nTk = Tk.next()
                    P.op('dve', (lambda pv3=pv3, nT=nT, cT=cT: V.tensor_tensor(out=nT[:], in0=cT[:], in1=pv3, op=ALU.subtract)), r=pk + [cTk], w=[nTk])
                    if si == 0:
                        p1t, p1tk = P1r.next()
                        pv3, pk = mm8(of_, cTT, [ofk, cTTk])
                        P.op('act', (lambda pv3=pv3, p1t=p1t: A.copy(out=p1t[:], in_=pv3)), r=pk, w=[p1tk])
                        pv3, pk = mm8(cT, p1t, [cTk, p1tk])
                        nTT, nTTk = TTk.next()
                        P.op('dve', (lambda pv3=pv3, nTT=nTT, cTT=cTT: V.tensor_tensor(out=nTT[:], in0=cTT[:], in1=pv3, op=ALU.subtract)), r=pk + [cTTk], w=[nTTk])
                        cTT, cTTk = nTT, nTTk
                    cT, cTk = nT, nTk
                yield
                u_, uk = up.next(); w_, wk_ = wT.next(); kg, kgk = KG.next(); kd, kdk = Kd.next()
                P.op('pool', lambda: G.tensor_tensor(out=kg[:], in0=kk[:], in1=bc_last(egc), op=ALU.mult), r=[kkk, sk], w=[kgk])
                P.op('pool', lambda: G.tensor_tensor(out=kd[:], in0=kk[:], in1=bc_last(edec), op=ALU.mult), r=[kkk, sk], w=[kdk])
                p, pv, pv3, pk = pair()
                for h in range(8):
                    P.op('pe', (lambda h=h, pv=pv: nc.tensor.matmul(pv[:, h * 128:(h + 1) * 128], cT[:, h, :], vv[:, h, :], start=True, stop=True)),
                         r=[cTk, vvk], w=pk)
                P.op('act', (lambda pv3=pv3: A.copy(out=u_[:], in_=pv3)), r=pk, w=[uk])
                p, pv, pv3, pk = pair()
                for h in range(8):
                    P.op('pe', (lambda h=h, pv=pv: nc.tensor.matmul(pv[:, h * 128:(h + 1) * 128], kg[:, h, :], cT[:, h, :], start=True, stop=True)),
                         r=[cTk, kgk], w=pk)
                P.op('act', (lambda pv3=pv3: A.copy(out=w_[:], in_=pv3)), r=pk, w=[wk_])
                yield
                S = Sst[d]; Sb = Sbf[d]; Sk = ("S", d); Sbk = ("Sb", d)
                vn_, vnk = vn.next(); t2, t2k = tf.next()
                p, pv, pv3, pk = pair()
                for h in range(8):
                    P.op('pe', (lambda h=h, pv=pv: nc.tensor.matmul(pv[:, h * 128:(h + 1) * 128], w_[:, h, :], Sb[:, h, :], start=True, stop=True)),
                         r=[wk_, Sbk], w=pk)
                yield
                P.op('dve', (lambda pv3=pv3: V.tensor_tensor(out=t2[:], in0=u_[:], in1=pv3, op=ALU.subtract)), r=pk + [uk], w=[t2k])
                P.op('pool', lambda: G.tensor_tensor(out=vn_[:], in0=t2[:], in1=bc_last(beta), op=ALU.mult), r=[t2k, gbk], w=[vnk])
                if need_o:
                    o_, ok_ = ob.next()
                    p, pv, pv3, pk = pair()
                    for h in range(8):
                        P.op('pe', (lambda h=h, pv=pv: nc.tensor.matmul(pv[:, h * 128:(h + 1) * 128], qt[:, h, :], Sb[:, h, :], start=True, stop=True)),
                             r=[qtk, Sbk], w=pk)
                    P.op('dve', (lambda pv3=pv3: V.tensor_tensor(out=o_[:], in0=pv3, in1=bc_last(egc), op=ALU.mult)), r=pk + [sk], w=[ok_])
                    p, pv, pv3, pk = pair()
                    for h in range(8):
                        P.op('pe', (lambda h=h, pv=pv: nc.tensor.matmul(pv[:, h * 128:(h + 1) * 128], qk_[:, h, :], vn_[:, h, :], start=True, stop=True)),
                             r=[qkk, vnk], w=pk)
                    P.op('dve', (lambda pv3=pv3: V.tensor_tensor(out=o_[:], in0=o_[:], in1=pv3, op=ALU.add)), r=pk + [ok_], w=[ok_])
                    P.dma('sp', OF_d[d, lt], o_[:].rearrange("p h k -> p (h k)"), r=[ok_], w=["d_OF"])
                yield
                p, pv, pv3, pk = pair()
                for h in range(8):
                    P.op('pe', (lambda h=h, pv=pv: nc.tensor.matmul(pv[:, h * 128:(h + 1) * 128], kd[:, h, :], vn_[:, h, :], start=True, stop=True)),
                         r=[kdk, vnk], w=pk)
                P.op('dve', lambda: V.tensor_tensor(out=S[:], in0=S[:], in1=bc_last(egl), op=ALU.mult), r=[Sk, sk], w=[Sk])
                P.op('dve', (lambda pv3=pv3: V.tensor_tensor(out=S[:], in0=S[:], in1=pv3, op=ALU.add)), r=pk + [Sk], w=[Sk])
                P.op('act', lambda: A.copy(out=Sb[:], in_=S[:]), r=[Sk], w=[Sbk])
                import os
                if os.environ.get("DN_DBG") and d == int(os.environ["DN_DBG"]) and gt == int(os.environ.get("DN_DBG_GT", "0")):
                    dump("dbg_sc", sc[:], [sk]); dump("dbg_Ei", ei[:], [eik]); dump("dbg_Es", es_[:], [esk])
                    dump("dbg_a0", a0[:], [a0k]); dump("dbg_b0", b0[:], [b0k]); dump("dbg_T", cT[:], [cTk])
                    dump("dbg_up", u_[:], [uk]); dump("dbg_wT", w_[:], [wk_]); dump("dbg_vn", vn_[:], [vnk]); dump("dbg_S", S[:], [Sk])
                    dump("dbg_l16", l16[:], [lk])
                    if need_o:
                        dump("dbg_qk", qk_[:], [qkk]); dump("dbg_o", o_[:], [ok_])

            order_f = [(t, t, False) for t in range(NCT)] + [(NCT + t, t, True) for t in range(NLT)]
            order_b = [(t, t, False) for t in reversed(range(NCT))] + [(NCT + t, t, True) for t in reversed(range(NLT))]
            import os
            nstep = int(os.environ.get("DN_STEPS", str(NT)))
            for i in range(nstep):
                g0 = dn_step(0, *order_f[i])
                g1 = dn_step(1, *order_b[i])
                run_interleaved(P, [g0, g1], 2)
            P.barrier()
        if stop_after == "c2":
            P.finish()
            return nc

        with contextlib.ExitStack() as ph:
            R = mk_rings(ph)
            load_mods(ph, [("L", 2)])
            of0 = Ring(nc, ph, "of0", 2, [128, D], F32); of1 = Ring(nc, ph, "of1", 2, [128, D], F32)
            sqt = Ring(nc, ph, "sqt", 1, [128, D], F32)
            zl = Ring(nc, ph, "zl", 2, [128, D], BF16)
            ms = Ring(nc, ph, "ms", 2, [128, 8], F32)
            onT = Ring(nc, ph, "onT", 2, [128, 8, 128], BF16)
            yb = Ring(nc, ph, "yb", 2, [128, D], F32)
            wo = sb("wo", [128, 8, D], BF16, ph)
            dnw = sb("dnw", [128, 128], F32, ph)
            P.dma('pool', wo[:], w_dn_out.rearrange("(k p) n -> p k n", p=128), r=["d_wo"], w=["wo"])
            P.dma('sp', dnw[:], dn_norm.partition_broadcast(128), r=["d_dnn"], w=["dnw"])

            def c3_tile(lt):
                gt = NCT + lt
                V = nc.vector; G = nc.gpsimd; A = nc.scalar
                a0_, a0k = of0.next(); a1_, a1k = of1.next(); z_, zk = zl.next(); m_, mk_ = ms.next(); sq, sqk = sqt.next()
                P.dma('sp', a0_[:], OF_d[0, lt], r=["d_OF"], w=[a0k])
                P.dma('sp', a1_[:], OF_d[1, lt], r=["d_OF"], w=[a1k])
                P.dma('sp', z_[:], ZZ_d[gt], r=["d_ZZ"], w=[zk])
                P.op('pool', lambda: G.tensor_tensor(out=a0_[:], in0=a0_[:], in1=a1_[:], op=ALU.add), r=[a0k, a1k], w=[a0k])
                P.op('act', lambda: A.activation(out=sq[:], in_=a0_[:], func=AF.Square), r=[a0k], w=[sqk])
                P.op('dve', lambda: V.reduce_sum(out=m_[:], in_=sq[:].rearrange("p (h k) -> p h k", h=8), axis=AX.X), r=[sqk], w=[mk_])
                P.op('dve', lambda: V.tensor_scalar(out=m_[:], in0=m_[:], scalar1=1.0 / 128, scalar2=EPS, op0=ALU.mult, op1=ALU.add), r=[mk_], w=[mk_])
                P.op('act', lambda: A.activation(out=m_[:], in_=m_[:], func=AF.Sqrt), r=[mk_], w=[mk_])
                P.op('dve', lambda: V.reciprocal(out=m_[:], in_=m_[:]), r=[mk_], w=[mk_])
                o3 = a0_[:].rearrange("p (h k) -> p h k", h=8)
                P.op('dve', lambda: V.tensor_tensor(out=o3, in0=o3, in1=m_[:].unsqueeze(2).to_broadcast([128, 8, 128]), op=ALU.mult), r=[a0k, mk_], w=[a0k])
                P.op('pool', lambda: G.tensor_tensor(out=o3, in0=o3, in1=dnw[:].unsqueeze(1).to_broadcast([128, 8, 128]), op=ALU.mult), r=[a0k, "dnw"], w=[a0k])
                P.op('pool', lambda: G.tensor_tensor(out=a0_[:], in0=a0_[:], in1=z_[:], op=ALU.mult), r=[a0k, zk], w=[a0k])
                for k in range(8):
                    b = 6 + k // 4
                    P.op('pe', (lambda b=b, k=k: nc.tensor.transpose(PS[b][:, (k % 4) * 128:(k % 4) * 128 + 128], a0_[:, k * 128:(k + 1) * 128], ident)),
                         r=[a0k, "msk"], w=[PK[b]])
                t_, tk = onT.next()
                for h in range(2):
                    b = 6 + h
                    P.op('act', (lambda b=b, h=h: A.copy(out=t_[:, 4 * h:4 * h + 4, :], in_=PS[b].rearrange("p (k t) -> p k t", k=4))),
                         r=[PK[b]], w=[(tk, h)])
                y_, yk = yb.next()
                xt, xk = R['xs'].next()
                P.dma('sp', xt[:], XS2_t[gt], r=["d_dst0"], w=[xk])
                for half in range(2):
                    b = 4 + half
                    for k in range(8):
                        P.op('pe', (lambda b=b, k=k, half=half: nc.tensor.matmul(PS[b], t_[:, k, :], wo[:, k, half * 512:(half + 1) * 512],
                                                                             start=(k == 0), stop=(k == 7))), r=[(tk, 0), (tk, 1), "wo"], w=[PK[b]])
                    sl = slice(half * 512, (half + 1) * 512)
                    P.op('dve', (lambda b=b, sl=sl: V.tensor_tensor(out=y_[:, sl], in0=PS[b], in1=mv("L", 2)[:, sl], op=ALU.mult)),
                         r=[PK[b], "mvec"], w=[(yk, half)])
                    P.op('pool', (lambda sl=sl: G.tensor_tensor(out=y_[:, sl], in0=y_[:, sl], in1=xt[:, sl], op=ALU.add)), r=[(yk, half), xk], w=[(yk, half)])
                P.dma('sp', XS3_t[gt], y_[:], r=[(yk, 0), (yk, 1)], w=["d_XS3"])

            for lt in range(NLT):
                c3_tile(lt)
            P.barrier()
        if stop_after == "c3":
            P.finish()
            return nc
        MOE(1, XS3_t, out_t, list(range(NCT, NT)), True)
        P.finish()
    return nc


_CACHE = {}


def _core_inputs(b, inp, consts):
    m = {}
    m['xin'] = np.ascontiguousarray(np.concatenate([inp['ctx'][b], inp['x'][b]], 0), dtype=np.float32)
    cc = np.stack([inp['c'][b], inp['c_ctx']], 0)
    m['ccol'] = np.ascontiguousarray(cc.reshape(2, 8, 128).transpose(2, 0, 1), dtype=np.float32)
    for k in ('w_ada', 'b_ada', 'norm_mix', 'norm_ffn', 'w_e_gate', 'w_e_up', 'w_e_down', 'norm_final'):
        m[k] = np.ascontiguousarray(inp[k], dtype=np.float32)
    m['w_pool'] = np.ascontiguousarray(inp['w_pool'][0]); m['b_pool'] = np.ascontiguousarray(inp['b_pool'][0])
    m['pool_scale'] = np.ascontiguousarray(inp['pool_scale'][0])
    m['w_dn_in'] = np.ascontiguousarray(inp['w_dn_in'][0])
    m['wconv'] = np.ascontiguousarray(inp['w_dn_conv'][0].reshape(4, 24, 128).transpose(2, 1, 0))
    m['dn_a_log'] = np.ascontiguousarray(inp['dn_a_log'][0].reshape(16))
    m['dn_dt_bias'] = np.ascontiguousarray(inp['dn_dt_bias'][0].reshape(16))
    m['dn_norm'] = np.ascontiguousarray(inp['dn_norm'][0]); m['w_dn_out'] = np.ascontiguousarray(inp['w_dn_out'][0])
    m['w_r'] = np.ascontiguousarray(np.concatenate([inp['w_rg'], inp['w_re']], -1))
    m['b_r'] = np.ascontiguousarray(np.concatenate([inp['b_rg'], inp['b_re']], -1))
    m.update(consts)
    return m


def kernel(**inputs):
    inp = {k: np.asarray(v) for k, v in inputs.items()}
    if 'nc' not in _CACHE:
        _CACHE['nc'] = build()
        _CACHE['consts'] = host_constants()
    nc = _CACHE['nc']
    consts = _CACHE['consts']
    B = inp['x'].shape[0]
    in_maps = [_core_inputs(b, inp, consts) for b in range(B)]
    res = run_bass_kernel_spmd(nc, in_maps, core_ids=list(range(B)))
    out = np.stack([np.asarray(res.results[b]['out']).reshape(NLT * 128, D) for b in range(B)], 0)
    return out.astype(inp['x'].dtype)
```

```python
import os
from concourse.bass_utils import run_bass_kernel_spmd
import numpy as np, contextlib
import concourse.bass as bass
import concourse.mybir as mybir

F32 = mybir.dt.float32
BF16 = mybir.dt.bfloat16
AF = mybir.ActivationFunctionType
ALU = mybir.AluOpType
AX = mybir.AxisListType


class Prog:
    NS = 16

    def __init__(self, nc, es):
        self.nc = nc
        self.E = {'pe': nc.tensor, 'act': nc.scalar, 'dve': nc.vector, 'pool': nc.gpsimd, 'sp': nc.sync}
        self.sem = {e: es.enter_context(nc.semaphore("s_" + e)) for e in ('pe', 'act', 'dve', 'pool')}
        self.dsem = {q: [es.enter_context(nc.semaphore("d_%s%d" % (q, i))) for i in range(self.NS)]
                     for q in ('sp', 'act', 'pool')}
        self.sigcount = {e: 0 for e in self.sem}
        self.dmacount = {q: 0 for q in self.dsem}
        self.waited = {}
        self.ops = []
        self.last_w = {}
        self.readers = {}
        self.n_inst = 0

    def op(self, eng, fn, r=(), w=(), dma=False):
        self.ops.append((eng, fn, tuple(r), tuple(w), dma))

    def dma(self, q, out, in_, r, w, **kw):
        e = self.E[q]
        self.op(q, lambda: e.dma_start(out=out, in_=in_, **kw), r, w, dma=True)

    @staticmethod
    def _needs_wait(oj_eng, oj_dma, oi_eng, oi_dma, typ):
        if oj_dma:
            return True
        if oj_eng == oi_eng and not oi_dma:
            if oi_eng == 'pe':
                return False
            return typ == 'raw'
        return True

    def flush(self):
        ops = self.ops
        n = len(ops)
        deps = [None] * n
        last_w, readers = self.last_w, self.readers
        for i, (eng, fn, r, w, dma) in enumerate(ops):
            d = {}
            for k in r:
                t = last_w.get(k)
                if t is not None:
                    d[t] = 'raw'
                if isinstance(k, tuple) and k and k[0] == 'ps':
                    for e2, t in readers.get(k, {}).items():
                        if e2 != eng and t not in d:
                            d[t] = 'raw'
            for k in w:
                t = last_w.get(k)
                if t is not None and t not in d:
                    d[t] = 'waw'
                for t in readers.get(k, {}).values():
                    if t not in d:
                        d[t] = 'war'
            d.pop(('p', i), None)
            deps[i] = d
            me = ('p', i)
            for k in r:
                rk = readers.setdefault(k, {})
                if dma:
                    rk[('d', i)] = me
                else:
                    rk[eng] = me
            for k in w:
                last_w[k] = me
                readers[k] = {}
        need_sig = [False] * n
        for i, (eng, fn, r, w, dma) in enumerate(ops):
            for t, typ in deps[i].items():
                if t[0] == 'p':
                    j = t[1]
                    ej, _, _, _, dj = ops[j]
                    if not dj and self._needs_wait(ej, dj, eng, dma, typ):
                        need_sig[j] = True
        last_of = {}
        for i, (eng, fn, r, w, dma) in enumerate(ops):
            if not dma:
                last_of[eng] = i
        for e, i in last_of.items():
            need_sig[i] = True
        resolved = [None] * n
        for i, (eng, fn, r, w, dma) in enumerate(ops):
            E = self.E[eng]
            waits = {}
            for t, typ in deps[i].items():
                if t[0] == 'p':
                    t2 = resolved[t[1]]
                    ej, dj = ops[t[1]][0], ops[t[1]][4]
                else:
                    t2 = t
                    ej, dj = t[1], t[0] == 'd'
                if not self._needs_wait(ej, dj, eng, dma, typ):
                    continue
                if t2[0] == 'c':
                    key = ('c', t2[1]); val = t2[2]
                else:
                    key = ('d', t2[1], t2[2]); val = t2[3]
                if waits.get(key, 0) < val:
                    waits[key] = val
            if dma:
                k = self.dmacount[eng]
                slot = k % self.NS
                if k >= self.NS:
                    key = ('d', eng, slot); val = 16 * (k // self.NS)
                    if waits.get(key, 0) < val:
                        waits[key] = val
            for key, val in waits.items():
                wk = (eng, key)
                if self.waited.get(wk, 0) >= val:
                    continue
                self.waited[wk] = val
                s = self.sem[key[1]] if key[0] == 'c' else self.dsem[key[1]][key[2]]
                E.wait_ge(s, val)
                self.n_inst += 1
            inst = fn()
            self.n_inst += 1
            if dma:
                k = self.dmacount[eng]
                slot = k % self.NS
                val = 16 * (k // self.NS + 1)
                inst.then_inc(self.dsem[eng][slot], 16)
                self.dmacount[eng] = k + 1
                resolved[i] = ('d', eng, slot, val)
            else:
                if need_sig[i]:
                    self.sigcount[eng] += 1
                    inst.then_inc(self.sem[eng], 1)
                    resolved[i] = ('c', eng, self.sigcount[eng])
        nxt = {}
        for i in range(n - 1, -1, -1):
            eng, dma = ops[i][0], ops[i][4]
            if dma:
                continue
            if resolved[i] is not None:
                nxt[eng] = resolved[i]
            else:
                resolved[i] = nxt[eng]
        for k in list(last_w.keys()):
            t = last_w[k]
            if t[0] == 'p':
                last_w[k] = resolved[t[1]]
        for k in list(readers.keys()):
            rk = readers[k]
            for kk in list(rk.keys()):
                t = rk[kk]
                if t[0] == 'p':
                    rk[kk] = resolved[t[1]]
        self.ops = []

    def barrier(self):
        self.flush()
        for eng in ('pe', 'act', 'dve', 'pool', 'sp'):
            E = self.E[eng]
            for e, c in self.sigcount.items():
                if c > 0 and e != eng and self.waited.get((eng, ('c', e)), 0) < c:
                    E.wait_ge(self.sem[e], c)
                    self.waited[(eng, ('c', e))] = c
            for q, k in self.dmacount.items():
                for slot in range(self.NS):
                    if k > slot:
                        val = 16 * ((k - 1 - slot) // self.NS + 1)
                        if self.waited.get((eng, ('d', q, slot)), 0) < val:
                            E.wait_ge(self.dsem[q][slot], val)
                            self.waited[(eng, ('d', q, slot))] = val

    def finish(self):
        self.flush()
        sp = self.E['sp']
        for e, c in self.sigcount.items():
            if c > 0:
                sp.wait_ge(self.sem[e], c)
        for q, k in self.dmacount.items():
            for slot in range(self.NS):
                if k > slot:
                    cnt = (k - 1 - slot) // self.NS + 1
                    sp.wait_ge(self.dsem[q][slot], 16 * cnt)


D = 1024
NCT = 2
NLT = 64
NT = NCT + NLT
EPS = 1e-6
POOL_WINDOWS = (2, 4, 8, 16)
JREL = {0: list(range(-1, 4)), 1: list(range(-1, 5)), 2: list(range(-2, 6)), 3: list(range(-4, 8))}
BAND_IDX = {}
_i = 0
for _g in range(4):
    for _j in JREL[_g]:
        BAND_IDX[(_g, _j)] = _i
        _i += 1
NBAND = _i


def host_constants():
    c = {}
    def axis_w(n, win):
        t = np.arange(n)
        lo = np.maximum(t - win // 2, 0); hi = np.minimum(t + win // 2, n)
        W = np.zeros((n, n), np.float64)
        for o in range(n):
            W[lo[o]:hi[o], o] = 1.0 / (hi[o] - lo[o])
        return W
    band = np.zeros((3, 128, NBAND, 512), np.float32)
    for g, win in enumerate(POOL_WINDOWS):
        Wr = axis_w(128, win); Wc = axis_w(64, win)
        for ti, b in enumerate((0, 7, 15)):
            for j in JREL[g]:
                jt = 4 * b + j
                if jt < 0 or jt >= 64:
                    continue
                M = np.einsum('ab,cd->acbd', Wr[2 * jt:2 * jt + 2, 8 * b:8 * b + 8], Wc).reshape(128, 512)
                if 0 <= j < 4:
                    M[:, j * 128:(j + 1) * 128] -= np.eye(128)
                band[ti, :, BAND_IDX[(g, j)], :] = M
    c['band'] = band
    cb = np.zeros((128, 8, 256), np.float32)
    for g, win in enumerate(POOL_WINDOWS):
        W = axis_w(256, win) - np.eye(256)
        for j in range(2):
            cb[:, g * 2 + j, :] = W[j * 128:(j + 1) * 128, :]
    c['cband'] = cb
    idx = np.arange(128)
    m = np.zeros((128, 13, 128), np.float32)
    m[:, 0] = np.eye(128)
    m[:, 1] = (idx[:, None] <= idx[None, :])
    m[:, 2] = (idx[:, None] >= idx[None, :])
    m[:, 3] = 1.0
    m[:, 4] = np.where(idx[:, None] <= idx[None, :], 0.0, -30000.0)
    m[:, 5] = np.where(idx[:, None] >= idx[None, :], 0.0, -30000.0)
    m[:, 6] = (idx[:, None] < idx[None, :])
    m[:, 7] = (idx[:, None] > idx[None, :])
    m[:, 8] = np.where(idx[:, None] > idx[None, :], 0.0, -30000.0)
    m[:, 9] = np.where(idx[:, None] < idx[None, :], 0.0, -30000.0)
    bd32 = (idx[:, None] // 32 == idx[None, :] // 32); bd64 = (idx[:, None] // 64 == idx[None, :] // 64)
    m[:, 10] = bd32; m[:, 11] = bd64 & ~bd32; m[:, 12] = ~bd64
    c['masks'] = m
    bm = np.zeros((16, 8, 128), np.float32)
    for r in range(16):
        bm[r, r % 8, :] = 1.0
    c['blockmask'] = bm
    cm = np.zeros((128, 160), np.float32)
    cm[:, 0:32] = np.arange(32)[None, :]
    cm[:, 32:40] = np.arange(8)[None, :] * 128 + np.arange(128)[:, None]
    cm[:, 40:120] = np.arange(80)[None, :] * 512
    c['cmisc'] = cm
    return c


_UNIQ = [0]


def run_interleaved(P, gens, width):
    active = []
    it = iter(gens)
    while True:
        while len(active) < width:
            try:
                active.append(next(it))
            except StopIteration:
                break
        if not active:
            break
        for g in list(active):
            try:
                next(g)
            except StopIteration:
                active.remove(g)
        P.flush()


class Ring:
    def __init__(self, nc, es, name, n, shape, dtype):
        _UNIQ[0] += 1
        self.tiles = [es.enter_context(nc.sbuf_tensor("%s%d_u%d" % (name, i, _UNIQ[0]), shape, dtype)) for i in range(n)]
        self.name = name
        self.i = 0

    def next(self):
        k = self.i % len(self.tiles)
        self.i += 1
        return self.tiles[k], "%s%d" % (self.name, k)


def build(stop_after=None, debug=False):
    nc = bass.Bass("TRN2", target_bir_lowering=False)
    es = contextlib.ExitStack()

    def din(name, shape, dt=F32):
        return nc.dram_tensor(name, list(shape), dt, kind="ExternalInput").ap()

    def dscr(name, shape, dt=F32):
        kind = "ExternalOutput" if debug else "Internal"
        return nc.dram_tensor(name, list(shape), dt, kind=kind).ap()

    xin = din("xin", [NT * 128, D])
    ccol = din("ccol", [128, 2, 8])
    w_ada = din("w_ada", [2, D, 6 * D]); b_ada = din("b_ada", [2, 6 * D])
    norm_mix = din("norm_mix", [2, D]); norm_ffn = din("norm_ffn", [2, D])
    w_pool = din("w_pool", [4, 256, 256]); b_pool = din("b_pool", [D]); pool_scale = din("pool_scale", [D])
    w_dn_in = din("w_dn_in", [D, 4128]); wconv = din("wconv", [128, 24, 4])
    dn_a_log = din("dn_a_log", [16]); dn_dt_bias = din("dn_dt_bias", [16]); dn_norm = din("dn_norm", [128])
    w_dn_out = din("w_dn_out", [D, D])
    w_r = din("w_r", [2, D, 36]); b_r = din("b_r", [2, 36])
    w_e_gate = din("w_e_gate", [2, 32, D, 512]); w_e_up = din("w_e_up", [2, 32, D, 512])
    w_e_down = din("w_e_down", [2, 32, 512, D]); norm_final = din("norm_final", [D])
    band = din("band", [3, 128, NBAND, 512]); cband = din("cband", [128, 8, 256])
    masks = din("masks", [128, 13, 128]); blockmask = din("blockmask", [16, 8, 128])
    cmisc = din("cmisc", [128, 160])
    out = nc.dram_tensor("out", [NLT * 128, D], F32, kind="ExternalOutput").ap()
    XS1 = dscr("XS1", [NT * 128, D])
    XS2 = dscr("XS2", [NT * 128, D])
    XS3 = dscr("XS3", [NT * 128, D])

    with es:
        P = Prog(nc, es)
        psum = es.enter_context(nc.psum_tensor("psum", [128, 4096], F32))
        PS = [psum[:, b * 512:(b + 1) * 512] for b in range(8)]
        PK = [("ps", b) for b in range(8)]

        def sb(name, shape, dt=F32, stack=es):
            _UNIQ[0] += 1
            return stack.enter_context(nc.sbuf_tensor("%s_u%d" % (name, _UNIQ[0]), list(shape), dt))

        msk = sb("msk", [128, 13, 128])
        P.dma('sp', msk[:], masks, r=["d_masks"], w=["msk"])
        ident = msk[:, 0, :]
        identb = sb("identb", [128, 128], BF16)
        P.op('dve', lambda: nc.vector.tensor_copy(out=identb[:], in_=msk[:, 0, :]), r=["msk"], w=["identb"])
        MODS_d = dscr("MODS_d", [2, 128, 6 * D])
        MVEC = {}

        def load_mods(ph, need):
            buf = sb("mvec", [128, len(need), D], F32, ph)
            MVEC.clear()
            for j, (st, ix) in enumerate(need):
                P.dma('sp', buf[:, j, :], MODS_d[0 if st == 'L' else 1][:, ix * D:(ix + 1) * D], r=["d_mods"], w=["mvec"])
                MVEC[(st, ix)] = buf[:, j, :]
        csb = sb("csb", [128, 2, 8]); sil = sb("sil", [128, 2, 8])
        rep = sb("rep", [128, 2, 8, 128], BF16)
        P.dma('sp', csb[:], ccol, r=["d_ccol"], w=["csb"])
        P.op('act', lambda: nc.scalar.activation(out=sil[:], in_=csb[:], func=AF.Silu), r=["csb"], w=["sil"])
        P.op('dve', lambda: nc.vector.tensor_copy(out=rep[:], in_=sil[:].unsqueeze(3).to_broadcast([128, 2, 8, 128])),
             r=["sil"], w=["rep"])

        def adaln(layer, ph):
            wr = Ring(nc, ph, "adaw", 2, [128, 8, 512], BF16)
            nw = sb("nw", [128, 2, D], F32, ph)
            modL = sb("modL", [128, 6 * D], F32, ph); modC = sb("modC", [128, 6 * D], F32, ph)
            P.dma('sp', modL[:], b_ada[layer].partition_broadcast(128), r=["d_b_ada"], w=["modL"])
            P.dma('sp', modC[:], b_ada[layer].partition_broadcast(128), r=["d_b_ada"], w=["modC"])
            P.dma('sp', nw[:, 0, :], norm_mix[layer].partition_broadcast(128), r=["d_nm"], w=["nw"])
            P.dma('sp', nw[:, 1, :], norm_ffn[layer].partition_broadcast(128), r=["d_nm"], w=["nw"])
            wv = w_ada[layer].rearrange("(k p) n -> p k n", p=128)
            for blk in range(12):
                wt, wk = wr.next()
                P.dma('pool', wt[:], wv[:, :, blk * 512:(blk + 1) * 512], r=["d_w_ada"], w=[wk])
                for s, (mod, mk) in enumerate(((modL, "modL"), (modC, "modC"))):
                    b = (blk * 2 + s) % 8
                    for k in range(8):
                        P.op('pe', (lambda b=b, s=s, k=k, wt=wt: nc.tensor.matmul(
                            PS[b], rep[:, s, k, :], wt[:, k, :], start=(k == 0), stop=(k == 7))),
                            r=["rep", wk], w=[PK[b]])
                    sl = slice(blk * 512, (blk + 1) * 512)
                    P.op('dve', (lambda b=b, mod=mod, sl=sl: nc.vector.tensor_tensor(
                        out=mod[:, sl], in0=PS[b], in1=mod[:, sl], op=ALU.add)), r=[PK[b], mk], w=[mk])
            for s, (mod, mk) in enumerate(((modL, "modL"), (modC, "modC"))):
                for j, col in enumerate((1, 4)):
                    sl = slice(col * D, (col + 1) * D)
                    P.op('dve', (lambda mod=mod, sl=sl, j=j: nc.vector.scalar_tensor_tensor(
                        out=mod[:, sl], in0=mod[:, sl], scalar=1.0, in1=nw[:, j, :], op0=ALU.add, op1=ALU.mult)),
                        r=[mk, "nw"], w=[mk])
            P.dma('sp', MODS_d[0], modL[:], r=["modL"], w=["d_mods"])
            P.dma('sp', MODS_d[1], modC[:], r=["modC"], w=["d_mods"])

        def mv(mod, i):
            return MVEC[(mod, i)]

        def rms_mod(ph_rings, xs_ap, xs_key, A_ap, sh_ap, mod_keys, out_ap, out_key, eps_scale=1.0 / D):
            ss, sk = ph_rings['ss'].next()
            tmp, tk = ph_rings['hxtmp'].next()
            P.op('act', lambda: nc.scalar.activation(out=tmp[:], in_=xs_ap, func=AF.Square), r=[xs_key], w=[tk])
            P.op('dve', lambda: nc.vector.reduce_sum(out=ss[:, 0:1], in_=tmp[:], axis=AX.X), r=[tk], w=[sk])
            P.op('dve', lambda: nc.vector.tensor_scalar(out=ss[:, 1:2], in0=ss[:, 0:1], scalar1=eps_scale, scalar2=EPS,
                                                        op0=ALU.mult, op1=ALU.add), r=[sk], w=[sk])
            P.op('act', lambda: nc.scalar.activation(out=ss[:, 2:3], in_=ss[:, 1:2], func=AF.Sqrt), r=[sk], w=[sk])
            P.op('dve', lambda: nc.vector.reciprocal(out=ss[:, 3:4], in_=ss[:, 2:3]), r=[sk], w=[sk])
            P.op('dve', lambda: nc.vector.scalar_tensor_tensor(out=tmp[:], in0=xs_ap, scalar=ss[:, 3:4], in1=A_ap,
                                                               op0=ALU.mult, op1=ALU.mult),
                 r=[xs_key, sk] + mod_keys, w=[tk])
            if sh_ap is None:
                P.op('pool', lambda: nc.gpsimd.tensor_copy(out=out_ap, in_=tmp[:]), r=[tk], w=[out_key])
            else:
                P.op('pool', lambda: nc.gpsimd.tensor_tensor(out=out_ap, in0=tmp[:], in1=sh_ap, op=ALU.add),
                     r=[tk] + mod_keys, w=[out_key])

        def mk_rings(ph):
            return {'ss': Ring(nc, ph, "ss", 4, [128, 4], F32),
                    'hxtmp': Ring(nc, ph, "hxtmp", 2, [128, D], F32),
                    'xs': Ring(nc, ph, "xsr", 2, [128, D], F32)}

        xin_t = xin.rearrange("(t p) d -> t p d", p=128)
        XS1_t = XS1.rearrange("(t p) d -> t p d", p=128)
        XS2_t = XS2.rearrange("(t p) d -> t p d", p=128)
        XS3_t = XS3.rearrange("(t p) d -> t p d", p=128)
        out_t = out.rearrange("(t p) d -> t p d", p=128)

        with contextlib.ExitStack() as ph:
            adaln(0, ph)
            P.barrier()
        def dump(name, ap, keys):
            shape = list(ap.shape)
            t = nc.dram_tensor(name, shape, ap.dtype, kind="ExternalOutput").ap()
            P.dma('sp', t, ap, r=keys, w=["dbg_" + name])
        if stop_after == "ada":
            dump("dbg_modL", modL[:], ["modL"]); dump("dbg_modC", modC[:], ["modC"]); dump("dbg_rep", rep[:], ["rep"])
            P.finish()
            return nc
        with contextlib.ExitStack() as ph:
            R = mk_rings(ph)
            hx0 = sb("hx0", [128, 24, D], BF16, ph)
            bandsb = sb("bandsb", [128, NBAND, 512], BF16, ph)
            cbandsb = sb("cbandsb", [128, 8, 256], BF16, ph)
            wpl = sb("wpl", [128, 8, 256], BF16, ph)
            vecs = sb("vecs", [128, 2, D], F32, ph)
            AB = sb("AB", [128, 4, D], F32, ph)
            dT = Ring(nc, ph, "dT", 1, [128, 8, 512], BF16)
            yt = Ring(nc, ph, "yt", 2, [128, D], F32)
            P.dma('pool', cbandsb[:], cband, r=["d_cband"], w=["cbandsb"])
            P.dma('pool', wpl[:], w_pool.rearrange("g (c p) e -> p (g c) e", p=128), r=["d_wpool"], w=["wpl"])
            P.dma('sp', vecs[:, 0, :], pool_scale.partition_broadcast(128), r=["d_ps"], w=["vecs"])
            P.dma('sp', vecs[:, 1, :], b_pool.partition_broadcast(128), r=["d_bp"], w=["vecs"])
            load_mods(ph, [(st, ix) for st in "LC" for ix in (0, 1, 2)])
            for s, (mod, mkey) in enumerate((("L", "mvec"), ("C", "mvec"))):
                P.op('dve', (lambda s=s, mod=mod: nc.vector.tensor_tensor(out=AB[:, 2 * s, :], in0=mv(mod, 2), in1=vecs[:, 0, :],
                                                                          op=ALU.mult)), r=[mkey, "vecs"], w=["AB"])
                P.op('dve', (lambda s=s: nc.vector.tensor_tensor(out=AB[:, 2 * s + 1, :], in0=AB[:, 2 * s, :], in1=vecs[:, 1, :],
                                                                 op=ALU.mult)), r=["AB", "vecs"], w=["AB"])

            def pool_segment(is_ctx, in_tiles, base, out_blocks):
                mod, mkey = ("C", "mvec") if is_ctx else ("L", "mvec")
                seq0 = 0 if is_ctx else NCT
                for lt in in_tiles:
                    xt, xk = R['xs'].next()
                    P.dma('sp', xt[:], xin_t[seq0 + lt], r=["d_xin"], w=[xk])
                    rms_mod(R, xt[:], xk, mv(mod, 1), mv(mod, 0), [mkey], hx0[:, lt - base, :], ("hx0", lt - base))
                cur_band = [None]
                for (ot0, ntl, btype) in out_blocks:
                    ncol = ntl * 128
                    if not is_ctx and cur_band[0] != btype:
                        P.dma('pool', bandsb[:], band[btype], r=["d_band"], w=["bandsb"])
                        cur_band[0] = btype
                    dt_, dk = dT.next()
                    for g in range(4):
                        for cc in range(2):
                            b = 2 * g + cc
                            if is_ctx:
                                lst = [(j, cbandsb[:, g * 2 + j, 0:ncol]) for j in range(2)]
                                bk = "cbandsb"
                            else:
                                lst = []
                                for j in JREL[g]:
                                    jt = ot0 + j
                                    if jt < 0 or jt >= NLT:
                                        continue
                                    lst.append((jt, bandsb[:, BAND_IDX[(g, j)], 0:ncol]))
                                bk = "bandsb"
                            for n_, (jt, rhs) in enumerate(lst):
                                P.op('pe', (lambda b=b, jt=jt, rhs=rhs, n_=n_, L=len(lst), ch=b, ncol=ncol: nc.tensor.matmul(
                                    PS[b][:, 0:ncol], hx0[:, jt - base, ch * 128:(ch + 1) * 128], rhs,
                                    start=(n_ == 0), stop=(n_ == L - 1))),
                                    r=[("hx0", jt - base), bk], w=[PK[b]])
                            if b % 2 == 0:
                                P.op('act', (lambda b=b, ncol=ncol, dt_=dt_: nc.scalar.copy(out=dt_[:, b, 0:ncol], in_=PS[b][:, 0:ncol])),
                                     r=[PK[b]], w=[(dk, b)])
                            else:
                                P.op('dve', (lambda b=b, ncol=ncol, dt_=dt_: nc.vector.tensor_copy(out=dt_[:, b, 0:ncol], in_=PS[b][:, 0:ncol])),
                                     r=[PK[b]], w=[(dk, b)])
                    for t in range(ntl):
                        gt = seq0 + ot0 + t
                        b0 = 2 * (t % 4)
                        for g in range(4):
                            pb = b0 + g // 2
                            for cc in range(2):
                                P.op('pe', (lambda pb=pb, g=g, cc=cc, t=t, dt_=dt_: nc.tensor.matmul(
                                    PS[pb][:, (g % 2) * 256:(g % 2) * 256 + 256], dt_[:, 2 * g + cc, t * 128:(t + 1) * 128],
                                    wpl[:, 2 * g + cc, :], start=(cc == 0), stop=(cc == 1))),
                                    r=[(dk, 2 * g + cc), "wpl"], w=[PK[pb]])
                        xt, xk = R['xs'].next()
                        P.dma('sp', xt[:], xin_t[gt], r=["d_xin"], w=[xk])
                        ai = 2 if is_ctx else 0
                        y, yk = yt.next()
                        for h in range(2):
                            sl = slice(h * 512, (h + 1) * 512)
                            P.op('dve', (lambda y=y, sl=sl, h=h, b0=b0, ai=ai: nc.vector.tensor_tensor(
                                out=y[:, sl], in0=PS[b0 + h], in1=AB[:, ai, sl], op=ALU.mult)), r=[PK[b0 + h], "AB"], w=[(yk, h)])
                            P.op('pool', (lambda y=y, sl=sl, xt=xt: nc.gpsimd.tensor_tensor(
                                out=y[:, sl], in0=y[:, sl], in1=xt[:, sl], op=ALU.add)), r=[(yk, h), xk], w=[(yk, h)])
                            P.op('pool', (lambda y=y, sl=sl, ai=ai: nc.gpsimd.tensor_tensor(
                                out=y[:, sl], in0=y[:, sl], in1=AB[:, ai + 1, sl], op=ALU.add)), r=[(yk, h), "AB"], w=[(yk, h)])
                        P.dma('sp', XS1_t[gt], y[:], r=[(yk, 0), (yk, 1)], w=["d_XS1"])

            pool_segment(True, [0, 1], 0, [(0, 2, 0)])
            if stop_after == "pool_dbg":
                dump("dbg_hx0", hx0[:, 0:2, :], [("hx0", 0), ("hx0", 1)])
                dump("dbg_dT", dT.tiles[0][:], [("dT0", b) for b in range(8)])
                dump("dbg_AB", AB[:], ["AB"])
                dump("dbg_cb", cbandsb[:], ["cbandsb"])
                dump("dbg_wpl", wpl[:], ["wpl"])
                P.finish()
                return nc
            for seg in range(4):
                lo = max(0, 16 * seg - 4); hi = min(NLT, 16 * seg + 20)
                blocks = [(4 * b, 4, 0 if b == 0 else (2 if b == 15 else 1)) for b in range(4 * seg, 4 * seg + 4)]
                pool_segment(False, list(range(lo, hi)), lo, blocks)
            P.flush()
        if stop_after == "pool":
            P.finish()
            return nc
        P.barrier()

        def moe_phase(layer, src_t, dst_t, tiles, final):
            SBT = 8 if final else 10
            with contextlib.ExitStack() as ph:
                R = mk_rings(ph)
                load_mods(ph, [(st, ix) for st in ("LC" if not final else "L") for ix in (3, 4, 5)])
                hxf = Ring(nc, ph, "hxf", 2, [128, D], F32)
                hxTf = Ring(nc, ph, "hxTf", 1, [128, 8, 128], F32)
                hxTb = sb("hxTb", [128, 8, SBT * 128], BF16, ph)
                acc = sb("acc", [128, SBT, D], F32, ph)
                gates = sb("gates", [128, SBT, 32], F32, ph)
                wrs = sb("wrs", [128, 8, 36], F32, ph)
                brb = sb("brb", [128, 36], F32, ph)
                rt = Ring(nc, ph, "rt", 2, [128, 96], F32)
                wg = Ring(nc, ph, "wg", 2, [128, 8, 512], BF16)
                wu = Ring(nc, ph, "wu", 2, [128, 8, 512], BF16)
                wd = Ring(nc, ph, "wd", 2, [128, 4, D], BF16)
                sgb = Ring(nc, ph, "sgb", 1, [128, 512], BF16)
                hidT = Ring(nc, ph, "hidT", 2, [128, 4, 512], BF16)
                nfb = None
                if final:
                    nfb = sb("nfb", [128, D], F32, ph)
                    P.dma('sp', nfb[:], norm_final.partition_broadcast(128), r=["d_nf"], w=["nfb"])
                P.dma('sp', wrs[:], w_r[layer].rearrange("(k p) n -> p k n", p=128), r=["d_wr"], w=["wrs"])
                P.dma('sp', brb[:], b_r[layer].partition_broadcast(128), r=["d_br"], w=["brb"])
                def route(i, r_, rk, lvl, sparse_info=None):
                    lg = r_[:, 0:36]; m4 = r_[:, 36:37]; nm4 = r_[:, 37:38]; e4 = r_[:, 40:44]; s4 = r_[:, 38:39]
                    pg = r_[:, 39:40]; ohg = r_[:, 44:48]; sel = r_[:, 48:56]; m8 = r_[:, 56:64]; d21 = r_[:, 64:65]
                    e21 = r_[:, 65:66]; w1 = r_[:, 66:67]; w2 = r_[:, 67:68]; c1 = r_[:, 72:80]; c2 = r_[:, 80:88]
                    V = nc.vector
                    def dv(fn, rk=rk):
                        P.op('dve', fn, r=[rk], w=[rk])
                    P.op('dve', lambda lg=lg: V.tensor_tensor(out=lg, in0=PS[5][:, 0:36], in1=brb[:], op=ALU.add),
                         r=[PK[5], "brb"], w=[rk])
                    if lvl <= 2:
                        P.op('dve', (lambda i=i, lg=lg: V.tensor_copy(out=gates[:, i, :], in_=lg[:, 0:32])), r=[rk], w=[("gates", i)])
                        return
                    dv(lambda: V.reduce_max(out=m4, in_=lg[:, 0:4], axis=AX.X))
                    dv(lambda: V.tensor_scalar(out=nm4, in0=m4, scalar1=-1.0, scalar2=None, op0=ALU.mult))
                    P.op('act', lambda: nc.scalar.activation(out=e4, in_=lg[:, 0:4], func=AF.Exp, bias=nm4, scale=1.0),
                         r=[rk], w=[rk])
                    dv(lambda: V.reduce_sum(out=s4, in_=e4, axis=AX.X))
                    dv(lambda: V.reciprocal(out=pg, in_=s4))
                    dv(lambda: V.tensor_scalar(out=ohg, in0=lg[:, 0:4], scalar1=m4, scalar2=None, op0=ALU.is_equal))
                    dv(lambda: V.tensor_scalar(out=sel, in0=lg[:, 4:12], scalar1=ohg[:, 0:1], scalar2=None, op0=ALU.mult))
                    for g in range(1, 4):
                        dv(lambda g=g: V.scalar_tensor_tensor(out=sel, in0=lg[:, 4 + 8 * g:12 + 8 * g], scalar=ohg[:, g:g + 1],
                                                              in1=sel, op0=ALU.mult, op1=ALU.add))
                    dv(lambda: V.max(out=m8, in_=sel))
                    dv(lambda: V.tensor_tensor(out=d21, in0=m8[:, 1:2], in1=m8[:, 0:1], op=ALU.subtract))
                    P.op('act', lambda: nc.scalar.activation(out=e21, in_=d21, func=AF.Exp), r=[rk], w=[rk])
                    dv(lambda: V.tensor_scalar(out=e21, in0=e21, scalar1=1.0, scalar2=None, op0=ALU.add))
                    dv(lambda: V.reciprocal(out=w1, in_=e21))
                    dv(lambda: V.tensor_tensor(out=w1, in0=w1, in1=pg, op=ALU.mult))
                    dv(lambda: V.tensor_tensor(out=w2, in0=pg, in1=w1, op=ALU.subtract))
                    if sparse_info is not None:
                        sparse_info(r_, rk, sel, m8, ohg, w1, w2, dv)
                        return
                    dv(lambda: V.tensor_scalar(out=c1, in0=sel, scalar1=m8[:, 0:1], scalar2=w1, op0=ALU.is_equal, op1=ALU.mult))
                    dv(lambda: V.tensor_scalar(out=c2, in0=sel, scalar1=m8[:, 1:2], scalar2=w2, op0=ALU.is_equal, op1=ALU.mult))
                    dv(lambda: V.tensor_tensor(out=c1, in0=c1, in1=c2, op=ALU.add))
                    P.op('dve', (lambda i=i, c1=c1, ohg=ohg: V.tensor_tensor(
                        out=gates[:, i, :].rearrange("p (g e) -> p g e", g=4),
                        in0=c1.unsqueeze(1).to_broadcast([128, 4, 8]), in1=ohg.unsqueeze(2).to_broadcast([128, 4, 8]),
                        op=ALU.mult)), r=[rk], w=[("gates", i)])
                for s0 in range(0, len(tiles), SBT):
                    sbt = tiles[s0:s0 + SBT]
                    import os
                    if os.environ.get("MOE_NT"):
                        sbt = sbt[:int(os.environ["MOE_NT"])]
                    n_sb = len(sbt)
                    for i, gt in enumerate(sbt):
                        mod, mkey = ("C", "mvec") if gt < NCT else ("L", "mvec")
                        xt, xk = R['xs'].next()
                        P.dma('sp', xt[:], src_t[gt], r=["d_XS1" if layer == 0 else "d_XS3"], w=[xk])
                        hx, hk = hxf.next()
                        rms_mod(R, xt[:], xk, mv(mod, 4), mv(mod, 3), [mkey], hx[:], hk)
                        import os
                        if int(os.environ.get("MOE_LVL", "9")) == 0:
                            dump("dbg_hx%d" % i, hx[:], [hk])
                            continue
                        for k in range(8):
                            b = 6 + k // 4
                            P.op('pe', (lambda b=b, k=k, hx=hx: nc.tensor.transpose(
                                PS[b][:, (k % 4) * 128:(k % 4) * 128 + 128], hx[:, k * 128:(k + 1) * 128], ident)),
                                r=[hk, "msk"], w=[PK[b]])
                        hT, hTk = hxTf.next()
                        for h in range(2):
                            b = 6 + h
                            P.op('act', (lambda b=b, h=h, hT=hT: nc.scalar.copy(
                                out=hT[:, 4 * h:4 * h + 4, :], in_=PS[b].rearrange("p (k t) -> p k t", k=4))),
                                r=[PK[b]], w=[hTk])
                        P.op('pool', (lambda i=i, hT=hT: nc.gpsimd.tensor_copy(out=hxTb[:, :, i * 128:(i + 1) * 128], in_=hT[:])),
                             r=[hTk], w=[("hxTb", i)])
                        import os
                        lvl = int(os.environ.get("MOE_LVL", "9"))
                        if lvl <= 1:
                            continue
                        for k in range(8):
                            P.op('pe', (lambda k=k, hT=hT: nc.tensor.matmul(PS[5][:, 0:36], hT[:, k, :], wrs[:, k, :],
                                                                          start=(k == 0), stop=(k == 7))),
                                 r=[hTk, "wrs"], w=[PK[5]])
                        r_, rk = rt.next()
                        route(i, r_, rk, lvl)
                    if stop_after == "moe_b1":
                        lvl = int(os.environ.get("MOE_LVL", "9"))
                        if lvl == 0:
                            P.finish()
                            return "STOP"
                        if lvl > 1:
                            dump("dbg_rt0", rt.tiles[0][:], [rt.name + "0"]); dump("dbg_rt1", rt.tiles[1][:], [rt.name + "1"])
                            dump("dbg_gates", gates[:], [("gates", i) for i in range(n_sb)])
                        if os.environ.get("NO_HXTB") is None:
                            dump("dbg_hxTb", hxTb[:], [("hxTb", i) for i in range(n_sb)])
                        else:
                            dump("dbg_hxTf", hxTf.tiles[0][:], [hxTf.name + "0"])
                        P.finish()
                        return "STOP"
                    blocks = [(t0, min(4, n_sb - t0)) for t0 in range(0, n_sb, 4)]
                    hcn = 0
                    yn = 0
                    for e in range(32):
                        wgt, wgk = wg.next(); wut, wuk = wu.next(); wdt, wdk = wd.next()
                        P.dma('pool', wgt[:], w_e_gate[layer, e].rearrange("(k p) n -> p k n", p=128), r=["d_weg"], w=[wgk])
                        P.dma('pool', wut[:], w_e_up[layer, e].rearrange("(k p) n -> p k n", p=128), r=["d_weu"], w=[wuk])
                        P.dma('pool', wdt[:], w_e_down[layer, e].rearrange("(k p) n -> p k n", p=128), r=["d_wed"], w=[wdk])
                        for (t0, ntl) in blocks:
                            ncol = ntl * 128
                            cs = slice(t0 * 128, t0 * 128 + ncol)
                            rk_h = [("hxTb", t0 + j) for j in range(ntl)]
                            hid, hidk = hidT.next()
                            for hc in range(4):
                                gb = (hcn % 2) * 2; ub = gb + 1; hcn += 1
                                for k in range(8):
                                    P.op('pe', (lambda gb=gb, k=k, hc=hc, wgt=wgt, cs=cs, ncol=ncol: nc.tensor.matmul(
                                        PS[gb][:, 0:ncol], wgt[:, k, hc * 128:(hc + 1) * 128], hxTb[:, k, cs],
                                        start=(k == 0), stop=(k == 7))), r=[wgk] + rk_h, w=[PK[gb]])
                                for k in range(8):
                                    P.op('pe', (lambda ub=ub, k=k, hc=hc, wut=wut, cs=cs, ncol=ncol: nc.tensor.matmul(
                                        PS[ub][:, 0:ncol], wut[:, k, hc * 128:(hc + 1) * 128], hxTb[:, k, cs],
                                        start=(k == 0), stop=(k == 7))), r=[wuk] + rk_h, w=[PK[ub]])
                                sg, sgk = sgb.next()
                                P.op('act', (lambda sg=sg, gb=gb, ncol=ncol: nc.scalar.activation(
                                    out=sg[:, 0:ncol], in_=PS[gb][:, 0:ncol], func=AF.Silu)), r=[PK[gb]], w=[sgk])
                                P.op('dve', (lambda sg=sg, ub=ub, ncol=ncol, hid=hid, hc=hc: nc.vector.tensor_tensor(
                                    out=hid[:, hc, 0:ncol], in0=sg[:, 0:ncol], in1=PS[ub][:, 0:ncol], op=ALU.mult)),
                                    r=[sgk, PK[ub]], w=[(hidk, hc)])
                            for t in range(ntl):
                                i = t0 + t
                                for half in range(2):
                                    yb = 4 + yn % 2; yn += 1
                                    for hc in range(4):
                                        P.op('pe', (lambda yb=yb, hc=hc, t=t, half=half, hid=hid, wdt=wdt: nc.tensor.matmul(
                                            PS[yb], hid[:, hc, t * 128:(t + 1) * 128], wdt[:, hc, half * 512:(half + 1) * 512],
                                            start=(hc == 0), stop=(hc == 3))), r=[(hidk, hc), wdk], w=[PK[yb]])
                                    sl = slice(half * 512, (half + 1) * 512)
                                    if e == 0:
                                        P.op('dve', (lambda yb=yb, i=i, sl=sl, e=e: nc.vector.tensor_scalar(
                                            out=acc[:, i, sl], in0=PS[yb], scalar1=gates[:, i, e:e + 1], scalar2=None, op0=ALU.mult)),
                                            r=[PK[yb], ("gates", i)], w=[("acc", i, half)])
                                    else:
                                        P.op('dve', (lambda yb=yb, i=i, sl=sl, e=e: nc.vector.scalar_tensor_tensor(
                                            out=acc[:, i, sl], in0=PS[yb], scalar=gates[:, i, e:e + 1], in1=acc[:, i, sl],
                                            op0=ALU.mult, op1=ALU.add)), r=[PK[yb], ("gates", i), ("acc", i, half)], w=[("acc", i, half)])
                    for i, gt in enumerate(sbt):
                        mod, mkey = ("C", "mvec") if gt < NCT else ("L", "mvec")
                        xt, xk = R['xs'].next()
                        P.dma('sp', xt[:], src_t[gt], r=["d_XS1" if layer == 0 else "d_XS3"], w=[xk])
                        P.op('pool', (lambda i=i, mod=mod: nc.gpsimd.tensor_tensor(out=acc[:, i, :], in0=acc[:, i, :], in1=mv(mod, 5), op=ALU.mult)),
                             r=[("acc", i, 0), ("acc", i, 1), mkey], w=[("acc", i, 0), ("acc", i, 1)])
                        P.op('pool', (lambda i=i, xt=xt: nc.gpsimd.tensor_tensor(out=acc[:, i, :], in0=acc[:, i, :], in1=xt[:], op=ALU.add)),
                             r=[("acc", i, 0), ("acc", i, 1), xk], w=[("acc", i, 0), ("acc", i, 1)])
                        if not final:
                            P.dma('sp', dst_t[gt], acc[:, i, :], r=[("acc", i, 0), ("acc", i, 1)], w=["d_dst%d" % layer])
                        else:
                            o_, ok = hxf.next()
                            rms_mod(R, acc[:, i, :], ("acc", i, 0), nfb[:], None, ["nfb", ("acc", i, 1)], o_[:], ok)
                            P.dma('sp', dst_t[gt - NCT], o_[:], r=[ok], w=["d_out"])
                P.barrier()

        I32 = mybir.dt.int32
        NTS = 65
        NSLOT = NTS * 512
        HXB = dscr("HXB", [NT * 128, D], BF16); XG = dscr("XG", [NSLOT, D], BF16)
        WG = dscr("WG", [NSLOT, 1]); YG = dscr("YG", [NSLOT, D])
        HXB_t = HXB.rearrange("(t p) d -> t p d", p=128)
        W2 = {"g": w_e_gate.rearrange("l e (p k) n -> (l e p) (k n)", k=8), "u": w_e_up.rearrange("l e (p k) n -> (l e p) (k n)", k=8),
              "d": w_e_down.rearrange("l e d n -> (l e d) n")}

        def moe_sparse(layer, src_t, dst_t, tiles, final):
            T_ = len(tiles)
            skey = "d_XS1" if layer == 0 else "d_XS3"
            es2 = contextlib.ExitStack()
            with es2:
                cms = sb("cms", [128, 160], F32, es2)
                info = sb("info", [128, NT, 8], F32, es2)
                posi = sb("posi", [128, NT, 2], I32, es2)
                cum = sb("cum", [128, 32], F32, es2)
                offs = sb("offs", [128, 32], F32, es2)
                te = sb("te", [128, 80], F32, es2)
                tec = sb("tec", [128, 80], F32, es2)
                P.dma('sp', cms[:], cmisc, r=["d_cm"], w=["cms"])
                P.op('pool', lambda: nc.gpsimd.memset(cum[:], 0.0), r=[], w=["cum"])
                iota = cms[:, 0:32]; base = cms[:, 32:40]; svals = cms[:, 40:120]
                V = nc.vector; G = nc.gpsimd; A = nc.scalar
                with contextlib.ExitStack() as ph:
                    R = mk_rings(ph)
                    load_mods(ph, [(st, ix) for st in ("LC" if not final else "L") for ix in (3, 4)])
                    hxf = Ring(nc, ph, "hxf", 2, [128, D], F32)
                    hxb = Ring(nc, ph, "hxb", 2, [128, D], BF16)
                    hxTf = Ring(nc, ph, "hxTf", 2, [128, 8, 128], F32)
                    wrs = sb("wrs", [128, 8, 36], F32, ph); brb = sb("brb", [128, 36], F32, ph)
                    rt = Ring(nc, ph, "rt", 2, [128, 288], F32)
                    gates = None
                    P.dma('sp', wrs[:], w_r[layer].rearrange("(k p) n -> p k n", p=128), r=["d_wr"], w=["wrs"])
                    P.dma('sp', brb[:], b_r[layer].partition_broadcast(128), r=["d_br"], w=["brb"])

                    def route(i, r_, rk):
                        lg = r_[:, 0:36]; m4 = r_[:, 36:37]; nm4 = r_[:, 37:38]; e4 = r_[:, 40:44]; s4 = r_[:, 38:39]
                        pg = r_[:, 39:40]; ohg = r_[:, 44:48]; sel = r_[:, 48:56]; m8 = r_[:, 56:64]; d21 = r_[:, 64:65]
                        e21 = r_[:, 65:66]; w1 = r_[:, 66:67]; w2 = r_[:, 67:68]; eq = r_[:, 72:88]
                        oh1 = r_[:, 96:128]; oh2 = r_[:, 128:160]; ohs = r_[:, 160:192]; rkt = r_[:, 192:224]; tmp = r_[:, 224:256]

                        def dv(fn):
                            P.op('dve', fn, r=[rk], w=[rk])
                        P.op('dve', lambda: V.tensor_tensor(out=lg, in0=PS[5 - 4 * (i % 2)][:, 0:36], in1=brb[:], op=ALU.add), r=[PK[5 - 4 * (i % 2)], "brb"], w=[rk])
                        dv(lambda: V.reduce_max(out=m4, in_=lg[:, 0:4], axis=AX.X))
                        dv(lambda: V.tensor_scalar(out=nm4, in0=m4, scalar1=-1.0, scalar2=None, op0=ALU.mult))
                        yield
                        P.op('act', lambda: A.activation(out=e4, in_=lg[:, 0:4], func=AF.Exp, bias=nm4, scale=1.0), r=[rk], w=[rk])
                        yield
                        dv(lambda: V.reduce_sum(out=s4, in_=e4, axis=AX.X))
                        dv(lambda: V.reciprocal(out=pg, in_=s4))
                        dv(lambda: V.tensor_scalar(out=ohg, in0=lg[:, 0:4], scalar1=m4, scalar2=None, op0=ALU.is_equal))
                        dv(lambda: V.tensor_scalar(out=sel, in0=lg[:, 4:12], scalar1=ohg[:, 0:1], scalar2=None, op0=ALU.mult))
                        for g in range(1, 4):
                            dv(lambda g=g: V.scalar_tensor_tensor(out=sel, in0=lg[:, 4 + 8 * g:12 + 8 * g], scalar=ohg[:, g:g + 1],
                                                                  in1=sel, op0=ALU.mult, op1=ALU.add))
                        yield
                        dv(lambda: V.max(out=m8, in_=sel))
                        dv(lambda: V.tensor_tensor(out=d21, in0=m8[:, 1:2], in1=m8[:, 0:1], op=ALU.subtract))
                        yield
                        P.op('act', lambda: A.activation(out=e21, in_=d21, func=AF.Exp), r=[rk], w=[rk])
                        yield
                        dv(lambda: V.tensor_scalar(out=e21, in0=e21, scalar1=1.0, scalar2=None, op0=ALU.add))
                        dv(lambda: V.reciprocal(out=w1, in_=e21))
                        dv(lambda: V.tensor_tensor(out=info[:, i, 2:3], in0=w1, in1=pg, op=ALU.mult))
                        dv(lambda: V.tensor_tensor(out=info[:, i, 5:6], in0=pg, in1=info[:, i, 2:3], op=ALU.subtract))
                        dv(lambda: V.tensor_scalar(out=eq[:, 0:8], in0=sel, scalar1=m8[:, 0:1], scalar2=None, op0=ALU.is_equal))
                        dv(lambda: V.tensor_scalar(out=eq[:, 8:16], in0=sel, scalar1=m8[:, 1:2], scalar2=None, op0=ALU.is_equal))
                        for j, oh in enumerate((oh1, oh2)):
                            dv(lambda j=j, oh=oh: V.tensor_tensor(out=oh.rearrange("p (g e) -> p g e", g=4),
                                                                  in0=eq[:, 8 * j:8 * j + 8].unsqueeze(1).to_broadcast([128, 4, 8]),
                                                                  in1=ohg.unsqueeze(2).to_broadcast([128, 4, 8]), op=ALU.mult))
                        dv(lambda: V.tensor_tensor(out=ohs, in0=oh1, in1=oh2, op=ALU.add))
                        yield
                        P.op('pe', lambda: nc.tensor.matmul(PS[4 - 4 * (i % 2)][:, 0:32], msk[:, 6, :], ohs, start=True, stop=True), r=[rk, "msk"], w=[PK[4 - 4 * (i % 2)]])
                        P.op('pe', lambda: nc.tensor.matmul(PS[4 - 4 * (i % 2)][:, 32:64], msk[:, 3, :], ohs, start=True, stop=True), r=[rk, "msk"], w=[PK[4 - 4 * (i % 2)]])
                        yield
                        P.op('dve', lambda: V.tensor_tensor(out=rkt, in0=PS[4 - 4 * (i % 2)][:, 0:32], in1=cum[:], op=ALU.add), r=[PK[4 - 4 * (i % 2)], "cum", rk], w=[rk])
                        P.op('dve', lambda: V.tensor_tensor(out=cum[:], in0=cum[:], in1=PS[4 - 4 * (i % 2)][:, 32:64], op=ALU.add), r=[PK[4 - 4 * (i % 2)], "cum", rk], w=["cum"])
                        for j, oh in enumerate((oh1, oh2)):
                            dv(lambda oh=oh: V.tensor_tensor(out=tmp, in0=oh, in1=rkt, op=ALU.mult))
                            dv(lambda j=j: V.reduce_sum(out=info[:, i, 3 * j + 1:3 * j + 2], in_=tmp, axis=AX.X))
                            dv(lambda oh=oh: V.tensor_tensor(out=tmp, in0=oh, in1=iota, op=ALU.mult))
                            dv(lambda j=j: V.reduce_sum(out=info[:, i, 3 * j:3 * j + 1], in_=tmp, axis=AX.X))

                    def sa_tile(i, gt):
                        mod, mkey = ("C", "mvec") if gt < NCT else ("L", "mvec")
                        xt, xk = R['xs'].next()
                        P.dma('sp', xt[:], src_t[gt], r=[skey], w=[xk])
                        hx, hk = hxf.next()
                        rms_mod(R, xt[:], xk, mv(mod, 4), mv(mod, 3), [mkey], hx[:], hk)
                        yield
                        hb, hbk = hxb.next()
                        P.op('pool', (lambda hb=hb, hx=hx: G.tensor_copy(out=hb[:], in_=hx[:])), r=[hk], w=[hbk])
                        P.dma('sp', HXB_t[gt], hb[:], r=[hbk], w=["d_HXB"])
                        for k in range(8):
                            b = (6 if i % 2 == 0 else 2) + k // 4
                            P.op('pe', (lambda b=b, k=k, hx=hx: nc.tensor.transpose(
                                PS[b][:, (k % 4) * 128:(k % 4) * 128 + 128], hx[:, k * 128:(k + 1) * 128], ident)),
                                r=[hk, "msk"], w=[PK[b]])
                        yield
                        hT, hTk = hxTf.next()
                        for h in range(2):
                            b = (6 if i % 2 == 0 else 2) + h
                            P.op('act', (lambda b=b, h=h, hT=hT: A.copy(
                                out=hT[:, 4 * h:4 * h + 4, :], in_=PS[b].rearrange("p (k t) -> p k t", k=4))), r=[PK[b]], w=[hTk])
                        for k in range(8):
                            P.op('pe', (lambda k=k, hT=hT: nc.tensor.matmul(PS[5 - 4 * (i % 2)][:, 0:36], hT[:, k, :], wrs[:, k, :],
                                                                          start=(k == 0), stop=(k == 7))), r=[hTk, "wrs"], w=[PK[5 - 4 * (i % 2)]])
                        yield
                        r_, rk = rt.next()
                        yield from route(i, r_, rk)

                    run_interleaved(P, (sa_tile(i, gt) for i, gt in enumerate(tiles)), 2)
                    ci = sb("ci", [128, 32], I32, ph); pn = sb("pn", [128, 32], F32, ph)
                    sa = sb("sa", [128, 32], F32, ph); sb_ = sb("sb_", [128, 32], F32, ph)
                    P.op('dve', lambda: V.tensor_copy(out=ci[:], in_=cum[:]), r=["cum"], w=["ci"])
                    P.op('dve', lambda: V.tensor_scalar(out=ci[:], in0=ci[:], scalar1=511, scalar2=None, op0=ALU.add), r=["ci"], w=["ci"])
                    P.op('dve', lambda: V.tensor_scalar(out=ci[:], in0=ci[:], scalar1=9, scalar2=None, op0=ALU.arith_shift_right), r=["ci"], w=["ci"])
                    P.op('dve', lambda: V.tensor_scalar(out=ci[:], in0=ci[:], scalar1=9, scalar2=None, op0=ALU.logical_shift_left), r=["ci"], w=["ci"])
                    P.op('dve', lambda: V.tensor_copy(out=pn[:], in_=ci[:]), r=["ci"], w=["pn"])
                    P.op('dve', lambda: V.tensor_copy(out=sa[:], in_=pn[:]), r=["pn"], w=["sa"])
                    a_, b_ = sa, sb_
                    ak, bk = "sa", "sb_"
                    for sh in (1, 2, 4, 8, 16):
                        P.op('dve', (lambda a_=a_, b_=b_, sh=sh: V.tensor_copy(out=b_[:, 0:sh], in_=a_[:, 0:sh])), r=[ak], w=[bk])
                        P.op('dve', (lambda a_=a_, b_=b_, sh=sh: V.tensor_tensor(out=b_[:, sh:32], in0=a_[:, sh:32], in1=a_[:, 0:32 - sh], op=ALU.add)),
                             r=[ak, bk], w=[bk])
                        a_, b_, ak, bk = b_, a_, bk, ak
                    incl, inclk = a_, ak
                    P.op('dve', lambda: V.tensor_tensor(out=offs[:], in0=incl[:], in1=pn[:], op=ALU.subtract), r=[inclk, "pn"], w=["offs"])
                    P.op('pool', lambda: G.memset(te[:], 0.0), r=[], w=["te"])
                    for e in range(32):
                        P.op('dve', (lambda e=e: V.scalar_tensor_tensor(out=te[:], in0=svals, scalar=incl[:, e:e + 1], in1=te[:], op0=ALU.is_ge, op1=ALU.add)),
                             r=[inclk, "te", "cms"], w=["te"])
                    P.op('dve', lambda: V.tensor_scalar(out=tec[:], in0=te[:], scalar1=31.0, scalar2=None, op0=ALU.min), r=["te"], w=["tec"])
                    P.barrier()
                if stop_after == "sA":
                    P.finish(); return "STOP"
                with contextlib.ExitStack() as ph:
                    zt = sb("zt", [128, 4096], BF16, ph)
                    hb2 = Ring(nc, ph, "hb2", 3, [128, D], BF16)
                    pt = Ring(nc, ph, "pt", 2, [128, 72], F32)
                    P.op('pool', lambda: G.memset(zt[:], 0.0), r=[], w=["zt"])
                    XGz = XG.rearrange("(q p f) d -> q p (f d)", p=128, f=4)
                    for q in range(NTS):
                        P.dma('sp', XGz[q], zt[:], r=["zt"], w=["d_XG"])
                    for i, gt in enumerate(tiles):
                        p_, pk_ = pt.next()
                        for j in range(2):
                            P.op('dve', (lambda p_=p_, i=i, j=j: V.tensor_scalar(out=p_[:, 0:32], in0=iota, scalar1=info[:, i, 3 * j:3 * j + 1], scalar2=None, op0=ALU.is_equal)),
                                 r=["info", "cms", pk_], w=[pk_])
                            P.op('dve', (lambda p_=p_: V.tensor_tensor(out=p_[:, 0:32], in0=p_[:, 0:32], in1=offs[:], op=ALU.mult)), r=[pk_, "offs"], w=[pk_])
                            P.op('dve', (lambda p_=p_, j=j: V.reduce_sum(out=p_[:, 32 + j:33 + j], in_=p_[:, 0:32], axis=AX.X)), r=[pk_], w=[pk_])
                            P.op('dve', (lambda p_=p_, i=i, j=j: V.tensor_tensor(out=p_[:, 32 + j:33 + j], in0=p_[:, 32 + j:33 + j], in1=info[:, i, 3 * j + 1:3 * j + 2], op=ALU.add)),
                                 r=[pk_, "info"], w=[pk_])
                        P.op('dve', (lambda p_=p_, i=i: V.tensor_copy(out=posi[:, i, :], in_=p_[:, 32:34])), r=[pk_], w=[("posi", i)])
                        hb, hbk = hb2.next()
                        P.dma('sp', hb[:], HXB_t[gt], r=["d_HXB"], w=[hbk])
                        for j in range(2):
                            P.op('pool', (lambda hb=hb, i=i, j=j: G.indirect_dma_start(
                                out=XG[:, :], out_offset=bass.IndirectOffsetOnAxis(ap=posi[:, i, j:j + 1], axis=0), in_=hb[:, :], in_offset=None)),
                                r=[hbk, ("posi", i), "d_XG"], w=["d_XGs"], dma=True)
                            P.op('pool', (lambda i=i, j=j: G.indirect_dma_start(
                                out=WG[:, :], out_offset=bass.IndirectOffsetOnAxis(ap=posi[:, i, j:j + 1], axis=0), in_=info[:, i, 3 * j + 2:3 * j + 3], in_offset=None)),
                                r=["info", ("posi", i)], w=["d_WG"], dma=True)
                        P.flush()
                    P.barrier()
                if stop_after == "sC":
                    P.finish(); return "STOP"
                with contextlib.ExitStack() as ph:
                    wg = Ring(nc, ph, "wg", 2, [128, 8, 512], BF16); wu = Ring(nc, ph, "wu", 2, [128, 8, 512], BF16)
                    wd = Ring(nc, ph, "wd", 2, [128, 4, D], BF16)
                    xg = Ring(nc, ph, "xg", 2, [128, 4, D], BF16); xT = Ring(nc, ph, "xT", 2, [128, 8, 512], BF16)
                    wgt = Ring(nc, ph, "wgt", 2, [128, 4], F32)
                    idf = Ring(nc, ph, "idf", 2, [128, 12], F32); idi = Ring(nc, ph, "idi", 2, [128, 12], I32)
                    sgb = Ring(nc, ph, "sgb", 2, [128, 512], BF16); hidT = Ring(nc, ph, "hidT", 2, [128, 4, 512], BF16)
                    yg = Ring(nc, ph, "yg", 2, [128, D], F32)
                    hcn = [0]; yn = [0]

                    def slot_tile(s_):
                        f_, fk = idf.next(); ii, ik = idi.next()
                        P.op('dve', lambda: V.scalar_tensor_tensor(out=f_[:, 0:1], in0=tec[:, s_:s_ + 1], scalar=128.0, in1=base[:, 0:1],
                                                                   op0=ALU.mult, op1=ALU.add), r=["tec", "cms"], w=[fk])
                        P.op('dve', lambda: V.scalar_tensor_tensor(out=f_[:, 8:12], in0=tec[:, s_:s_ + 1].to_broadcast([128, 4]), scalar=512.0, in1=base[:, 0:4],
                                                                   op0=ALU.mult, op1=ALU.add), r=["tec", "cms", fk], w=[fk])
                        if layer > 0:
                            P.op('dve', lambda: V.tensor_scalar(out=f_[:, 0:1], in0=f_[:, 0:1], scalar1=float(layer * 32 * 128), scalar2=None, op0=ALU.add), r=[fk], w=[fk])
                            P.op('dve', lambda: V.tensor_scalar(out=f_[:, 8:12], in0=f_[:, 8:12], scalar1=float(layer * 32 * 512), scalar2=None, op0=ALU.add), r=[fk], w=[fk])
                        P.op('dve', lambda: V.tensor_copy(out=ii[:], in_=f_[:]), r=[fk], w=[ik])
                        wgt_, wgk = wg.next(); wut, wuk = wu.next(); wdt, wdk = wd.next()
                        P.op('pool', lambda: G.indirect_dma_start(out=wgt_[:].rearrange("p k n -> p (k n)"), out_offset=None, in_=W2["g"],
                                                                  in_offset=bass.IndirectOffsetOnAxis(ap=ii[:, 0:1], axis=0)),
                             r=[ik], w=[(wgk, k) for k in range(8)], dma=True)
                        P.op('pool', lambda: G.indirect_dma_start(out=wut[:].rearrange("p k n -> p (k n)"), out_offset=None, in_=W2["u"],
                                                                  in_offset=bass.IndirectOffsetOnAxis(ap=ii[:, 0:1], axis=0)),
                             r=[ik], w=[(wuk, k) for k in range(8)], dma=True)
                        for k in range(4):
                            P.op('pool', (lambda k=k: G.indirect_dma_start(out=wdt[:, k, :], out_offset=None, in_=W2["d"],
                                                                         in_offset=bass.IndirectOffsetOnAxis(ap=ii[:, 8 + k:9 + k], axis=0))),
                                 r=[ik], w=[(wdk, k)], dma=True)
                        yield
                        x_, xk_ = xg.next(); xt_, xtk = xT.next(); g4, g4k = wgt.next()
                        P.dma('sp', x_[:], XG[s_ * 512:(s_ + 1) * 512, :].rearrange("(t p) d -> p t d", p=128), r=["d_XGs", "d_XG"], w=[xk_])
                        P.dma('sp', g4[:], WG[s_ * 512:(s_ + 1) * 512, :].rearrange("(t p) o -> p (t o)", p=128), r=["d_WG"], w=[g4k], allow_slow_non_contiguous=True)
                        yield
                        for t in range(4):
                            b = 6 + t % 2
                            psb = PS[b].bitcast(BF16)
                            for k in range(8):
                                P.op('pe', (lambda psb=psb, k=k, t=t: nc.tensor.transpose(psb[:, k * 128:(k + 1) * 128], x_[:, t, k:D:8], identb[:])),
                                     r=[xk_, "identb"], w=[PK[b]])
                            P.op('act', (lambda psb=psb, t=t: A.copy(out=xt_[:, :, t * 128:(t + 1) * 128], in_=psb.rearrange("p (k c) -> p k c", k=8))),
                                 r=[PK[b]], w=[(xtk, t)])
                        yield
                        xtkeys = [(xtk, t) for t in range(4)]
                        hid, hidk = hidT.next()
                        for hc in range(4):
                            gb = (hcn[0] % 2) * 2; ub = gb + 1; hcn[0] += 1
                            for k in range(8):
                                P.op('pe', (lambda gb=gb, k=k, hc=hc: nc.tensor.matmul(PS[gb], wgt_[:, k, hc * 128:(hc + 1) * 128], xt_[:, k, :],
                                                                                    start=(k == 0), stop=(k == 7))), r=[(wgk, k)] + xtkeys, w=[PK[gb]])
                            for k in range(8):
                                P.op('pe', (lambda ub=ub, k=k, hc=hc: nc.tensor.matmul(PS[ub], wut[:, k, hc * 128:(hc + 1) * 128], xt_[:, k, :],
                                                                                    start=(k == 0), stop=(k == 7))), r=[(wuk, k)] + xtkeys, w=[PK[ub]])
                            yield
                            sg, sgk = sgb.next()
                            P.op('act', (lambda sg=sg, gb=gb: A.activation(out=sg[:], in_=PS[gb], func=AF.Silu)), r=[PK[gb]], w=[sgk])
                            P.op('dve', (lambda sg=sg, ub=ub, hc=hc: V.tensor_tensor(out=hid[:, hc, :], in0=sg[:], in1=PS[ub], op=ALU.mult)),
                                 r=[sgk, PK[ub]], w=[(hidk, hc)])
                        for t in range(4):
                            yield
                            y_, yk = yg.next()
                            for half in range(2):
                                yb_ = 4 + yn[0] % 2; yn[0] += 1
                                for hc in range(4):
                                    P.op('pe', (lambda yb_=yb_, hc=hc, t=t, half=half: nc.tensor.matmul(
                                        PS[yb_], hid[:, hc, t * 128:(t + 1) * 128], wdt[:, hc, half * 512:(half + 1) * 512],
                                        start=(hc == 0), stop=(hc == 3))), r=[(hidk, hc), (wdk, hc)], w=[PK[yb_]])
                                P.op('act', (lambda yb_=yb_, half=half, t=t, y_=y_: A.activation(out=y_[:, half * 512:(half + 1) * 512], in_=PS[yb_], func=AF.Copy,
                                                                                            scale=g4[:, t:t + 1])), r=[PK[yb_], g4k], w=[(yk, half)])
                            P.dma('sp', YG[s_ * 512 + t * 128:s_ * 512 + (t + 1) * 128, :], y_[:], r=[(yk, 0), (yk, 1)], w=["d_YG"])

                    run_interleaved(P, (slot_tile(s_) for s_ in range(NTS)), 2)
                    P.barrier()
                if stop_after == "sD":
                    P.finish(); return "STOP"
                with contextlib.ExitStack() as ph:
                    R = mk_rings(ph)
                    load_mods(ph, [(st, 5) for st in ("LC" if not final else "L")])
                    y1 = Ring(nc, ph, "y1", 2, [128, D], F32); y2 = Ring(nc, ph, "y2", 2, [128, D], F32)
                    ofin = Ring(nc, ph, "ofin", 2, [128, D], F32)
                    nfb = None
                    if final:
                        nfb = sb("nfb", [128, D], F32, ph)
                        P.dma('sp', nfb[:], norm_final.partition_broadcast(128), r=["d_nf"], w=["nfb"])

                    def comb(i, gt):
                        mod, mkey = ("C", "mvec") if gt < NCT else ("L", "mvec")
                        a_, ak_ = y1.next(); b_, bk_ = y2.next()
                        for j, (dst, dk_) in enumerate(((a_, ak_), (b_, bk_))):
                            P.op('pool', (lambda dst=dst, j=j: G.indirect_dma_start(out=dst[:, :], out_offset=None, in_=YG[:, :],
                                                                                  in_offset=bass.IndirectOffsetOnAxis(ap=posi[:, i, j:j + 1], axis=0))),
                                 r=[("posi", i), "d_YG"], w=[dk_], dma=True)
                        xt, xk = R['xs'].next()
                        P.dma('sp', xt[:], src_t[gt], r=[skey], w=[xk])
                        P.op('pool', lambda: G.tensor_tensor(out=a_[:], in0=a_[:], in1=b_[:], op=ALU.add), r=[ak_, bk_], w=[ak_])
                        P.op('dve', lambda: V.tensor_tensor(out=a_[:], in0=a_[:], in1=mv(mod, 5), op=ALU.mult), r=[ak_, mkey], w=[ak_])
                        P.op('pool', lambda: G.tensor_tensor(out=a_[:], in0=a_[:], in1=xt[:], op=ALU.add), r=[ak_, xk], w=[ak_])
                        if not final:
                            P.dma('sp', dst_t[gt], a_[:], r=[ak_], w=["d_dst%d" % layer])
                        else:
                            o_, ok = ofin.next()
                            rms_mod(R, a_[:], ak_, nfb[:], None, ["nfb"], o_[:], ok)
                            P.dma('sp', dst_t[gt - NCT], o_[:], r=[ok], w=["d_out"])

                    for i, gt in enumerate(tiles):
                        comb(i, gt)
                    P.barrier()

        import os
        MOE = moe_sparse if os.environ.get("DENSE_MOE") is None else moe_phase
        if MOE(0, XS1_t, XS2_t, list(range(NT)), False) == "STOP":
            return nc
        if stop_after == "moe0":
            P.finish()
            return nc

        bfd = lambda name, shape: dscr(name, shape, BF16)
        QT_d = bfd("QT_d", [NT, 128, 8, 128]); KT_d = bfd("KT_d", [NT, 128, 8, 128])
        KK_d = bfd("KK_d", [NT, 128, 8, 128]); VV_d = bfd("VV_d", [NT, 128, 8, 128])
        ZZ_d = bfd("ZZ_d", [NT, 128, D]); GB_d = dscr("GB_d", [NT, 128, 32])
        OF_d = dscr("OF_d", [2, NLT, 128, D])
        with contextlib.ExitStack() as ph:
            adaln(1, ph)
            P.barrier()

        with contextlib.ExitStack() as ph:
            R = mk_rings(ph)
            load_mods(ph, [(st, ix) for st in "LC" for ix in (0, 1)])
            SBT1 = 4
            hxf = Ring(nc, ph, "c1hx", 1, [128, D], F32)
            hxT = sb("c1hxT", [128, 8, (SBT1 + 2) * 128], BF16, ph)
            pT = Ring(nc, ph, "pT", 4, [128, SBT1 * 128 + 4], F32)
            cv = Ring(nc, ph, "cv", 4, [128, SBT1 * 128], F32)
            sqb = Ring(nc, ph, "sqb", 3, [128, SBT1 * 128], BF16)
            rsb = Ring(nc, ph, "rsb", 3, [128, SBT1 * 128], F32)
            kf = Ring(nc, ph, "kf", 3, [128, SBT1 * 128], F32)
            qst = sb("qst", [128, SBT1, 8, 128], BF16, ph); kst = sb("kst", [128, SBT1, 8, 128], BF16, ph)
            ktst = sb("ktst", [128, SBT1, 8, 128], BF16, ph); vtst = sb("vtst", [128, SBT1, 8, 128], BF16, ph)
            win_sb = sb("win_sb", [128, 8, 3072], BF16, ph)
            wz = sb("wz", [128, 8, 1056], BF16, ph)
            wcs = sb("wcs", [128, 24, 4], F32, ph)
            onesb = sb("onesb", [128, 128], BF16, ph)
            c16 = sb("c16", [128, 2, 16], F32, ph)
            zsb = Ring(nc, ph, "zsb", 1, [128, D], BF16)
            gbr = Ring(nc, ph, "gbr", 2, [128, 64], F32)
            P.dma('pool', wz[:], w_dn_in.rearrange("(k p) n -> p k n", p=128)[:, :, 3072:4128], r=["d_win"], w=["wz"])
            P.dma('sp', wcs[:], wconv, r=["d_wconv"], w=["wcs"])
            P.op('dve', lambda: nc.vector.tensor_copy(out=onesb[:], in_=msk[:, 3, :]), r=["msk"], w=["onesb"])
            P.dma('sp', c16[:, 0, :], dn_dt_bias.partition_broadcast(128), r=["d_dtb"], w=["c16"])
            P.dma('sp', c16[:, 1, :], dn_a_log.partition_broadcast(128), r=["d_alog"], w=["c16"])
            P.op('act', lambda: nc.scalar.activation(out=c16[:, 1, :], in_=c16[:, 1, :], func=AF.Exp), r=["c16"], w=["c16"])
            P.op('dve', lambda: nc.vector.tensor_scalar(out=c16[:, 1, :], in0=c16[:, 1, :], scalar1=-1.0, scalar2=None, op0=ALU.mult),
                 r=["c16"], w=["c16"])
            win_v = w_dn_in.rearrange("(k p) n -> p k n", p=128)
            for j6 in range(6):
                P.dma('pool', win_sb[:, :, j6 * 512:(j6 + 1) * 512], win_v[:, :, j6 * 512:(j6 + 1) * 512], r=["d_win"], w=[("win_sb", j6)])

            def c1_tile_norm(gt, slot, mod, mkey):
                xt, xk = R['xs'].next()
                P.dma('sp', xt[:], XS2_t[gt], r=["d_dst0"], w=[xk])
                hx, hk = hxf.next()
                rms_mod(R, xt[:], xk, mv(mod, 1), mv(mod, 0), [mkey], hx[:], hk)
                for k in range(8):
                    b = 6 + k // 4
                    P.op('pe', (lambda b=b, k=k, hx=hx: nc.tensor.transpose(
                        PS[b][:, (k % 4) * 128:(k % 4) * 128 + 128], hx[:, k * 128:(k + 1) * 128], ident)),
                        r=[hk, "msk"], w=[PK[b]])
                for h in range(2):
                    b = 6 + h
                    P.op('act', (lambda b=b, h=h, slot=slot: nc.scalar.copy(
                        out=hxT[:, 4 * h:4 * h + 4, slot * 128:(slot + 1) * 128],
                        in_=PS[b].rearrange("p (k t) -> p k t", k=4))), r=[PK[b]], w=[("hxT", slot)])

            def c1_zab(gt, slot, is_ctx):
                hk = [("hxT", slot)]
                if not is_ctx:
                    zs, zk = zsb.next()
                    for half in range(2):
                        b = 4 + half
                        for k in range(8):
                            P.op('pe', (lambda b=b, k=k, half=half, slot=slot: nc.tensor.matmul(
                                PS[b], hxT[:, k, slot * 128:(slot + 1) * 128], wz[:, k, half * 512:(half + 1) * 512],
                                start=(k == 0), stop=(k == 7))), r=hk + ["wz"], w=[PK[b]])
                        P.op('act', (lambda b=b, half=half, zs=zs: nc.scalar.activation(
                            out=zs[:, half * 512:(half + 1) * 512], in_=PS[b], func=AF.Silu)), r=[PK[b]], w=[(zk, half)])
                    P.dma('sp', ZZ_d[gt], zs[:], r=[(zk, 0), (zk, 1)], w=["d_ZZ"])
                for k in range(8):
                    P.op('pe', (lambda k=k, slot=slot: nc.tensor.matmul(
                        PS[3][:, 0:32], hxT[:, k, slot * 128:(slot + 1) * 128], wz[:, k, 1024:1056],
                        start=(k == 0), stop=(k == 7))), r=hk + ["wz"], w=[PK[3]])
                g_, gk = gbr.next()
                V = nc.vector
                ab = g_[:, 0:32].rearrange("p (f h) -> p f h", f=4)
                o4 = g_[:, 32:64].rearrange("p (f h) -> p f h", f=4)
                P.op('dve', lambda: V.tensor_copy(out=g_[:, 0:32], in_=PS[3][:, 0:32]), r=[PK[3]], w=[gk])
                P.op('dve', lambda: V.tensor_tensor(out=o4[:, 0::2, :], in0=ab[:, 0::2, :],
                                                    in1=c16[:, 0, :].rearrange("p (d h) -> p d h", d=2), op=ALU.add),
                     r=[gk, "c16"], w=[gk])
                P.op('act', lambda: nc.scalar.activation(out=o4[:, 0::2, :], in_=o4[:, 0::2, :], func=AF.Exp), r=[gk], w=[gk])
                P.op('dve', lambda: V.tensor_scalar(out=o4[:, 0::2, :], in0=o4[:, 0::2, :], scalar1=1.0, scalar2=None, op0=ALU.add),
                     r=[gk], w=[gk])
                P.op('act', lambda: nc.scalar.activation(out=o4[:, 0::2, :], in_=o4[:, 0::2, :], func=AF.Ln), r=[gk], w=[gk])
                P.op('dve', lambda: V.tensor_tensor(out=o4[:, 0::2, :], in0=o4[:, 0::2, :],
                                                    in1=c16[:, 1, :].rearrange("p (d h) -> p d h", d=2), op=ALU.mult),
                     r=[gk, "c16"], w=[gk])
                P.op('act', lambda: nc.scalar.activation(out=o4[:, 1::2, :], in_=ab[:, 1::2, :], func=AF.Sigmoid), r=[gk], w=[gk])
                P.dma('sp', GB_d[gt], g_[:, 32:64], r=[gk], w=["d_GB"])

            def c1_chunk(cc, seq0, nseq, t0, t1, hbase_tile):
                W = (t1 - t0) * 128
                ntile = t1 - t0
                wk = ("win_sb", cc // 4)
                p_, pk = pT.next()
                tok0 = t0 * 128 - 2
                lo = max(tok0, 0); hi = min(t1 * 128 + 1, nseq * 128)
                if lo > tok0:
                    P.op('pool', lambda: nc.gpsimd.memset(p_[:, 0:lo - tok0], 0.0), r=[], w=[(pk, 'l')])
                if hi < t1 * 128 + 1:
                    P.op('pool', lambda: nc.gpsimd.memset(p_[:, hi - tok0:W + 3], 0.0), r=[], w=[(pk, 'r')])
                a = lo
                wi = 0
                while a < hi:
                    b_ = min(a + 512, hi)
                    bank = (wi + cc) % 3
                    hk = [("hxT", s_) for s_ in range((a // 128) - hbase_tile, ((b_ - 1) // 128) - hbase_tile + 1)]
                    for k in range(8):
                        P.op('pe', (lambda k=k, bank=bank, a=a, b_=b_: nc.tensor.matmul(
                            PS[bank][:, 0:b_ - a], win_sb[:, k, cc * 128:(cc + 1) * 128], hxT[:, k, a - hbase_tile * 128:b_ - hbase_tile * 128],
                            start=(k == 0), stop=(k == 7))), r=[wk] + hk, w=[PK[bank]])
                    P.op('act', (lambda bank=bank, a=a, b_=b_: nc.scalar.copy(out=p_[:, a - tok0:b_ - tok0], in_=PS[bank][:, 0:b_ - a])),
                         r=[PK[bank]], w=[(pk, wi)])
                    a = b_; wi += 1
                yield
                pkeys = [(pk, j) for j in range(wi)] + [(pk, 'l'), (pk, 'r')]
                c_, ck = cv.next()
                P.op('dve', lambda: nc.vector.tensor_scalar(out=c_[:, 0:W], in0=p_[:, 0:W], scalar1=wcs[:, cc, 0:1], scalar2=None, op0=ALU.mult),
                     r=pkeys + ["wcs"], w=[ck])
                for tap in range(1, 4):
                    eng = 'dve'
                    E_ = nc.gpsimd if eng == 'pool' else nc.vector
                    P.op(eng, (lambda tap=tap, E_=E_: E_.scalar_tensor_tensor(out=c_[:, 0:W], in0=p_[:, tap:tap + W], scalar=wcs[:, cc, tap:tap + 1],
                                                                             in1=c_[:, 0:W], op0=ALU.mult, op1=ALU.add)),
                         r=pkeys + ["wcs", ck], w=[ck])
                yield
                P.op('act', lambda: nc.scalar.activation(out=c_[:, 0:W], in_=c_[:, 0:W], func=AF.Silu), r=[ck], w=[ck])
                yield
                kind = cc // 8; h = cc % 8
                src = c_
                srck = ck
                if kind < 2:
                    sq, sqk = sqb.next(); rs, rk_ = rsb.next()
                    P.op('pool', lambda: nc.gpsimd.tensor_tensor(out=sq[:, 0:W], in0=c_[:, 0:W], in1=c_[:, 0:W], op=ALU.mult), r=[ck], w=[sqk])
                    for j in range(0, W, 512):
                        n_ = min(512, W - j)
                        bank = 3 + (j // 512) % 2
                        P.op('pe', (lambda j=j, n_=n_, bank=bank: nc.tensor.matmul(PS[bank][:, 0:n_], onesb[:], sq[:, j:j + n_], start=True, stop=True)),
                             r=[sqk, "onesb"], w=[PK[bank]])
                        P.op('dve', (lambda j=j, n_=n_, bank=bank: nc.vector.tensor_scalar(out=rs[:, j:j + n_], in0=PS[bank][:, 0:n_], scalar1=EPS, scalar2=None, op0=ALU.add)),
                             r=[PK[bank]], w=[(rk_, j)])
                    yield
                    rkeys = [(rk_, j) for j in range(0, W, 512)]
                    P.op('act', lambda: nc.scalar.activation(out=rs[:, 0:W], in_=rs[:, 0:W], func=AF.Sqrt), r=rkeys, w=rkeys)
                    yield
                    P.op('dve', lambda: nc.vector.reciprocal(out=rs[:, 0:W], in_=rs[:, 0:W]), r=rkeys, w=rkeys)
                    if kind == 0:
                        P.op('dve', lambda: nc.vector.scalar_tensor_tensor(
                            out=qst[:, 0:ntile, h, :], in0=c_[:, 0:W].rearrange("p (t k) -> p t k", k=128), scalar=float(128 ** -0.5),
                            in1=rs[:, 0:W].rearrange("p (t k) -> p t k", k=128), op0=ALU.mult, op1=ALU.mult), r=[ck] + rkeys, w=[("qst", h)])
                        return
                    kf_, kfk = kf.next()
                    P.op('dve', lambda: nc.vector.tensor_tensor(out=kf_[:, 0:W], in0=c_[:, 0:W], in1=rs[:, 0:W], op=ALU.mult), r=[ck] + rkeys, w=[kfk])
                    P.op('pool', lambda: nc.gpsimd.tensor_copy(out=kst[:, 0:ntile, h, :], in_=kf_[:, 0:W].rearrange("p (t k) -> p t k", k=128)),
                         r=[kfk], w=[("kst", h)])
                    src = kf_; srck = kfk
                yield
                dst = ktst if kind == 1 else vtst
                dkey = "ktst" if kind == 1 else "vtst"
                for j in range(0, ntile, 4):
                    n_ = min(4, ntile - j)
                    bank = 5 + (j // 4) % 2
                    for t in range(n_):
                        P.op('pe', (lambda t=t, j=j, bank=bank: nc.tensor.transpose(
                            PS[bank][:, t * 128:(t + 1) * 128], src[:, (j + t) * 128:(j + t + 1) * 128], ident)),
                            r=[srck, "msk"], w=[PK[bank]])
                    P.op('act', (lambda j=j, n_=n_, bank=bank: nc.scalar.copy(
                        out=dst[:, j:j + n_, h, :], in_=PS[bank][:, 0:n_ * 128].rearrange("p (t k) -> p t k", k=128))),
                        r=[PK[bank]], w=[(dkey, h, j)])

            def c1_superblock(seq0, nseq, t0, t1, is_ctx):
                mod, mkey = ("C", "mvec") if is_ctx else ("L", "mvec")
                hb = max(t0 - 1, 0); he = min(t1 + 1, nseq)
                for lt in range(hb, he):
                    c1_tile_norm(seq0 + lt, lt - hb, mod, mkey)
                for lt in range(t0, t1):
                    c1_zab(seq0 + lt, lt - hb, is_ctx)
                run_interleaved(P, (c1_chunk(cc, seq0, nseq, t0, t1, hb) for cc in range(24)), 3)
                nt_ = t1 - t0
                g0 = seq0 + t0
                for (dst, st, key) in ((QT_d, qst, "qst"), (KT_d, kst, "kst")):
                    P.dma('sp', dst[g0:g0 + nt_].rearrange("t p h k -> p t h k"), st[:, 0:nt_, :, :],
                          r=[(key, h) for h in range(8)], w=["d_" + key])
                for (dst, st, key) in ((KK_d, ktst, "ktst"), (VV_d, vtst, "vtst")):
                    P.dma('sp', dst[g0:g0 + nt_].rearrange("t p h k -> p t h k"), st[:, 0:nt_, :, :],
                          r=[(key, h, j) for h in range(8) for j in range(0, nt_, 4)], w=["d_" + key])

            c1_superblock(0, NCT, 0, NCT, True)
            for t0 in range(0, NLT, SBT1):
                c1_superblock(NCT, NLT, t0, t0 + SBT1, False)
            P.barrier()
        if stop_after == "c1":
            P.finish()
            return nc

        with contextlib.ExitStack() as ph:
            HS = [128, 8, 128]
            lKT = Ring(nc, ph, "lKT", 2, HS, BF16); lQT = Ring(nc, ph, "lQT", 2, HS, BF16)
            lKK = Ring(nc, ph, "lKK", 2, HS, BF16); lVV = Ring(nc, ph, "lVV", 2, HS, BF16)
            gbl = Ring(nc, ph, "gbl", 2, [128, 32], F32)
            scr = Ring(nc, ph, "scr", 2, [128, 80], F32)
            L16 = Ring(nc, ph, "L16", 2, [16, 4, 128], F32)
            rE = Ring(nc, ph, "rE", 2, [16, 2, 8, 128], F32)
            bmask = sb("bmask", [16, 8, 128], F32, ph)
            Ei = Ring(nc, ph, "Ei", 2, HS, F32); Es = Ring(nc, ph, "Es", 2, HS, F32)
            SBm = Ring(nc, ph, "SBm", 2, HS, F32); tf = Ring(nc, ph, "tf", 2, HS, F32)
            Ak = Ring(nc, ph, "Ak", 4, HS, BF16); Bk = Ring(nc, ph, "Bk", 4, HS, BF16); Tk = Ring(nc, ph, "Tk", 4, HS, BF16)
            TTk = Ring(nc, ph, "TTk", 4, HS, BF16); A0r = Ring(nc, ph, "A0r", 2, HS, BF16); B0r = Ring(nc, ph, "B0r", 2, HS, BF16)
            Of = Ring(nc, ph, "Of", 8, HS, BF16); P1r = Ring(nc, ph, "P1r", 4, HS, BF16)
            qkT = Ring(nc, ph, "qkT", 2, HS, BF16); KG = Ring(nc, ph, "KG", 2, HS, BF16); Kd = Ring(nc, ph, "Kd", 2, HS, BF16)
            up = Ring(nc, ph, "up", 2, HS, F32); wT = Ring(nc, ph, "wT", 2, HS, BF16); vn = Ring(nc, ph, "vn", 2, HS, BF16)
            ob = Ring(nc, ph, "ob", 2, HS, F32)
            Sst = [sb("S%d" % d, HS, F32, ph) for d in range(2)]
            Sbf = [sb("Sb%d" % d, HS, BF16, ph) for d in range(2)]
            P.dma('sp', bmask[:], blockmask, r=["d_bm"], w=["bmask"])
            mskb = sb("mskb", [128, 3, 128], BF16, ph)
            P.op('dve', lambda: nc.vector.tensor_copy(out=mskb[:], in_=msk[:, 10:13, :]), r=["msk"], w=["mskb"])
            for d in range(2):
                P.op('pool', (lambda d=d: nc.gpsimd.memset(Sst[d][:], 0.0)), r=[], w=[("S", d)])
                P.op('pool', (lambda d=d: nc.gpsimd.memset(Sbf[d][:], 0.0)), r=[], w=[("Sb", d)])
            for t_ in scr.tiles:
                P.op('pool', (lambda t_=t_: nc.gpsimd.memset(t_[:], 1.0)), r=[], w=[])
            P.flush()
            P.barrier()
            ppc = [0]

            def pair():
                p = ppc[0] % 4
                ppc[0] += 1
                v = psum[:, p * 1024:(p + 1) * 1024]
                return p, v, v.rearrange("p (h k) -> p h k", h=8), [PK[2 * p], PK[2 * p + 1]]

            def bc_mid(ap2d, n=128):
                return ap2d.unsqueeze(1).to_broadcast([ap2d.shape[0], 8, ap2d.shape[1]])

            def bc_last(ap2d):
                return ap2d.unsqueeze(2).to_broadcast([ap2d.shape[0], 8, 128])

            def dn_step(d, gt, lt, need_o):
                V = nc.vector; G = nc.gpsimd; A = nc.scalar
                kt, ktk = lKT.next(); kk, kkk = lKK.next(); vv, vvk = lVV.next(); gb, gbk = gbl.next()
                P.dma('sp', kt[:], KT_d[gt], r=["d_kst"], w=[ktk])
                P.dma('sp', kk[:], KK_d[gt], r=["d_ktst"], w=[kkk])
                P.dma('sp', vv[:], VV_d[gt], r=["d_vtst"], w=[vvk])
                P.dma('sp', gb[:], GB_d[gt], r=["d_GB"], w=[gbk])
                if need_o:
                    qt, qtk = lQT.next()
                    P.dma('sp', qt[:], QT_d[gt], r=["d_qst"], w=[qtk])
                g = gb[:, 16 * d:16 * d + 8]; beta = gb[:, 16 * d + 8:16 * d + 16]
                sc, sk = scr.next()
                p, pv, pv3, pk = pair()
                P.op('pe', lambda: nc.tensor.matmul(pv[:, 0:8], msk[:, 1 + d, :], g, start=True, stop=True), r=[gbk, "msk"], w=pk)
                P.op('pe', lambda: nc.tensor.matmul(pv[:, 8:16], msk[:, 3, :], g, start=True, stop=True), r=[gbk, "msk"], w=pk)
                P.op('dve', lambda: V.tensor_copy(out=sc[:, 0:16], in_=pv[:, 0:16]), r=pk, w=[sk])
                yield
                P.op('act', lambda: A.activation(out=sc[:, 16:24], in_=sc[:, 0:8], func=AF.Exp), r=[sk], w=[sk])
                P.op('dve', lambda: V.tensor_tensor(out=sc[:, 24:32], in0=sc[:, 8:16], in1=sc[:, 0:8], op=ALU.subtract), r=[sk], w=[sk])
                P.op('act', lambda: A.activation(out=sc[:, 24:32], in_=sc[:, 24:32], func=AF.Exp), r=[sk], w=[sk])
                P.op('act', lambda: A.activation(out=sc[:, 32:40], in_=sc[:, 8:16], func=AF.Exp), r=[sk], w=[sk])
                P.op('dve', lambda: V.tensor_scalar(out=sc[:, 40:48], in0=sc[:, 0:8], scalar1=-1.0, scalar2=None, op0=ALU.mult), r=[sk], w=[sk])
                P.op('dve', lambda: V.tensor_copy(out=sc[:, 56:64], in_=sc[:, 0:8]), r=[sk], w=[sk])
                P.op('act', lambda: A.activation(out=sc[:, 72:80], in_=beta, func=AF.Ln), r=[gbk], w=[sk])
                P.op('dve', lambda: V.tensor_tensor(out=sc[:, 72:80], in0=sc[:, 72:80], in1=sc[:, 40:48], op=ALU.add), r=[sk], w=[sk])
                yield
                egc = sc[:, 16:24]; edec = sc[:, 24:32]; egl = sc[:, 32:40]
                l16, lk = L16.next(); re, rek = rE.next()
                p, pv, pv3, pk = pair()
                for q in range(4):
                    P.op('pe', (lambda q=q: nc.tensor.transpose(pv[0:16, q * 128:(q + 1) * 128], sc[:, 40 + 8 * q:56 + 8 * q], ident)),
                         r=[sk, "msk"], w=pk)
                yield
                P.op('act', lambda: A.copy(out=l16[:], in_=pv[0:16, 0:512].rearrange("p (q k) -> p q k", q=4)), r=pk, w=[lk])
                P.op('pool', lambda: G.tensor_tensor(out=re[:, 0, :, :], in0=bc_mid(l16[:, 1, :]), in1=bmask[:], op=ALU.mult), r=[lk, "bmask"], w=[(rek, 0)])
                P.op('pool', lambda: G.tensor_tensor(out=re[:, 1, :, :], in0=bc_mid(l16[:, 3, :]), in1=bmask[:], op=ALU.mult), r=[lk, "bmask"], w=[(rek, 1)])
                yield
                ei, eik = Ei.next(); es_, esk = Es.next()
                for which, (lq, dst, dk_, mi) in enumerate(((0, ei, eik, 4 + d), (2, es_, esk, 8 + d))):
                    yield
                    p, pv, pv3, pk = pair()
                    for half in range(2):
                        P.op('pe', (lambda half=half, which=which, lq=lq, pv=pv: nc.tensor.matmul(
                            pv[:, half * 512:(half + 1) * 512], l16[:, lq, :],
                            re[:, which, 4 * half:4 * half + 4, :].rearrange("p h k -> p (h k)"), start=True, stop=True)),
                            r=[lk, (rek, which)], w=pk)
                    P.op('dve', (lambda pv3=pv3, dst=dst, mi=mi: V.scalar_tensor_tensor(
                        out=dst[:], in0=pv3, scalar=0.0, in1=bc_mid(msk[:, mi, :]), op0=ALU.min, op1=ALU.add)), r=pk + ["msk"], w=[dk_])
                    P.op('act', (lambda dst=dst: A.activation(out=dst[:], in_=dst[:], func=AF.Exp)), r=[dk_], w=[dk_])
                yield
                sbm, sbk = SBm.next()
                P.op('pool', lambda: G.tensor_tensor(out=sbm[:], in0=bc_mid(msk[:, 6 + d, :]), in1=bc_last(beta), op=ALU.mult), r=["msk", gbk], w=[sbk])
                p, pv, pv3, pk = pair()
                for h in range(8):
                    P.op('pe', (lambda h=h, pv=pv: nc.tensor.matmul(pv[:, h * 128:(h + 1) * 128], kt[:, h, :], kt[:, h, :], start=True, stop=True)),
                         r=[ktk], w=pk)
                yield
                t1, t1k = tf.next()
                a0, a0k = A0r.next(); b0, b0k = B0r.next()
                P.op('dve', (lambda pv3=pv3: V.tensor_tensor(out=t1[:], in0=pv3, in1=ei[:], op=ALU.mult)), r=pk + [eik], w=[t1k])
                P.op('dve', lambda: V.tensor_tensor(out=a0[:], in0=t1[:], in1=sbm[:], op=ALU.mult), r=[t1k, sbk], w=[a0k])
                P.op('dve', (lambda pv3=pv3: V.tensor_tensor(out=b0[:], in0=pv3, in1=es_[:], op=ALU.mult)), r=pk + [esk], w=[b0k])
                if need_o:
                    qk_, qkk = qkT.next()
                    p, pv, pv3, pk = pair()
                    for h in range(8):
                        P.op('pe', (lambda h=h, pv=pv: nc.tensor.matmul(pv[:, h * 128:(h + 1) * 128], kt[:, h, :], qt[:, h, :], start=True, stop=True)),
                             r=[ktk, qtk], w=pk)
                    P.op('dve', (lambda pv3=pv3: V.tensor_tensor(out=qk_[:], in0=pv3, in1=ei[:], op=ALU.mult)), r=pk + [eik], w=[qkk])
                yield
                def mm8(lhs, rhs, keys):
                    p, pv, pv3, pk = pair()
                    for h in range(8):
                        P.op('pe', (lambda h=h, pv=pv: nc.tensor.matmul(pv[:, h * 128:(h + 1) * 128], lhs[:, h, :], rhs[:, h, :], start=True, stop=True)),
                             r=keys, w=pk)
                    return pv3, pk
                ca, cak = Ak.next(); cb, cbk = Bk.next(); cT, cTk = Tk.next(); cTT, cTTk = TTk.next()
                P.op('dve', lambda ca=ca: V.tensor_tensor(out=ca[:], in0=a0[:], in1=bc_mid(mskb[:, 0, :]), op=ALU.mult), r=[a0k, "mskb"], w=[cak])
                P.op('dve', lambda cb=cb: V.tensor_tensor(out=cb[:], in0=b0[:], in1=bc_mid(mskb[:, 0, :]), op=ALU.mult), r=[b0k, "mskb"], w=[cbk])
                P.op('dve', lambda ca=ca, cT=cT: V.tensor_tensor(out=cT[:], in0=bc_mid(identb[:]), in1=ca[:], op=ALU.subtract), r=["identb", cak], w=[cTk])
                P.op('dve', lambda cb=cb, cTT=cTT: V.tensor_tensor(out=cTT[:], in0=bc_mid(identb[:]), in1=cb[:], op=ALU.subtract), r=["identb", cbk], w=[cTTk])
                yield
                offs = []
                for mi in (11, 12):
                    of_, ofk = Of.next(); oft, oftk = Of.next()
                    P.op('dve', (lambda of_=of_, mi=mi: V.tensor_tensor(out=of_[:], in0=a0[:], in1=bc_mid(mskb[:, mi - 10, :]), op=ALU.mult)), r=[a0k, "mskb"], w=[ofk])
                    P.op('dve', (lambda oft=oft, mi=mi: V.tensor_tensor(out=oft[:], in0=b0[:], in1=bc_mid(mskb[:, mi - 10, :]), op=ALU.mult)), r=[b0k, "mskb"], w=[oftk])
                    offs.append((of_, ofk, oft, oftk))
                for lev in range(4):
                    yield
                    na, nak = Ak.next(); nb, nbk = Bk.next(); nT, nTk = Tk.next(); nTT, nTTk = TTk.next()
                    pv3, pk = mm8(cb, ca, [cak, cbk])
                    P.op('act', (lambda pv3=pv3, na=na: A.copy(out=na[:], in_=pv3)), r=pk, w=[nak])
                    pv3, pk = mm8(ca, cb, [cak, cbk])
                    P.op('dve', (lambda pv3=pv3, nb=nb: V.tensor_copy(out=nb[:], in_=pv3)), r=pk, w=[nbk])
                    yield
                    pv3, pk = mm8(nb, cT, [nbk, cTk])
                    P.op('dve', (lambda pv3=pv3, nT=nT, cT=cT: V.tensor_tensor(out=nT[:], in0=pv3, in1=cT[:], op=ALU.add)), r=pk + [cTk], w=[nTk])
                    pv3, pk = mm8(na, cTT, [nak, cTTk])
                    P.op('dve', (lambda pv3=pv3, nTT=nTT, cTT=cTT: V.tensor_tensor(out=nTT[:], in0=pv3, in1=cTT[:], op=ALU.add)), r=pk + [cTTk], w=[nTTk])
                    ca, cak, cb, cbk, cT, cTk, cTT, cTTk = na, nak, nb, nbk, nT, nTk, nTT, nTTk
                for si, (of_, ofk, oft, oftk) in enumerate(offs):
                    yield
                    p1, p1k = P1r.next()
                    pv3, pk = mm8(oft, cT, [oftk, cTk])
                    P.op('act', (lambda pv3=pv3, p1=p1: A.copy(out=p1[:], in_=pv3)), r=pk, w=[p1k])
                    yield
                    pv3, pk = mm8(cTT, p1, [cTTk, p1k])
                    nT, nTk = Tk.next()
                    P.op('dve', (lambda pv3=pv3, nT=nT, cT=cT: V.tensor_tensor(out=nT[:], in0=cT[:], in1=pv3, op=ALU.subtract)), r=pk + [cTk], w=[nTk])
                    if si == 0:
                        p1t, p1tk = P1r.next()
                        pv3, pk = mm8(of_, cTT, [ofk, cTTk])
                        P.op('act', (lambda pv3=pv3, p1t=p1t: A.copy(out=p1t[:], in_=pv3)), r=pk, w=[p1tk])
                        pv3, pk = mm8(cT, p1t, [cTk, p1tk])
                        nTT, nTTk = TTk.next()
                        P.op('dve', (lambda pv3=pv3, nTT=nTT, cTT=cTT: V.tensor_tensor(out=nTT[:], in0=cTT[:], in1=pv3, op=ALU.subtract)), r=pk + [cTTk], w=[nTTk])
                        cTT, cTTk = nTT, nTTk
                    cT, cTk = nT, nTk
                yield
                u_, uk = up.next(); w_, wk_ = wT.next(); kg, kgk = KG.next(); kd, kdk = Kd.next()
                P.op('pool', lambda: G.tensor_tensor(out=kg[:], in0=kk[:], in1=bc_last(egc), op=ALU.mult), r=[kkk, sk], w=[kgk])
                P.op('pool', lambda: G.tensor_tensor(out=kd[:], in0=kk[:], in1=bc_last(edec), op=ALU.mult), r=[kkk, sk], w=[kdk])
                p, pv, pv3, pk = pair()
                for h in range(8):
                    P.op('pe', (lambda h=h, pv=pv: nc.tensor.matmul(pv[:, h * 128:(h + 1) * 128], cT[:, h, :], vv[:, h, :], start=True, stop=True)),
                         r=[cTk, vvk], w=pk)
                P.op('act', (lambda pv3=pv3: A.copy(out=u_[:], in_=pv3)), r=pk, w=[uk])
                p, pv, pv3, pk = pair()
                for h in range(8):
                    P.op('pe', (lambda h=h, pv=pv: nc.tensor.matmul(pv[:, h * 128:(h + 1) * 128], kg[:, h, :], cT[:, h, :], start=True, stop=True)),
                         r=[cTk, kgk], w=pk)
                P.op('act', (lambda pv3=pv3: A.copy(out=w_[:], in_=pv3)), r=pk, w=[wk_])
                yield
                S = Sst[d]; Sb = Sbf[d]; Sk = ("S", d); Sbk = ("Sb", d)
                vn_, vnk = vn.next(); t2, t2k = tf.next()
                p, pv, pv3, pk = pair()
                for h in range(8):
                    P.op('pe', (lambda h=h, pv=pv: nc.tensor.matmul(pv[:, h * 128:(h + 1) * 128], w_[:, h, :], Sb[:, h, :], start=True, stop=True)),
                         r=[wk_, Sbk], w=pk)
                yield
                P.op('dve', (lambda pv3=pv3: V.tensor_tensor(out=t2[:], in0=u_[:], in1=pv3, op=ALU.subtract)), r=pk + [uk], w=[t2k])
                P.op('dve', lambda: V.tensor_tensor(out=vn_[:], in0=t2[:], in1=bc_last(beta), op=ALU.mult), r=[t2k, gbk], w=[vnk])
                if need_o:
                    o_, ok_ = ob.next()
                    p, pv, pv3, pk = pair()
                    for h in range(8):
                        P.op('pe', (lambda h=h, pv=pv: nc.tensor.matmul(pv[:, h * 128:(h + 1) * 128], qt[:, h, :], Sb[:, h, :], start=True, stop=True)),
                             r=[qtk, Sbk], w=pk)
                    P.op('dve', (lambda pv3=pv3: V.tensor_tensor(out=o_[:], in0=pv3, in1=bc_last(egc), op=ALU.mult)), r=pk + [sk], w=[ok_])
                    p, pv, pv3, pk = pair()
                    for h in range(8):
                        P.op('pe', (lambda h=h, pv=pv: nc.tensor.matmul(pv[:, h * 128:(h + 1) * 128], qk_[:, h, :], vn_[:, h, :], start=True, stop=True)),
                             r=[qkk, vnk], w=pk)
                    P.op('dve', (lambda pv3=pv3: V.tensor_tensor(out=o_[:], in0=o_[:], in1=pv3, op=ALU.add)), r=pk + [ok_], w=[ok_])
                    P.dma('sp', OF_d[d, lt], o_[:].rearrange("p h k -> p (h k)"), r=[ok_], w=["d_OF"])
                yield
                p, pv, pv3, pk = pair()
                for h in range(8):
                    P.op('pe', (lambda h=h, pv=pv: nc.tensor.matmul(pv[:, h * 128:(h + 1) * 128], kd[:, h, :], vn_[:, h, :], start=True, stop=True)),
                         r=[kdk, vnk], w=pk)
                P.op('dve', lambda: V.tensor_tensor(out=S[:], in0=S[:], in1=bc_last(egl), op=ALU.mult), r=[Sk, sk], w=[Sk])
                P.op('dve', (lambda pv3=pv3: V.tensor_tensor(out=S[:], in0=S[:], in1=pv3, op=ALU.add)), r=pk + [Sk], w=[Sk])
                P.op('act', lambda: A.copy(out=Sb[:], in_=S[:]), r=[Sk], w=[Sbk])
                import os
                if os.environ.get("DN_DBG") and d == int(os.environ["DN_DBG"]) and gt == int(os.environ.get("DN_DBG_GT", "0")):
                    dump("dbg_sc", sc[:], [sk]); dump("dbg_Ei", ei[:], [eik]); dump("dbg_Es", es_[:], [esk])
                    dump("dbg_a0", a0[:], [a0k]); dump("dbg_b0", b0[:], [b0k]); dump("dbg_T", cT[:], [cTk])
                    dump("dbg_up", u_[:], [uk]); dump("dbg_wT", w_[:], [wk_]); dump("dbg_vn", vn_[:], [vnk]); dump("dbg_S", S[:], [Sk])
                    dump("dbg_l16", l16[:], [lk])
                    if need_o:
                        dump("dbg_qk", qk_[:], [qkk]); dump("dbg_o", o_[:], [ok_])

            order_f = [(t, t, False) for t in range(NCT)] + [(NCT + t, t, True) for t in range(NLT)]
            order_b = [(t, t, False) for t in reversed(range(NCT))] + [(NCT + t, t, True) for t in reversed(range(NLT))]
            import os
            nstep = int(os.environ.get("DN_STEPS", str(NT)))
            for i in range(nstep):
                g0 = dn_step(0, *order_f[i])
                g1 = dn_step(1, *order_b[i])
                run_interleaved(P, [g0, g1], 2)
            P.barrier()
        if stop_after == "c2":
            P.finish()
            return nc

        with contextlib.ExitStack() as ph:
            R = mk_rings(ph)
            load_mods(ph, [("L", 2)])
            of0 = Ring(nc, ph, "of0", 2, [128, D], F32); of1 = Ring(nc, ph, "of1", 2, [128, D], F32)
            sqt = Ring(nc, ph, "sqt", 2, [128, D], F32)
            zl = Ring(nc, ph, "zl", 2, [128, D], BF16)
            ms = Ring(nc, ph, "ms", 2, [128, 8], F32)
            onT = Ring(nc, ph, "onT", 2, [128, 8, 128], BF16)
            yb = Ring(nc, ph, "yb", 2, [128, D], F32)
            wo = sb("wo", [128, 8, D], BF16, ph)
            dnw = sb("dnw", [128, 128], F32, ph)
            P.dma('pool', wo[:], w_dn_out.rearrange("(k p) n -> p k n", p=128), r=["d_wo"], w=["wo"])
            P.dma('sp', dnw[:], dn_norm.partition_broadcast(128), r=["d_dnn"], w=["dnw"])

            def c3_tile(lt):
                gt = NCT + lt
                V = nc.vector; G = nc.gpsimd; A = nc.scalar
                a0_, a0k = of0.next(); a1_, a1k = of1.next(); z_, zk = zl.next(); m_, mk_ = ms.next(); sq, sqk = sqt.next()
                P.dma('sp', a0_[:], OF_d[0, lt], r=["d_OF"], w=[a0k])
                P.dma('sp', a1_[:], OF_d[1, lt], r=["d_OF"], w=[a1k])
                P.dma('sp', z_[:], ZZ_d[gt], r=["d_ZZ"], w=[zk])
                P.op('pool', lambda: G.tensor_tensor(out=a0_[:], in0=a0_[:], in1=a1_[:], op=ALU.add), r=[a0k, a1k], w=[a0k])
                yield
                P.op('act', lambda: A.activation(out=sq[:], in_=a0_[:], func=AF.Square), r=[a0k], w=[sqk])
                P.op('dve', lambda: V.reduce_sum(out=m_[:], in_=sq[:].rearrange("p (h k) -> p h k", h=8), axis=AX.X), r=[sqk], w=[mk_])
                P.op('dve', lambda: V.tensor_scalar(out=m_[:], in0=m_[:], scalar1=1.0 / 128, scalar2=EPS, op0=ALU.mult, op1=ALU.add), r=[mk_], w=[mk_])
                yield
                P.op('act', lambda: A.activation(out=m_[:], in_=m_[:], func=AF.Sqrt), r=[mk_], w=[mk_])
                yield
                P.op('dve', lambda: V.reciprocal(out=m_[:], in_=m_[:]), r=[mk_], w=[mk_])
                o3 = a0_[:].rearrange("p (h k) -> p h k", h=8)
                P.op('dve', lambda: V.tensor_tensor(out=o3, in0=o3, in1=m_[:].unsqueeze(2).to_broadcast([128, 8, 128]), op=ALU.mult), r=[a0k, mk_], w=[a0k])
                P.op('pool', lambda: G.tensor_tensor(out=o3, in0=o3, in1=dnw[:].unsqueeze(1).to_broadcast([128, 8, 128]), op=ALU.mult), r=[a0k, "dnw"], w=[a0k])
                P.op('pool', lambda: G.tensor_tensor(out=a0_[:], in0=a0_[:], in1=z_[:], op=ALU.mult), r=[a0k, zk], w=[a0k])
                yield
                for k in range(8):
                    b = (6 if lt % 2 == 0 else 2) + k // 4
                    P.op('pe', (lambda b=b, k=k: nc.tensor.transpose(PS[b][:, (k % 4) * 128:(k % 4) * 128 + 128], a0_[:, k * 128:(k + 1) * 128], ident)),
                         r=[a0k, "msk"], w=[PK[b]])
                t_, tk = onT.next()
                yield
                for h in range(2):
                    b = (6 if lt % 2 == 0 else 2) + h
                    P.op('act', (lambda b=b, h=h: A.copy(out=t_[:, 4 * h:4 * h + 4, :], in_=PS[b].rearrange("p (k t) -> p k t", k=4))),
                         r=[PK[b]], w=[(tk, h)])
                y_, yk = yb.next()
                xt, xk = R['xs'].next()
                P.dma('sp', xt[:], XS2_t[gt], r=["d_dst0"], w=[xk])
                yield
                for half in range(2):
                    b = (4 if lt % 2 == 0 else 0) + half
                    for k in range(8):
                        P.op('pe', (lambda b=b, k=k, half=half: nc.tensor.matmul(PS[b], t_[:, k, :], wo[:, k, half * 512:(half + 1) * 512],
                                                                             start=(k == 0), stop=(k == 7))), r=[(tk, 0), (tk, 1), "wo"], w=[PK[b]])
                    sl = slice(half * 512, (half + 1) * 512)
                    P.op('dve', (lambda b=b, sl=sl: V.tensor_tensor(out=y_[:, sl], in0=PS[b], in1=mv("L", 2)[:, sl], op=ALU.mult)),
                         r=[PK[b], "mvec"], w=[(yk, half)])
                    P.op('pool', (lambda sl=sl: G.tensor_tensor(out=y_[:, sl], in0=y_[:, sl], in1=xt[:, sl], op=ALU.add)), r=[(yk, half), xk], w=[(yk, half)])
                P.dma('sp', XS3_t[gt], y_[:], r=[(yk, 0), (yk, 1)], w=["d_XS3"])

            run_interleaved(P, (c3_tile(lt) for lt in range(NLT)), 2)
            P.barrier()
        if stop_after == "c3":
            P.finish()
            return nc
        MOE(1, XS3_t, out_t, list(range(NCT, NT)), True)
        P.finish()
    return nc


_CACHE = {}


def _core_inputs(b, inp, consts):
    m = {}
    m['xin'] = np.ascontiguousarray(np.concatenate([inp['ctx'][b], inp['x'][b]], 0), dtype=np.float32)
    cc = np.stack([inp['c'][b], inp['c_ctx']], 0)
    m['ccol'] = np.ascontiguousarray(cc.reshape(2, 8, 128).transpose(2, 0, 1), dtype=np.float32)
    for k in ('w_ada', 'b_ada', 'norm_mix', 'norm_ffn', 'w_e_gate', 'w_e_up', 'w_e_down', 'norm_final'):
        m[k] = np.ascontiguousarray(inp[k], dtype=np.float32)
    m['w_pool'] = np.ascontiguousarray(inp['w_pool'][0]); m['b_pool'] = np.ascontiguousarray(inp['b_pool'][0])
    m['pool_scale'] = np.ascontiguousarray(inp['pool_scale'][0])
    m['w_dn_in'] = np.ascontiguousarray(inp['w_dn_in'][0])
    m['wconv'] = np.ascontiguousarray(inp['w_dn_conv'][0].reshape(4, 24, 128).transpose(2, 1, 0))
    m['dn_a_log'] = np.ascontiguousarray(inp['dn_a_log'][0].reshape(16))
    m['dn_dt_bias'] = np.ascontiguousarray(inp['dn_dt_bias'][0].reshape(16))
    m['dn_norm'] = np.ascontiguousarray(inp['dn_norm'][0]); m['w_dn_out'] = np.ascontiguousarray(inp['w_dn_out'][0])
    m['w_r'] = np.ascontiguousarray(np.concatenate([inp['w_rg'], inp['w_re']], -1))
    m['b_r'] = np.ascontiguousarray(np.concatenate([inp['b_rg'], inp['b_re']], -1))
    m.update(consts)
    return m


def kernel(**inputs):
    inp = {k: np.asarray(v) for k, v in inputs.items()}
    if 'nc' not in _CACHE:
        _CACHE['nc'] = build()
        _CACHE['consts'] = host_constants()
    nc = _CACHE['nc']
    consts = _CACHE['consts']
    B = inp['x'].shape[0]
    in_maps = [_core_inputs(b, inp, consts) for b in range(B)]
    res = run_bass_kernel_spmd(nc, in_maps, core_ids=list(range(B)))
    out = np.stack([np.asarray(res.results[b]['out']).reshape(NLT * 128, D) for b in range(B)], 0)
    return out.astype(inp['x'].dtype)
```

```python
import os
from concourse.bass_utils import run_bass_kernel_spmd
import numpy as np, contextlib
import concourse.bass as bass
import concourse.mybir as mybir

F32 = mybir.dt.float32
BF16 = mybir.dt.bfloat16
AF = mybir.ActivationFunctionType
ALU = mybir.AluOpType
AX = mybir.AxisListType


class Prog:
    NS = 16

    def __init__(self, nc, es):
        self.nc = nc
        self.E = {'pe': nc.tensor, 'act': nc.scalar, 'dve': nc.vector, 'pool': nc.gpsimd, 'sp': nc.sync}
        self.sem = {e: es.enter_context(nc.semaphore("s_" + e)) for e in ('pe', 'act', 'dve', 'pool')}
        self.dsem = {q: [es.enter_context(nc.semaphore("d_%s%d" % (q, i))) for i in range(self.NS)]
                     for q in ('sp', 'act', 'pool')}
        self.sigcount = {e: 0 for e in self.sem}
        self.dmacount = {q: 0 for q in self.dsem}
        self.waited = {}
        self.ops = []
        self.last_w = {}
        self.readers = {}
        self.n_inst = 0

    def op(self, eng, fn, r=(), w=(), dma=False):
        self.ops.append((eng, fn, tuple(r), tuple(w), dma))

    def dma(self, q, out, in_, r, w, **kw):
        e = self.E[q]
        self.op(q, lambda: e.dma_start(out=out, in_=in_, **kw), r, w, dma=True)

    @staticmethod
    def _needs_wait(oj_eng, oj_dma, oi_eng, oi_dma, typ):
        if oj_dma:
            return True
        if oj_eng == oi_eng and not oi_dma:
            if oi_eng == 'pe':
                return False
            return typ == 'raw'
        return True

    def flush(self):
        ops = self.ops
        n = len(ops)
        deps = [None] * n
        last_w, readers = self.last_w, self.readers
        for i, (eng, fn, r, w, dma) in enumerate(ops):
            d = {}
            for k in r:
                t = last_w.get(k)
                if t is not None:
                    d[t] = 'raw'
                if isinstance(k, tuple) and k and k[0] == 'ps':
                    for e2, t in readers.get(k, {}).items():
                        if e2 != eng and t not in d:
                            d[t] = 'raw'
            for k in w:
                t = last_w.get(k)
                if t is not None and t not in d:
                    d[t] = 'waw'
                for t in readers.get(k, {}).values():
                    if t not in d:
                        d[t] = 'war'
            d.pop(('p', i), None)
            deps[i] = d
            me = ('p', i)
            for k in r:
                rk = readers.setdefault(k, {})
                if dma:
                    rk[('d', i)] = me
                else:
                    rk[eng] = me
            for k in w:
                last_w[k] = me
                readers[k] = {}
        need_sig = [False] * n
        for i, (eng, fn, r, w, dma) in enumerate(ops):
            for t, typ in deps[i].items():
                if t[0] == 'p':
                    j = t[1]
                    ej, _, _, _, dj = ops[j]
                    if not dj and self._needs_wait(ej, dj, eng, dma, typ):
                        need_sig[j] = True
        last_of = {}
        for i, (eng, fn, r, w, dma) in enumerate(ops):
            if not dma:
                last_of[eng] = i
        for e, i in last_of.items():
            need_sig[i] = True
        resolved = [None] * n
        for i, (eng, fn, r, w, dma) in enumerate(ops):
            E = self.E[eng]
            waits = {}
            for t, typ in deps[i].items():
                if t[0] == 'p':
                    t2 = resolved[t[1]]
                    ej, dj = ops[t[1]][0], ops[t[1]][4]
                else:
                    t2 = t
                    ej, dj = t[1], t[0] == 'd'
                if not self._needs_wait(ej, dj, eng, dma, typ):
                    continue
                if t2[0] == 'c':
                    key = ('c', t2[1]); val = t2[2]
                else:
                    key = ('d', t2[1], t2[2]); val = t2[3]
                if waits.get(key, 0) < val:
                    waits[key] = val
            if dma:
                k = self.dmacount[eng]
                slot = k % self.NS
                if k >= self.NS:
                    key = ('d', eng, slot); val = 16 * (k // self.NS)
                    if waits.get(key, 0) < val:
                        waits[key] = val
            for key, val in waits.items():
                wk = (eng, key)
                if self.waited.get(wk, 0) >= val:
                    continue
                self.waited[wk] = val
                s = self.sem[key[1]] if key[0] == 'c' else self.dsem[key[1]][key[2]]
                E.wait_ge(s, val)
                self.n_inst += 1
            inst = fn()
            self.n_inst += 1
            if dma:
                k = self.dmacount[eng]
                slot = k % self.NS
                val = 16 * (k // self.NS + 1)
                inst.then_inc(self.dsem[eng][slot], 16)
                self.dmacount[eng] = k + 1
                resolved[i] = ('d', eng, slot, val)
            else:
                if need_sig[i]:
                    self.sigcount[eng] += 1
                    inst.then_inc(self.sem[eng], 1)
                    resolved[i] = ('c', eng, self.sigcount[eng])
        nxt = {}
        for i in range(n - 1, -1, -1):
            eng, dma = ops[i][0], ops[i][4]
            if dma:
                continue
            if resolved[i] is not None:
                nxt[eng] = resolved[i]
            else:
                resolved[i] = nxt[eng]
        for k in list(last_w.keys()):
            t = last_w[k]
            if t[0] == 'p':
                last_w[k] = resolved[t[1]]
        for k in list(readers.keys()):
            rk = readers[k]
            for kk in list(rk.keys()):
                t = rk[kk]
                if t[0] == 'p':
                    rk[kk] = resolved[t[1]]
        self.ops = []

    def barrier(self):
        self.flush()
        for eng in ('pe', 'act', 'dve', 'pool', 'sp'):
            E = self.E[eng]
            for e, c in self.sigcount.items():
                if c > 0 and e != eng and self.waited.get((eng, ('c', e)), 0) < c:
                    E.wait_ge(self.sem[e], c)
                    self.waited[(eng, ('c', e))] = c
            for q, k in self.dmacount.items():
                for slot in range(self.NS):
                    if k > slot:
                        val = 16 * ((k - 1 - slot) // self.NS + 1)
                        if self.waited.get((eng, ('d', q, slot)), 0) < val:
                            E.wait_ge(self.dsem[q][slot], val)
                            self.waited[(eng, ('d', q, slot))] = val

    def finish(self):
        self.flush()
        sp = self.E['sp']
        for e, c in self.sigcount.items():
            if c > 0:
                sp.wait_ge(self.sem[e], c)
        for q, k in self.dmacount.items():
            for slot in range(self.NS):
                if k > slot:
                    cnt = (k - 1 - slot) // self.NS + 1
                    sp.wait_ge(self.dsem[q][slot], 16 * cnt)


D = 1024
NCT = 2
NLT = 64
NT = NCT + NLT
EPS = 1e-6
POOL_WINDOWS = (2, 4, 8, 16)
JREL = {0: list(range(-1, 4)), 1: list(range(-1, 5)), 2: list(range(-2, 6)), 3: list(range(-4, 8))}
BAND_IDX = {}
_i = 0
for _g in range(4):
    for _j in JREL[_g]:
        BAND_IDX[(_g, _j)] = _i
        _i += 1
NBAND = _i


def host_constants():
    c = {}
    def axis_w(n, win):
        t = np.arange(n)
        lo = np.maximum(t - win // 2, 0); hi = np.minimum(t + win // 2, n)
        W = np.zeros((n, n), np.float64)
        for o in range(n):
            W[lo[o]:hi[o], o] = 1.0 / (hi[o] - lo[o])
        return W
    band = np.zeros((3, 128, NBAND, 512), np.float32)
    for g, win in enumerate(POOL_WINDOWS):
        Wr = axis_w(128, win); Wc = axis_w(64, win)
        for ti, b in enumerate((0, 7, 15)):
            for j in JREL[g]:
                jt = 4 * b + j
                if jt < 0 or jt >= 64:
                    continue
                M = np.einsum('ab,cd->acbd', Wr[2 * jt:2 * jt + 2, 8 * b:8 * b + 8], Wc).reshape(128, 512)
                if 0 <= j < 4:
                    M[:, j * 128:(j + 1) * 128] -= np.eye(128)
                band[ti, :, BAND_IDX[(g, j)], :] = M
    c['band'] = band
    cb = np.zeros((128, 8, 256), np.float32)
    for g, win in enumerate(POOL_WINDOWS):
        W = axis_w(256, win) - np.eye(256)
        for j in range(2):
            cb[:, g * 2 + j, :] = W[j * 128:(j + 1) * 128, :]
    c['cband'] = cb
    idx = np.arange(128)
    m = np.zeros((128, 13, 128), np.float32)
    m[:, 0] = np.eye(128)
    m[:, 1] = (idx[:, None] <= idx[None, :])
    m[:, 2] = (idx[:, None] >= idx[None, :])
    m[:, 3] = 1.0
    m[:, 4] = np.where(idx[:, None] <= idx[None, :], 0.0, -30000.0)
    m[:, 5] = np.where(idx[:, None] >= idx[None, :], 0.0, -30000.0)
    m[:, 6] = (idx[:, None] < idx[None, :])
    m[:, 7] = (idx[:, None] > idx[None, :])
    m[:, 8] = np.where(idx[:, None] > idx[None, :], 0.0, -30000.0)
    m[:, 9] = np.where(idx[:, None] < idx[None, :], 0.0, -30000.0)
    bd32 = (idx[:, None] // 32 == idx[None, :] // 32); bd64 = (idx[:, None] // 64 == idx[None, :] // 64)
    m[:, 10] = bd32; m[:, 11] = bd64 & ~bd32; m[:, 12] = ~bd64
    c['masks'] = m
    bm = np.zeros((16, 8, 128), np.float32)
    for r in range(16):
        bm[r, r % 8, :] = 1.0
    c['blockmask'] = bm
    cm = np.zeros((128, 160), np.float32)
    cm[:, 0:32] = np.arange(32)[None, :]
    cm[:, 32:40] = np.arange(8)[None, :] * 128 + np.arange(128)[:, None]
    cm[:, 40:120] = np.arange(80)[None, :] * 512
    c['cmisc'] = cm
    return c


_UNIQ = [0]


def run_interleaved(P, gens, width, rare=True):
    active = []
    rounds = 0
    it = iter(gens)
    while True:
        while len(active) < width:
            try:
                active.append(next(it))
            except StopIteration:
                break
        if not active:
            break
        for g in list(active):
            try:
                next(g)
            except StopIteration:
                active.remove(g)
        rounds += 1
        if not rare or rounds % 200 == 0:
            P.flush()
    P.flush()


class Ring:
    def __init__(self, nc, es, name, n, shape, dtype):
        _UNIQ[0] += 1
        self.tiles = [es.enter_context(nc.sbuf_tensor("%s%d_u%d" % (name, i, _UNIQ[0]), shape, dtype)) for i in range(n)]
        self.name = name
        self.i = 0

    def next(self):
        k = self.i % len(self.tiles)
        self.i += 1
        return self.tiles[k], "%s%d" % (self.name, k)


def build(stop_after=None, debug=False):
    nc = bass.Bass("TRN2", target_bir_lowering=False)
    es = contextlib.ExitStack()

    def din(name, shape, dt=F32):
        return nc.dram_tensor(name, list(shape), dt, kind="ExternalInput").ap()

    def dscr(name, shape, dt=F32):
        kind = "ExternalOutput" if debug else "Internal"
        return nc.dram_tensor(name, list(shape), dt, kind=kind).ap()

    xin = din("xin", [NT * 128, D])
    ccol = din("ccol", [128, 2, 8])
    w_ada = din("w_ada", [2, D, 6 * D]); b_ada = din("b_ada", [2, 6 * D])
    norm_mix = din("norm_mix", [2, D]); norm_ffn = din("norm_ffn", [2, D])
    w_pool = din("w_pool", [4, 256, 256]); b_pool = din("b_pool", [D]); pool_scale = din("pool_scale", [D])
    w_dn_in = din("w_dn_in", [D, 4128]); wconv = din("wconv", [128, 24, 4])
    dn_a_log = din("dn_a_log", [16]); dn_dt_bias = din("dn_dt_bias", [16]); dn_norm = din("dn_norm", [128])
    w_dn_out = din("w_dn_out", [D, D])
    w_r = din("w_r", [2, D, 36]); b_r = din("b_r", [2, 36])
    w_e_gate = din("w_e_gate", [2, 32, D, 512]); w_e_up = din("w_e_up", [2, 32, D, 512])
    w_e_down = din("w_e_down", [2, 32, 512, D]); norm_final = din("norm_final", [D])
    band = din("band", [3, 128, NBAND, 512]); cband = din("cband", [128, 8, 256])
    masks = din("masks", [128, 13, 128]); blockmask = din("blockmask", [16, 8, 128])
    cmisc = din("cmisc", [128, 160])
    out = nc.dram_tensor("out", [NLT * 128, D], F32, kind="ExternalOutput").ap()
    XS1 = dscr("XS1", [NT * 128, D])
    XS2 = dscr("XS2", [NT * 128, D])
    XS3 = dscr("XS3", [NT * 128, D])

    with es:
        P = Prog(nc, es)
        psum = es.enter_context(nc.psum_tensor("psum", [128, 4096], F32))
        PS = [psum[:, b * 512:(b + 1) * 512] for b in range(8)]
        PK = [("ps", b) for b in range(8)]

        def sb(name, shape, dt=F32, stack=es):
            _UNIQ[0] += 1
            return stack.enter_context(nc.sbuf_tensor("%s_u%d" % (name, _UNIQ[0]), list(shape), dt))

        msk = sb("msk", [128, 13, 128])
        P.dma('sp', msk[:], masks, r=["d_masks"], w=["msk"])
        ident = msk[:, 0, :]
        identb = sb("identb", [128, 128], BF16)
        P.op('dve', lambda: nc.vector.tensor_copy(out=identb[:], in_=msk[:, 0, :]), r=["msk"], w=["identb"])
        MODS_d = dscr("MODS_d", [2, 128, 6 * D])
        MVEC = {}

        def load_mods(ph, need):
            buf = sb("mvec", [128, len(need), D], F32, ph)
            MVEC.clear()
            for j, (st, ix) in enumerate(need):
                P.dma('sp', buf[:, j, :], MODS_d[0 if st == 'L' else 1][:, ix * D:(ix + 1) * D], r=["d_mods"], w=["mvec"])
                MVEC[(st, ix)] = buf[:, j, :]
        csb = sb("csb", [128, 2, 8]); sil = sb("sil", [128, 2, 8])
        rep = sb("rep", [128, 2, 8, 128], BF16)
        P.dma('sp', csb[:], ccol, r=["d_ccol"], w=["csb"])
        P.op('act', lambda: nc.scalar.activation(out=sil[:], in_=csb[:], func=AF.Silu), r=["csb"], w=["sil"])
        P.op('dve', lambda: nc.vector.tensor_copy(out=rep[:], in_=sil[:].unsqueeze(3).to_broadcast([128, 2, 8, 128])),
             r=["sil"], w=["rep"])

        def adaln(layer, ph):
            wr = Ring(nc, ph, "adaw", 2, [128, 8, 512], BF16)
            nw = sb("nw", [128, 2, D], F32, ph)
            modL = sb("modL", [128, 6 * D], F32, ph); modC = sb("modC", [128, 6 * D], F32, ph)
            P.dma('sp', modL[:], b_ada[layer].partition_broadcast(128), r=["d_b_ada"], w=["modL"])
            P.dma('sp', modC[:], b_ada[layer].partition_broadcast(128), r=["d_b_ada"], w=["modC"])
            P.dma('sp', nw[:, 0, :], norm_mix[layer].partition_broadcast(128), r=["d_nm"], w=["nw"])
            P.dma('sp', nw[:, 1, :], norm_ffn[layer].partition_broadcast(128), r=["d_nm"], w=["nw"])
            wv = w_ada[layer].rearrange("(k p) n -> p k n", p=128)
            for blk in range(12):
                wt, wk = wr.next()
                P.dma('pool', wt[:], wv[:, :, blk * 512:(blk + 1) * 512], r=["d_w_ada"], w=[wk])
                for s, (mod, mk) in enumerate(((modL, "modL"), (modC, "modC"))):
                    b = (blk * 2 + s) % 8
                    for k in range(8):
                        P.op('pe', (lambda b=b, s=s, k=k, wt=wt: nc.tensor.matmul(
                            PS[b], rep[:, s, k, :], wt[:, k, :], start=(k == 0), stop=(k == 7))),
                            r=["rep", wk], w=[PK[b]])
                    sl = slice(blk * 512, (blk + 1) * 512)
                    P.op('dve', (lambda b=b, mod=mod, sl=sl: nc.vector.tensor_tensor(
                        out=mod[:, sl], in0=PS[b], in1=mod[:, sl], op=ALU.add)), r=[PK[b], mk], w=[mk])
            for s, (mod, mk) in enumerate(((modL, "modL"), (modC, "modC"))):
                for j, col in enumerate((1, 4)):
                    sl = slice(col * D, (col + 1) * D)
                    P.op('dve', (lambda mod=mod, sl=sl, j=j: nc.vector.scalar_tensor_tensor(
                        out=mod[:, sl], in0=mod[:, sl], scalar=1.0, in1=nw[:, j, :], op0=ALU.add, op1=ALU.mult)),
                        r=[mk, "nw"], w=[mk])
            P.dma('sp', MODS_d[0], modL[:], r=["modL"], w=["d_mods"])
            P.dma('sp', MODS_d[1], modC[:], r=["modC"], w=["d_mods"])

        def mv(mod, i):
            return MVEC[(mod, i)]

        def rms_mod(ph_rings, xs_ap, xs_key, A_ap, sh_ap, mod_keys, out_ap, out_key, eps_scale=1.0 / D):
            ss, sk = ph_rings['ss'].next()
            tmp, tk = ph_rings['hxtmp'].next()
            P.op('act', lambda: nc.scalar.activation(out=tmp[:], in_=xs_ap, func=AF.Square), r=[xs_key], w=[tk])
            P.op('dve', lambda: nc.vector.reduce_sum(out=ss[:, 0:1], in_=tmp[:], axis=AX.X), r=[tk], w=[sk])
            P.op('dve', lambda: nc.vector.tensor_scalar(out=ss[:, 1:2], in0=ss[:, 0:1], scalar1=eps_scale, scalar2=EPS,
                                                        op0=ALU.mult, op1=ALU.add), r=[sk], w=[sk])
            P.op('act', lambda: nc.scalar.activation(out=ss[:, 2:3], in_=ss[:, 1:2], func=AF.Sqrt), r=[sk], w=[sk])
            P.op('dve', lambda: nc.vector.reciprocal(out=ss[:, 3:4], in_=ss[:, 2:3]), r=[sk], w=[sk])
            P.op('dve', lambda: nc.vector.scalar_tensor_tensor(out=tmp[:], in0=xs_ap, scalar=ss[:, 3:4], in1=A_ap,
                                                               op0=ALU.mult, op1=ALU.mult),
                 r=[xs_key, sk] + mod_keys, w=[tk])
            if sh_ap is None:
                P.op('pool', lambda: nc.gpsimd.tensor_copy(out=out_ap, in_=tmp[:]), r=[tk], w=[out_key])
            else:
                P.op('pool', lambda: nc.gpsimd.tensor_tensor(out=out_ap, in0=tmp[:], in1=sh_ap, op=ALU.add),
                     r=[tk] + mod_keys, w=[out_key])

        def mk_rings(ph):
            return {'ss': Ring(nc, ph, "ss", 4, [128, 4], F32),
                    'hxtmp': Ring(nc, ph, "hxtmp", 2, [128, D], F32),
                    'xs': Ring(nc, ph, "xsr", 2, [128, D], F32)}

        xin_t = xin.rearrange("(t p) d -> t p d", p=128)
        XS1_t = XS1.rearrange("(t p) d -> t p d", p=128)
        XS2_t = XS2.rearrange("(t p) d -> t p d", p=128)
        XS3_t = XS3.rearrange("(t p) d -> t p d", p=128)
        out_t = out.rearrange("(t p) d -> t p d", p=128)

        with contextlib.ExitStack() as ph:
            adaln(0, ph)
            P.barrier()
        def dump(name, ap, keys):
            shape = list(ap.shape)
            t = nc.dram_tensor(name, shape, ap.dtype, kind="ExternalOutput").ap()
            P.dma('sp', t, ap, r=keys, w=["dbg_" + name])
        if stop_after == "ada":
            dump("dbg_modL", modL[:], ["modL"]); dump("dbg_modC", modC[:], ["modC"]); dump("dbg_rep", rep[:], ["rep"])
            P.finish()
            return nc
        with contextlib.ExitStack() as ph:
            R = mk_rings(ph)
            hx0 = sb("hx0", [128, 24, D], BF16, ph)
            bandsb = sb("bandsb", [128, NBAND, 512], BF16, ph)
            cbandsb = sb("cbandsb", [128, 8, 256], BF16, ph)
            wpl = sb("wpl", [128, 8, 256], BF16, ph)
            vecs = sb("vecs", [128, 2, D], F32, ph)
            AB = sb("AB", [128, 4, D], F32, ph)
            dT = Ring(nc, ph, "dT", 1, [128, 8, 512], BF16)
            yt = Ring(nc, ph, "yt", 2, [128, D], F32)
            P.dma('pool', cbandsb[:], cband, r=["d_cband"], w=["cbandsb"])
            P.dma('pool', wpl[:], w_pool.rearrange("g (c p) e -> p (g c) e", p=128), r=["d_wpool"], w=["wpl"])
            P.dma('sp', vecs[:, 0, :], pool_scale.partition_broadcast(128), r=["d_ps"], w=["vecs"])
            P.dma('sp', vecs[:, 1, :], b_pool.partition_broadcast(128), r=["d_bp"], w=["vecs"])
            load_mods(ph, [(st, ix) for st in "LC" for ix in (0, 1, 2)])
            for s, (mod, mkey) in enumerate((("L", "mvec"), ("C", "mvec"))):
                P.op('dve', (lambda s=s, mod=mod: nc.vector.tensor_tensor(out=AB[:, 2 * s, :], in0=mv(mod, 2), in1=vecs[:, 0, :],
                                                                          op=ALU.mult)), r=[mkey, "vecs"], w=["AB"])
                P.op('dve', (lambda s=s: nc.vector.tensor_tensor(out=AB[:, 2 * s + 1, :], in0=AB[:, 2 * s, :], in1=vecs[:, 1, :],
                                                                 op=ALU.mult)), r=["AB", "vecs"], w=["AB"])

            def pool_segment(is_ctx, in_tiles, base, out_blocks):
                mod, mkey = ("C", "mvec") if is_ctx else ("L", "mvec")
                seq0 = 0 if is_ctx else NCT
                for lt in in_tiles:
                    xt, xk = R['xs'].next()
                    P.dma('sp', xt[:], xin_t[seq0 + lt], r=["d_xin"], w=[xk])
                    rms_mod(R, xt[:], xk, mv(mod, 1), mv(mod, 0), [mkey], hx0[:, lt - base, :], ("hx0", lt - base))
                cur_band = [None]
                for (ot0, ntl, btype) in out_blocks:
                    ncol = ntl * 128
                    if not is_ctx and cur_band[0] != btype:
                        P.dma('pool', bandsb[:], band[btype], r=["d_band"], w=["bandsb"])
                        cur_band[0] = btype
                    dt_, dk = dT.next()
                    for g in range(4):
                        for cc in range(2):
                            b = 2 * g + cc
                            if is_ctx:
                                lst = [(j, cbandsb[:, g * 2 + j, 0:ncol]) for j in range(2)]
                                bk = "cbandsb"
                            else:
                                lst = []
                                for j in JREL[g]:
                                    jt = ot0 + j
                                    if jt < 0 or jt >= NLT:
                                        continue
                                    lst.append((jt, bandsb[:, BAND_IDX[(g, j)], 0:ncol]))
                                bk = "bandsb"
                            for n_, (jt, rhs) in enumerate(lst):
                                P.op('pe', (lambda b=b, jt=jt, rhs=rhs, n_=n_, L=len(lst), ch=b, ncol=ncol: nc.tensor.matmul(
                                    PS[b][:, 0:ncol], hx0[:, jt - base, ch * 128:(ch + 1) * 128], rhs,
                                    start=(n_ == 0), stop=(n_ == L - 1))),
                                    r=[("hx0", jt - base), bk], w=[PK[b]])
                            if b % 2 == 0:
                                P.op('act', (lambda b=b, ncol=ncol, dt_=dt_: nc.scalar.copy(out=dt_[:, b, 0:ncol], in_=PS[b][:, 0:ncol])),
                                     r=[PK[b]], w=[(dk, b)])
                            else:
                                P.op('dve', (lambda b=b, ncol=ncol, dt_=dt_: nc.vector.tensor_copy(out=dt_[:, b, 0:ncol], in_=PS[b][:, 0:ncol])),
                                     r=[PK[b]], w=[(dk, b)])
                    for t in range(ntl):
                        gt = seq0 + ot0 + t
                        b0 = 2 * (t % 4)
                        for g in range(4):
                            pb = b0 + g // 2
                            for cc in range(2):
                                P.op('pe', (lambda pb=pb, g=g, cc=cc, t=t, dt_=dt_: nc.tensor.matmul(
                                    PS[pb][:, (g % 2) * 256:(g % 2) * 256 + 256], dt_[:, 2 * g + cc, t * 128:(t + 1) * 128],
                                    wpl[:, 2 * g + cc, :], start=(cc == 0), stop=(cc == 1))),
                                    r=[(dk, 2 * g + cc), "wpl"], w=[PK[pb]])
                        xt, xk = R['xs'].next()
                        P.dma('sp', xt[:], xin_t[gt], r=["d_xin"], w=[xk])
                        ai = 2 if is_ctx else 0
                        y, yk = yt.next()
                        for h in range(2):
                            sl = slice(h * 512, (h + 1) * 512)
                            P.op('dve', (lambda y=y, sl=sl, h=h, b0=b0, ai=ai: nc.vector.tensor_tensor(
                                out=y[:, sl], in0=PS[b0 + h], in1=AB[:, ai, sl], op=ALU.mult)), r=[PK[b0 + h], "AB"], w=[(yk, h)])
                            P.op('pool', (lambda y=y, sl=sl, xt=xt: nc.gpsimd.tensor_tensor(
                                out=y[:, sl], in0=y[:, sl], in1=xt[:, sl], op=ALU.add)), r=[(yk, h), xk], w=[(yk, h)])
                            P.op('pool', (lambda y=y, sl=sl, ai=ai: nc.gpsimd.tensor_tensor(
                                out=y[:, sl], in0=y[:, sl], in1=AB[:, ai + 1, sl], op=ALU.add)), r=[(yk, h), "AB"], w=[(yk, h)])
                        P.dma('sp', XS1_t[gt], y[:], r=[(yk, 0), (yk, 1)], w=["d_XS1"])

            pool_segment(True, [0, 1], 0, [(0, 2, 0)])
            if stop_after == "pool_dbg":
                dump("dbg_hx0", hx0[:, 0:2, :], [("hx0", 0), ("hx0", 1)])
                dump("dbg_dT", dT.tiles[0][:], [("dT0", b) for b in range(8)])
                dump("dbg_AB", AB[:], ["AB"])
                dump("dbg_cb", cbandsb[:], ["cbandsb"])
                dump("dbg_wpl", wpl[:], ["wpl"])
                P.finish()
                return nc
            for seg in range(4):
                lo = max(0, 16 * seg - 4); hi = min(NLT, 16 * seg + 20)
                blocks = [(4 * b, 4, 0 if b == 0 else (2 if b == 15 else 1)) for b in range(4 * seg, 4 * seg + 4)]
                pool_segment(False, list(range(lo, hi)), lo, blocks)
            P.flush()
        if stop_after == "pool":
            P.finish()
            return nc
        P.barrier()

        def moe_phase(layer, src_t, dst_t, tiles, final):
            SBT = 8 if final else 10
            with contextlib.ExitStack() as ph:
                R = mk_rings(ph)
                load_mods(ph, [(st, ix) for st in ("LC" if not final else "L") for ix in (3, 4, 5)])
                hxf = Ring(nc, ph, "hxf", 2, [128, D], F32)
                hxTf = Ring(nc, ph, "hxTf", 1, [128, 8, 128], F32)
                hxTb = sb("hxTb", [128, 8, SBT * 128], BF16, ph)
                acc = sb("acc", [128, SBT, D], F32, ph)
                gates = sb("gates", [128, SBT, 32], F32, ph)
                wrs = sb("wrs", [128, 8, 36], F32, ph)
                brb = sb("brb", [128, 36], F32, ph)
                rt = Ring(nc, ph, "rt", 2, [128, 96], F32)
                wg = Ring(nc, ph, "wg", 2, [128, 8, 512], BF16)
                wu = Ring(nc, ph, "wu", 2, [128, 8, 512], BF16)
                wd = Ring(nc, ph, "wd", 2, [128, 4, D], BF16)
                sgb = Ring(nc, ph, "sgb", 1, [128, 512], BF16)
                hidT = Ring(nc, ph, "hidT", 2, [128, 4, 512], BF16)
                nfb = None
                if final:
                    nfb = sb("nfb", [128, D], F32, ph)
                    P.dma('sp', nfb[:], norm_final.partition_broadcast(128), r=["d_nf"], w=["nfb"])
                P.dma('sp', wrs[:], w_r[layer].rearrange("(k p) n -> p k n", p=128), r=["d_wr"], w=["wrs"])
                P.dma('sp', brb[:], b_r[layer].partition_broadcast(128), r=["d_br"], w=["brb"])
                def route(i, r_, rk, lvl, sparse_info=None):
                    lg = r_[:, 0:36]; m4 = r_[:, 36:37]; nm4 = r_[:, 37:38]; e4 = r_[:, 40:44]; s4 = r_[:, 38:39]
                    pg = r_[:, 39:40]; ohg = r_[:, 44:48]; sel = r_[:, 48:56]; m8 = r_[:, 56:64]; d21 = r_[:, 64:65]
                    e21 = r_[:, 65:66]; w1 = r_[:, 66:67]; w2 = r_[:, 67:68]; c1 = r_[:, 72:80]; c2 = r_[:, 80:88]
                    V = nc.vector
                    def dv(fn, rk=rk):
                        P.op('dve', fn, r=[rk], w=[rk])
                    P.op('dve', lambda lg=lg: V.tensor_tensor(out=lg, in0=PS[5][:, 0:36], in1=brb[:], op=ALU.add),
                         r=[PK[5], "brb"], w=[rk])
                    if lvl <= 2:
                        P.op('dve', (lambda i=i, lg=lg: V.tensor_copy(out=gates[:, i, :], in_=lg[:, 0:32])), r=[rk], w=[("gates", i)])
                        return
                    dv(lambda: V.reduce_max(out=m4, in_=lg[:, 0:4], axis=AX.X))
                    dv(lambda: V.tensor_scalar(out=nm4, in0=m4, scalar1=-1.0, scalar2=None, op0=ALU.mult))
                    P.op('act', lambda: nc.scalar.activation(out=e4, in_=lg[:, 0:4], func=AF.Exp, bias=nm4, scale=1.0),
                         r=[rk], w=[rk])
                    dv(lambda: V.reduce_sum(out=s4, in_=e4, axis=AX.X))
                    dv(lambda: V.reciprocal(out=pg, in_=s4))
                    dv(lambda: V.tensor_scalar(out=ohg, in0=lg[:, 0:4], scalar1=m4, scalar2=None, op0=ALU.is_equal))
                    dv(lambda: V.tensor_scalar(out=sel, in0=lg[:, 4:12], scalar1=ohg[:, 0:1], scalar2=None, op0=ALU.mult))
                    for g in range(1, 4):
                        dv(lambda g=g: V.scalar_tensor_tensor(out=sel, in0=lg[:, 4 + 8 * g:12 + 8 * g], scalar=ohg[:, g:g + 1],
                                                              in1=sel, op0=ALU.mult, op1=ALU.add))
                    dv(lambda: V.max(out=m8, in_=sel))
                    dv(lambda: V.tensor_tensor(out=d21, in0=m8[:, 1:2], in1=m8[:, 0:1], op=ALU.subtract))
                    P.op('act', lambda: nc.scalar.activation(out=e21, in_=d21, func=AF.Exp), r=[rk], w=[rk])
                    dv(lambda: V.tensor_scalar(out=e21, in0=e21, scalar1=1.0, scalar2=None, op0=ALU.add))
                    dv(lambda: V.reciprocal(out=w1, in_=e21))
                    dv(lambda: V.tensor_tensor(out=w1, in0=w1, in1=pg, op=ALU.mult))
                    dv(lambda: V.tensor_tensor(out=w2, in0=pg, in1=w1, op=ALU.subtract))
                    if sparse_info is not None:
                        sparse_info(r_, rk, sel, m8, ohg, w1, w2, dv)
                        return
                    dv(lambda: V.tensor_scalar(out=c1, in0=sel, scalar1=m8[:, 0:1], scalar2=w1, op0=ALU.is_equal, op1=ALU.mult))
                    dv(lambda: V.tensor_scalar(out=c2, in0=sel, scalar1=m8[:, 1:2], scalar2=w2, op0=ALU.is_equal, op1=ALU.mult))
                    dv(lambda: V.tensor_tensor(out=c1, in0=c1, in1=c2, op=ALU.add))
                    P.op('dve', (lambda i=i, c1=c1, ohg=ohg: V.tensor_tensor(
                        out=gates[:, i, :].rearrange("p (g e) -> p g e", g=4),
                        in0=c1.unsqueeze(1).to_broadcast([128, 4, 8]), in1=ohg.unsqueeze(2).to_broadcast([128, 4, 8]),
                        op=ALU.mult)), r=[rk], w=[("gates", i)])
                for s0 in range(0, len(tiles), SBT):
                    sbt = tiles[s0:s0 + SBT]
                    import os
                    if os.environ.get("MOE_NT"):
                        sbt = sbt[:int(os.environ["MOE_NT"])]
                    n_sb = len(sbt)
                    for i, gt in enumerate(sbt):
                        mod, mkey = ("C", "mvec") if gt < NCT else ("L", "mvec")
                        xt, xk = R['xs'].next()
                        P.dma('sp', xt[:], src_t[gt], r=["d_XS1" if layer == 0 else "d_XS3"], w=[xk])
                        hx, hk = hxf.next()
                        rms_mod(R, xt[:], xk, mv(mod, 4), mv(mod, 3), [mkey], hx[:], hk)
                        import os
                        if int(os.environ.get("MOE_LVL", "9")) == 0:
                            dump("dbg_hx%d" % i, hx[:], [hk])
                            continue
                        for k in range(8):
                            b = 6 + k // 4
                            P.op('pe', (lambda b=b, k=k, hx=hx: nc.tensor.transpose(
                                PS[b][:, (k % 4) * 128:(k % 4) * 128 + 128], hx[:, k * 128:(k + 1) * 128], ident)),
                                r=[hk, "msk"], w=[PK[b]])
                        hT, hTk = hxTf.next()
                        for h in range(2):
                            b = 6 + h
                            P.op('act', (lambda b=b, h=h, hT=hT: nc.scalar.copy(
                                out=hT[:, 4 * h:4 * h + 4, :], in_=PS[b].rearrange("p (k t) -> p k t", k=4))),
                                r=[PK[b]], w=[hTk])
                        P.op('pool', (lambda i=i, hT=hT: nc.gpsimd.tensor_copy(out=hxTb[:, :, i * 128:(i + 1) * 128], in_=hT[:])),
                             r=[hTk], w=[("hxTb", i)])
                        import os
                        lvl = int(os.environ.get("MOE_LVL", "9"))
                        if lvl <= 1:
                            continue
                        for k in range(8):
                            P.op('pe', (lambda k=k, hT=hT: nc.tensor.matmul(PS[5][:, 0:36], hT[:, k, :], wrs[:, k, :],
                                                                          start=(k == 0), stop=(k == 7))),
                                 r=[hTk, "wrs"], w=[PK[5]])
                        r_, rk = rt.next()
                        route(i, r_, rk, lvl)
                    if stop_after == "moe_b1":
                        lvl = int(os.environ.get("MOE_LVL", "9"))
                        if lvl == 0:
                            P.finish()
                            return "STOP"
                        if lvl > 1:
                            dump("dbg_rt0", rt.tiles[0][:], [rt.name + "0"]); dump("dbg_rt1", rt.tiles[1][:], [rt.name + "1"])
                            dump("dbg_gates", gates[:], [("gates", i) for i in range(n_sb)])
                        if os.environ.get("NO_HXTB") is None:
                            dump("dbg_hxTb", hxTb[:], [("hxTb", i) for i in range(n_sb)])
                        else:
                            dump("dbg_hxTf", hxTf.tiles[0][:], [hxTf.name + "0"])
                        P.finish()
                        return "STOP"
                    blocks = [(t0, min(4, n_sb - t0)) for t0 in range(0, n_sb, 4)]
                    hcn = 0
                    yn = 0
                    for e in range(32):
                        wgt, wgk = wg.next(); wut, wuk = wu.next(); wdt, wdk = wd.next()
                        P.dma('pool', wgt[:], w_e_gate[layer, e].rearrange("(k p) n -> p k n", p=128), r=["d_weg"], w=[wgk])
                        P.dma('pool', wut[:], w_e_up[layer, e].rearrange("(k p) n -> p k n", p=128), r=["d_weu"], w=[wuk])
                        P.dma('pool', wdt[:], w_e_down[layer, e].rearrange("(k p) n -> p k n", p=128), r=["d_wed"], w=[wdk])
                        for (t0, ntl) in blocks:
                            ncol = ntl * 128
                            cs = slice(t0 * 128, t0 * 128 + ncol)
                            rk_h = [("hxTb", t0 + j) for j in range(ntl)]
                            hid, hidk = hidT.next()
                            for hc in range(4):
                                gb = (hcn % 2) * 2; ub = gb + 1; hcn += 1
                                for k in range(8):
                                    P.op('pe', (lambda gb=gb, k=k, hc=hc, wgt=wgt, cs=cs, ncol=ncol: nc.tensor.matmul(
                                        PS[gb][:, 0:ncol], wgt[:, k, hc * 128:(hc + 1) * 128], hxTb[:, k, cs],
                                        start=(k == 0), stop=(k == 7))), r=[wgk] + rk_h, w=[PK[gb]])
                                for k in range(8):
                                    P.op('pe', (lambda ub=ub, k=k, hc=hc, wut=wut, cs=cs, ncol=ncol: nc.tensor.matmul(
                                        PS[ub][:, 0:ncol], wut[:, k, hc * 128:(hc + 1) * 128], hxTb[:, k, cs],
                                        start=(k == 0), stop=(k == 7))), r=[wuk] + rk_h, w=[PK[ub]])
                                sg, sgk = sgb.next()
                                P.op('act', (lambda sg=sg, gb=gb, ncol=ncol: nc.scalar.activation(
                                    out=sg[:, 0:ncol], in_=PS[gb][:, 0:ncol], func=AF.Silu)), r=[PK[gb]], w=[sgk])
                                P.op('dve', (lambda sg=sg, ub=ub, ncol=ncol, hid=hid, hc=hc: nc.vector.tensor_tensor(
                                    out=hid[:, hc, 0:ncol], in0=sg[:, 0:ncol], in1=PS[ub][:, 0:ncol], op=ALU.mult)),
                                    r=[sgk, PK[ub]], w=[(hidk, hc)])
                            for t in range(ntl):
                                i = t0 + t
                                for half in range(2):
                                    yb = 4 + yn % 2; yn += 1
                                    for hc in range(4):
                                        P.op('pe', (lambda yb=yb, hc=hc, t=t, half=half, hid=hid, wdt=wdt: nc.tensor.matmul(
                                            PS[yb], hid[:, hc, t * 128:(t + 1) * 128], wdt[:, hc, half * 512:(half + 1) * 512],
                                            start=(hc == 0), stop=(hc == 3))), r=[(hidk, hc), wdk], w=[PK[yb]])
                                    sl = slice(half * 512, (half + 1) * 512)
                                    if e == 0:
                                        P.op('dve', (lambda yb=yb, i=i, sl=sl, e=e: nc.vector.tensor_scalar(
                                            out=acc[:, i, sl], in0=PS[yb], scalar1=gates[:, i, e:e + 1], scalar2=None, op0=ALU.mult)),
                                            r=[PK[yb], ("gates", i)], w=[("acc", i, half)])
                                    else:
                                        P.op('dve', (lambda yb=yb, i=i, sl=sl, e=e: nc.vector.scalar_tensor_tensor(
                                            out=acc[:, i, sl], in0=PS[yb], scalar=gates[:, i, e:e + 1], in1=acc[:, i, sl],
                                            op0=ALU.mult, op1=ALU.add)), r=[PK[yb], ("gates", i), ("acc", i, half)], w=[("acc", i, half)])
                    for i, gt in enumerate(sbt):
                        mod, mkey = ("C", "mvec") if gt < NCT else ("L", "mvec")
                        xt, xk = R['xs'].next()
                        P.dma('sp', xt[:], src_t[gt], r=["d_XS1" if layer == 0 else "d_XS3"], w=[xk])
                        P.op('pool', (lambda i=i, mod=mod: nc.gpsimd.tensor_tensor(out=acc[:, i, :], in0=acc[:, i, :], in1=mv(mod, 5), op=ALU.mult)),
                             r=[("acc", i, 0), ("acc", i, 1), mkey], w=[("acc", i, 0), ("acc", i, 1)])
                        P.op('pool', (lambda i=i, xt=xt: nc.gpsimd.tensor_tensor(out=acc[:, i, :], in0=acc[:, i, :], in1=xt[:], op=ALU.add)),
                             r=[("acc", i, 0), ("acc", i, 1), xk], w=[("acc", i, 0), ("acc", i, 1)])
                        if not final:
                            P.dma('sp', dst_t[gt], acc[:, i, :], r=[("acc", i, 0), ("acc", i, 1)], w=["d_dst%d" % layer])
                        else:
                            o_, ok = hxf.next()
                            rms_mod(R, acc[:, i, :], ("acc", i, 0), nfb[:], None, ["nfb", ("acc", i, 1)], o_[:], ok)
                            P.dma('sp', dst_t[gt - NCT], o_[:], r=[ok], w=["d_out"])
                P.barrier()

        I32 = mybir.dt.int32
        NTS = 65
        NSLOT = NTS * 512
        HXB = dscr("HXB", [NT * 128, D], BF16); XG = dscr("XG", [NSLOT, D], BF16)
        WG = dscr("WG", [NSLOT, 1]); YG = dscr("YG", [NSLOT, D])
        HXB_t = HXB.rearrange("(t p) d -> t p d", p=128)
        W2 = {"g": w_e_gate.rearrange("l e (p k) n -> (l e p) (k n)", k=8), "u": w_e_up.rearrange("l e (p k) n -> (l e p) (k n)", k=8),
              "d": w_e_down.rearrange("l e d n -> (l e d) n")}

        def moe_sparse(layer, src_t, dst_t, tiles, final):
            T_ = len(tiles)
            skey = "d_XS1" if layer == 0 else "d_XS3"
            es2 = contextlib.ExitStack()
            with es2:
                cms = sb("cms", [128, 160], F32, es2)
                info = sb("info", [128, NT, 8], F32, es2)
                posi = sb("posi", [128, NT, 2], I32, es2)
                cum = sb("cum", [128, 32], F32, es2)
                offs = sb("offs", [128, 32], F32, es2)
                te = sb("te", [128, 80], F32, es2)
                tec = sb("tec", [128, 80], F32, es2)
                P.dma('sp', cms[:], cmisc, r=["d_cm"], w=["cms"])
                P.op('pool', lambda: nc.gpsimd.memset(cum[:], 0.0), r=[], w=["cum"])
                iota = cms[:, 0:32]; base = cms[:, 32:40]; svals = cms[:, 40:120]
                V = nc.vector; G = nc.gpsimd; A = nc.scalar
                with contextlib.ExitStack() as ph:
                    R = mk_rings(ph)
                    load_mods(ph, [(st, ix) for st in ("LC" if not final else "L") for ix in (3, 4)])
                    hxf = Ring(nc, ph, "hxf", 2, [128, D], F32)
                    hxb = Ring(nc, ph, "hxb", 2, [128, D], BF16)
                    hxTf = Ring(nc, ph, "hxTf", 2, [128, 8, 128], F32)
                    wrs = sb("wrs", [128, 8, 36], F32, ph); brb = sb("brb", [128, 36], F32, ph)
                    rt = Ring(nc, ph, "rt", 2, [128, 288], F32)
                    gates = None
                    P.dma('sp', wrs[:], w_r[layer].rearrange("(k p) n -> p k n", p=128), r=["d_wr"], w=["wrs"])
                    P.dma('sp', brb[:], b_r[layer].partition_broadcast(128), r=["d_br"], w=["brb"])

                    def route(i, r_, rk):
                        lg = r_[:, 0:36]; m4 = r_[:, 36:37]; nm4 = r_[:, 37:38]; e4 = r_[:, 40:44]; s4 = r_[:, 38:39]
                        pg = r_[:, 39:40]; ohg = r_[:, 44:48]; sel = r_[:, 48:56]; m8 = r_[:, 56:64]; d21 = r_[:, 64:65]
                        e21 = r_[:, 65:66]; w1 = r_[:, 66:67]; w2 = r_[:, 67:68]; eq = r_[:, 72:88]
                        oh1 = r_[:, 96:128]; oh2 = r_[:, 128:160]; ohs = r_[:, 160:192]; rkt = r_[:, 192:224]; tmp = r_[:, 224:256]

                        def dv(fn):
                            P.op('dve', fn, r=[rk], w=[rk])
                        P.op('dve', lambda: V.tensor_tensor(out=lg, in0=PS[5 - 4 * (i % 2)][:, 0:36], in1=brb[:], op=ALU.add), r=[PK[5 - 4 * (i % 2)], "brb"], w=[rk])
                        dv(lambda: V.reduce_max(out=m4, in_=lg[:, 0:4], axis=AX.X))
                        dv(lambda: V.tensor_scalar(out=nm4, in0=m4, scalar1=-1.0, scalar2=None, op0=ALU.mult))
                        yield
                        P.op('act', lambda: A.activation(out=e4, in_=lg[:, 0:4], func=AF.Exp, bias=nm4, scale=1.0), r=[rk], w=[rk])
                        yield
                        dv(lambda: V.reduce_sum(out=s4, in_=e4, axis=AX.X))
                        dv(lambda: V.reciprocal(out=pg, in_=s4))
                        dv(lambda: V.tensor_scalar(out=ohg, in0=lg[:, 0:4], scalar1=m4, scalar2=None, op0=ALU.is_equal))
                        dv(lambda: V.tensor_scalar(out=sel, in0=lg[:, 4:12], scalar1=ohg[:, 0:1], scalar2=None, op0=ALU.mult))
                        for g in range(1, 4):
                            dv(lambda g=g: V.scalar_tensor_tensor(out=sel, in0=lg[:, 4 + 8 * g:12 + 8 * g], scalar=ohg[:, g:g + 1],
                                                                  in1=sel, op0=ALU.mult, op1=ALU.add))
                        yield
                        dv(lambda: V.max(out=m8, in_=sel))
                        dv(lambda: V.tensor_tensor(out=d21, in0=m8[:, 1:2], in1=m8[:, 0:1], op=ALU.subtract))
                        yield
                        P.op('act', lambda: A.activation(out=e21, in_=d21, func=AF.Exp), r=[rk], w=[rk])
                        yield
                        dv(lambda: V.tensor_scalar(out=e21, in0=e21, scalar1=1.0, scalar2=None, op0=ALU.add))
                        dv(lambda: V.reciprocal(out=w1, in_=e21))
                        dv(lambda: V.tensor_tensor(out=info[:, i, 2:3], in0=w1, in1=pg, op=ALU.mult))
                        dv(lambda: V.tensor_tensor(out=info[:, i, 5:6], in0=pg, in1=info[:, i, 2:3], op=ALU.subtract))
                        dv(lambda: V.tensor_scalar(out=eq[:, 0:8], in0=sel, scalar1=m8[:, 0:1], scalar2=None, op0=ALU.is_equal))
                        dv(lambda: V.tensor_scalar(out=eq[:, 8:16], in0=sel, scalar1=m8[:, 1:2], scalar2=None, op0=ALU.is_equal))
                        for j, oh in enumerate((oh1, oh2)):
                            dv(lambda j=j, oh=oh: V.tensor_tensor(out=oh.rearrange("p (g e) -> p g e", g=4),
                                                                  in0=eq[:, 8 * j:8 * j + 8].unsqueeze(1).to_broadcast([128, 4, 8]),
                                                                  in1=ohg.unsqueeze(2).to_broadcast([128, 4, 8]), op=ALU.mult))
                        dv(lambda: V.tensor_tensor(out=ohs, in0=oh1, in1=oh2, op=ALU.add))
                        yield
                        P.op('pe', lambda: nc.tensor.matmul(PS[4 - 4 * (i % 2)][:, 0:32], msk[:, 6, :], ohs, start=True, stop=True), r=[rk, "msk"], w=[PK[4 - 4 * (i % 2)]])
                        P.op('pe', lambda: nc.tensor.matmul(PS[4 - 4 * (i % 2)][:, 32:64], msk[:, 3, :], ohs, start=True, stop=True), r=[rk, "msk"], w=[PK[4 - 4 * (i % 2)]])
                        yield
                        P.op('dve', lambda: V.tensor_tensor(out=rkt, in0=PS[4 - 4 * (i % 2)][:, 0:32], in1=cum[:], op=ALU.add), r=[PK[4 - 4 * (i % 2)], "cum", rk], w=[rk])
                        P.op('dve', lambda: V.tensor_tensor(out=cum[:], in0=cum[:], in1=PS[4 - 4 * (i % 2)][:, 32:64], op=ALU.add), r=[PK[4 - 4 * (i % 2)], "cum", rk], w=["cum"])
                        for j, oh in enumerate((oh1, oh2)):
                            dv(lambda oh=oh: V.tensor_tensor(out=tmp, in0=oh, in1=rkt, op=ALU.mult))
                            dv(lambda j=j: V.reduce_sum(out=info[:, i, 3 * j + 1:3 * j + 2], in_=tmp, axis=AX.X))
                            dv(lambda oh=oh: V.tensor_tensor(out=tmp, in0=oh, in1=iota, op=ALU.mult))
                            dv(lambda j=j: V.reduce_sum(out=info[:, i, 3 * j:3 * j + 1], in_=tmp, axis=AX.X))

                    def sa_tile(i, gt):
                        mod, mkey = ("C", "mvec") if gt < NCT else ("L", "mvec")
                        xt, xk = R['xs'].next()
                        P.dma('sp', xt[:], src_t[gt], r=[skey], w=[xk])
                        hx, hk = hxf.next()
                        rms_mod(R, xt[:], xk, mv(mod, 4), mv(mod, 3), [mkey], hx[:], hk)
                        yield
                        hb, hbk = hxb.next()
                        P.op('pool', (lambda hb=hb, hx=hx: G.tensor_copy(out=hb[:], in_=hx[:])), r=[hk], w=[hbk])
                        P.dma('sp', HXB_t[gt], hb[:], r=[hbk], w=["d_HXB"])
                        for k in range(8):
                            b = (6 if i % 2 == 0 else 2) + k // 4
                            P.op('pe', (lambda b=b, k=k, hx=hx: nc.tensor.transpose(
                                PS[b][:, (k % 4) * 128:(k % 4) * 128 + 128], hx[:, k * 128:(k + 1) * 128], ident)),
                                r=[hk, "msk"], w=[PK[b]])
                        yield
                        hT, hTk = hxTf.next()
                        for h in range(2):
                            b = (6 if i % 2 == 0 else 2) + h
                            P.op('act', (lambda b=b, h=h, hT=hT: A.copy(
                                out=hT[:, 4 * h:4 * h + 4, :], in_=PS[b].rearrange("p (k t) -> p k t", k=4))), r=[PK[b]], w=[hTk])
                        for k in range(8):
                            P.op('pe', (lambda k=k, hT=hT: nc.tensor.matmul(PS[5 - 4 * (i % 2)][:, 0:36], hT[:, k, :], wrs[:, k, :],
                                                                          start=(k == 0), stop=(k == 7))), r=[hTk, "wrs"], w=[PK[5 - 4 * (i % 2)]])
                        yield
                        r_, rk = rt.next()
                        yield from route(i, r_, rk)

                    run_interleaved(P, (sa_tile(i, gt) for i, gt in enumerate(tiles)), 2)
                    ci = sb("ci", [128, 32], I32, ph); pn = sb("pn", [128, 32], F32, ph)
                    sa = sb("sa", [128, 32], F32, ph); sb_ = sb("sb_", [128, 32], F32, ph)
                    P.op('dve', lambda: V.tensor_copy(out=ci[:], in_=cum[:]), r=["cum"], w=["ci"])
                    P.op('dve', lambda: V.tensor_scalar(out=ci[:], in0=ci[:], scalar1=511, scalar2=None, op0=ALU.add), r=["ci"], w=["ci"])
                    P.op('dve', lambda: V.tensor_scalar(out=ci[:], in0=ci[:], scalar1=9, scalar2=None, op0=ALU.arith_shift_right), r=["ci"], w=["ci"])
                    P.op('dve', lambda: V.tensor_scalar(out=ci[:], in0=ci[:], scalar1=9, scalar2=None, op0=ALU.logical_shift_left), r=["ci"], w=["ci"])
                    P.op('dve', lambda: V.tensor_copy(out=pn[:], in_=ci[:]), r=["ci"], w=["pn"])
                    P.op('dve', lambda: V.tensor_copy(out=sa[:], in_=pn[:]), r=["pn"], w=["sa"])
                    a_, b_ = sa, sb_
                    ak, bk = "sa", "sb_"
                    for sh in (1, 2, 4, 8, 16):
                        P.op('dve', (lambda a_=a_, b_=b_, sh=sh: V.tensor_copy(out=b_[:, 0:sh], in_=a_[:, 0:sh])), r=[ak], w=[bk])
                        P.op('dve', (lambda a_=a_, b_=b_, sh=sh: V.tensor_tensor(out=b_[:, sh:32], in0=a_[:, sh:32], in1=a_[:, 0:32 - sh], op=ALU.add)),
                             r=[ak, bk], w=[bk])
                        a_, b_, ak, bk = b_, a_, bk, ak
                    incl, inclk = a_, ak
                    P.op('dve', lambda: V.tensor_tensor(out=offs[:], in0=incl[:], in1=pn[:], op=ALU.subtract), r=[inclk, "pn"], w=["offs"])
                    P.op('pool', lambda: G.memset(te[:], 0.0), r=[], w=["te"])
                    for e in range(32):
                        P.op('dve', (lambda e=e: V.scalar_tensor_tensor(out=te[:], in0=svals, scalar=incl[:, e:e + 1], in1=te[:], op0=ALU.is_ge, op1=ALU.add)),
                             r=[inclk, "te", "cms"], w=["te"])
                    P.op('dve', lambda: V.tensor_scalar(out=tec[:], in0=te[:], scalar1=31.0, scalar2=None, op0=ALU.min), r=["te"], w=["tec"])
                    P.barrier()
                if stop_after == "sA":
                    P.finish(); return "STOP"
                with contextlib.ExitStack() as ph:
                    zt = sb("zt", [128, 4096], BF16, ph)
                    hb2 = Ring(nc, ph, "hb2", 3, [128, D], BF16)
                    pt = Ring(nc, ph, "pt", 2, [128, 72], F32)
                    P.op('pool', lambda: G.memset(zt[:], 0.0), r=[], w=["zt"])
                    XGz = XG.rearrange("(q p f) d -> q p (f d)", p=128, f=4)
                    for q in range(NTS):
                        P.dma('sp', XGz[q], zt[:], r=["zt"], w=["d_XG"])
                    for i, gt in enumerate(tiles):
                        p_, pk_ = pt.next()
                        for j in range(2):
                            P.op('dve', (lambda p_=p_, i=i, j=j: V.tensor_scalar(out=p_[:, 0:32], in0=iota, scalar1=info[:, i, 3 * j:3 * j + 1], scalar2=None, op0=ALU.is_equal)),
                                 r=["info", "cms", pk_], w=[pk_])
                            P.op('dve', (lambda p_=p_: V.tensor_tensor(out=p_[:, 0:32], in0=p_[:, 0:32], in1=offs[:], op=ALU.mult)), r=[pk_, "offs"], w=[pk_])
                            P.op('dve', (lambda p_=p_, j=j: V.reduce_sum(out=p_[:, 32 + j:33 + j], in_=p_[:, 0:32], axis=AX.X)), r=[pk_], w=[pk_])
                            P.op('dve', (lambda p_=p_, i=i, j=j: V.tensor_tensor(out=p_[:, 32 + j:33 + j], in0=p_[:, 32 + j:33 + j], in1=info[:, i, 3 * j + 1:3 * j + 2], op=ALU.add)),
                                 r=[pk_, "info"], w=[pk_])
                        P.op('dve', (lambda p_=p_, i=i: V.tensor_copy(out=posi[:, i, :], in_=p_[:, 32:34])), r=[pk_], w=[("posi", i)])
                        hb, hbk = hb2.next()
                        P.dma('sp', hb[:], HXB_t[gt], r=["d_HXB"], w=[hbk])
                        for j in range(2):
                            P.op('pool', (lambda hb=hb, i=i, j=j: G.indirect_dma_start(
                                out=XG[:, :], out_offset=bass.IndirectOffsetOnAxis(ap=posi[:, i, j:j + 1], axis=0), in_=hb[:, :], in_offset=None)),
                                r=[hbk, ("posi", i), "d_XG"], w=["d_XGs"], dma=True)
                            P.op('pool', (lambda i=i, j=j: G.indirect_dma_start(
                                out=WG[:, :], out_offset=bass.IndirectOffsetOnAxis(ap=posi[:, i, j:j + 1], axis=0), in_=info[:, i, 3 * j + 2:3 * j + 3], in_offset=None)),
                                r=["info", ("posi", i)], w=["d_WG"], dma=True)
                        P.flush()
                    P.barrier()
                if stop_after == "sC":
                    P.finish(); return "STOP"
                with contextlib.ExitStack() as ph:
                    wg = Ring(nc, ph, "wg", 2, [128, 8, 512], BF16); wu = Ring(nc, ph, "wu", 2, [128, 8, 512], BF16)
                    wd = Ring(nc, ph, "wd", 2, [128, 4, D], BF16)
                    xg = Ring(nc, ph, "xg", 2, [128, 4, D], BF16); xT = Ring(nc, ph, "xT", 2, [128, 8, 512], BF16)
                    wgt = Ring(nc, ph, "wgt", 2, [128, 4], F32)
                    idf = Ring(nc, ph, "idf", 2, [128, 12], F32); idi = Ring(nc, ph, "idi", 2, [128, 12], I32)
                    sgb = Ring(nc, ph, "sgb", 2, [128, 512], BF16); hidT = Ring(nc, ph, "hidT", 2, [128, 4, 512], BF16)
                    yg = Ring(nc, ph, "yg", 2, [128, D], F32)
                    hcn = [0]; yn = [0]

                    def slot_tile(s_):
                        f_, fk = idf.next(); ii, ik = idi.next()
                        P.op('dve', lambda: V.scalar_tensor_tensor(out=f_[:, 0:1], in0=tec[:, s_:s_ + 1], scalar=128.0, in1=base[:, 0:1],
                                                                   op0=ALU.mult, op1=ALU.add), r=["tec", "cms"], w=[fk])
                        P.op('dve', lambda: V.scalar_tensor_tensor(out=f_[:, 8:12], in0=tec[:, s_:s_ + 1].to_broadcast([128, 4]), scalar=512.0, in1=base[:, 0:4],
                                                                   op0=ALU.mult, op1=ALU.add), r=["tec", "cms", fk], w=[fk])
                        if layer > 0:
                            P.op('dve', lambda: V.tensor_scalar(out=f_[:, 0:1], in0=f_[:, 0:1], scalar1=float(layer * 32 * 128), scalar2=None, op0=ALU.add), r=[fk], w=[fk])
                            P.op('dve', lambda: V.tensor_scalar(out=f_[:, 8:12], in0=f_[:, 8:12], scalar1=float(layer * 32 * 512), scalar2=None, op0=ALU.add), r=[fk], w=[fk])
                        P.op('dve', lambda: V.tensor_copy(out=ii[:], in_=f_[:]), r=[fk], w=[ik])
                        wgt_, wgk = wg.next(); wut, wuk = wu.next(); wdt, wdk = wd.next()
                        P.op('pool', lambda: G.indirect_dma_start(out=wgt_[:].rearrange("p k n -> p (k n)"), out_offset=None, in_=W2["g"],
                                                                  in_offset=bass.IndirectOffsetOnAxis(ap=ii[:, 0:1], axis=0)),
                             r=[ik], w=[(wgk, k) for k in range(8)], dma=True)
                        P.op('pool', lambda: G.indirect_dma_start(out=wut[:].rearrange("p k n -> p (k n)"), out_offset=None, in_=W2["u"],
                                                                  in_offset=bass.IndirectOffsetOnAxis(ap=ii[:, 0:1], axis=0)),
                             r=[ik], w=[(wuk, k) for k in range(8)], dma=True)
                        for k in range(4):
                            P.op('pool', (lambda k=k: G.indirect_dma_start(out=wdt[:, k, :], out_offset=None, in_=W2["d"],
                                                                         in_offset=bass.IndirectOffsetOnAxis(ap=ii[:, 8 + k:9 + k], axis=0))),
                                 r=[ik], w=[(wdk, k)], dma=True)
                        yield
                        x_, xk_ = xg.next(); xt_, xtk = xT.next(); g4, g4k = wgt.next()
                        P.dma('sp', x_[:], XG[s_ * 512:(s_ + 1) * 512, :].rearrange("(t p) d -> p t d", p=128), r=["d_XGs", "d_XG"], w=[xk_])
                        P.dma('sp', g4[:], WG[s_ * 512:(s_ + 1) * 512, :].rearrange("(t p) o -> p (t o)", p=128), r=["d_WG"], w=[g4k], allow_slow_non_contiguous=True)
                        yield
                        for t in range(4):
                            b = 6 + t % 2
                            psb = PS[b].bitcast(BF16)
                            for k in range(8):
                                P.op('pe', (lambda psb=psb, k=k, t=t: nc.tensor.transpose(psb[:, k * 128:(k + 1) * 128], x_[:, t, k:D:8], identb[:])),
                                     r=[xk_, "identb"], w=[PK[b]])
                            P.op('act', (lambda psb=psb, t=t: A.copy(out=xt_[:, :, t * 128:(t + 1) * 128], in_=psb.rearrange("p (k c) -> p k c", k=8))),
                                 r=[PK[b]], w=[(xtk, t)])
                        yield
                        xtkeys = [(xtk, t) for t in range(4)]
                        hid, hidk = hidT.next()
                        for hc in range(4):
                            gb = (hcn[0] % 2) * 2; ub = gb + 1; hcn[0] += 1
                            for k in range(8):
                                P.op('pe', (lambda gb=gb, k=k, hc=hc: nc.tensor.matmul(PS[gb], wgt_[:, k, hc * 128:(hc + 1) * 128], xt_[:, k, :],
                                                                                    start=(k == 0), stop=(k == 7))), r=[(wgk, k)] + xtkeys, w=[PK[gb]])
                            for k in range(8):
                                P.op('pe', (lambda ub=ub, k=k, hc=hc: nc.tensor.matmul(PS[ub], wut[:, k, hc * 128:(hc + 1) * 128], xt_[:, k, :],
                                                                                    start=(k == 0), stop=(k == 7))), r=[(wuk, k)] + xtkeys, w=[PK[ub]])
                            yield
                            sg, sgk = sgb.next()
                            P.op('act', (lambda sg=sg, gb=gb: A.activation(out=sg[:], in_=PS[gb], func=AF.Silu)), r=[PK[gb]], w=[sgk])
                            P.op('dve', (lambda sg=sg, ub=ub, hc=hc: V.tensor_tensor(out=hid[:, hc, :], in0=sg[:], in1=PS[ub], op=ALU.mult)),
                                 r=[sgk, PK[ub]], w=[(hidk, hc)])
                        for t in range(4):
                            yield
                            y_, yk = yg.next()
                            for half in range(2):
                                yb_ = 4 + yn[0] % 2; yn[0] += 1
                                for hc in range(4):
                                    P.op('pe', (lambda yb_=yb_, hc=hc, t=t, half=half: nc.tensor.matmul(
                                        PS[yb_], hid[:, hc, t * 128:(t + 1) * 128], wdt[:, hc, half * 512:(half + 1) * 512],
                                        start=(hc == 0), stop=(hc == 3))), r=[(hidk, hc), (wdk, hc)], w=[PK[yb_]])
                                P.op('act', (lambda yb_=yb_, half=half, t=t, y_=y_: A.activation(out=y_[:, half * 512:(half + 1) * 512], in_=PS[yb_], func=AF.Copy,
                                                                                            scale=g4[:, t:t + 1])), r=[PK[yb_], g4k], w=[(yk, half)])
                            P.dma('sp', YG[s_ * 512 + t * 128:s_ * 512 + (t + 1) * 128, :], y_[:], r=[(yk, 0), (yk, 1)], w=["d_YG"])

                    run_interleaved(P, (slot_tile(s_) for s_ in range(NTS)), 2)
                    P.barrier()
                if stop_after == "sD":
                    P.finish(); return "STOP"
                with contextlib.ExitStack() as ph:
                    R = mk_rings(ph)
                    load_mods(ph, [(st, 5) for st in ("LC" if not final else "L")])
                    y1 = Ring(nc, ph, "y1", 2, [128, D], F32); y2 = Ring(nc, ph, "y2", 2, [128, D], F32)
                    ofin = Ring(nc, ph, "ofin", 2, [128, D], F32)
                    nfb = None
                    if final:
                        nfb = sb("nfb", [128, D], F32, ph)
                        P.dma('sp', nfb[:], norm_final.partition_broadcast(128), r=["d_nf"], w=["nfb"])

                    def comb(i, gt):
                        mod, mkey = ("C", "mvec") if gt < NCT else ("L", "mvec")
                        a_, ak_ = y1.next(); b_, bk_ = y2.next()
                        for j, (dst, dk_) in enumerate(((a_, ak_), (b_, bk_))):
                            P.op('pool', (lambda dst=dst, j=j: G.indirect_dma_start(out=dst[:, :], out_offset=None, in_=YG[:, :],
                                                                                  in_offset=bass.IndirectOffsetOnAxis(ap=posi[:, i, j:j + 1], axis=0))),
                                 r=[("posi", i), "d_YG"], w=[dk_], dma=True)
                        xt, xk = R['xs'].next()
                        P.dma('sp', xt[:], src_t[gt], r=[skey], w=[xk])
                        P.op('pool', lambda: G.tensor_tensor(out=a_[:], in0=a_[:], in1=b_[:], op=ALU.add), r=[ak_, bk_], w=[ak_])
                        P.op('dve', lambda: V.tensor_tensor(out=a_[:], in0=a_[:], in1=mv(mod, 5), op=ALU.mult), r=[ak_, mkey], w=[ak_])
                        P.op('pool', lambda: G.tensor_tensor(out=a_[:], in0=a_[:], in1=xt[:], op=ALU.add), r=[ak_, xk], w=[ak_])
                        if not final:
                            P.dma('sp', dst_t[gt], a_[:], r=[ak_], w=["d_dst%d" % layer])
                        else:
                            o_, ok = ofin.next()
                            rms_mod(R, a_[:], ak_, nfb[:], None, ["nfb"], o_[:], ok)
                            P.dma('sp', dst_t[gt - NCT], o_[:], r=[ok], w=["d_out"])

                    for i, gt in enumerate(tiles):
                        comb(i, gt)
                    P.barrier()

        import os
        MOE = moe_sparse if os.environ.get("DENSE_MOE") is None else moe_phase
        if MOE(0, XS1_t, XS2_t, list(range(NT)), False) == "STOP":
            return nc
        if stop_after == "moe0":
            P.finish()
            return nc

        bfd = lambda name, shape: dscr(name, shape, BF16)
        QT_d = bfd("QT_d", [NT, 128, 8, 128]); KT_d = bfd("KT_d", [NT, 128, 8, 128])
        KK_d = bfd("KK_d", [NT, 128, 8, 128]); VV_d = bfd("VV_d", [NT, 128, 8, 128])
        ZZ_d = bfd("ZZ_d", [NT, 128, D]); GB_d = dscr("GB_d", [NT, 128, 32])
        OF_d = dscr("OF_d", [2, NLT, 128, D])
        with contextlib.ExitStack() as ph:
            adaln(1, ph)
            P.barrier()

        with contextlib.ExitStack() as ph:
            R = mk_rings(ph)
            load_mods(ph, [(st, ix) for st in "LC" for ix in (0, 1)])
            SBT1 = 4
            hxf = Ring(nc, ph, "c1hx", 1, [128, D], F32)
            hxT = sb("c1hxT", [128, 8, (SBT1 + 2) * 128], BF16, ph)
            pT = Ring(nc, ph, "pT", 4, [128, SBT1 * 128 + 4], F32)
            cv = Ring(nc, ph, "cv", 4, [128, SBT1 * 128], F32)
            sqb = Ring(nc, ph, "sqb", 3, [128, SBT1 * 128], BF16)
            rsb = Ring(nc, ph, "rsb", 3, [128, SBT1 * 128], F32)
            kf = Ring(nc, ph, "kf", 3, [128, SBT1 * 128], F32)
            qst = sb("qst", [128, SBT1, 8, 128], BF16, ph); kst = sb("kst", [128, SBT1, 8, 128], BF16, ph)
            ktst = sb("ktst", [128, SBT1, 8, 128], BF16, ph); vtst = sb("vtst", [128, SBT1, 8, 128], BF16, ph)
            win_sb = sb("win_sb", [128, 8, 3072], BF16, ph)
            wz = sb("wz", [128, 8, 1056], BF16, ph)
            wcs = sb("wcs", [128, 24, 4], F32, ph)
            onesb = sb("onesb", [128, 128], BF16, ph)
            c16 = sb("c16", [128, 2, 16], F32, ph)
            zsb = Ring(nc, ph, "zsb", 1, [128, D], BF16)
            gbr = Ring(nc, ph, "gbr", 2, [128, 64], F32)
            P.dma('pool', wz[:], w_dn_in.rearrange("(k p) n -> p k n", p=128)[:, :, 3072:4128], r=["d_win"], w=["wz"])
            P.dma('sp', wcs[:], wconv, r=["d_wconv"], w=["wcs"])
            P.op('dve', lambda: nc.vector.tensor_copy(out=onesb[:], in_=msk[:, 3, :]), r=["msk"], w=["onesb"])
            P.dma('sp', c16[:, 0, :], dn_dt_bias.partition_broadcast(128), r=["d_dtb"], w=["c16"])
            P.dma('sp', c16[:, 1, :], dn_a_log.partition_broadcast(128), r=["d_alog"], w=["c16"])
            P.op('act', lambda: nc.scalar.activation(out=c16[:, 1, :], in_=c16[:, 1, :], func=AF.Exp), r=["c16"], w=["c16"])
            P.op('dve', lambda: nc.vector.tensor_scalar(out=c16[:, 1, :], in0=c16[:, 1, :], scalar1=-1.0, scalar2=None, op0=ALU.mult),
                 r=["c16"], w=["c16"])
            win_v = w_dn_in.rearrange("(k p) n -> p k n", p=128)
            for j6 in range(6):
                P.dma('pool', win_sb[:, :, j6 * 512:(j6 + 1) * 512], win_v[:, :, j6 * 512:(j6 + 1) * 512], r=["d_win"], w=[("win_sb", j6)])

            def c1_tile_norm(gt, slot, mod, mkey):
                xt, xk = R['xs'].next()
                P.dma('sp', xt[:], XS2_t[gt], r=["d_dst0"], w=[xk])
                hx, hk = hxf.next()
                rms_mod(R, xt[:], xk, mv(mod, 1), mv(mod, 0), [mkey], hx[:], hk)
                for k in range(8):
                    b = 6 + k // 4
                    P.op('pe', (lambda b=b, k=k, hx=hx: nc.tensor.transpose(
                        PS[b][:, (k % 4) * 128:(k % 4) * 128 + 128], hx[:, k * 128:(k + 1) * 128], ident)),
                        r=[hk, "msk"], w=[PK[b]])
                for h in range(2):
                    b = 6 + h
                    P.op('act', (lambda b=b, h=h, slot=slot: nc.scalar.copy(
                        out=hxT[:, 4 * h:4 * h + 4, slot * 128:(slot + 1) * 128],
                        in_=PS[b].rearrange("p (k t) -> p k t", k=4))), r=[PK[b]], w=[("hxT", slot)])

            def c1_zab(gt, slot, is_ctx):
                hk = [("hxT", slot)]
                if not is_ctx:
                    zs, zk = zsb.next()
                    for half in range(2):
                        b = 4 + half
                        for k in range(8):
                            P.op('pe', (lambda b=b, k=k, half=half, slot=slot: nc.tensor.matmul(
                                PS[b], hxT[:, k, slot * 128:(slot + 1) * 128], wz[:, k, half * 512:(half + 1) * 512],
                                start=(k == 0), stop=(k == 7))), r=hk + ["wz"], w=[PK[b]])
                        P.op('act', (lambda b=b, half=half, zs=zs: nc.scalar.activation(
                            out=zs[:, half * 512:(half + 1) * 512], in_=PS[b], func=AF.Silu)), r=[PK[b]], w=[(zk, half)])
                    P.dma('sp', ZZ_d[gt], zs[:], r=[(zk, 0), (zk, 1)], w=["d_ZZ"])
                for k in range(8):
                    P.op('pe', (lambda k=k, slot=slot: nc.tensor.matmul(
                        PS[3][:, 0:32], hxT[:, k, slot * 128:(slot + 1) * 128], wz[:, k, 1024:1056],
                        start=(k == 0), stop=(k == 7))), r=hk + ["wz"], w=[PK[3]])
                g_, gk = gbr.next()
                V = nc.vector
                ab = g_[:, 0:32].rearrange("p (f h) -> p f h", f=4)
                o4 = g_[:, 32:64].rearrange("p (f h) -> p f h", f=4)
                P.op('dve', lambda: V.tensor_copy(out=g_[:, 0:32], in_=PS[3][:, 0:32]), r=[PK[3]], w=[gk])
                P.op('dve', lambda: V.tensor_tensor(out=o4[:, 0::2, :], in0=ab[:, 0::2, :],
                                                    in1=c16[:, 0, :].rearrange("p (d h) -> p d h", d=2), op=ALU.add),
                     r=[gk, "c16"], w=[gk])
                P.op('act', lambda: nc.scalar.activation(out=o4[:, 0::2, :], in_=o4[:, 0::2, :], func=AF.Exp), r=[gk], w=[gk])
                P.op('dve', lambda: V.tensor_scalar(out=o4[:, 0::2, :], in0=o4[:, 0::2, :], scalar1=1.0, scalar2=None, op0=ALU.add),
                     r=[gk], w=[gk])
                P.op('act', lambda: nc.scalar.activation(out=o4[:, 0::2, :], in_=o4[:, 0::2, :], func=AF.Ln), r=[gk], w=[gk])
                P.op('dve', lambda: V.tensor_tensor(out=o4[:, 0::2, :], in0=o4[:, 0::2, :],
                                                    in1=c16[:, 1, :].rearrange("p (d h) -> p d h", d=2), op=ALU.mult),
                     r=[gk, "c16"], w=[gk])
                P.op('act', lambda: nc.scalar.activation(out=o4[:, 1::2, :], in_=ab[:, 1::2, :], func=AF.Sigmoid), r=[gk], w=[gk])
                P.dma('sp', GB_d[gt], g_[:, 32:64], r=[gk], w=["d_GB"])

            def c1_chunk(cc, seq0, nseq, t0, t1, hbase_tile):
                W = (t1 - t0) * 128
                ntile = t1 - t0
                wk = ("win_sb", cc // 4)
                p_, pk = pT.next()
                tok0 = t0 * 128 - 2
                lo = max(tok0, 0); hi = min(t1 * 128 + 1, nseq * 128)
                if lo > tok0:
                    P.op('pool', lambda: nc.gpsimd.memset(p_[:, 0:lo - tok0], 0.0), r=[], w=[(pk, 'l')])
                if hi < t1 * 128 + 1:
                    P.op('pool', lambda: nc.gpsimd.memset(p_[:, hi - tok0:W + 3], 0.0), r=[], w=[(pk, 'r')])
                a = lo
                wi = 0
                while a < hi:
                    b_ = min(a + 512, hi)
                    bank = (wi + cc) % 3
                    hk = [("hxT", s_) for s_ in range((a // 128) - hbase_tile, ((b_ - 1) // 128) - hbase_tile + 1)]
                    for k in range(8):
                        P.op('pe', (lambda k=k, bank=bank, a=a, b_=b_: nc.tensor.matmul(
                            PS[bank][:, 0:b_ - a], win_sb[:, k, cc * 128:(cc + 1) * 128], hxT[:, k, a - hbase_tile * 128:b_ - hbase_tile * 128],
                            start=(k == 0), stop=(k == 7))), r=[wk] + hk, w=[PK[bank]])
                    P.op('act', (lambda bank=bank, a=a, b_=b_: nc.scalar.copy(out=p_[:, a - tok0:b_ - tok0], in_=PS[bank][:, 0:b_ - a])),
                         r=[PK[bank]], w=[(pk, wi)])
                    a = b_; wi += 1
                yield
                pkeys = [(pk, j) for j in range(wi)] + [(pk, 'l'), (pk, 'r')]
                c_, ck = cv.next()
                P.op('dve', lambda: nc.vector.tensor_scalar(out=c_[:, 0:W], in0=p_[:, 0:W], scalar1=wcs[:, cc, 0:1], scalar2=None, op0=ALU.mult),
                     r=pkeys + ["wcs"], w=[ck])
                for tap in range(1, 4):
                    eng = 'dve'
                    E_ = nc.gpsimd if eng == 'pool' else nc.vector
                    P.op(eng, (lambda tap=tap, E_=E_: E_.scalar_tensor_tensor(out=c_[:, 0:W], in0=p_[:, tap:tap + W], scalar=wcs[:, cc, tap:tap + 1],
                                                                             in1=c_[:, 0:W], op0=ALU.mult, op1=ALU.add)),
                         r=pkeys + ["wcs", ck], w=[ck])
                yield
                P.op('act', lambda: nc.scalar.activation(out=c_[:, 0:W], in_=c_[:, 0:W], func=AF.Silu), r=[ck], w=[ck])
                yield
                kind = cc // 8; h = cc % 8
                src = c_
                srck = ck
                if kind < 2:
                    sq, sqk = sqb.next(); rs, rk_ = rsb.next()
                    P.op('pool', lambda: nc.gpsimd.tensor_tensor(out=sq[:, 0:W], in0=c_[:, 0:W], in1=c_[:, 0:W], op=ALU.mult), r=[ck], w=[sqk])
                    for j in range(0, W, 512):
                        n_ = min(512, W - j)
                        bank = 3 + (j // 512) % 2
                        P.op('pe', (lambda j=j, n_=n_, bank=bank: nc.tensor.matmul(PS[bank][:, 0:n_], onesb[:], sq[:, j:j + n_], start=True, stop=True)),
                             r=[sqk, "onesb"], w=[PK[bank]])
                        P.op('dve', (lambda j=j, n_=n_, bank=bank: nc.vector.tensor_scalar(out=rs[:, j:j + n_], in0=PS[bank][:, 0:n_], scalar1=EPS, scalar2=None, op0=ALU.add)),
                             r=[PK[bank]], w=[(rk_, j)])
                    yield
                    rkeys = [(rk_, j) for j in range(0, W, 512)]
                    P.op('act', lambda: nc.scalar.activation(out=rs[:, 0:W], in_=rs[:, 0:W], func=AF.Sqrt), r=rkeys, w=rkeys)
                    yield
                    P.op('dve', lambda: nc.vector.reciprocal(out=rs[:, 0:W], in_=rs[:, 0:W]), r=rkeys, w=rkeys)
                    if kind == 0:
                        P.op('dve', lambda: nc.vector.scalar_tensor_tensor(
                            out=qst[:, 0:ntile, h, :], in0=c_[:, 0:W].rearrange("p (t k) -> p t k", k=128), scalar=float(128 ** -0.5),
                            in1=rs[:, 0:W].rearrange("p (t k) -> p t k", k=128), op0=ALU.mult, op1=ALU.mult), r=[ck] + rkeys, w=[("qst", h)])
                        return
                    kf_, kfk = kf.next()
                    P.op('dve', lambda: nc.vector.tensor_tensor(out=kf_[:, 0:W], in0=c_[:, 0:W], in1=rs[:, 0:W], op=ALU.mult), r=[ck] + rkeys, w=[kfk])
                    P.op('pool', lambda: nc.gpsimd.tensor_copy(out=kst[:, 0:ntile, h, :], in_=kf_[:, 0:W].rearrange("p (t k) -> p t k", k=128)),
                         r=[kfk], w=[("kst", h)])
                    src = kf_; srck = kfk
                yield
                dst = ktst if kind == 1 else vtst
                dkey = "ktst" if kind == 1 else "vtst"
                for j in range(0, ntile, 4):
                    n_ = min(4, ntile - j)
                    bank = 5 + (j // 4) % 2
                    for t in range(n_):
                        P.op('pe', (lambda t=t, j=j, bank=bank: nc.tensor.transpose(
                            PS[bank][:, t * 128:(t + 1) * 128], src[:, (j + t) * 128:(j + t + 1) * 128], ident)),
                            r=[srck, "msk"], w=[PK[bank]])
                    P.op('act', (lambda j=j, n_=n_, bank=bank: nc.scalar.copy(
                        out=dst[:, j:j + n_, h, :], in_=PS[bank][:, 0:n_ * 128].rearrange("p (t k) -> p t k", k=128))),
                        r=[PK[bank]], w=[(dkey, h, j)])

            def c1_superblock(seq0, nseq, t0, t1, is_ctx):
                mod, mkey = ("C", "mvec") if is_ctx else ("L", "mvec")
                hb = max(t0 - 1, 0); he = min(t1 + 1, nseq)
                for lt in range(hb, he):
                    c1_tile_norm(seq0 + lt, lt - hb, mod, mkey)
                for lt in range(t0, t1):
                    c1_zab(seq0 + lt, lt - hb, is_ctx)
                run_interleaved(P, (c1_chunk(cc, seq0, nseq, t0, t1, hb) for cc in range(24)), 3)
                nt_ = t1 - t0
                g0 = seq0 + t0
                for (dst, st, key) in ((QT_d, qst, "qst"), (KT_d, kst, "kst")):
                    P.dma('sp', dst[g0:g0 + nt_].rearrange("t p h k -> p t h k"), st[:, 0:nt_, :, :],
                          r=[(key, h) for h in range(8)], w=["d_" + key])
                for (dst, st, key) in ((KK_d, ktst, "ktst"), (VV_d, vtst, "vtst")):
                    P.dma('sp', dst[g0:g0 + nt_].rearrange("t p h k -> p t h k"), st[:, 0:nt_, :, :],
                          r=[(key, h, j) for h in range(8) for j in range(0, nt_, 4)], w=["d_" + key])

            c1_superblock(0, NCT, 0, NCT, True)
            for t0 in range(0, NLT, SBT1):
                c1_superblock(NCT, NLT, t0, t0 + SBT1, False)
            P.barrier()
        if stop_after == "c1":
            P.finish()
            return nc

        with contextlib.ExitStack() as ph:
            HS = [128, 8, 128]
            lKT = Ring(nc, ph, "lKT", 2, HS, BF16); lQT = Ring(nc, ph, "lQT", 2, HS, BF16)
            lKK = Ring(nc, ph, "lKK", 2, HS, BF16); lVV = Ring(nc, ph, "lVV", 2, HS, BF16)
            gbl = Ring(nc, ph, "gbl", 2, [128, 32], F32)
            scr = Ring(nc, ph, "scr", 2, [128, 80], F32)
            L16 = Ring(nc, ph, "L16", 2, [16, 4, 128], F32)
            rE = Ring(nc, ph, "rE", 2, [16, 2, 8, 128], F32)
            bmask = sb("bmask", [16, 8, 128], F32, ph)
            Ei = Ring(nc, ph, "Ei", 2, HS, F32); Es = Ring(nc, ph, "Es", 2, HS, F32)
            SBm = Ring(nc, ph, "SBm", 2, HS, F32); tf = Ring(nc, ph, "tf", 2, HS, F32)
            Ak = Ring(nc, ph, "Ak", 4, HS, BF16); Bk = Ring(nc, ph, "Bk", 4, HS, BF16); Tk = Ring(nc, ph, "Tk", 4, HS, BF16)
            TTk = Ring(nc, ph, "TTk", 4, HS, BF16); A0r = Ring(nc, ph, "A0r", 2, HS, BF16); B0r = Ring(nc, ph, "B0r", 2, HS, BF16)
            Of = Ring(nc, ph, "Of", 8, HS, BF16); P1r = Ring(nc, ph, "P1r", 4, HS, BF16)
            qkT = Ring(nc, ph, "qkT", 2, HS, BF16); KG = Ring(nc, ph, "KG", 2, HS, BF16); Kd = Ring(nc, ph, "Kd", 2, HS, BF16)
            up = Ring(nc, ph, "up", 2, HS, F32); wT = Ring(nc, ph, "wT", 2, HS, BF16); vn = Ring(nc, ph, "vn", 2, HS, BF16)
            ob = Ring(nc, ph, "ob", 2, HS, F32)
            Sst = [sb("S%d" % d, HS, F32, ph) for d in range(2)]
            Sbf = [sb("Sb%d" % d, HS, BF16, ph) for d in range(2)]
            P.dma('sp', bmask[:], blockmask, r=["d_bm"], w=["bmask"])
            mskb = sb("mskb", [128, 3, 128], BF16, ph)
            P.op('dve', lambda: nc.vector.tensor_copy(out=mskb[:], in_=msk[:, 10:13, :]), r=["msk"], w=["mskb"])
            for d in range(2):
                P.op('pool', (lambda d=d: nc.gpsimd.memset(Sst[d][:], 0.0)), r=[], w=[("S", d)])
                P.op('pool', (lambda d=d: nc.gpsimd.memset(Sbf[d][:], 0.0)), r=[], w=[("Sb", d)])
            for t_ in scr.tiles:
                P.op('pool', (lambda t_=t_: nc.gpsimd.memset(t_[:], 1.0)), r=[], w=[])
            P.flush()
            P.barrier()
            ppc = [0]

            def pair():
                p = ppc[0] % 4
                ppc[0] += 1
                v = psum[:, p * 1024:(p + 1) * 1024]
                return p, v, v.rearrange("p (h k) -> p h k", h=8), [PK[2 * p], PK[2 * p + 1]]

            def bc_mid(ap2d, n=128):
                return ap2d.unsqueeze(1).to_broadcast([ap2d.shape[0], 8, ap2d.shape[1]])

            def bc_last(ap2d):
                return ap2d.unsqueeze(2).to_broadcast([ap2d.shape[0], 8, 128])

            def dn_step(d, gt, lt, need_o):
                V = nc.vector; G = nc.gpsimd; A = nc.scalar
                kt, ktk = lKT.next(); kk, kkk = lKK.next(); vv, vvk = lVV.next(); gb, gbk = gbl.next()
                P.dma('sp', kt[:], KT_d[gt], r=["d_kst"], w=[ktk])
                P.dma('sp', kk[:], KK_d[gt], r=["d_ktst"], w=[kkk])
                P.dma('sp', vv[:], VV_d[gt], r=["d_vtst"], w=[vvk])
                P.dma('sp', gb[:], GB_d[gt], r=["d_GB"], w=[gbk])
                if need_o:
                    qt, qtk = lQT.next()
                    P.dma('sp', qt[:], QT_d[gt], r=["d_qst"], w=[qtk])
                g = gb[:, 16 * d:16 * d + 8]; beta = gb[:, 16 * d + 8:16 * d + 16]
                sc, sk = scr.next()
                p, pv, pv3, pk = pair()
                P.op('pe', lambda: nc.tensor.matmul(pv[:, 0:8], msk[:, 1 + d, :], g, start=True, stop=True), r=[gbk, "msk"], w=pk)
                P.op('pe', lambda: nc.tensor.matmul(pv[:, 8:16], msk[:, 3, :], g, start=True, stop=True), r=[gbk, "msk"], w=pk)
                P.op('dve', lambda: V.tensor_copy(out=sc[:, 0:16], in_=pv[:, 0:16]), r=pk, w=[sk])
                yield
                P.op('act', lambda: A.activation(out=sc[:, 16:24], in_=sc[:, 0:8], func=AF.Exp), r=[sk], w=[sk])
                P.op('dve', lambda: V.tensor_tensor(out=sc[:, 24:32], in0=sc[:, 8:16], in1=sc[:, 0:8], op=ALU.subtract), r=[sk], w=[sk])
                P.op('act', lambda: A.activation(out=sc[:, 24:32], in_=sc[:, 24:32], func=AF.Exp), r=[sk], w=[sk])
                P.op('act', lambda: A.activation(out=sc[:, 32:40], in_=sc[:, 8:16], func=AF.Exp), r=[sk], w=[sk])
                P.op('dve', lambda: V.tensor_scalar(out=sc[:, 40:48], in0=sc[:, 0:8], scalar1=-1.0, scalar2=None, op0=ALU.mult), r=[sk], w=[sk])
                P.op('dve', lambda: V.tensor_copy(out=sc[:, 56:64], in_=sc[:, 0:8]), r=[sk], w=[sk])
                P.op('act', lambda: A.activation(out=sc[:, 72:80], in_=beta, func=AF.Ln), r=[gbk], w=[sk])
                P.op('dve', lambda: V.tensor_tensor(out=sc[:, 72:80], in0=sc[:, 72:80], in1=sc[:, 40:48], op=ALU.add), r=[sk], w=[sk])
                yield
                egc = sc[:, 16:24]; edec = sc[:, 24:32]; egl = sc[:, 32:40]
                l16, lk = L16.next(); re, rek = rE.next()
                p, pv, pv3, pk = pair()
                for q in range(4):
                    P.op('pe', (lambda q=q: nc.tensor.transpose(pv[0:16, q * 128:(q + 1) * 128], sc[:, 40 + 8 * q:56 + 8 * q], ident)),
                         r=[sk, "msk"], w=pk)
                yield
                P.op('act', lambda: A.copy(out=l16[:], in_=pv[0:16, 0:512].rearrange("p (q k) -> p q k", q=4)), r=pk, w=[lk])
                P.op('pool', lambda: G.tensor_tensor(out=re[:, 0, :, :], in0=bc_mid(l16[:, 1, :]), in1=bmask[:], op=ALU.mult), r=[lk, "bmask"], w=[(rek, 0)])
                P.op('pool', lambda: G.tensor_tensor(out=re[:, 1, :, :], in0=bc_mid(l16[:, 3, :]), in1=bmask[:], op=ALU.mult), r=[lk, "bmask"], w=[(rek, 1)])
                yield
                ei, eik = Ei.next(); es_, esk = Es.next()
                for which, (lq, dst, dk_, mi) in enumerate(((0, ei, eik, 4 + d), (2, es_, esk, 8 + d))):
                    yield
                    p, pv, pv3, pk = pair()
                    for half in range(2):
                        P.op('pe', (lambda half=half, which=which, lq=lq, pv=pv: nc.tensor.matmul(
                            pv[:, half * 512:(half + 1) * 512], l16[:, lq, :],
                            re[:, which, 4 * half:4 * half + 4, :].rearrange("p h k -> p (h k)"), start=True, stop=True)),
                            r=[lk, (rek, which)], w=pk)
                    P.op('dve', (lambda pv3=pv3, dst=dst, mi=mi: V.scalar_tensor_tensor(
                        out=dst[:], in0=pv3, scalar=0.0, in1=bc_mid(msk[:, mi, :]), op0=ALU.min, op1=ALU.add)), r=pk + ["msk"], w=[dk_])
                    P.op('act', (lambda dst=dst: A.activation(out=dst[:], in_=dst[:], func=AF.Exp)), r=[dk_], w=[dk_])
                yield
                sbm, sbk = SBm.next()
                P.op('pool', lambda: G.tensor_tensor(out=sbm[:], in0=bc_mid(msk[:, 6 + d, :]), in1=bc_last(beta), op=ALU.mult), r=["msk", gbk], w=[sbk])
                p, pv, pv3, pk = pair()
                for h in range(8):
                    P.op('pe', (lambda h=h, pv=pv: nc.tensor.matmul(pv[:, h * 128:(h + 1) * 128], kt[:, h, :], kt[:, h, :], start=True, stop=True)),
                         r=[ktk], w=pk)
                yield
                t1, t1k = tf.next()
                a0, a0k = A0r.next(); b0, b0k = B0r.next()
                P.op('dve', (lambda pv3=pv3: V.tensor_tensor(out=t1[:], in0=pv3, in1=ei[:], op=ALU.mult)), r=pk + [eik], w=[t1k])
                P.op('dve', lambda: V.tensor_tensor(out=a0[:], in0=t1[:], in1=sbm[:], op=ALU.mult), r=[t1k, sbk], w=[a0k])
                P.op('dve', (lambda pv3=pv3: V.tensor_tensor(out=b0[:], in0=pv3, in1=es_[:], op=ALU.mult)), r=pk + [esk], w=[b0k])
                if need_o:
                    qk_, qkk = qkT.next()
                    p, pv, pv3, pk = pair()
                    for h in range(8):
                        P.op('pe', (lambda h=h, pv=pv: nc.tensor.matmul(pv[:, h * 128:(h + 1) * 128], kt[:, h, :], qt[:, h, :], start=True, stop=True)),
                             r=[ktk, qtk], w=pk)
                    P.op('dve', (lambda pv3=pv3: V.tensor_tensor(out=qk_[:], in0=pv3, in1=ei[:], op=ALU.mult)), r=pk + [eik], w=[qkk])
                yield
                def mm8(lhs, rhs, keys):
                    p, pv, pv3, pk = pair()
                    for h in range(8):
                        P.op('pe', (lambda h=h, pv=pv: nc.tensor.matmul(pv[:, h * 128:(h + 1) * 128], lhs[:, h, :], rhs[:, h, :], start=True, stop=True)),
                             r=keys, w=pk)
                    return pv3, pk
                ca, cak = Ak.next(); cb, cbk = Bk.next(); cT, cTk = Tk.next(); cTT, cTTk = TTk.next()
                P.op('dve', lambda ca=ca: V.tensor_tensor(out=ca[:], in0=a0[:], in1=bc_mid(mskb[:, 0, :]), op=ALU.mult), r=[a0k, "mskb"], w=[cak])
                P.op('dve', lambda cb=cb: V.tensor_tensor(out=cb[:], in0=b0[:], in1=bc_mid(mskb[:, 0, :]), op=ALU.mult), r=[b0k, "mskb"], w=[cbk])
                P.op('dve', lambda ca=ca, cT=cT: V.tensor_tensor(out=cT[:], in0=bc_mid(identb[:]), in1=ca[:], op=ALU.subtract), r=["identb", cak], w=[cTk])
                P.op('dve', lambda cb=cb, cTT=cTT: V.tensor_tensor(out=cTT[:], in0=bc_mid(identb[:]), in1=cb[:], op=ALU.subtract), r=["identb", cbk], w=[cTTk])
                yield
                offs = []
                for mi in (11, 12):
                    of_, ofk = Of.next(); oft, oftk = Of.next()
                    P.op('dve', (lambda of_=of_, mi=mi: V.tensor_tensor(out=of_[:], in0=a0[:], in1=bc_mid(mskb[:, mi - 10, :]), op=ALU.mult)), r=[a0k, "mskb"], w=[ofk])
                    P.op('dve', (lambda oft=oft, mi=mi: V.tensor_tensor(out=oft[:], in0=b0[:], in1=bc_mid(mskb[:, mi - 10, :]), op=ALU.mult)), r=[b0k, "mskb"], w=[oftk])
                    offs.append((of_, ofk, oft, oftk))
                for lev in range(4):
                    yield
                    na, nak = Ak.next(); nb, nbk = Bk.next(); nT, nTk = Tk.next(); nTT, nTTk = TTk.next()
                    pv3, pk = mm8(cb, ca, [cak, cbk])
                    P.op('act', (lambda pv3=pv3, na=na: A.copy(out=na[:], in_=pv3)), r=pk, w=[nak])
                    pv3, pk = mm8(ca, cb, [cak, cbk])
                    P.op('dve', (lambda pv3=pv3, nb=nb: V.tensor_copy(out=nb[:], in_=pv3)), r=pk, w=[nbk])
                    yield
                    pv3, pk = mm8(nb, cT, [nbk, cTk])
                    P.op('dve', (lambda pv3=pv3, nT=nT, cT=cT: V.tensor_tensor(out=nT[:], in0=pv3, in1=cT[:], op=ALU.add)), r=pk + [cTk], w=[nTk])
                    pv3, pk = mm8(na, cTT, [nak, cTTk])
                    P.op('dve', (lambda pv3=pv3, nTT=nTT, cTT=cTT: V.tensor_tensor(out=nTT[:], in0=pv3, in1=cTT[:], op=ALU.add)), r=pk + [cTTk], w=[nTTk])
                    ca, cak, cb, cbk, cT, cTk, cTT, cTTk = na, nak, nb, nbk, nT, nTk, nTT, nTTk
                for si, (of_, ofk, oft, oftk) in enumerate(offs):
                    yield
                    p1, p1k = P1r.next()
                    pv3, pk = mm8(oft, cT, [oftk, cTk])
                    P.op('act', (lambda pv3=pv3, p1=p1: A.copy(out=p1[:], in_=pv3)), r=pk, w=[p1k])
                    yield
                    pv3, pk = mm8(cTT, p1, [cTTk, p1k])
                    nT, nTk = Tk.next()
                    P.op('dve', (lambda pv3=pv3, nT=nT, cT=cT: V.tensor_tensor(out=nT[:], in0=cT[:], in1=pv3, op=ALU.subtract)), r=pk + [cTk], w=[nTk])
                    if si == 0:
                        p1t, p1tk = P1r.next()
                        pv3, pk = mm8(of_, cTT, [ofk, cTTk])
                        P.op('act', (lambda pv3=pv3, p1t=p1t: A.copy(out=p1t[:], in_=pv3)), r=pk, w=[p1tk])
                        pv3, pk = mm8(cT, p1t, [cTk, p1tk])
                        nTT, nTTk = TTk.next()
                        P.op('dve', (lambda pv3=pv3, nTT=nTT, cTT=cTT: V.tensor_tensor(out=nTT[:], in0=cTT[:], in1=pv3, op=ALU.subtract)), r=pk + [cTTk], w=[nTTk])
                        cTT, cTTk = nTT, nTTk
                    cT, cTk = nT, nTk
                yield
                u_, uk = up.next(); w_, wk_ = wT.next(); kg, kgk = KG.next(); kd, kdk = Kd.next()
                P.op('pool', lambda: G.tensor_tensor(out=kg[:], in0=kk[:], in1=bc_last(egc), op=ALU.mult), r=[kkk, sk], w=[kgk])
                P.op('pool', lambda: G.tensor_tensor(out=kd[:], in0=kk[:], in1=bc_last(edec), op=ALU.mult), r=[kkk, sk], w=[kdk])
                p, pv, pv3, pk = pair()
                for h in range(8):
                    P.op('pe', (lambda h=h, pv=pv: nc.tensor.matmul(pv[:, h * 128:(h + 1) * 128], cT[:, h, :], vv[:, h, :], start=True, stop=True)),
                         r=[cTk, vvk], w=pk)
                P.op('act', (lambda pv3=pv3: A.copy(out=u_[:], in_=pv3)), r=pk, w=[uk])
                p, pv, pv3, pk = pair()
                for h in range(8):
                    P.op('pe', (lambda h=h, pv=pv: nc.tensor.matmul(pv[:, h * 128:(h + 1) * 128], kg[:, h, :], cT[:, h, :], start=True, stop=True)),
                         r=[cTk, kgk], w=pk)
                P.op('act', (lambda pv3=pv3: A.copy(out=w_[:], in_=pv3)), r=pk, w=[wk_])
                yield
                S = Sst[d]; Sb = Sbf[d]; Sk = ("S", d); Sbk = ("Sb", d)
                vn_, vnk = vn.next(); t2, t2k = tf.next()
                p, pv, pv3, pk = pair()
                for h in range(8):
                    P.op('pe', (lambda h=h, pv=pv: nc.tensor.matmul(pv[:, h * 128:(h + 1) * 128], w_[:, h, :], Sb[:, h, :], start=True, stop=True)),
                         r=[wk_, Sbk], w=pk)
                yield
                P.op('dve', (lambda pv3=pv3: V.tensor_tensor(out=t2[:], in0=u_[:], in1=pv3, op=ALU.subtract)), r=pk + [uk], w=[t2k])
                P.op('dve', lambda: V.tensor_tensor(out=vn_[:], in0=t2[:], in1=bc_last(beta), op=ALU.mult), r=[t2k, gbk], w=[vnk])
                if need_o:
                    o_, ok_ = ob.next()
                    p, pv, pv3, pk = pair()
                    for h in range(8):
                        P.op('pe', (lambda h=h, pv=pv: nc.tensor.matmul(pv[:, h * 128:(h + 1) * 128], qt[:, h, :], Sb[:, h, :], start=True, stop=True)),
                             r=[qtk, Sbk], w=pk)
                    P.op('dve', (lambda pv3=pv3: V.tensor_tensor(out=o_[:], in0=pv3, in1=bc_last(egc), op=ALU.mult)), r=pk + [sk], w=[ok_])
                    p, pv, pv3, pk = pair()
                    for h in range(8):
                        P.op('pe', (lambda h=h, pv=pv: nc.tensor.matmul(pv[:, h * 128:(h + 1) * 128], qk_[:, h, :], vn_[:, h, :], start=True, stop=True)),
                             r=[qkk, vnk], w=pk)
                    P.op('dve', (lambda pv3=pv3: V.tensor_tensor(out=o_[:], in0=o_[:], in1=pv3, op=ALU.add)), r=pk + [ok_], w=[ok_])
                    P.dma('sp', OF_d[d, lt], o_[:].rearrange("p h k -> p (h k)"), r=[ok_], w=["d_OF"])
                yield
                p, pv, pv3, pk = pair()
                for h in range(8):
                    P.op('pe', (lambda h=h, pv=pv: nc.tensor.matmul(pv[:, h * 128:(h + 1) * 128], kd[:, h, :], vn_[:, h, :], start=True, stop=True)),
                         r=[kdk, vnk], w=pk)
                P.op('dve', lambda: V.tensor_tensor(out=S[:], in0=S[:], in1=bc_last(egl), op=ALU.mult), r=[Sk, sk], w=[Sk])
                P.op('dve', (lambda pv3=pv3: V.tensor_tensor(out=S[:], in0=S[:], in1=pv3, op=ALU.add)), r=pk + [Sk], w=[Sk])
                P.op('act', lambda: A.copy(out=Sb[:], in_=S[:]), r=[Sk], w=[Sbk])
                import os
                if os.environ.get("DN_DBG") and d == int(os.environ["DN_DBG"]) and gt == int(os.environ.get("DN_DBG_GT", "0")):
                    dump("dbg_sc", sc[:], [sk]); dump("dbg_Ei", ei[:], [eik]); dump("dbg_Es", es_[:], [esk])
                    dump("dbg_a0", a0[:], [a0k]); dump("dbg_b0", b0[:], [b0k]); dump("dbg_T", cT[:], [cTk])
                    dump("dbg_up", u_[:], [uk]); dump("dbg_wT", w_[:], [wk_]); dump("dbg_vn", vn_[:], [vnk]); dump("dbg_S", S[:], [Sk])
                    dump("dbg_l16", l16[:], [lk])
                    if need_o:
                        dump("dbg_qk", qk_[:], [qkk]); dump("dbg_o", o_[:], [ok_])

            order_f = [(t, t, False) for t in range(NCT)] + [(NCT + t, t, True) for t in range(NLT)]
            order_b = [(t, t, False) for t in reversed(range(NCT))] + [(NCT + t, t, True) for t in reversed(range(NLT))]
            import os
            nstep = int(os.environ.get("DN_STEPS", str(NT)))
            for i in range(nstep):
                g0 = dn_step(0, *order_f[i])
                g1 = dn_step(1, *order_b[i])
                run_interleaved(P, [g0, g1], 2, rare=False)
            P.barrier()
        if stop_after == "c2":
            P.finish()
            return nc

        with contextlib.ExitStack() as ph:
            R = mk_rings(ph)
            load_mods(ph, [("L", 2)])
            of0 = Ring(nc, ph, "of0", 2, [128, D], F32); of1 = Ring(nc, ph, "of1", 2, [128, D], F32)
            sqt = Ring(nc, ph, "sqt", 2, [128, D], F32)
            zl = Ring(nc, ph, "zl", 2, [128, D], BF16)
            ms = Ring(nc, ph, "ms", 2, [128, 8], F32)
            onT = Ring(nc, ph, "onT", 2, [128, 8, 128], BF16)
            yb = Ring(nc, ph, "yb", 2, [128, D], F32)
            wo = sb("wo", [128, 8, D], BF16, ph)
            dnw = sb("dnw", [128, 128], F32, ph)
            P.dma('pool', wo[:], w_dn_out.rearrange("(k p) n -> p k n", p=128), r=["d_wo"], w=["wo"])
            P.dma('sp', dnw[:], dn_norm.partition_broadcast(128), r=["d_dnn"], w=["dnw"])

            def c3_tile(lt):
                gt = NCT + lt
                V = nc.vector; G = nc.gpsimd; A = nc.scalar
                a0_, a0k = of0.next(); a1_, a1k = of1.next(); z_, zk = zl.next(); m_, mk_ = ms.next(); sq, sqk = sqt.next()
                P.dma('sp', a0_[:], OF_d[0, lt], r=["d_OF"], w=[a0k])
                P.dma('sp', a1_[:], OF_d[1, lt], r=["d_OF"], w=[a1k])
                P.dma('sp', z_[:], ZZ_d[gt], r=["d_ZZ"], w=[zk])
                P.op('pool', lambda: G.tensor_tensor(out=a0_[:], in0=a0_[:], in1=a1_[:], op=ALU.add), r=[a0k, a1k], w=[a0k])
                yield
                P.op('act', lambda: A.activation(out=sq[:], in_=a0_[:], func=AF.Square), r=[a0k], w=[sqk])
                P.op('dve', lambda: V.reduce_sum(out=m_[:], in_=sq[:].rearrange("p (h k) -> p h k", h=8), axis=AX.X), r=[sqk], w=[mk_])
                P.op('dve', lambda: V.tensor_scalar(out=m_[:], in0=m_[:], scalar1=1.0 / 128, scalar2=EPS, op0=ALU.mult, op1=ALU.add), r=[mk_], w=[mk_])
                yield
                P.op('act', lambda: A.activation(out=m_[:], in_=m_[:], func=AF.Sqrt), r=[mk_], w=[mk_])
                yield
                P.op('dve', lambda: V.reciprocal(out=m_[:], in_=m_[:]), r=[mk_], w=[mk_])
                o3 = a0_[:].rearrange("p (h k) -> p h k", h=8)
                P.op('dve', lambda: V.tensor_tensor(out=o3, in0=o3, in1=m_[:].unsqueeze(2).to_broadcast([128, 8, 128]), op=ALU.mult), r=[a0k, mk_], w=[a0k])
                P.op('pool', lambda: G.tensor_tensor(out=o3, in0=o3, in1=dnw[:].unsqueeze(1).to_broadcast([128, 8, 128]), op=ALU.mult), r=[a0k, "dnw"], w=[a0k])
                P.op('pool', lambda: G.tensor_tensor(out=a0_[:], in0=a0_[:], in1=z_[:], op=ALU.mult), r=[a0k, zk], w=[a0k])
                yield
                for k in range(8):
                    b = (6 if lt % 2 == 0 else 2) + k // 4
                    P.op('pe', (lambda b=b, k=k: nc.tensor.transpose(PS[b][:, (k % 4) * 128:(k % 4) * 128 + 128], a0_[:, k * 128:(k + 1) * 128], ident)),
                         r=[a0k, "msk"], w=[PK[b]])
                t_, tk = onT.next()
                yield
                for h in range(2):
                    b = (6 if lt % 2 == 0 else 2) + h
                    P.op('act', (lambda b=b, h=h: A.copy(out=t_[:, 4 * h:4 * h + 4, :], in_=PS[b].rearrange("p (k t) -> p k t", k=4))),
                         r=[PK[b]], w=[(tk, h)])
                y_, yk = yb.next()
                xt, xk = R['xs'].next()
                P.dma('sp', xt[:], XS2_t[gt], r=["d_dst0"], w=[xk])
                yield
                for half in range(2):
                    b = (4 if lt % 2 == 0 else 0) + half
                    for k in range(8):
                        P.op('pe', (lambda b=b, k=k, half=half: nc.tensor.matmul(PS[b], t_[:, k, :], wo[:, k, half * 512:(half + 1) * 512],
                                                                             start=(k == 0), stop=(k == 7))), r=[(tk, 0), (tk, 1), "wo"], w=[PK[b]])
                    sl = slice(half * 512, (half + 1) * 512)
                    P.op('dve', (lambda b=b, sl=sl: V.tensor_tensor(out=y_[:, sl], in0=PS[b], in1=mv("L", 2)[:, sl], op=ALU.mult)),
                         r=[PK[b], "mvec"], w=[(yk, half)])
                    P.op('pool', (lambda sl=sl: G.tensor_tensor(out=y_[:, sl], in0=y_[:, sl], in1=xt[:, sl], op=ALU.add)), r=[(yk, half), xk], w=[(yk, half)])
                P.dma('sp', XS3_t[gt], y_[:], r=[(yk, 0), (yk, 1)], w=["d_XS3"])

            run_interleaved(P, (c3_tile(lt) for lt in range(NLT)), 2)
            P.barrier()
        if stop_after == "c3":
            P.finish()
            return nc
        MOE(1, XS3_t, out_t, list(range(NCT, NT)), True)
        P.finish()
    return nc


_CACHE = {}


def _core_inputs(b, inp, consts):
    m = {}
    m['xin'] = np.ascontiguousarray(np.concatenate([inp['ctx'][b], inp['x'][b]], 0), dtype=np.float32)
    cc = np.stack([inp['c'][b], inp['c_ctx']], 0)
    m['ccol'] = np.ascontiguousarray(cc.reshape(2, 8, 128).transpose(2, 0, 1), dtype=np.float32)
    for k in ('w_ada', 'b_ada', 'norm_mix', 'norm_ffn', 'w_e_gate', 'w_e_up', 'w_e_down', 'norm_final'):
        m[k] = np.ascontiguousarray(inp[k], dtype=np.float32)
    m['w_pool'] = np.ascontiguousarray(inp['w_pool'][0]); m['b_pool'] = np.ascontiguousarray(inp['b_pool'][0])
    m['pool_scale'] = np.ascontiguousarray(inp['pool_scale'][0])
    m['w_dn_in'] = np.ascontiguousarray(inp['w_dn_in'][0])
    m['wconv'] = np.ascontiguousarray(inp['w_dn_conv'][0].reshape(4, 24, 128).transpose(2, 1, 0))
    m['dn_a_log'] = np.ascontiguousarray(inp['dn_a_log'][0].reshape(16))
    m['dn_dt_bias'] = np.ascontiguousarray(inp['dn_dt_bias'][0].reshape(16))
    m['dn_norm'] = np.ascontiguousarray(inp['dn_norm'][0]); m['w_dn_out'] = np.ascontiguousarray(inp['w_dn_out'][0])
    m['w_r'] = np.ascontiguousarray(np.concatenate([inp['w_rg'], inp['w_re']], -1))
    m['b_r'] = np.ascontiguousarray(np.concatenate([inp['b_rg'], inp['b_re']], -1))
    m.update(consts)
    return m


def kernel(**inputs):
    inp = {k: np.asarray(v) for k, v in inputs.items()}
    if 'nc' not in _CACHE:
        _CACHE['nc'] = build()
        _CACHE['consts'] = host_constants()
    nc = _CACHE['nc']
    consts = _CACHE['consts']
    B = inp['x'].shape[0]
    in_maps = [_core_inputs(b, inp, consts) for b in range(B)]
    res = run_bass_kernel_spmd(nc, in_maps, core_ids=list(range(B)))
    out = np.stack([np.asarray(res.results[b]['out']).reshape(NLT * 128, D) for b in range(B)], 0)
    return out.astype(inp['x'].dtype)
```

```python
import os
from concourse.bass_utils import run_bass_kernel_spmd
import numpy as np, contextlib
import concourse.bass as bass
import concourse.mybir as mybir

F32 = mybir.dt.float32
BF16 = mybir.dt.bfloat16
AF = mybir.ActivationFunctionType
ALU = mybir.AluOpType
AX = mybir.AxisListType


class Prog:
    NS = 16

    def __init__(self, nc, es):
        self.nc = nc
        self.E = {'pe': nc.tensor, 'act': nc.scalar, 'dve': nc.vector, 'pool': nc.gpsimd, 'sp': nc.sync}
        self.sem = {e: es.enter_context(nc.semaphore("s_" + e)) for e in ('pe', 'act', 'dve', 'pool')}
        self.dsem = {q: [es.enter_context(nc.semaphore("d_%s%d" % (q, i))) for i in range(self.NS)]
                     for q in ('sp', 'act', 'pool')}
        self.sigcount = {e: 0 for e in self.sem}
        self.dmacount = {q: 0 for q in self.dsem}
        self.waited = {}
        self.ops = []
        self.last_w = {}
        self.readers = {}
        self.n_inst = 0

    def op(self, eng, fn, r=(), w=(), dma=False):
        self.ops.append((eng, fn, tuple(r), tuple(w), dma))

    def dma(self, q, out, in_, r, w, **kw):
        e = self.E[q]
        self.op(q, lambda: e.dma_start(out=out, in_=in_, **kw), r, w, dma=True)

    @staticmethod
    def _needs_wait(oj_eng, oj_dma, oi_eng, oi_dma, typ):
        if oj_dma:
            return True
        if oj_eng == oi_eng and not oi_dma:
            if oi_eng == 'pe':
                return False
            return typ == 'raw'
        return True

    def flush(self):
        ops = self.ops
        n = len(ops)
        deps = [None] * n
        last_w, readers = self.last_w, self.readers
        for i, (eng, fn, r, w, dma) in enumerate(ops):
            d = {}
            for k in r:
                t = last_w.get(k)
                if t is not None:
                    d[t] = 'raw'
                if isinstance(k, tuple) and k and k[0] == 'ps':
                    for e2, t in readers.get(k, {}).items():
                        if e2 != eng and t not in d:
                            d[t] = 'raw'
            for k in w:
                t = last_w.get(k)
                if t is not None and t not in d:
                    d[t] = 'waw'
                for t in readers.get(k, {}).values():
                    if t not in d:
                        d[t] = 'war'
            d.pop(('p', i), None)
            deps[i] = d
            me = ('p', i)
            for k in r:
                rk = readers.setdefault(k, {})
                if dma:
                    rk[('d', i)] = me
                else:
                    rk[eng] = me
            for k in w:
                last_w[k] = me
                readers[k] = {}
        need_sig = [False] * n
        for i, (eng, fn, r, w, dma) in enumerate(ops):
            for t, typ in deps[i].items():
                if t[0] == 'p':
                    j = t[1]
                    ej, _, _, _, dj = ops[j]
                    if not dj and self._needs_wait(ej, dj, eng, dma, typ):
                        need_sig[j] = True
        last_of = {}
        for i, (eng, fn, r, w, dma) in enumerate(ops):
            if not dma:
                last_of[eng] = i
        for e, i in last_of.items():
            need_sig[i] = True
        resolved = [None] * n
        for i, (eng, fn, r, w, dma) in enumerate(ops):
            E = self.E[eng]
            waits = {}
            for t, typ in deps[i].items():
                if t[0] == 'p':
                    t2 = resolved[t[1]]
                    ej, dj = ops[t[1]][0], ops[t[1]][4]
                else:
                    t2 = t
                    ej, dj = t[1], t[0] == 'd'
                if not self._needs_wait(ej, dj, eng, dma, typ):
                    continue
                if t2[0] == 'c':
                    key = ('c', t2[1]); val = t2[2]
                else:
                    key = ('d', t2[1], t2[2]); val = t2[3]
                if waits.get(key, 0) < val:
                    waits[key] = val
            if dma:
                k = self.dmacount[eng]
                slot = k % self.NS
                if k >= self.NS:
                    key = ('d', eng, slot); val = 16 * (k // self.NS)
                    if waits.get(key, 0) < val:
                        waits[key] = val
            for key, val in waits.items():
                wk = (eng, key)
                if self.waited.get(wk, 0) >= val:
                    continue
                self.waited[wk] = val
                s = self.sem[key[1]] if key[0] == 'c' else self.dsem[key[1]][key[2]]
                E.wait_ge(s, val)
                self.n_inst += 1
            inst = fn()
            self.n_inst += 1
            if dma:
                k = self.dmacount[eng]
                slot = k % self.NS
                val = 16 * (k // self.NS + 1)
                inst.then_inc(self.dsem[eng][slot], 16)
                self.dmacount[eng] = k + 1
                resolved[i] = ('d', eng, slot, val)
            else:
                if need_sig[i]:
                    self.sigcount[eng] += 1
                    inst.then_inc(self.sem[eng], 1)
                    resolved[i] = ('c', eng, self.sigcount[eng])
        nxt = {}
        for i in range(n - 1, -1, -1):
            eng, dma = ops[i][0], ops[i][4]
            if dma:
                continue
            if resolved[i] is not None:
                nxt[eng] = resolved[i]
            else:
                resolved[i] = nxt[eng]
        for k in list(last_w.keys()):
            t = last_w[k]
            if t[0] == 'p':
                last_w[k] = resolved[t[1]]
        for k in list(readers.keys()):
            rk = readers[k]
            for kk in list(rk.keys()):
                t = rk[kk]
                if t[0] == 'p':
                    rk[kk] = resolved[t[1]]
        self.ops = []

    def barrier(self):
        self.flush()
        for eng in ('pe', 'act', 'dve', 'pool', 'sp'):
            E = self.E[eng]
            for e, c in self.sigcount.items():
                if c > 0 and e != eng and self.waited.get((eng, ('c', e)), 0) < c:
                    E.wait_ge(self.sem[e], c)
                    self.waited[(eng, ('c', e))] = c
            for q, k in self.dmacount.items():
                for slot in range(self.NS):
                    if k > slot:
                        val = 16 * ((k - 1 - slot) // self.NS + 1)
                        if self.waited.get((eng, ('d', q, slot)), 0) < val:
                            E.wait_ge(self.dsem[q][slot], val)
                            self.waited[(eng, ('d', q, slot))] = val

    def finish(self):
        self.flush()
        sp = self.E['sp']
        for e, c in self.sigcount.items():
            if c > 0:
                sp.wait_ge(self.sem[e], c)
        for q, k in self.dmacount.items():
            for slot in range(self.NS):
                if k > slot:
                    cnt = (k - 1 - slot) // self.NS + 1
                    sp.wait_ge(self.dsem[q][slot], 16 * cnt)


D = 1024
NCT = 2
NLT = 64
NT = NCT + NLT
EPS = 1e-6
POOL_WINDOWS = (2, 4, 8, 16)
JREL = {0: list(range(-1, 4)), 1: list(range(-1, 5)), 2: list(range(-2, 6)), 3: list(range(-4, 8))}
BAND_IDX = {}
_i = 0
for _g in range(4):
    for _j in JREL[_g]:
        BAND_IDX[(_g, _j)] = _i
        _i += 1
NBAND = _i


def host_constants():
    c = {}
    def axis_w(n, win):
        t = np.arange(n)
        lo = np.maximum(t - win // 2, 0); hi = np.minimum(t + win // 2, n)
        W = np.zeros((n, n), np.float64)
        for o in range(n):
            W[lo[o]:hi[o], o] = 1.0 / (hi[o] - lo[o])
        return W
    band = np.zeros((3, 128, NBAND, 512), np.float32)
    for g, win in enumerate(POOL_WINDOWS):
        Wr = axis_w(128, win); Wc = axis_w(64, win)
        for ti, b in enumerate((0, 7, 15)):
            for j in JREL[g]:
                jt = 4 * b + j
                if jt < 0 or jt >= 64:
                    continue
                M = np.einsum('ab,cd->acbd', Wr[2 * jt:2 * jt + 2, 8 * b:8 * b + 8], Wc).reshape(128, 512)
                if 0 <= j < 4:
                    M[:, j * 128:(j + 1) * 128] -= np.eye(128)
                band[ti, :, BAND_IDX[(g, j)], :] = M
    c['band'] = band
    cb = np.zeros((128, 8, 256), np.float32)
    for g, win in enumerate(POOL_WINDOWS):
        W = axis_w(256, win) - np.eye(256)
        for j in range(2):
            cb[:, g * 2 + j, :] = W[j * 128:(j + 1) * 128, :]
    c['cband'] = cb
    idx = np.arange(128)
    m = np.zeros((128, 13, 128), np.float32)
    m[:, 0] = np.eye(128)
    m[:, 1] = (idx[:, None] <= idx[None, :])
    m[:, 2] = (idx[:, None] >= idx[None, :])
    m[:, 3] = 1.0
    m[:, 4] = np.where(idx[:, None] <= idx[None, :], 0.0, -30000.0)
    m[:, 5] = np.where(idx[:, None] >= idx[None, :], 0.0, -30000.0)
    m[:, 6] = (idx[:, None] < idx[None, :])
    m[:, 7] = (idx[:, None] > idx[None, :])
    m[:, 8] = np.where(idx[:, None] > idx[None, :], 0.0, -30000.0)
    m[:, 9] = np.where(idx[:, None] < idx[None, :], 0.0, -30000.0)
    bd32 = (idx[:, None] // 32 == idx[None, :] // 32); bd64 = (idx[:, None] // 64 == idx[None, :] // 64)
    m[:, 10] = bd32; m[:, 11] = bd64 & ~bd32; m[:, 12] = ~bd64
    c['masks'] = m
    bm = np.zeros((16, 8, 128), np.float32)
    for r in range(16):
        bm[r, r % 8, :] = 1.0
    c['blockmask'] = bm
    cm = np.zeros((128, 160), np.float32)
    cm[:, 0:32] = np.arange(32)[None, :]
    cm[:, 32:40] = np.arange(8)[None, :] * 128 + np.arange(128)[:, None]
    cm[:, 40:120] = np.arange(80)[None, :] * 512
    c['cmisc'] = cm
    return c


_UNIQ = [0]


def run_interleaved(P, gens, width, rare=True):
    active = []
    rounds = 0
    it = iter(gens)
    while True:
        while len(active) < width:
            try:
                active.append(next(it))
            except StopIteration:
                break
        if not active:
            break
        for g in list(active):
            try:
                next(g)
            except StopIteration:
                active.remove(g)
        rounds += 1
        if not rare or rounds % 200 == 0:
            P.flush()
    P.flush()


class Ring:
    def __init__(self, nc, es, name, n, shape, dtype):
        _UNIQ[0] += 1
        self.tiles = [es.enter_context(nc.sbuf_tensor("%s%d_u%d" % (name, i, _UNIQ[0]), shape, dtype)) for i in range(n)]
        self.name = name
        self.i = 0

    def next(self):
        k = self.i % len(self.tiles)
        self.i += 1
        return self.tiles[k], "%s%d" % (self.name, k)


def build(stop_after=None, debug=False):
    nc = bass.Bass("TRN2", target_bir_lowering=False)
    es = contextlib.ExitStack()

    def din(name, shape, dt=F32):
        return nc.dram_tensor(name, list(shape), dt, kind="ExternalInput").ap()

    def dscr(name, shape, dt=F32):
        kind = "ExternalOutput" if debug else "Internal"
        return nc.dram_tensor(name, list(shape), dt, kind=kind).ap()

    xin = din("xin", [NT * 128, D])
    ccol = din("ccol", [128, 2, 8])
    w_ada = din("w_ada", [2, D, 6 * D]); b_ada = din("b_ada", [2, 6 * D])
    norm_mix = din("norm_mix", [2, D]); norm_ffn = din("norm_ffn", [2, D])
    w_pool = din("w_pool", [4, 256, 256]); b_pool = din("b_pool", [D]); pool_scale = din("pool_scale", [D])
    w_dn_in = din("w_dn_in", [D, 4128]); wconv = din("wconv", [128, 24, 4])
    dn_a_log = din("dn_a_log", [16]); dn_dt_bias = din("dn_dt_bias", [16]); dn_norm = din("dn_norm", [128])
    w_dn_out = din("w_dn_out", [D, D])
    w_r = din("w_r", [2, D, 36]); b_r = din("b_r", [2, 36])
    w_e_gate = din("w_e_gate", [2, 32, D, 512]); w_e_up = din("w_e_up", [2, 32, D, 512])
    w_e_down = din("w_e_down", [2, 32, 512, D]); norm_final = din("norm_final", [D])
    band = din("band", [3, 128, NBAND, 512]); cband = din("cband", [128, 8, 256])
    masks = din("masks", [128, 13, 128]); blockmask = din("blockmask", [16, 8, 128])
    cmisc = din("cmisc", [128, 160])
    out = nc.dram_tensor("out", [NLT * 128, D], F32, kind="ExternalOutput").ap()
    XS1 = dscr("XS1", [NT * 128, D])
    XS2 = dscr("XS2", [NT * 128, D])
    XS3 = dscr("XS3", [NT * 128, D])

    with es:
        P = Prog(nc, es)
        psum = es.enter_context(nc.psum_tensor("psum", [128, 4096], F32))
        PS = [psum[:, b * 512:(b + 1) * 512] for b in range(8)]
        PK = [("ps", b) for b in range(8)]

        def sb(name, shape, dt=F32, stack=es):
            _UNIQ[0] += 1
            return stack.enter_context(nc.sbuf_tensor("%s_u%d" % (name, _UNIQ[0]), list(shape), dt))

        msk = sb("msk", [128, 13, 128])
        P.dma('sp', msk[:], masks, r=["d_masks"], w=["msk"])
        ident = msk[:, 0, :]
        identb = sb("identb", [128, 128], BF16)
        P.op('dve', lambda: nc.vector.tensor_copy(out=identb[:], in_=msk[:, 0, :]), r=["msk"], w=["identb"])
        MODS_d = dscr("MODS_d", [2, 128, 6 * D])
        MVEC = {}

        def load_mods(ph, need):
            buf = sb("mvec", [128, len(need), D], F32, ph)
            MVEC.clear()
            for j, (st, ix) in enumerate(need):
                P.dma('sp', buf[:, j, :], MODS_d[0 if st == 'L' else 1][:, ix * D:(ix + 1) * D], r=["d_mods"], w=["mvec"])
                MVEC[(st, ix)] = buf[:, j, :]
        csb = sb("csb", [128, 2, 8]); sil = sb("sil", [128, 2, 8])
        rep = sb("rep", [128, 2, 8, 128], BF16)
        P.dma('sp', csb[:], ccol, r=["d_ccol"], w=["csb"])
        P.op('act', lambda: nc.scalar.activation(out=sil[:], in_=csb[:], func=AF.Silu), r=["csb"], w=["sil"])
        P.op('dve', lambda: nc.vector.tensor_copy(out=rep[:], in_=sil[:].unsqueeze(3).to_broadcast([128, 2, 8, 128])),
             r=["sil"], w=["rep"])

        def adaln(layer, ph):
            wr = Ring(nc, ph, "adaw", 2, [128, 8, 512], BF16)
            nw = sb("nw", [128, 2, D], F32, ph)
            modL = sb("modL", [128, 6 * D], F32, ph); modC = sb("modC", [128, 6 * D], F32, ph)
            P.dma('sp', modL[:], b_ada[layer].partition_broadcast(128), r=["d_b_ada"], w=["modL"])
            P.dma('sp', modC[:], b_ada[layer].partition_broadcast(128), r=["d_b_ada"], w=["modC"])
            P.dma('sp', nw[:, 0, :], norm_mix[layer].partition_broadcast(128), r=["d_nm"], w=["nw"])
            P.dma('sp', nw[:, 1, :], norm_ffn[layer].partition_broadcast(128), r=["d_nm"], w=["nw"])
            wv = w_ada[layer].rearrange("(k p) n -> p k n", p=128)
            for blk in range(12):
                wt, wk = wr.next()
                P.dma('pool', wt[:], wv[:, :, blk * 512:(blk + 1) * 512], r=["d_w_ada"], w=[wk])
                for s, (mod, mk) in enumerate(((modL, "modL"), (modC, "modC"))):
                    b = (blk * 2 + s) % 8
                    for k in range(8):
                        P.op('pe', (lambda b=b, s=s, k=k, wt=wt: nc.tensor.matmul(
                            PS[b], rep[:, s, k, :], wt[:, k, :], start=(k == 0), stop=(k == 7))),
                            r=["rep", wk], w=[PK[b]])
                    sl = slice(blk * 512, (blk + 1) * 512)
                    P.op('dve', (lambda b=b, mod=mod, sl=sl: nc.vector.tensor_tensor(
                        out=mod[:, sl], in0=PS[b], in1=mod[:, sl], op=ALU.add)), r=[PK[b], mk], w=[mk])
            for s, (mod, mk) in enumerate(((modL, "modL"), (modC, "modC"))):
                for j, col in enumerate((1, 4)):
                    sl = slice(col * D, (col + 1) * D)
                    P.op('dve', (lambda mod=mod, sl=sl, j=j: nc.vector.scalar_tensor_tensor(
                        out=mod[:, sl], in0=mod[:, sl], scalar=1.0, in1=nw[:, j, :], op0=ALU.add, op1=ALU.mult)),
                        r=[mk, "nw"], w=[mk])
            P.dma('sp', MODS_d[0], modL[:], r=["modL"], w=["d_mods"])
            P.dma('sp', MODS_d[1], modC[:], r=["modC"], w=["d_mods"])

        def mv(mod, i):
            return MVEC[(mod, i)]

        def rms_mod(ph_rings, xs_ap, xs_key, A_ap, sh_ap, mod_keys, out_ap, out_key, eps_scale=1.0 / D):
            ss, sk = ph_rings['ss'].next()
            tmp, tk = ph_rings['hxtmp'].next()
            P.op('act', lambda: nc.scalar.activation(out=tmp[:], in_=xs_ap, func=AF.Square), r=[xs_key], w=[tk])
            P.op('dve', lambda: nc.vector.reduce_sum(out=ss[:, 0:1], in_=tmp[:], axis=AX.X), r=[tk], w=[sk])
            P.op('dve', lambda: nc.vector.tensor_scalar(out=ss[:, 1:2], in0=ss[:, 0:1], scalar1=eps_scale, scalar2=EPS,
                                                        op0=ALU.mult, op1=ALU.add), r=[sk], w=[sk])
            P.op('act', lambda: nc.scalar.activation(out=ss[:, 2:3], in_=ss[:, 1:2], func=AF.Sqrt), r=[sk], w=[sk])
            P.op('dve', lambda: nc.vector.reciprocal(out=ss[:, 3:4], in_=ss[:, 2:3]), r=[sk], w=[sk])
            P.op('dve', lambda: nc.vector.scalar_tensor_tensor(out=tmp[:], in0=xs_ap, scalar=ss[:, 3:4], in1=A_ap,
                                                               op0=ALU.mult, op1=ALU.mult),
                 r=[xs_key, sk] + mod_keys, w=[tk])
            if sh_ap is None:
                P.op('pool', lambda: nc.gpsimd.tensor_copy(out=out_ap, in_=tmp[:]), r=[tk], w=[out_key])
            else:
                P.op('pool', lambda: nc.gpsimd.tensor_tensor(out=out_ap, in0=tmp[:], in1=sh_ap, op=ALU.add),
                     r=[tk] + mod_keys, w=[out_key])

        def mk_rings(ph):
            return {'ss': Ring(nc, ph, "ss", 4, [128, 4], F32),
                    'hxtmp': Ring(nc, ph, "hxtmp", 2, [128, D], F32),
                    'xs': Ring(nc, ph, "xsr", 2, [128, D], F32)}

        xin_t = xin.rearrange("(t p) d -> t p d", p=128)
        XS1_t = XS1.rearrange("(t p) d -> t p d", p=128)
        XS2_t = XS2.rearrange("(t p) d -> t p d", p=128)
        XS3_t = XS3.rearrange("(t p) d -> t p d", p=128)
        out_t = out.rearrange("(t p) d -> t p d", p=128)

        with contextlib.ExitStack() as ph:
            adaln(0, ph)
            P.barrier()
        def dump(name, ap, keys):
            shape = list(ap.shape)
            t = nc.dram_tensor(name, shape, ap.dtype, kind="ExternalOutput").ap()
            P.dma('sp', t, ap, r=keys, w=["dbg_" + name])
        if stop_after == "ada":
            dump("dbg_modL", modL[:], ["modL"]); dump("dbg_modC", modC[:], ["modC"]); dump("dbg_rep", rep[:], ["rep"])
            P.finish()
            return nc
        with contextlib.ExitStack() as ph:
            R = mk_rings(ph)
            hx0 = sb("hx0", [128, 24, D], BF16, ph)
            bandsb = sb("bandsb", [128, NBAND, 512], BF16, ph)
            cbandsb = sb("cbandsb", [128, 8, 256], BF16, ph)
            wpl = sb("wpl", [128, 8, 256], BF16, ph)
            vecs = sb("vecs", [128, 2, D], F32, ph)
            AB = sb("AB", [128, 4, D], F32, ph)
            dT = Ring(nc, ph, "dT", 1, [128, 8, 512], BF16)
            yt = Ring(nc, ph, "yt", 2, [128, D], F32)
            P.dma('pool', cbandsb[:], cband, r=["d_cband"], w=["cbandsb"])
            P.dma('pool', wpl[:], w_pool.rearrange("g (c p) e -> p (g c) e", p=128), r=["d_wpool"], w=["wpl"])
            P.dma('sp', vecs[:, 0, :], pool_scale.partition_broadcast(128), r=["d_ps"], w=["vecs"])
            P.dma('sp', vecs[:, 1, :], b_pool.partition_broadcast(128), r=["d_bp"], w=["vecs"])
            load_mods(ph, [(st, ix) for st in "LC" for ix in (0, 1, 2)])
            for s, (mod, mkey) in enumerate((("L", "mvec"), ("C", "mvec"))):
                P.op('dve', (lambda s=s, mod=mod: nc.vector.tensor_tensor(out=AB[:, 2 * s, :], in0=mv(mod, 2), in1=vecs[:, 0, :],
                                                                          op=ALU.mult)), r=[mkey, "vecs"], w=["AB"])
                P.op('dve', (lambda s=s: nc.vector.tensor_tensor(out=AB[:, 2 * s + 1, :], in0=AB[:, 2 * s, :], in1=vecs[:, 1, :],
                                                                 op=ALU.mult)), r=["AB", "vecs"], w=["AB"])

            def pool_segment(is_ctx, in_tiles, base, out_blocks):
                mod, mkey = ("C", "mvec") if is_ctx else ("L", "mvec")
                seq0 = 0 if is_ctx else NCT
                for lt in in_tiles:
                    xt, xk = R['xs'].next()
                    P.dma('sp', xt[:], xin_t[seq0 + lt], r=["d_xin"], w=[xk])
                    rms_mod(R, xt[:], xk, mv(mod, 1), mv(mod, 0), [mkey], hx0[:, lt - base, :], ("hx0", lt - base))
                cur_band = [None]
                for (ot0, ntl, btype) in out_blocks:
                    ncol = ntl * 128
                    if not is_ctx and cur_band[0] != btype:
                        P.dma('pool', bandsb[:], band[btype], r=["d_band"], w=["bandsb"])
                        cur_band[0] = btype
                    dt_, dk = dT.next()
                    for g in range(4):
                        for cc in range(2):
                            b = 2 * g + cc
                            if is_ctx:
                                lst = [(j, cbandsb[:, g * 2 + j, 0:ncol]) for j in range(2)]
                                bk = "cbandsb"
                            else:
                                lst = []
                                for j in JREL[g]:
                                    jt = ot0 + j
                                    if jt < 0 or jt >= NLT:
                                        continue
                                    lst.append((jt, bandsb[:, BAND_IDX[(g, j)], 0:ncol]))
                                bk = "bandsb"
                            for n_, (jt, rhs) in enumerate(lst):
                                P.op('pe', (lambda b=b, jt=jt, rhs=rhs, n_=n_, L=len(lst), ch=b, ncol=ncol: nc.tensor.matmul(
                                    PS[b][:, 0:ncol], hx0[:, jt - base, ch * 128:(ch + 1) * 128], rhs,
                                    start=(n_ == 0), stop=(n_ == L - 1))),
                                    r=[("hx0", jt - base), bk], w=[PK[b]])
                            if b % 2 == 0:
                                P.op('act', (lambda b=b, ncol=ncol, dt_=dt_: nc.scalar.copy(out=dt_[:, b, 0:ncol], in_=PS[b][:, 0:ncol])),
                                     r=[PK[b]], w=[(dk, b)])
                            else:
                                P.op('dve', (lambda b=b, ncol=ncol, dt_=dt_: nc.vector.tensor_copy(out=dt_[:, b, 0:ncol], in_=PS[b][:, 0:ncol])),
                                     r=[PK[b]], w=[(dk, b)])
                    for t in range(ntl):
                        gt = seq0 + ot0 + t
                        b0 = 2 * (t % 4)
                        for g in range(4):
                            pb = b0 + g // 2
                            for cc in range(2):
                                P.op('pe', (lambda pb=pb, g=g, cc=cc, t=t, dt_=dt_: nc.tensor.matmul(
                                    PS[pb][:, (g % 2) * 256:(g % 2) * 256 + 256], dt_[:, 2 * g + cc, t * 128:(t + 1) * 128],
                                    wpl[:, 2 * g + cc, :], start=(cc == 0), stop=(cc == 1))),
                                    r=[(dk, 2 * g + cc), "wpl"], w=[PK[pb]])
                        xt, xk = R['xs'].next()
                        P.dma('sp', xt[:], xin_t[gt], r=["d_xin"], w=[xk])
                        ai = 2 if is_ctx else 0
                        y, yk = yt.next()
                        for h in range(2):
                            sl = slice(h * 512, (h + 1) * 512)
                            P.op('dve', (lambda y=y, sl=sl, h=h, b0=b0, ai=ai: nc.vector.tensor_tensor(
                                out=y[:, sl], in0=PS[b0 + h], in1=AB[:, ai, sl], op=ALU.mult)), r=[PK[b0 + h], "AB"], w=[(yk, h)])
                            P.op('pool', (lambda y=y, sl=sl, xt=xt: nc.gpsimd.tensor_tensor(
                                out=y[:, sl], in0=y[:, sl], in1=xt[:, sl], op=ALU.add)), r=[(yk, h), xk], w=[(yk, h)])
                            P.op('pool', (lambda y=y, sl=sl, ai=ai: nc.gpsimd.tensor_tensor(
                                out=y[:, sl], in0=y[:, sl], in1=AB[:, ai + 1, sl], op=ALU.add)), r=[(yk, h), "AB"], w=[(yk, h)])
                        P.dma('sp', XS1_t[gt], y[:], r=[(yk, 0), (yk, 1)], w=["d_XS1"])

            pool_segment(True, [0, 1], 0, [(0, 2, 0)])
            if stop_after == "pool_dbg":
                dump("dbg_hx0", hx0[:, 0:2, :], [("hx0", 0), ("hx0", 1)])
                dump("dbg_dT", dT.tiles[0][:], [("dT0", b) for b in range(8)])
                dump("dbg_AB", AB[:], ["AB"])
                dump("dbg_cb", cbandsb[:], ["cbandsb"])
                dump("dbg_wpl", wpl[:], ["wpl"])
                P.finish()
                return nc
            for seg in range(4):
                lo = max(0, 16 * seg - 4); hi = min(NLT, 16 * seg + 20)
                blocks = [(4 * b, 4, 0 if b == 0 else (2 if b == 15 else 1)) for b in range(4 * seg, 4 * seg + 4)]
                pool_segment(False, list(range(lo, hi)), lo, blocks)
            P.flush()
        if stop_after == "pool":
            P.finish()
            return nc
        P.barrier()

        def moe_phase(layer, src_t, dst_t, tiles, final):
            SBT = 8 if final else 10
            with contextlib.ExitStack() as ph:
                R = mk_rings(ph)
                load_mods(ph, [(st, ix) for st in ("LC" if not final else "L") for ix in (3, 4, 5)])
                hxf = Ring(nc, ph, "hxf", 2, [128, D], F32)
                hxTf = Ring(nc, ph, "hxTf", 1, [128, 8, 128], F32)
                hxTb = sb("hxTb", [128, 8, SBT * 128], BF16, ph)
                acc = sb("acc", [128, SBT, D], F32, ph)
                gates = sb("gates", [128, SBT, 32], F32, ph)
                wrs = sb("wrs", [128, 8, 36], F32, ph)
                brb = sb("brb", [128, 36], F32, ph)
                rt = Ring(nc, ph, "rt", 2, [128, 96], F32)
                wg = Ring(nc, ph, "wg", 2, [128, 8, 512], BF16)
                wu = Ring(nc, ph, "wu", 2, [128, 8, 512], BF16)
                wd = Ring(nc, ph, "wd", 2, [128, 4, D], BF16)
                sgb = Ring(nc, ph, "sgb", 1, [128, 512], BF16)
                hidT = Ring(nc, ph, "hidT", 2, [128, 4, 512], BF16)
                nfb = None
                if final:
                    nfb = sb("nfb", [128, D], F32, ph)
                    P.dma('sp', nfb[:], norm_final.partition_broadcast(128), r=["d_nf"], w=["nfb"])
                P.dma('sp', wrs[:], w_r[layer].rearrange("(k p) n -> p k n", p=128), r=["d_wr"], w=["wrs"])
                P.dma('sp', brb[:], b_r[layer].partition_broadcast(128), r=["d_br"], w=["brb"])
                def route(i, r_, rk, lvl, sparse_info=None):
                    lg = r_[:, 0:36]; m4 = r_[:, 36:37]; nm4 = r_[:, 37:38]; e4 = r_[:, 40:44]; s4 = r_[:, 38:39]
                    pg = r_[:, 39:40]; ohg = r_[:, 44:48]; sel = r_[:, 48:56]; m8 = r_[:, 56:64]; d21 = r_[:, 64:65]
                    e21 = r_[:, 65:66]; w1 = r_[:, 66:67]; w2 = r_[:, 67:68]; c1 = r_[:, 72:80]; c2 = r_[:, 80:88]
                    V = nc.vector
                    def dv(fn, rk=rk):
                        P.op('dve', fn, r=[rk], w=[rk])
                    P.op('dve', lambda lg=lg: V.tensor_tensor(out=lg, in0=PS[5][:, 0:36], in1=brb[:], op=ALU.add),
                         r=[PK[5], "brb"], w=[rk])
                    if lvl <= 2:
                        P.op('dve', (lambda i=i, lg=lg: V.tensor_copy(out=gates[:, i, :], in_=lg[:, 0:32])), r=[rk], w=[("gates", i)])
                        return
                    dv(lambda: V.reduce_max(out=m4, in_=lg[:, 0:4], axis=AX.X))
                    dv(lambda: V.tensor_scalar(out=nm4, in0=m4, scalar1=-1.0, scalar2=None, op0=ALU.mult))
                    P.op('act', lambda: nc.scalar.activation(out=e4, in_=lg[:, 0:4], func=AF.Exp, bias=nm4, scale=1.0),
                         r=[rk], w=[rk])
                    dv(lambda: V.reduce_sum(out=s4, in_=e4, axis=AX.X))
                    dv(lambda: V.reciprocal(out=pg, in_=s4))
                    dv(lambda: V.tensor_scalar(out=ohg, in0=lg[:, 0:4], scalar1=m4, scalar2=None, op0=ALU.is_equal))
                    dv(lambda: V.tensor_scalar(out=sel, in0=lg[:, 4:12], scalar1=ohg[:, 0:1], scalar2=None, op0=ALU.mult))
                    for g in range(1, 4):
                        dv(lambda g=g: V.scalar_tensor_tensor(out=sel, in0=lg[:, 4 + 8 * g:12 + 8 * g], scalar=ohg[:, g:g + 1],
                                                              in1=sel, op0=ALU.mult, op1=ALU.add))
                    dv(lambda: V.max(out=m8, in_=sel))
                    dv(lambda: V.tensor_tensor(out=d21, in0=m8[:, 1:2], in1=m8[:, 0:1], op=ALU.subtract))
                    P.op('act', lambda: nc.scalar.activation(out=e21, in_=d21, func=AF.Exp), r=[rk], w=[rk])
                    dv(lambda: V.tensor_scalar(out=e21, in0=e21, scalar1=1.0, scalar2=None, op0=ALU.add))
                    dv(lambda: V.reciprocal(out=w1, in_=e21))
                    dv(lambda: V.tensor_tensor(out=w1, in0=w1, in1=pg, op=ALU.mult))
                    dv(lambda: V.tensor_tensor(out=w2, in0=pg, in1=w1, op=ALU.subtract))
                    if sparse_info is not None:
                        sparse_info(r_, rk, sel, m8, ohg, w1, w2, dv)
                        return
                    dv(lambda: V.tensor_scalar(out=c1, in0=sel, scalar1=m8[:, 0:1], scalar2=w1, op0=ALU.is_equal, op1=ALU.mult))
                    dv(lambda: V.tensor_scalar(out=c2, in0=sel, scalar1=m8[:, 1:2], scalar2=w2, op0=ALU.is_equal, op1=ALU.mult))
                    dv(lambda: V.tensor_tensor(out=c1, in0=c1, in1=c2, op=ALU.add))
                    P.op('dve', (lambda i=i, c1=c1, ohg=ohg: V.tensor_tensor(
                        out=gates[:, i, :].rearrange("p (g e) -> p g e", g=4),
                        in0=c1.unsqueeze(1).to_broadcast([128, 4, 8]), in1=ohg.unsqueeze(2).to_broadcast([128, 4, 8]),
                        op=ALU.mult)), r=[rk], w=[("gates", i)])
                for s0 in range(0, len(tiles), SBT):
                    sbt = tiles[s0:s0 + SBT]
                    import os
                    if os.environ.get("MOE_NT"):
                        sbt = sbt[:int(os.environ["MOE_NT"])]
                    n_sb = len(sbt)
                    for i, gt in enumerate(sbt):
                        mod, mkey = ("C", "mvec") if gt < NCT else ("L", "mvec")
                        xt, xk = R['xs'].next()
                        P.dma('sp', xt[:], src_t[gt], r=["d_XS1" if layer == 0 else "d_XS3"], w=[xk])
                        hx, hk = hxf.next()
                        rms_mod(R, xt[:], xk, mv(mod, 4), mv(mod, 3), [mkey], hx[:], hk)
                        import os
                        if int(os.environ.get("MOE_LVL", "9")) == 0:
                            dump("dbg_hx%d" % i, hx[:], [hk])
                            continue
                        for k in range(8):
                            b = 6 + k // 4
                            P.op('pe', (lambda b=b, k=k, hx=hx: nc.tensor.transpose(
                                PS[b][:, (k % 4) * 128:(k % 4) * 128 + 128], hx[:, k * 128:(k + 1) * 128], ident)),
                                r=[hk, "msk"], w=[PK[b]])
                        hT, hTk = hxTf.next()
                        for h in range(2):
                            b = 6 + h
                            P.op('act', (lambda b=b, h=h, hT=hT: nc.scalar.copy(
                                out=hT[:, 4 * h:4 * h + 4, :], in_=PS[b].rearrange("p (k t) -> p k t", k=4))),
                                r=[PK[b]], w=[hTk])
                        P.op('pool', (lambda i=i, hT=hT: nc.gpsimd.tensor_copy(out=hxTb[:, :, i * 128:(i + 1) * 128], in_=hT[:])),
                             r=[hTk], w=[("hxTb", i)])
                        import os
                        lvl = int(os.environ.get("MOE_LVL", "9"))
                        if lvl <= 1:
                            continue
                        for k in range(8):
                            P.op('pe', (lambda k=k, hT=hT: nc.tensor.matmul(PS[5][:, 0:36], hT[:, k, :], wrs[:, k, :],
                                                                          start=(k == 0), stop=(k == 7))),
                                 r=[hTk, "wrs"], w=[PK[5]])
                        r_, rk = rt.next()
                        route(i, r_, rk, lvl)
                    if stop_after == "moe_b1":
                        lvl = int(os.environ.get("MOE_LVL", "9"))
                        if lvl == 0:
                            P.finish()
                            return "STOP"
                        if lvl > 1:
                            dump("dbg_rt0", rt.tiles[0][:], [rt.name + "0"]); dump("dbg_rt1", rt.tiles[1][:], [rt.name + "1"])
                            dump("dbg_gates", gates[:], [("gates", i) for i in range(n_sb)])
                        if os.environ.get("NO_HXTB") is None:
                            dump("dbg_hxTb", hxTb[:], [("hxTb", i) for i in range(n_sb)])
                        else:
                            dump("dbg_hxTf", hxTf.tiles[0][:], [hxTf.name + "0"])
                        P.finish()
                        return "STOP"
                    blocks = [(t0, min(4, n_sb - t0)) for t0 in range(0, n_sb, 4)]
                    hcn = 0
                    yn = 0
                    for e in range(32):
                        wgt, wgk = wg.next(); wut, wuk = wu.next(); wdt, wdk = wd.next()
                        P.dma('pool', wgt[:], w_e_gate[layer, e].rearrange("(k p) n -> p k n", p=128), r=["d_weg"], w=[wgk])
                        P.dma('pool', wut[:], w_e_up[layer, e].rearrange("(k p) n -> p k n", p=128), r=["d_weu"], w=[wuk])
                        P.dma('pool', wdt[:], w_e_down[layer, e].rearrange("(k p) n -> p k n", p=128), r=["d_wed"], w=[wdk])
                        for (t0, ntl) in blocks:
                            ncol = ntl * 128
                            cs = slice(t0 * 128, t0 * 128 + ncol)
                            rk_h = [("hxTb", t0 + j) for j in range(ntl)]
                            hid, hidk = hidT.next()
                            for hc in range(4):
                                gb = (hcn % 2) * 2; ub = gb + 1; hcn += 1
                                for k in range(8):
                                    P.op('pe', (lambda gb=gb, k=k, hc=hc, wgt=wgt, cs=cs, ncol=ncol: nc.tensor.matmul(
                                        PS[gb][:, 0:ncol], wgt[:, k, hc * 128:(hc + 1) * 128], hxTb[:, k, cs],
                                        start=(k == 0), stop=(k == 7))), r=[wgk] + rk_h, w=[PK[gb]])
                                for k in range(8):
                                    P.op('pe', (lambda ub=ub, k=k, hc=hc, wut=wut, cs=cs, ncol=ncol: nc.tensor.matmul(
                                        PS[ub][:, 0:ncol], wut[:, k, hc * 128:(hc + 1) * 128], hxTb[:, k, cs],
                                        start=(k == 0), stop=(k == 7))), r=[wuk] + rk_h, w=[PK[ub]])
                                sg, sgk = sgb.next()
                                P.op('act', (lambda sg=sg, gb=gb, ncol=ncol: nc.scalar.activation(
                                    out=sg[:, 0:ncol], in_=PS[gb][:, 0:ncol], func=AF.Silu)), r=[PK[gb]], w=[sgk])
                                P.op('dve', (lambda sg=sg, ub=ub, ncol=ncol, hid=hid, hc=hc: nc.vector.tensor_tensor(
                                    out=hid[:, hc, 0:ncol], in0=sg[:, 0:ncol], in1=PS[ub][:, 0:ncol], op=ALU.mult)),
                                    r=[sgk, PK[ub]], w=[(hidk, hc)])
                            for t in range(ntl):
                                i = t0 + t
                                for half in range(2):
                                    yb = 4 + yn % 2; yn += 1
                                    for hc in range(4):
                                        P.op('pe', (lambda yb=yb, hc=hc, t=t, half=half, hid=hid, wdt=wdt: nc.tensor.matmul(
                                            PS[yb], hid[:, hc, t * 128:(t + 1) * 128], wdt[:, hc, half * 512:(half + 1) * 512],
                                            start=(hc == 0), stop=(hc == 3))), r=[(hidk, hc), wdk], w=[PK[yb]])
                                    sl = slice(half * 512, (half + 1) * 512)
                                    if e == 0:
                                        P.op('dve', (lambda yb=yb, i=i, sl=sl, e=e: nc.vector.tensor_scalar(
                                            out=acc[:, i, sl], in0=PS[yb], scalar1=gates[:, i, e:e + 1], scalar2=None, op0=ALU.mult)),
                                            r=[PK[yb], ("gates", i)], w=[("acc", i, half)])
                                    else:
                                        P.op('dve', (lambda yb=yb, i=i, sl=sl, e=e: nc.vector.scalar_tensor_tensor(
                                            out=acc[:, i, sl], in0=PS[yb], scalar=gates[:, i, e:e + 1], in1=acc[:, i, sl],
                                            op0=ALU.mult, op1=ALU.add)), r=[PK[yb], ("gates", i), ("acc", i, half)], w=[("acc", i, half)])
                    for i, gt in enumerate(sbt):
                        mod, mkey = ("C", "mvec") if gt < NCT else ("L", "mvec")
                        xt, xk = R['xs'].next()
                        P.dma('sp', xt[:], src_t[gt], r=["d_XS1" if layer == 0 else "d_XS3"], w=[xk])
                        P.op('pool', (lambda i=i, mod=mod: nc.gpsimd.tensor_tensor(out=acc[:, i, :], in0=acc[:, i, :], in1=mv(mod, 5), op=ALU.mult)),
                             r=[("acc", i, 0), ("acc", i, 1), mkey], w=[("acc", i, 0), ("acc", i, 1)])
                        P.op('pool', (lambda i=i, xt=xt: nc.gpsimd.tensor_tensor(out=acc[:, i, :], in0=acc[:, i, :], in1=xt[:], op=ALU.add)),
                             r=[("acc", i, 0), ("acc", i, 1), xk], w=[("acc", i, 0), ("acc", i, 1)])
                        if not final:
                            P.dma('sp', dst_t[gt], acc[:, i, :], r=[("acc", i, 0), ("acc", i, 1)], w=["d_dst%d" % layer])
                        else:
                            o_, ok = hxf.next()
                            rms_mod(R, acc[:, i, :], ("acc", i, 0), nfb[:], None, ["nfb", ("acc", i, 1)], o_[:], ok)
                            P.dma('sp', dst_t[gt - NCT], o_[:], r=[ok], w=["d_out"])
                P.barrier()

        I32 = mybir.dt.int32
        NTS = 65
        NSLOT = NTS * 512
        HXB = dscr("HXB", [NT * 128, D], BF16); XG = dscr("XG", [NSLOT, D], BF16)
        WG = dscr("WG", [NSLOT, 1]); YG = dscr("YG", [NSLOT, D])
        HXB_t = HXB.rearrange("(t p) d -> t p d", p=128)
        W2 = {"g": w_e_gate.rearrange("l e (p k) n -> (l e p) (k n)", k=8), "u": w_e_up.rearrange("l e (p k) n -> (l e p) (k n)", k=8),
              "d": w_e_down.rearrange("l e d n -> (l e d) n")}

        def moe_sparse(layer, src_t, dst_t, tiles, final):
            T_ = len(tiles)
            skey = "d_XS1" if layer == 0 else "d_XS3"
            es2 = contextlib.ExitStack()
            with es2:
                cms = sb("cms", [128, 160], F32, es2)
                info = sb("info", [128, NT, 8], F32, es2)
                posi = sb("posi", [128, NT, 2], I32, es2)
                cum = sb("cum", [128, 32], F32, es2)
                offs = sb("offs", [128, 32], F32, es2)
                te = sb("te", [128, 80], F32, es2)
                tec = sb("tec", [128, 80], F32, es2)
                P.dma('sp', cms[:], cmisc, r=["d_cm"], w=["cms"])
                P.op('pool', lambda: nc.gpsimd.memset(cum[:], 0.0), r=[], w=["cum"])
                iota = cms[:, 0:32]; base = cms[:, 32:40]; svals = cms[:, 40:120]
                V = nc.vector; G = nc.gpsimd; A = nc.scalar
                with contextlib.ExitStack() as ph:
                    R = mk_rings(ph)
                    load_mods(ph, [(st, ix) for st in ("LC" if not final else "L") for ix in (3, 4)])
                    hxf = Ring(nc, ph, "hxf", 2, [128, D], F32)
                    hxb = Ring(nc, ph, "hxb", 2, [128, D], BF16)
                    hxTf = Ring(nc, ph, "hxTf", 2, [128, 8, 128], F32)
                    wrs = sb("wrs", [128, 8, 36], F32, ph); brb = sb("brb", [128, 36], F32, ph)
                    rt = Ring(nc, ph, "rt", 2, [128, 288], F32)
                    gates = None
                    P.dma('sp', wrs[:], w_r[layer].rearrange("(k p) n -> p k n", p=128), r=["d_wr"], w=["wrs"])
                    P.dma('sp', brb[:], b_r[layer].partition_broadcast(128), r=["d_br"], w=["brb"])

                    def route(i, r_, rk):
                        lg = r_[:, 0:36]; m4 = r_[:, 36:37]; nm4 = r_[:, 37:38]; e4 = r_[:, 40:44]; s4 = r_[:, 38:39]
                        pg = r_[:, 39:40]; ohg = r_[:, 44:48]; sel = r_[:, 48:56]; m8 = r_[:, 56:64]; d21 = r_[:, 64:65]
                        e21 = r_[:, 65:66]; w1 = r_[:, 66:67]; w2 = r_[:, 67:68]; eq = r_[:, 72:88]
                        oh1 = r_[:, 96:128]; oh2 = r_[:, 128:160]; ohs = r_[:, 160:192]; rkt = r_[:, 192:224]; tmp = r_[:, 224:256]

                        def dv(fn):
                            P.op('dve', fn, r=[rk], w=[rk])
                        P.op('dve', lambda: V.tensor_tensor(out=lg, in0=PS[5 - 4 * (i % 2)][:, 0:36], in1=brb[:], op=ALU.add), r=[PK[5 - 4 * (i % 2)], "brb"], w=[rk])
                        dv(lambda: V.reduce_max(out=m4, in_=lg[:, 0:4], axis=AX.X))
                        dv(lambda: V.tensor_scalar(out=nm4, in0=m4, scalar1=-1.0, scalar2=None, op0=ALU.mult))
                        yield
                        P.op('act', lambda: A.activation(out=e4, in_=lg[:, 0:4], func=AF.Exp, bias=nm4, scale=1.0), r=[rk], w=[rk])
                        yield
                        dv(lambda: V.reduce_sum(out=s4, in_=e4, axis=AX.X))
                        dv(lambda: V.reciprocal(out=pg, in_=s4))
                        dv(lambda: V.tensor_scalar(out=ohg, in0=lg[:, 0:4], scalar1=m4, scalar2=None, op0=ALU.is_equal))
                        dv(lambda: V.tensor_scalar(out=sel, in0=lg[:, 4:12], scalar1=ohg[:, 0:1], scalar2=None, op0=ALU.mult))
                        for g in range(1, 4):
                            dv(lambda g=g: V.scalar_tensor_tensor(out=sel, in0=lg[:, 4 + 8 * g:12 + 8 * g], scalar=ohg[:, g:g + 1],
                                                                  in1=sel, op0=ALU.mult, op1=ALU.add))
                        yield
                        dv(lambda: V.max(out=m8, in_=sel))
                        dv(lambda: V.tensor_tensor(out=d21, in0=m8[:, 1:2], in1=m8[:, 0:1], op=ALU.subtract))
                        yield
                        P.op('act', lambda: A.activation(out=e21, in_=d21, func=AF.Exp), r=[rk], w=[rk])
                        yield
                        dv(lambda: V.tensor_scalar(out=e21, in0=e21, scalar1=1.0, scalar2=None, op0=ALU.add))
                        dv(lambda: V.reciprocal(out=w1, in_=e21))
                        dv(lambda: V.tensor_tensor(out=info[:, i, 2:3], in0=w1, in1=pg, op=ALU.mult))
                        dv(lambda: V.tensor_tensor(out=info[:, i, 5:6], in0=pg, in1=info[:, i, 2:3], op=ALU.subtract))
                        dv(lambda: V.tensor_scalar(out=eq[:, 0:8], in0=sel, scalar1=m8[:, 0:1], scalar2=None, op0=ALU.is_equal))
                        dv(lambda: V.tensor_scalar(out=eq[:, 8:16], in0=sel, scalar1=m8[:, 1:2], scalar2=None, op0=ALU.is_equal))
                        for j, oh in enumerate((oh1, oh2)):
                            dv(lambda j=j, oh=oh: V.tensor_tensor(out=oh.rearrange("p (g e) -> p g e", g=4),
                                                                  in0=eq[:, 8 * j:8 * j + 8].unsqueeze(1).to_broadcast([128, 4, 8]),
                                                                  in1=ohg.unsqueeze(2).to_broadcast([128, 4, 8]), op=ALU.mult))
                        dv(lambda: V.tensor_tensor(out=ohs, in0=oh1, in1=oh2, op=ALU.add))
                        yield
                        P.op('pe', lambda: nc.tensor.matmul(PS[4 - 4 * (i % 2)][:, 0:32], msk[:, 6, :], ohs, start=True, stop=True), r=[rk, "msk"], w=[PK[4 - 4 * (i % 2)]])
                        P.op('pe', lambda: nc.tensor.matmul(PS[4 - 4 * (i % 2)][:, 32:64], msk[:, 3, :], ohs, start=True, stop=True), r=[rk, "msk"], w=[PK[4 - 4 * (i % 2)]])
                        yield
                        P.op('dve', lambda: V.tensor_tensor(out=rkt, in0=PS[4 - 4 * (i % 2)][:, 0:32], in1=cum[:], op=ALU.add), r=[PK[4 - 4 * (i % 2)], "cum", rk], w=[rk])
                        P.op('dve', lambda: V.tensor_tensor(out=cum[:], in0=cum[:], in1=PS[4 - 4 * (i % 2)][:, 32:64], op=ALU.add), r=[PK[4 - 4 * (i % 2)], "cum", rk], w=["cum"])
                        for j, oh in enumerate((oh1, oh2)):
                            dv(lambda oh=oh: V.tensor_tensor(out=tmp, in0=oh, in1=rkt, op=ALU.mult))
                            dv(lambda j=j: V.reduce_sum(out=info[:, i, 3 * j + 1:3 * j + 2], in_=tmp, axis=AX.X))
                            dv(lambda oh=oh: V.tensor_tensor(out=tmp, in0=oh, in1=iota, op=ALU.mult))
                            dv(lambda j=j: V.reduce_sum(out=info[:, i, 3 * j:3 * j + 1], in_=tmp, axis=AX.X))

                    def sa_tile(i, gt):
                        mod, mkey = ("C", "mvec") if gt < NCT else ("L", "mvec")
                        xt, xk = R['xs'].next()
                        P.dma('sp', xt[:], src_t[gt], r=[skey], w=[xk])
                        hx, hk = hxf.next()
                        rms_mod(R, xt[:], xk, mv(mod, 4), mv(mod, 3), [mkey], hx[:], hk)
                        yield
                        hb, hbk = hxb.next()
                        P.op('pool', (lambda hb=hb, hx=hx: G.tensor_copy(out=hb[:], in_=hx[:])), r=[hk], w=[hbk])
                        P.dma('sp', HXB_t[gt], hb[:], r=[hbk], w=["d_HXB"])
                        for k in range(8):
                            b = (6 if i % 2 == 0 else 2) + k // 4
                            P.op('pe', (lambda b=b, k=k, hx=hx: nc.tensor.transpose(
                                PS[b][:, (k % 4) * 128:(k % 4) * 128 + 128], hx[:, k * 128:(k + 1) * 128], ident)),
                                r=[hk, "msk"], w=[PK[b]])
                        yield
                        hT, hTk = hxTf.next()
                        for h in range(2):
                            b = (6 if i % 2 == 0 else 2) + h
                            P.op('act', (lambda b=b, h=h, hT=hT: A.copy(
                                out=hT[:, 4 * h:4 * h + 4, :], in_=PS[b].rearrange("p (k t) -> p k t", k=4))), r=[PK[b]], w=[hTk])
                        for k in range(8):
                            P.op('pe', (lambda k=k, hT=hT: nc.tensor.matmul(PS[5 - 4 * (i % 2)][:, 0:36], hT[:, k, :], wrs[:, k, :],
                                                                          start=(k == 0), stop=(k == 7))), r=[hTk, "wrs"], w=[PK[5 - 4 * (i % 2)]])
                        yield
                        r_, rk = rt.next()
                        yield from route(i, r_, rk)

                    run_interleaved(P, (sa_tile(i, gt) for i, gt in enumerate(tiles)), 2)
                    ci = sb("ci", [128, 32], I32, ph); pn = sb("pn", [128, 32], F32, ph)
                    sa = sb("sa", [128, 32], F32, ph); sb_ = sb("sb_", [128, 32], F32, ph)
                    P.op('dve', lambda: V.tensor_copy(out=ci[:], in_=cum[:]), r=["cum"], w=["ci"])
                    P.op('dve', lambda: V.tensor_scalar(out=ci[:], in0=ci[:], scalar1=511, scalar2=None, op0=ALU.add), r=["ci"], w=["ci"])
                    P.op('dve', lambda: V.tensor_scalar(out=ci[:], in0=ci[:], scalar1=9, scalar2=None, op0=ALU.arith_shift_right), r=["ci"], w=["ci"])
                    P.op('dve', lambda: V.tensor_scalar(out=ci[:], in0=ci[:], scalar1=9, scalar2=None, op0=ALU.logical_shift_left), r=["ci"], w=["ci"])
                    P.op('dve', lambda: V.tensor_copy(out=pn[:], in_=ci[:]), r=["ci"], w=["pn"])
                    P.op('dve', lambda: V.tensor_copy(out=sa[:], in_=pn[:]), r=["pn"], w=["sa"])
                    a_, b_ = sa, sb_
                    ak, bk = "sa", "sb_"
                    for sh in (1, 2, 4, 8, 16):
                        P.op('dve', (lambda a_=a_, b_=b_, sh=sh: V.tensor_copy(out=b_[:, 0:sh], in_=a_[:, 0:sh])), r=[ak], w=[bk])
                        P.op('dve', (lambda a_=a_, b_=b_, sh=sh: V.tensor_tensor(out=b_[:, sh:32], in0=a_[:, sh:32], in1=a_[:, 0:32 - sh], op=ALU.add)),
                             r=[ak, bk], w=[bk])
                        a_, b_, ak, bk = b_, a_, bk, ak
                    incl, inclk = a_, ak
                    P.op('dve', lambda: V.tensor_tensor(out=offs[:], in0=incl[:], in1=pn[:], op=ALU.subtract), r=[inclk, "pn"], w=["offs"])
                    P.op('pool', lambda: G.memset(te[:], 0.0), r=[], w=["te"])
                    for e in range(32):
                        P.op('dve', (lambda e=e: V.scalar_tensor_tensor(out=te[:], in0=svals, scalar=incl[:, e:e + 1], in1=te[:], op0=ALU.is_ge, op1=ALU.add)),
                             r=[inclk, "te", "cms"], w=["te"])
                    P.op('dve', lambda: V.tensor_scalar(out=tec[:], in0=te[:], scalar1=31.0, scalar2=None, op0=ALU.min), r=["te"], w=["tec"])
                    P.barrier()
                if stop_after == "sA":
                    P.finish(); return "STOP"
                with contextlib.ExitStack() as ph:
                    zt = sb("zt", [128, 4096], BF16, ph)
                    hb2 = Ring(nc, ph, "hb2", 3, [128, D], BF16)
                    pt = Ring(nc, ph, "pt", 3, [128, 72], F32)
                    P.op('pool', lambda: G.memset(zt[:], 0.0), r=[], w=["zt"])
                    XGz = XG.rearrange("(q p f) d -> q p (f d)", p=128, f=4)
                    for q in range(NTS):
                        P.dma('sp', XGz[q], zt[:], r=["zt"], w=["d_XG"])
                    def sc_tile(i, gt):
                        p_, pk_ = pt.next()
                        for j in range(2):
                            P.op('dve', (lambda p_=p_, i=i, j=j: V.tensor_scalar(out=p_[:, 0:32], in0=iota, scalar1=info[:, i, 3 * j:3 * j + 1], scalar2=None, op0=ALU.is_equal)),
                                 r=["info", "cms", pk_], w=[pk_])
                            P.op('dve', (lambda p_=p_: V.tensor_tensor(out=p_[:, 0:32], in0=p_[:, 0:32], in1=offs[:], op=ALU.mult)), r=[pk_, "offs"], w=[pk_])
                            P.op('dve', (lambda p_=p_, j=j: V.reduce_sum(out=p_[:, 32 + j:33 + j], in_=p_[:, 0:32], axis=AX.X)), r=[pk_], w=[pk_])
                            P.op('dve', (lambda p_=p_, i=i, j=j: V.tensor_tensor(out=p_[:, 32 + j:33 + j], in0=p_[:, 32 + j:33 + j], in1=info[:, i, 3 * j + 1:3 * j + 2], op=ALU.add)),
                                 r=[pk_, "info"], w=[pk_])
                        yield
                        P.op('dve', (lambda p_=p_, i=i: V.tensor_copy(out=posi[:, i, :], in_=p_[:, 32:34])), r=[pk_], w=[("posi", i)])
                        hb, hbk = hb2.next()
                        P.dma('sp', hb[:], HXB_t[gt], r=["d_HXB"], w=[hbk])
                        yield
                        for j in range(2):
                            P.op('pool', (lambda hb=hb, i=i, j=j: G.indirect_dma_start(
                                out=XG[:, :], out_offset=bass.IndirectOffsetOnAxis(ap=posi[:, i, j:j + 1], axis=0), in_=hb[:, :], in_offset=None)),
                                r=[hbk, ("posi", i), "d_XG"], w=["d_XGs"], dma=True)
                            P.op('pool', (lambda i=i, j=j: G.indirect_dma_start(
                                out=WG[:, :], out_offset=bass.IndirectOffsetOnAxis(ap=posi[:, i, j:j + 1], axis=0), in_=info[:, i, 3 * j + 2:3 * j + 3], in_offset=None)),
                                r=["info", ("posi", i)], w=["d_WG"], dma=True)
                        yield

                    run_interleaved(P, (sc_tile(i, gt) for i, gt in enumerate(tiles)), 3)
                    P.barrier()
                if stop_after == "sC":
                    P.finish(); return "STOP"
                with contextlib.ExitStack() as ph:
                    wg = Ring(nc, ph, "wg", 2, [128, 8, 512], BF16); wu = Ring(nc, ph, "wu", 2, [128, 8, 512], BF16)
                    wd = Ring(nc, ph, "wd", 2, [128, 4, D], BF16)
                    xg = Ring(nc, ph, "xg", 2, [128, 4, D], BF16); xT = Ring(nc, ph, "xT", 2, [128, 8, 512], BF16)
                    wgt = Ring(nc, ph, "wgt", 2, [128, 4], F32)
                    idf = Ring(nc, ph, "idf", 2, [128, 12], F32); idi = Ring(nc, ph, "idi", 2, [128, 12], I32)
                    sgb = Ring(nc, ph, "sgb", 2, [128, 512], BF16); hidT = Ring(nc, ph, "hidT", 2, [128, 4, 512], BF16)
                    yg = Ring(nc, ph, "yg", 2, [128, D], F32)
                    hcn = [0]; yn = [0]

                    def slot_tile(s_):
                        f_, fk = idf.next(); ii, ik = idi.next()
                        P.op('dve', lambda: V.scalar_tensor_tensor(out=f_[:, 0:1], in0=tec[:, s_:s_ + 1], scalar=128.0, in1=base[:, 0:1],
                                                                   op0=ALU.mult, op1=ALU.add), r=["tec", "cms"], w=[fk])
                        P.op('dve', lambda: V.scalar_tensor_tensor(out=f_[:, 8:12], in0=tec[:, s_:s_ + 1].to_broadcast([128, 4]), scalar=512.0, in1=base[:, 0:4],
                                                                   op0=ALU.mult, op1=ALU.add), r=["tec", "cms", fk], w=[fk])
                        if layer > 0:
                            P.op('dve', lambda: V.tensor_scalar(out=f_[:, 0:1], in0=f_[:, 0:1], scalar1=float(layer * 32 * 128), scalar2=None, op0=ALU.add), r=[fk], w=[fk])
                            P.op('dve', lambda: V.tensor_scalar(out=f_[:, 8:12], in0=f_[:, 8:12], scalar1=float(layer * 32 * 512), scalar2=None, op0=ALU.add), r=[fk], w=[fk])
                        P.op('dve', lambda: V.tensor_copy(out=ii[:], in_=f_[:]), r=[fk], w=[ik])
                        wgt_, wgk = wg.next(); wut, wuk = wu.next(); wdt, wdk = wd.next()
                        P.op('pool', lambda: G.indirect_dma_start(out=wgt_[:].rearrange("p k n -> p (k n)"), out_offset=None, in_=W2["g"],
                                                                  in_offset=bass.IndirectOffsetOnAxis(ap=ii[:, 0:1], axis=0)),
                             r=[ik], w=[(wgk, k) for k in range(8)], dma=True)
                        P.op('pool', lambda: G.indirect_dma_start(out=wut[:].rearrange("p k n -> p (k n)"), out_offset=None, in_=W2["u"],
                                                                  in_offset=bass.IndirectOffsetOnAxis(ap=ii[:, 0:1], axis=0)),
                             r=[ik], w=[(wuk, k) for k in range(8)], dma=True)
                        for k in range(4):
                            P.op('pool', (lambda k=k: G.indirect_dma_start(out=wdt[:, k, :], out_offset=None, in_=W2["d"],
                                                                         in_offset=bass.IndirectOffsetOnAxis(ap=ii[:, 8 + k:9 + k], axis=0))),
                                 r=[ik], w=[(wdk, k)], dma=True)
                        yield
                        x_, xk_ = xg.next(); xt_, xtk = xT.next(); g4, g4k = wgt.next()
                        P.dma('sp', x_[:], XG[s_ * 512:(s_ + 1) * 512, :].rearrange("(t p) d -> p t d", p=128), r=["d_XGs", "d_XG"], w=[xk_])
                        P.dma('sp', g4[:], WG[s_ * 512:(s_ + 1) * 512, :].rearrange("(t p) o -> p (t o)", p=128), r=["d_WG"], w=[g4k], allow_slow_non_contiguous=True)
                        yield
                        for t in range(4):
                            b = 6 + t % 2
                            psb = PS[b].bitcast(BF16)
                            for k in range(8):
                                P.op('pe', (lambda psb=psb, k=k, t=t: nc.tensor.transpose(psb[:, k * 128:(k + 1) * 128], x_[:, t, k:D:8], identb[:])),
                                     r=[xk_, "identb"], w=[PK[b]])
                            P.op('act', (lambda psb=psb, t=t: A.copy(out=xt_[:, :, t * 128:(t + 1) * 128], in_=psb.rearrange("p (k c) -> p k c", k=8))),
                                 r=[PK[b]], w=[(xtk, t)])
                        yield
                        xtkeys = [(xtk, t) for t in range(4)]
                        hid, hidk = hidT.next()
                        for hc in range(4):
                            gb = (hcn[0] % 2) * 2; ub = gb + 1; hcn[0] += 1
                            for k in range(8):
                                P.op('pe', (lambda gb=gb, k=k, hc=hc: nc.tensor.matmul(PS[gb], wgt_[:, k, hc * 128:(hc + 1) * 128], xt_[:, k, :],
                                                                                    start=(k == 0), stop=(k == 7))), r=[(wgk, k)] + xtkeys, w=[PK[gb]])
                            for k in range(8):
                                P.op('pe', (lambda ub=ub, k=k, hc=hc: nc.tensor.matmul(PS[ub], wut[:, k, hc * 128:(hc + 1) * 128], xt_[:, k, :],
                                                                                    start=(k == 0), stop=(k == 7))), r=[(wuk, k)] + xtkeys, w=[PK[ub]])
                            yield
                            sg, sgk = sgb.next()
                            P.op('act', (lambda sg=sg, gb=gb: A.activation(out=sg[:], in_=PS[gb], func=AF.Silu)), r=[PK[gb]], w=[sgk])
                            P.op('dve', (lambda sg=sg, ub=ub, hc=hc: V.tensor_tensor(out=hid[:, hc, :], in0=sg[:], in1=PS[ub], op=ALU.mult)),
                                 r=[sgk, PK[ub]], w=[(hidk, hc)])
                        for t in range(4):
                            yield
                            y_, yk = yg.next()
                            for half in range(2):
                                yb_ = 4 + yn[0] % 2; yn[0] += 1
                                for hc in range(4):
                                    P.op('pe', (lambda yb_=yb_, hc=hc, t=t, half=half: nc.tensor.matmul(
                                        PS[yb_], hid[:, hc, t * 128:(t + 1) * 128], wdt[:, hc, half * 512:(half + 1) * 512],
                                        start=(hc == 0), stop=(hc == 3))), r=[(hidk, hc), (wdk, hc)], w=[PK[yb_]])
                                P.op('act', (lambda yb_=yb_, half=half, t=t, y_=y_: A.activation(out=y_[:, half * 512:(half + 1) * 512], in_=PS[yb_], func=AF.Copy,
                                                                                            scale=g4[:, t:t + 1])), r=[PK[yb_], g4k], w=[(yk, half)])
                            P.dma('sp', YG[s_ * 512 + t * 128:s_ * 512 + (t + 1) * 128, :], y_[:], r=[(yk, 0), (yk, 1)], w=["d_YG"])

                    run_interleaved(P, (slot_tile(s_) for s_ in range(NTS)), 2)
                    P.barrier()
                if stop_after == "sD":
                    P.finish(); return "STOP"
                with contextlib.ExitStack() as ph:
                    R = mk_rings(ph)
                    load_mods(ph, [(st, 5) for st in ("LC" if not final else "L")])
                    y1 = Ring(nc, ph, "y1", 3, [128, D], F32); y2 = Ring(nc, ph, "y2", 3, [128, D], F32)
                    ofin = Ring(nc, ph, "ofin", 3, [128, D], F32); xs3 = Ring(nc, ph, "xs3", 3, [128, D], F32)
                    nfb = None
                    if final:
                        nfb = sb("nfb", [128, D], F32, ph)
                        P.dma('sp', nfb[:], norm_final.partition_broadcast(128), r=["d_nf"], w=["nfb"])

                    def comb(i, gt):
                        mod, mkey = ("C", "mvec") if gt < NCT else ("L", "mvec")
                        a_, ak_ = y1.next(); b_, bk_ = y2.next()
                        for j, (dst, dk_) in enumerate(((a_, ak_), (b_, bk_))):
                            P.op('pool', (lambda dst=dst, j=j: G.indirect_dma_start(out=dst[:, :], out_offset=None, in_=YG[:, :],
                                                                                  in_offset=bass.IndirectOffsetOnAxis(ap=posi[:, i, j:j + 1], axis=0))),
                                 r=[("posi", i), "d_YG"], w=[dk_], dma=True)
                        xt, xk = xs3.next()
                        P.dma('sp', xt[:], src_t[gt], r=[skey], w=[xk])
                        yield
                        P.op('pool', lambda: G.tensor_tensor(out=a_[:], in0=a_[:], in1=b_[:], op=ALU.add), r=[ak_, bk_], w=[ak_])
                        yield
                        P.op('dve', lambda: V.tensor_tensor(out=a_[:], in0=a_[:], in1=mv(mod, 5), op=ALU.mult), r=[ak_, mkey], w=[ak_])
                        yield
                        P.op('pool', lambda: G.tensor_tensor(out=a_[:], in0=a_[:], in1=xt[:], op=ALU.add), r=[ak_, xk], w=[ak_])
                        yield
                        if not final:
                            P.dma('sp', dst_t[gt], a_[:], r=[ak_], w=["d_dst%d" % layer])
                        else:
                            o_, ok = ofin.next()
                            rms_mod(R, a_[:], ak_, nfb[:], None, ["nfb"], o_[:], ok)
                            P.dma('sp', dst_t[gt - NCT], o_[:], r=[ok], w=["d_out"])

                    run_interleaved(P, (comb(i, gt) for i, gt in enumerate(tiles)), 3)
                    P.barrier()

        import os
        MOE = moe_sparse if os.environ.get("DENSE_MOE") is None else moe_phase
        if MOE(0, XS1_t, XS2_t, list(range(NT)), False) == "STOP":
            return nc
        if stop_after == "moe0":
            P.finish()
            return nc

        bfd = lambda name, shape: dscr(name, shape, BF16)
        QT_d = bfd("QT_d", [NT, 128, 8, 128]); KT_d = bfd("KT_d", [NT, 128, 8, 128])
        KK_d = bfd("KK_d", [NT, 128, 8, 128]); VV_d = bfd("VV_d", [NT, 128, 8, 128])
        ZZ_d = bfd("ZZ_d", [NT, 128, D]); GB_d = dscr("GB_d", [NT, 128, 32])
        OF_d = dscr("OF_d", [2, NLT, 128, D])
        with contextlib.ExitStack() as ph:
            adaln(1, ph)
            P.barrier()

        with contextlib.ExitStack() as ph:
            R = mk_rings(ph)
            load_mods(ph, [(st, ix) for st in "LC" for ix in (0, 1)])
            SBT1 = 4
            hxf = Ring(nc, ph, "c1hx", 1, [128, D], F32)
            hxT = sb("c1hxT", [128, 8, (SBT1 + 2) * 128], BF16, ph)
            pT = Ring(nc, ph, "pT", 5, [128, SBT1 * 128 + 4], F32)
            cv = Ring(nc, ph, "cv", 5, [128, SBT1 * 128], F32)
            sqb = Ring(nc, ph, "sqb", 4, [128, SBT1 * 128], BF16)
            rsb = Ring(nc, ph, "rsb", 4, [128, SBT1 * 128], F32)
            kf = Ring(nc, ph, "kf", 4, [128, SBT1 * 128], F32)
            qst = sb("qst", [128, SBT1, 8, 128], BF16, ph); kst = sb("kst", [128, SBT1, 8, 128], BF16, ph)
            ktst = sb("ktst", [128, SBT1, 8, 128], BF16, ph); vtst = sb("vtst", [128, SBT1, 8, 128], BF16, ph)
            win_sb = sb("win_sb", [128, 8, 3072], BF16, ph)
            wz = sb("wz", [128, 8, 1056], BF16, ph)
            wcs = sb("wcs", [128, 24, 4], F32, ph)
            onesb = sb("onesb", [128, 128], BF16, ph)
            c16 = sb("c16", [128, 2, 16], F32, ph)
            zsb = Ring(nc, ph, "zsb", 1, [128, D], BF16)
            gbr = Ring(nc, ph, "gbr", 2, [128, 64], F32)
            P.dma('pool', wz[:], w_dn_in.rearrange("(k p) n -> p k n", p=128)[:, :, 3072:4128], r=["d_win"], w=["wz"])
            P.dma('sp', wcs[:], wconv, r=["d_wconv"], w=["wcs"])
            P.op('dve', lambda: nc.vector.tensor_copy(out=onesb[:], in_=msk[:, 3, :]), r=["msk"], w=["onesb"])
            P.dma('sp', c16[:, 0, :], dn_dt_bias.partition_broadcast(128), r=["d_dtb"], w=["c16"])
            P.dma('sp', c16[:, 1, :], dn_a_log.partition_broadcast(128), r=["d_alog"], w=["c16"])
            P.op('act', lambda: nc.scalar.activation(out=c16[:, 1, :], in_=c16[:, 1, :], func=AF.Exp), r=["c16"], w=["c16"])
            P.op('dve', lambda: nc.vector.tensor_scalar(out=c16[:, 1, :], in0=c16[:, 1, :], scalar1=-1.0, scalar2=None, op0=ALU.mult),
                 r=["c16"], w=["c16"])
            win_v = w_dn_in.rearrange("(k p) n -> p k n", p=128)
            for j6 in range(6):
                P.dma('pool', win_sb[:, :, j6 * 512:(j6 + 1) * 512], win_v[:, :, j6 * 512:(j6 + 1) * 512], r=["d_win"], w=[("win_sb", j6)])

            def c1_tile_norm(gt, slot, mod, mkey):
                xt, xk = R['xs'].next()
                P.dma('sp', xt[:], XS2_t[gt], r=["d_dst0"], w=[xk])
                hx, hk = hxf.next()
                rms_mod(R, xt[:], xk, mv(mod, 1), mv(mod, 0), [mkey], hx[:], hk)
                for k in range(8):
                    b = 6 + k // 4
                    P.op('pe', (lambda b=b, k=k, hx=hx: nc.tensor.transpose(
                        PS[b][:, (k % 4) * 128:(k % 4) * 128 + 128], hx[:, k * 128:(k + 1) * 128], ident)),
                        r=[hk, "msk"], w=[PK[b]])
                for h in range(2):
                    b = 6 + h
                    P.op('act', (lambda b=b, h=h, slot=slot: nc.scalar.copy(
                        out=hxT[:, 4 * h:4 * h + 4, slot * 128:(slot + 1) * 128],
                        in_=PS[b].rearrange("p (k t) -> p k t", k=4))), r=[PK[b]], w=[("hxT", slot)])

            def c1_zab(gt, slot, is_ctx):
                hk = [("hxT", slot)]
                if not is_ctx:
                    zs, zk = zsb.next()
                    for half in range(2):
                        b = 4 + half
                        for k in range(8):
                            P.op('pe', (lambda b=b, k=k, half=half, slot=slot: nc.tensor.matmul(
                                PS[b], hxT[:, k, slot * 128:(slot + 1) * 128], wz[:, k, half * 512:(half + 1) * 512],
                                start=(k == 0), stop=(k == 7))), r=hk + ["wz"], w=[PK[b]])
                        P.op('act', (lambda b=b, half=half, zs=zs: nc.scalar.activation(
                            out=zs[:, half * 512:(half + 1) * 512], in_=PS[b], func=AF.Silu)), r=[PK[b]], w=[(zk, half)])
                    P.dma('sp', ZZ_d[gt], zs[:], r=[(zk, 0), (zk, 1)], w=["d_ZZ"])
                for k in range(8):
                    P.op('pe', (lambda k=k, slot=slot: nc.tensor.matmul(
                        PS[3][:, 0:32], hxT[:, k, slot * 128:(slot + 1) * 128], wz[:, k, 1024:1056],
                        start=(k == 0), stop=(k == 7))), r=hk + ["wz"], w=[PK[3]])
                g_, gk = gbr.next()
                V = nc.vector
                ab = g_[:, 0:32].rearrange("p (f h) -> p f h", f=4)
                o4 = g_[:, 32:64].rearrange("p (f h) -> p f h", f=4)
                P.op('dve', lambda: V.tensor_copy(out=g_[:, 0:32], in_=PS[3][:, 0:32]), r=[PK[3]], w=[gk])
                P.op('dve', lambda: V.tensor_tensor(out=o4[:, 0::2, :], in0=ab[:, 0::2, :],
                                                    in1=c16[:, 0, :].rearrange("p (d h) -> p d h", d=2), op=ALU.add),
                     r=[gk, "c16"], w=[gk])
                P.op('act', lambda: nc.scalar.activation(out=o4[:, 0::2, :], in_=o4[:, 0::2, :], func=AF.Exp), r=[gk], w=[gk])
                P.op('dve', lambda: V.tensor_scalar(out=o4[:, 0::2, :], in0=o4[:, 0::2, :], scalar1=1.0, scalar2=None, op0=ALU.add),
                     r=[gk], w=[gk])
                P.op('act', lambda: nc.scalar.activation(out=o4[:, 0::2, :], in_=o4[:, 0::2, :], func=AF.Ln), r=[gk], w=[gk])
                P.op('dve', lambda: V.tensor_tensor(out=o4[:, 0::2, :], in0=o4[:, 0::2, :],
                                                    in1=c16[:, 1, :].rearrange("p (d h) -> p d h", d=2), op=ALU.mult),
                     r=[gk, "c16"], w=[gk])
                P.op('act', lambda: nc.scalar.activation(out=o4[:, 1::2, :], in_=ab[:, 1::2, :], func=AF.Sigmoid), r=[gk], w=[gk])
                P.dma('sp', GB_d[gt], g_[:, 32:64], r=[gk], w=["d_GB"])

            def c1_chunk(cc, seq0, nseq, t0, t1, hbase_tile):
                W = (t1 - t0) * 128
                ntile = t1 - t0
                wk = ("win_sb", cc // 4)
                p_, pk = pT.next()
                tok0 = t0 * 128 - 2
                lo = max(tok0, 0); hi = min(t1 * 128 + 1, nseq * 128)
                if lo > tok0:
                    P.op('pool', lambda: nc.gpsimd.memset(p_[:, 0:lo - tok0], 0.0), r=[], w=[(pk, 'l')])
                if hi < t1 * 128 + 1:
                    P.op('pool', lambda: nc.gpsimd.memset(p_[:, hi - tok0:W + 3], 0.0), r=[], w=[(pk, 'r')])
                a = lo
                wi = 0
                while a < hi:
                    b_ = min(a + 512, hi)
                    bank = (wi + cc) % 3
                    hk = [("hxT", s_) for s_ in range((a // 128) - hbase_tile, ((b_ - 1) // 128) - hbase_tile + 1)]
                    for k in range(8):
                        P.op('pe', (lambda k=k, bank=bank, a=a, b_=b_: nc.tensor.matmul(
                            PS[bank][:, 0:b_ - a], win_sb[:, k, cc * 128:(cc + 1) * 128], hxT[:, k, a - hbase_tile * 128:b_ - hbase_tile * 128],
                            start=(k == 0), stop=(k == 7))), r=[wk] + hk, w=[PK[bank]])
                    P.op('act', (lambda bank=bank, a=a, b_=b_: nc.scalar.copy(out=p_[:, a - tok0:b_ - tok0], in_=PS[bank][:, 0:b_ - a])),
                         r=[PK[bank]], w=[(pk, wi)])
                    a = b_; wi += 1
                yield
                pkeys = [(pk, j) for j in range(wi)] + [(pk, 'l'), (pk, 'r')]
                c_, ck = cv.next()
                P.op('dve', lambda: nc.vector.tensor_scalar(out=c_[:, 0:W], in0=p_[:, 0:W], scalar1=wcs[:, cc, 0:1], scalar2=None, op0=ALU.mult),
                     r=pkeys + ["wcs"], w=[ck])
                for tap in range(1, 4):
                    eng = 'dve'
                    E_ = nc.gpsimd if eng == 'pool' else nc.vector
                    P.op(eng, (lambda tap=tap, E_=E_: E_.scalar_tensor_tensor(out=c_[:, 0:W], in0=p_[:, tap:tap + W], scalar=wcs[:, cc, tap:tap + 1],
                                                                             in1=c_[:, 0:W], op0=ALU.mult, op1=ALU.add)),
                         r=pkeys + ["wcs", ck], w=[ck])
                yield
                P.op('act', lambda: nc.scalar.activation(out=c_[:, 0:W], in_=c_[:, 0:W], func=AF.Silu), r=[ck], w=[ck])
                yield
                kind = cc // 8; h = cc % 8
                src = c_
                srck = ck
                if kind < 2:
                    sq, sqk = sqb.next(); rs, rk_ = rsb.next()
                    P.op('pool', lambda: nc.gpsimd.tensor_tensor(out=sq[:, 0:W], in0=c_[:, 0:W], in1=c_[:, 0:W], op=ALU.mult), r=[ck], w=[sqk])
                    for j in range(0, W, 512):
                        n_ = min(512, W - j)
                        bank = 3 + (j // 512) % 2
                        P.op('pe', (lambda j=j, n_=n_, bank=bank: nc.tensor.matmul(PS[bank][:, 0:n_], onesb[:], sq[:, j:j + n_], start=True, stop=True)),
                             r=[sqk, "onesb"], w=[PK[bank]])
                        P.op('dve', (lambda j=j, n_=n_, bank=bank: nc.vector.tensor_scalar(out=rs[:, j:j + n_], in0=PS[bank][:, 0:n_], scalar1=EPS, scalar2=None, op0=ALU.add)),
                             r=[PK[bank]], w=[(rk_, j)])
                    yield
                    rkeys = [(rk_, j) for j in range(0, W, 512)]
                    P.op('act', lambda: nc.scalar.activation(out=rs[:, 0:W], in_=rs[:, 0:W], func=AF.Sqrt), r=rkeys, w=rkeys)
                    yield
                    P.op('dve', lambda: nc.vector.reciprocal(out=rs[:, 0:W], in_=rs[:, 0:W]), r=rkeys, w=rkeys)
                    if kind == 0:
                        P.op('dve', lambda: nc.vector.scalar_tensor_tensor(
                            out=qst[:, 0:ntile, h, :], in0=c_[:, 0:W].rearrange("p (t k) -> p t k", k=128), scalar=float(128 ** -0.5),
                            in1=rs[:, 0:W].rearrange("p (t k) -> p t k", k=128), op0=ALU.mult, op1=ALU.mult), r=[ck] + rkeys, w=[("qst", h)])
                        return
                    kf_, kfk = kf.next()
                    P.op('dve', lambda: nc.vector.tensor_tensor(out=kf_[:, 0:W], in0=c_[:, 0:W], in1=rs[:, 0:W], op=ALU.mult), r=[ck] + rkeys, w=[kfk])
                    P.op('pool', lambda: nc.gpsimd.tensor_copy(out=kst[:, 0:ntile, h, :], in_=kf_[:, 0:W].rearrange("p (t k) -> p t k", k=128)),
                         r=[kfk], w=[("kst", h)])
                    src = kf_; srck = kfk
                yield
                dst = ktst if kind == 1 else vtst
                dkey = "ktst" if kind == 1 else "vtst"
                for j in range(0, ntile, 4):
                    n_ = min(4, ntile - j)
                    bank = 5 + (j // 4) % 2
                    for t in range(n_):
                        P.op('pe', (lambda t=t, j=j, bank=bank: nc.tensor.transpose(
                            PS[bank][:, t * 128:(t + 1) * 128], src[:, (j + t) * 128:(j + t + 1) * 128], ident)),
                            r=[srck, "msk"], w=[PK[bank]])
                    P.op('act', (lambda j=j, n_=n_, bank=bank: nc.scalar.copy(
                        out=dst[:, j:j + n_, h, :], in_=PS[bank][:, 0:n_ * 128].rearrange("p (t k) -> p t k", k=128))),
                        r=[PK[bank]], w=[(dkey, h, j)])

            def c1_superblock(seq0, nseq, t0, t1, is_ctx):
                mod, mkey = ("C", "mvec") if is_ctx else ("L", "mvec")
                hb = max(t0 - 1, 0); he = min(t1 + 1, nseq)
                for lt in range(hb, he):
                    c1_tile_norm(seq0 + lt, lt - hb, mod, mkey)
                for lt in range(t0, t1):
                    c1_zab(seq0 + lt, lt - hb, is_ctx)
                run_interleaved(P, (c1_chunk(cc, seq0, nseq, t0, t1, hb) for cc in range(24)), 4)
                nt_ = t1 - t0
                g0 = seq0 + t0
                for (dst, st, key) in ((QT_d, qst, "qst"), (KT_d, kst, "kst")):
                    P.dma('sp', dst[g0:g0 + nt_].rearrange("t p h k -> p t h k"), st[:, 0:nt_, :, :],
                          r=[(key, h) for h in range(8)], w=["d_" + key])
                for (dst, st, key) in ((KK_d, ktst, "ktst"), (VV_d, vtst, "vtst")):
                    P.dma('sp', dst[g0:g0 + nt_].rearrange("t p h k -> p t h k"), st[:, 0:nt_, :, :],
                          r=[(key, h, j) for h in range(8) for j in range(0, nt_, 4)], w=["d_" + key])

            c1_superblock(0, NCT, 0, NCT, True)
            for t0 in range(0, NLT, SBT1):
                c1_superblock(NCT, NLT, t0, t0 + SBT1, False)
            P.barrier()
        if stop_after == "c1":
            P.finish()
            return nc

        with contextlib.ExitStack() as ph:
            HS = [128, 8, 128]
            lKT = Ring(nc, ph, "lKT", 2, HS, BF16); lQT = Ring(nc, ph, "lQT", 2, HS, BF16)
            lKK = Ring(nc, ph, "lKK", 2, HS, BF16); lVV = Ring(nc, ph, "lVV", 2, HS, BF16)
            gbl = Ring(nc, ph, "gbl", 2, [128, 32], F32)
            scr = Ring(nc, ph, "scr", 2, [128, 80], F32)
            L16 = Ring(nc, ph, "L16", 2, [16, 4, 128], F32)
            rE = Ring(nc, ph, "rE", 2, [16, 2, 8, 128], F32)
            bmask = sb("bmask", [16, 8, 128], F32, ph)
            Ei = Ring(nc, ph, "Ei", 2, HS, F32); Es = Ring(nc, ph, "Es", 2, HS, F32)
            SBm = Ring(nc, ph, "SBm", 2, HS, F32); tf = Ring(nc, ph, "tf", 2, HS, F32)
            Ak = Ring(nc, ph, "Ak", 4, HS, BF16); Bk = Ring(nc, ph, "Bk", 4, HS, BF16); Tk = Ring(nc, ph, "Tk", 4, HS, BF16)
            TTk = Ring(nc, ph, "TTk", 4, HS, BF16); A0r = Ring(nc, ph, "A0r", 2, HS, BF16); B0r = Ring(nc, ph, "B0r", 2, HS, BF16)
            Of = Ring(nc, ph, "Of", 8, HS, BF16); P1r = Ring(nc, ph, "P1r", 4, HS, BF16)
            qkT = Ring(nc, ph, "qkT", 2, HS, BF16); KG = Ring(nc, ph, "KG", 2, HS, BF16); Kd = Ring(nc, ph, "Kd", 2, HS, BF16)
            up = Ring(nc, ph, "up", 2, HS, F32); wT = Ring(nc, ph, "wT", 2, HS, BF16); vn = Ring(nc, ph, "vn", 2, HS, BF16)
            ob = Ring(nc, ph, "ob", 2, HS, F32)
            Sst = [sb("S%d" % d, HS, F32, ph) for d in range(2)]
            Sbf = [sb("Sb%d" % d, HS, BF16, ph) for d in range(2)]
            P.dma('sp', bmask[:], blockmask, r=["d_bm"], w=["bmask"])
            mskb = sb("mskb", [128, 3, 128], BF16, ph)
            P.op('dve', lambda: nc.vector.tensor_copy(out=mskb[:], in_=msk[:, 10:13, :]), r=["msk"], w=["mskb"])
            for d in range(2):
                P.op('pool', (lambda d=d: nc.gpsimd.memset(Sst[d][:], 0.0)), r=[], w=[("S", d)])
                P.op('pool', (lambda d=d: nc.gpsimd.memset(Sbf[d][:], 0.0)), r=[], w=[("Sb", d)])
            for t_ in scr.tiles:
                P.op('pool', (lambda t_=t_: nc.gpsimd.memset(t_[:], 1.0)), r=[], w=[])
            P.flush()
            P.barrier()
            ppc = [0]

            def pair():
                p = ppc[0] % 4
                ppc[0] += 1
                v = psum[:, p * 1024:(p + 1) * 1024]
                return p, v, v.rearrange("p (h k) -> p h k", h=8), [PK[2 * p], PK[2 * p + 1]]

            def bc_mid(ap2d, n=128):
                return ap2d.unsqueeze(1).to_broadcast([ap2d.shape[0], 8, ap2d.shape[1]])

            def bc_last(ap2d):
                return ap2d.unsqueeze(2).to_broadcast([ap2d.shape[0], 8, 128])

            def dn_step(d, gt, lt, need_o):
                V = nc.vector; G = nc.gpsimd; A = nc.scalar
                kt, ktk = lKT.next(); kk, kkk = lKK.next(); vv, vvk = lVV.next(); gb, gbk = gbl.next()
                P.dma('sp', kt[:], KT_d[gt], r=["d_kst"], w=[ktk])
                P.dma('sp', kk[:], KK_d[gt], r=["d_ktst"], w=[kkk])
                P.dma('sp', vv[:], VV_d[gt], r=["d_vtst"], w=[vvk])
                P.dma('sp', gb[:], GB_d[gt], r=["d_GB"], w=[gbk])
                if need_o:
                    qt, qtk = lQT.next()
                    P.dma('sp', qt[:], QT_d[gt], r=["d_qst"], w=[qtk])
                g = gb[:, 16 * d:16 * d + 8]; beta = gb[:, 16 * d + 8:16 * d + 16]
                sc, sk = scr.next()
                p, pv, pv3, pk = pair()
                P.op('pe', lambda: nc.tensor.matmul(pv[:, 0:8], msk[:, 1 + d, :], g, start=True, stop=True), r=[gbk, "msk"], w=pk)
                P.op('pe', lambda: nc.tensor.matmul(pv[:, 8:16], msk[:, 3, :], g, start=True, stop=True), r=[gbk, "msk"], w=pk)
                P.op('dve', lambda: V.tensor_copy(out=sc[:, 0:16], in_=pv[:, 0:16]), r=pk, w=[sk])
                yield
                P.op('act', lambda: A.activation(out=sc[:, 16:24], in_=sc[:, 0:8], func=AF.Exp), r=[sk], w=[sk])
                P.op('dve', lambda: V.tensor_tensor(out=sc[:, 24:32], in0=sc[:, 8:16], in1=sc[:, 0:8], op=ALU.subtract), r=[sk], w=[sk])
                P.op('act', lambda: A.activation(out=sc[:, 24:32], in_=sc[:, 24:32], func=AF.Exp), r=[sk], w=[sk])
                P.op('act', lambda: A.activation(out=sc[:, 32:40], in_=sc[:, 8:16], func=AF.Exp), r=[sk], w=[sk])
                P.op('dve', lambda: V.tensor_scalar(out=sc[:, 40:48], in0=sc[:, 0:8], scalar1=-1.0, scalar2=None, op0=ALU.mult), r=[sk], w=[sk])
                P.op('dve', lambda: V.tensor_copy(out=sc[:, 56:64], in_=sc[:, 0:8]), r=[sk], w=[sk])
                P.op('act', lambda: A.activation(out=sc[:, 72:80], in_=beta, func=AF.Ln), r=[gbk], w=[sk])
                P.op('dve', lambda: V.tensor_tensor(out=sc[:, 72:80], in0=sc[:, 72:80], in1=sc[:, 40:48], op=ALU.add), r=[sk], w=[sk])
                yield
                egc = sc[:, 16:24]; edec = sc[:, 24:32]; egl = sc[:, 32:40]
                l16, lk = L16.next(); re, rek = rE.next()
                p, pv, pv3, pk = pair()
                for q in range(4):
                    P.op('pe', (lambda q=q: nc.tensor.transpose(pv[0:16, q * 128:(q + 1) * 128], sc[:, 40 + 8 * q:56 + 8 * q], ident)),
                         r=[sk, "msk"], w=pk)
                yield
                P.op('act', lambda: A.copy(out=l16[:], in_=pv[0:16, 0:512].rearrange("p (q k) -> p q k", q=4)), r=pk, w=[lk])
                P.op('pool', lambda: G.tensor_tensor(out=re[:, 0, :, :], in0=bc_mid(l16[:, 1, :]), in1=bmask[:], op=ALU.mult), r=[lk, "bmask"], w=[(rek, 0)])
                P.op('pool', lambda: G.tensor_tensor(out=re[:, 1, :, :], in0=bc_mid(l16[:, 3, :]), in1=bmask[:], op=ALU.mult), r=[lk, "bmask"], w=[(rek, 1)])
                yield
                ei, eik = Ei.next(); es_, esk = Es.next()
                for which, (lq, dst, dk_, mi) in enumerate(((0, ei, eik, 4 + d), (2, es_, esk, 8 + d))):
                    yield
                    p, pv, pv3, pk = pair()
                    for half in range(2):
                        P.op('pe', (lambda half=half, which=which, lq=lq, pv=pv: nc.tensor.matmul(
                            pv[:, half * 512:(half + 1) * 512], l16[:, lq, :],
                            re[:, which, 4 * half:4 * half + 4, :].rearrange("p h k -> p (h k)"), start=True, stop=True)),
                            r=[lk, (rek, which)], w=pk)
                    P.op('dve', (lambda pv3=pv3, dst=dst, mi=mi: V.scalar_tensor_tensor(
                        out=dst[:], in0=pv3, scalar=0.0, in1=bc_mid(msk[:, mi, :]), op0=ALU.min, op1=ALU.add)), r=pk + ["msk"], w=[dk_])
                    P.op('act', (lambda dst=dst: A.activation(out=dst[:], in_=dst[:], func=AF.Exp)), r=[dk_], w=[dk_])
                yield
                sbm, sbk = SBm.next()
                P.op('pool', lambda: G.tensor_tensor(out=sbm[:], in0=bc_mid(msk[:, 6 + d, :]), in1=bc_last(beta), op=ALU.mult), r=["msk", gbk], w=[sbk])
                p, pv, pv3, pk = pair()
                for h in range(8):
                    P.op('pe', (lambda h=h, pv=pv: nc.tensor.matmul(pv[:, h * 128:(h + 1) * 128], kt[:, h, :], kt[:, h, :], start=True, stop=True)),
                         r=[ktk], w=pk)
                yield
                t1, t1k = tf.next()
                a0, a0k = A0r.next(); b0, b0k = B0r.next()
                P.op('dve', (lambda pv3=pv3: V.tensor_tensor(out=t1[:], in0=pv3, in1=ei[:], op=ALU.mult)), r=pk + [eik], w=[t1k])
                P.op('dve', lambda: V.tensor_tensor(out=a0[:], in0=t1[:], in1=sbm[:], op=ALU.mult), r=[t1k, sbk], w=[a0k])
                P.op('dve', (lambda pv3=pv3: V.tensor_tensor(out=b0[:], in0=pv3, in1=es_[:], op=ALU.mult)), r=pk + [esk], w=[b0k])
                if need_o:
                    qk_, qkk = qkT.next()
                    p, pv, pv3, pk = pair()
                    for h in range(8):
                        P.op('pe', (lambda h=h, pv=pv: nc.tensor.matmul(pv[:, h * 128:(h + 1) * 128], kt[:, h, :], qt[:, h, :], start=True, stop=True)),
                             r=[ktk, qtk], w=pk)
                    P.op('dve', (lambda pv3=pv3: V.tensor_tensor(out=qk_[:], in0=pv3, in1=ei[:], op=ALU.mult)), r=pk + [eik], w=[qkk])
                yield
                def mm8(lhs, rhs, keys):
                    p, pv, pv3, pk = pair()
                    for h in range(8):
                        P.op('pe', (lambda h=h, pv=pv: nc.tensor.matmul(pv[:, h * 128:(h + 1) * 128], lhs[:, h, :], rhs[:, h, :], start=True, stop=True)),
                             r=keys, w=pk)
                    return pv3, pk
                ca, cak = Ak.next(); cb, cbk = Bk.next(); cT, cTk = Tk.next(); cTT, cTTk = TTk.next()
                P.op('dve', lambda ca=ca: V.tensor_tensor(out=ca[:], in0=a0[:], in1=bc_mid(mskb[:, 0, :]), op=ALU.mult), r=[a0k, "mskb"], w=[cak])
                P.op('dve', lambda cb=cb: V.tensor_tensor(out=cb[:], in0=b0[:], in1=bc_mid(mskb[:, 0, :]), op=ALU.mult), r=[b0k, "mskb"], w=[cbk])
                P.op('dve', lambda ca=ca, cT=cT: V.tensor_tensor(out=cT[:], in0=bc_mid(identb[:]), in1=ca[:], op=ALU.subtract), r=["identb", cak], w=[cTk])
                P.op('dve', lambda cb=cb, cTT=cTT: V.tensor_tensor(out=cTT[:], in0=bc_mid(identb[:]), in1=cb[:], op=ALU.subtract), r=["identb", cbk], w=[cTTk])
                yield
                offs = []
                for mi in (11, 12):
                    of_, ofk = Of.next(); oft, oftk = Of.next()
                    P.op('dve', (lambda of_=of_, mi=mi: V.tensor_tensor(out=of_[:], in0=a0[:], in1=bc_mid(mskb[:, mi - 10, :]), op=ALU.mult)), r=[a0k, "mskb"], w=[ofk])
                    P.op('dve', (lambda oft=oft, mi=mi: V.tensor_tensor(out=oft[:], in0=b0[:], in1=bc_mid(mskb[:, mi - 10, :]), op=ALU.mult)), r=[b0k, "mskb"], w=[oftk])
                    offs.append((of_, ofk, oft, oftk))
                for lev in range(4):
                    yield
                    na, nak = Ak.next(); nb, nbk = Bk.next(); nT, nTk = Tk.next(); nTT, nTTk = TTk.next()
                    pv3, pk = mm8(cb, ca, [cak, cbk])
                    P.op('act', (lambda pv3=pv3, na=na: A.copy(out=na[:], in_=pv3)), r=pk, w=[nak])
                    pv3, pk = mm8(ca, cb, [cak, cbk])
                    P.op('dve', (lambda pv3=pv3, nb=nb: V.tensor_copy(out=nb[:], in_=pv3)), r=pk, w=[nbk])
                    yield
                    pv3, pk = mm8(nb, cT, [nbk, cTk])
                    P.op('dve', (lambda pv3=pv3, nT=nT, cT=cT: V.tensor_tensor(out=nT[:], in0=pv3, in1=cT[:], op=ALU.add)), r=pk + [cTk], w=[nTk])
                    pv3, pk = mm8(na, cTT, [nak, cTTk])
                    P.op('dve', (lambda pv3=pv3, nTT=nTT, cTT=cTT: V.tensor_tensor(out=nTT[:], in0=pv3, in1=cTT[:], op=ALU.add)), r=pk + [cTTk], w=[nTTk])
                    ca, cak, cb, cbk, cT, cTk, cTT, cTTk = na, nak, nb, nbk, nT, nTk, nTT, nTTk
                for si, (of_, ofk, oft, oftk) in enumerate(offs):
                    yield
                    p1, p1k = P1r.next()
                    pv3, pk = mm8(oft, cT, [oftk, cTk])
                    P.op('act', (lambda pv3=pv3, p1=p1: A.copy(out=p1[:], in_=pv3)), r=pk, w=[p1k])
                    yield
                    pv3, pk = mm8(cTT, p1, [cTTk, p1k])
                    nT, nTk = Tk.next()
                    P.op('dve', (lambda pv3=pv3, nT=nT, cT=cT: V.tensor_tensor(out=nT[:], in0=cT[:], in1=pv3, op=ALU.subtract)), r=pk + [cTk], w=[nTk])
                    if si == 0:
                        p1t, p1tk = P1r.next()
                        pv3, pk = mm8(of_, cTT, [ofk, cTTk])
                        P.op('act', (lambda pv3=pv3, p1t=p1t: A.copy(out=p1t[:], in_=pv3)), r=pk, w=[p1tk])
                        pv3, pk = mm8(cT, p1t, [cTk, p1tk])
                        nTT, nTTk = TTk.next()
                        P.op('dve', (lambda pv3=pv3, nTT=nTT, cTT=cTT: V.tensor_tensor(out=nTT[:], in0=cTT[:], in1=pv3, op=ALU.subtract)), r=pk + [cTTk], w=[nTTk])
                        cTT, cTTk = nTT, nTTk
                    cT, cTk = nT, nTk
                yield
                u_, uk = up.next(); w_, wk_ = wT.next(); kg, kgk = KG.next(); kd, kdk = Kd.next()
                P.op('pool', lambda: G.tensor_tensor(out=kg[:], in0=kk[:], in1=bc_last(egc), op=ALU.mult), r=[kkk, sk], w=[kgk])
                P.op('pool', lambda: G.tensor_tensor(out=kd[:], in0=kk[:], in1=bc_last(edec), op=ALU.mult), r=[kkk, sk], w=[kdk])
                p, pv, pv3, pk = pair()
                for h in range(8):
                    P.op('pe', (lambda h=h, pv=pv: nc.tensor.matmul(pv[:, h * 128:(h + 1) * 128], cT[:, h, :], vv[:, h, :], start=True, stop=True)),
                         r=[cTk, vvk], w=pk)
                P.op('act', (lambda pv3=pv3: A.copy(out=u_[:], in_=pv3)), r=pk, w=[uk])
                p, pv, pv3, pk = pair()
                for h in range(8):
                    P.op('pe', (lambda h=h, pv=pv: nc.tensor.matmul(pv[:, h * 128:(h + 1) * 128], kg[:, h, :], cT[:, h, :], start=True, stop=True)),
                         r=[cTk, kgk], w=pk)
                P.op('act', (lambda pv3=pv3: A.copy(out=w_[:], in_=pv3)), r=pk, w=[wk_])
                yield
                S = Sst[d]; Sb = Sbf[d]; Sk = ("S", d); Sbk = ("Sb", d)
                vn_, vnk = vn.next(); t2, t2k = tf.next()
                p, pv, pv3, pk = pair()
                for h in range(8):
                    P.op('pe', (lambda h=h, pv=pv: nc.tensor.matmul(pv[:, h * 128:(h + 1) * 128], w_[:, h, :], Sb[:, h, :], start=True, stop=True)),
                         r=[wk_, Sbk], w=pk)
                yield
                P.op('dve', (lambda pv3=pv3: V.tensor_tensor(out=t2[:], in0=u_[:], in1=pv3, op=ALU.subtract)), r=pk + [uk], w=[t2k])
                P.op('dve', lambda: V.tensor_tensor(out=vn_[:], in0=t2[:], in1=bc_last(beta), op=ALU.mult), r=[t2k, gbk], w=[vnk])
                if need_o:
                    o_, ok_ = ob.next()
                    p, pv, pv3, pk = pair()
                    for h in range(8):
                        P.op('pe', (lambda h=h, pv=pv: nc.tensor.matmul(pv[:, h * 128:(h + 1) * 128], qt[:, h, :], Sb[:, h, :], start=True, stop=True)),
                             r=[qtk, Sbk], w=pk)
                    P.op('dve', (lambda pv3=pv3: V.tensor_tensor(out=o_[:], in0=pv3, in1=bc_last(egc), op=ALU.mult)), r=pk + [sk], w=[ok_])
                    p, pv, pv3, pk = pair()
                    for h in range(8):
                        P.op('pe', (lambda h=h, pv=pv: nc.tensor.matmul(pv[:, h * 128:(h + 1) * 128], qk_[:, h, :], vn_[:, h, :], start=True, stop=True)),
                             r=[qkk, vnk], w=pk)
                    P.op('dve', (lambda pv3=pv3: V.tensor_tensor(out=o_[:], in0=o_[:], in1=pv3, op=ALU.add)), r=pk + [ok_], w=[ok_])
                    P.dma('sp', OF_d[d, lt], o_[:].rearrange("p h k -> p (h k)"), r=[ok_], w=["d_OF"])
                yield
                p, pv, pv3, pk = pair()
                for h in range(8):
                    P.op('pe', (lambda h=h, pv=pv: nc.tensor.matmul(pv[:, h * 128:(h + 1) * 128], kd[:, h, :], vn_[:, h, :], start=True, stop=True)),
                         r=[kdk, vnk], w=pk)
                P.op('dve', lambda: V.tensor_tensor(out=S[:], in0=S[:], in1=bc_last(egl), op=ALU.mult), r=[Sk, sk], w=[Sk])
                P.op('dve', (lambda pv3=pv3: V.tensor_tensor(out=S[:], in0=S[:], in1=pv3, op=ALU.add)), r=pk + [Sk], w=[Sk])
                P.op('act', lambda: A.copy(out=Sb[:], in_=S[:]), r=[Sk], w=[Sbk])
                import os
                if os.environ.get("DN_DBG") and d == int(os.environ["DN_DBG"]) and gt == int(os.environ.get("DN_DBG_GT", "0")):
                    dump("dbg_sc", sc[:], [sk]); dump("dbg_Ei", ei[:], [eik]); dump("dbg_Es", es_[:], [esk])
                    dump("dbg_a0", a0[:], [a0k]); dump("dbg_b0", b0[:], [b0k]); dump("dbg_T", cT[:], [cTk])
                    dump("dbg_up", u_[:], [uk]); dump("dbg_wT", w_[:], [wk_]); dump("dbg_vn", vn_[:], [vnk]); dump("dbg_S", S[:], [Sk])
                    dump("dbg_l16", l16[:], [lk])
                    if need_o:
                        dump("dbg_qk", qk_[:], [qkk]); dump("dbg_o", o_[:], [ok_])

            order_f = [(t, t, False) for t in range(NCT)] + [(NCT + t, t, True) for t in range(NLT)]
            order_b = [(t, t, False) for t in reversed(range(NCT))] + [(NCT + t, t, True) for t in reversed(range(NLT))]
            import os
            nstep = int(os.environ.get("DN_STEPS", str(NT)))
            for i in range(nstep):
                g0 = dn_step(0, *order_f[i])
                g1 = dn_step(1, *order_b[i])
                run_interleaved(P, [g0, g1], 2, rare=False)
            P.barrier()
        if stop_after == "c2":
            P.finish()
            return nc

        with contextlib.ExitStack() as ph:
            R = mk_rings(ph)
            load_mods(ph, [("L", 2)])
            of0 = Ring(nc, ph, "of0", 2, [128, D], F32); of1 = Ring(nc, ph, "of1", 2, [128, D], F32)
            sqt = Ring(nc, ph, "sqt", 2, [128, D], F32)
            zl = Ring(nc, ph, "zl", 2, [128, D], BF16)
            ms = Ring(nc, ph, "ms", 2, [128, 8], F32)
            onT = Ring(nc, ph, "onT", 2, [128, 8, 128], BF16)
            yb = Ring(nc, ph, "yb", 2, [128, D], F32)
            wo = sb("wo", [128, 8, D], BF16, ph)
            dnw = sb("dnw", [128, 128], F32, ph)
            P.dma('pool', wo[:], w_dn_out.rearrange("(k p) n -> p k n", p=128), r=["d_wo"], w=["wo"])
            P.dma('sp', dnw[:], dn_norm.partition_broadcast(128), r=["d_dnn"], w=["dnw"])

            def c3_tile(lt):
                gt = NCT + lt
                V = nc.vector; G = nc.gpsimd; A = nc.scalar
                a0_, a0k = of0.next(); a1_, a1k = of1.next(); z_, zk = zl.next(); m_, mk_ = ms.next(); sq, sqk = sqt.next()
                P.dma('sp', a0_[:], OF_d[0, lt], r=["d_OF"], w=[a0k])
                P.dma('sp', a1_[:], OF_d[1, lt], r=["d_OF"], w=[a1k])
                P.dma('sp', z_[:], ZZ_d[gt], r=["d_ZZ"], w=[zk])
                P.op('pool', lambda: G.tensor_tensor(out=a0_[:], in0=a0_[:], in1=a1_[:], op=ALU.add), r=[a0k, a1k], w=[a0k])
                yield
                P.op('act', lambda: A.activation(out=sq[:], in_=a0_[:], func=AF.Square), r=[a0k], w=[sqk])
                P.op('dve', lambda: V.reduce_sum(out=m_[:], in_=sq[:].rearrange("p (h k) -> p h k", h=8), axis=AX.X), r=[sqk], w=[mk_])
                P.op('dve', lambda: V.tensor_scalar(out=m_[:], in0=m_[:], scalar1=1.0 / 128, scalar2=EPS, op0=ALU.mult, op1=ALU.add), r=[mk_], w=[mk_])
                yield
                P.op('act', lambda: A.activation(out=m_[:], in_=m_[:], func=AF.Sqrt), r=[mk_], w=[mk_])
                yield
                P.op('dve', lambda: V.reciprocal(out=m_[:], in_=m_[:]), r=[mk_], w=[mk_])
                o3 = a0_[:].rearrange("p (h k) -> p h k", h=8)
                P.op('dve', lambda: V.tensor_tensor(out=o3, in0=o3, in1=m_[:].unsqueeze(2).to_broadcast([128, 8, 128]), op=ALU.mult), r=[a0k, mk_], w=[a0k])
                P.op('pool', lambda: G.tensor_tensor(out=o3, in0=o3, in1=dnw[:].unsqueeze(1).to_broadcast([128, 8, 128]), op=ALU.mult), r=[a0k, "dnw"], w=[a0k])
                P.op('pool', lambda: G.tensor_tensor(out=a0_[:], in0=a0_[:], in1=z_[:], op=ALU.mult), r=[a0k, zk], w=[a0k])
                yield
                for k in range(8):
                    b = (6 if lt % 2 == 0 else 2) + k // 4
                    P.op('pe', (lambda b=b, k=k: nc.tensor.transpose(PS[b][:, (k % 4) * 128:(k % 4) * 128 + 128], a0_[:, k * 128:(k + 1) * 128], ident)),
                         r=[a0k, "msk"], w=[PK[b]])
                t_, tk = onT.next()
                yield
                for h in range(2):
                    b = (6 if lt % 2 == 0 else 2) + h
                    P.op('act', (lambda b=b, h=h: A.copy(out=t_[:, 4 * h:4 * h + 4, :], in_=PS[b].rearrange("p (k t) -> p k t", k=4))),
                         r=[PK[b]], w=[(tk, h)])
                y_, yk = yb.next()
                xt, xk = R['xs'].next()
                P.dma('sp', xt[:], XS2_t[gt], r=["d_dst0"], w=[xk])
                yield
                for half in range(2):
                    b = (4 if lt % 2 == 0 else 0) + half
                    for k in range(8):
                        P.op('pe', (lambda b=b, k=k, half=half: nc.tensor.matmul(PS[b], t_[:, k, :], wo[:, k, half * 512:(half + 1) * 512],
                                                                             start=(k == 0), stop=(k == 7))), r=[(tk, 0), (tk, 1), "wo"], w=[PK[b]])
                    sl = slice(half * 512, (half + 1) * 512)
                    P.op('dve', (lambda b=b, sl=sl: V.tensor_tensor(out=y_[:, sl], in0=PS[b], in1=mv("L", 2)[:, sl], op=ALU.mult)),
                         r=[PK[b], "mvec"], w=[(yk, half)])
                    P.op('pool', (lambda sl=sl: G.tensor_tensor(out=y_[:, sl], in0=y_[:, sl], in1=xt[:, sl], op=ALU.add)), r=[(yk, half), xk], w=[(yk, half)])
                P.dma('sp', XS3_t[gt], y_[:], r=[(yk, 0), (yk, 1)], w=["d_XS3"])

            run_interleaved(P, (c3_tile(lt) for lt in range(NLT)), 2)
            P.barrier()
        if stop_after == "c3":
            P.finish()
            return nc
        MOE(1, XS3_t, out_t, list(range(NCT, NT)), True)
        P.finish()
    return nc


_CACHE = {}


def _core_inputs(b, inp, consts):
    m = {}
    m['xin'] = np.ascontiguousarray(np.concatenate([inp['ctx'][b], inp['x'][b]], 0), dtype=np.float32)
    cc = np.stack([inp['c'][b], inp['c_ctx']], 0)
    m['ccol'] = np.ascontiguousarray(cc.reshape(2, 8, 128).transpose(2, 0, 1), dtype=np.float32)
    for k in ('w_ada', 'b_ada', 'norm_mix', 'norm_ffn', 'w_e_gate', 'w_e_up', 'w_e_down', 'norm_final'):
        m[k] = np.ascontiguousarray(inp[k], dtype=np.float32)
    m['w_pool'] = np.ascontiguousarray(inp['w_pool'][0]); m['b_pool'] = np.ascontiguousarray(inp['b_pool'][0])
    m['pool_scale'] = np.ascontiguousarray(inp['pool_scale'][0])
    m['w_dn_in'] = np.ascontiguousarray(inp['w_dn_in'][0])
    m['wconv'] = np.ascontiguousarray(inp['w_dn_conv'][0].reshape(4, 24, 128).transpose(2, 1, 0))
    m['dn_a_log'] = np.ascontiguousarray(inp['dn_a_log'][0].reshape(16))
    m['dn_dt_bias'] = np.ascontiguousarray(inp['dn_dt_bias'][0].reshape(16))
    m['dn_norm'] = np.ascontiguousarray(inp['dn_norm'][0]); m['w_dn_out'] = np.ascontiguousarray(inp['w_dn_out'][0])
    m['w_r'] = np.ascontiguousarray(np.concatenate([inp['w_rg'], inp['w_re']], -1))
    m['b_r'] = np.ascontiguousarray(np.concatenate([inp['b_rg'], inp['b_re']], -1))
    m.update(consts)
    return m


def kernel(**inputs):
    inp = {k: np.asarray(v) for k, v in inputs.items()}
    if 'nc' not in _CACHE:
        _CACHE['nc'] = build()
        _CACHE['consts'] = host_constants()
    nc = _CACHE['nc']
    consts = _CACHE['consts']
    B = inp['x'].shape[0]
    in_maps = [_core_inputs(b, inp, consts) for b in range(B)]
    res = run_bass_kernel_spmd(nc, in_maps, core_ids=list(range(B)))
    out = np.stack([np.asarray(res.results[b]['out']).reshape(NLT * 128, D) for b in range(B)], 0)
    return out.astype(inp['x'].dtype)
```
